# Optimizing a Trainium2 kernel written in Bass

```python
import math
import jax, jax.numpy as jnp
from jax import lax
import numpy as np

D_MODEL = 1024
BATCH = 2
SEQ = 16384
DEPTH = 2

A_HEADS = 4
A_HEAD_DIM = 128
A_WIDTH = A_HEADS * A_HEAD_DIM
A_CHUNK = 128
B_WIDTH = 512
B_KERNEL = 31
C_HEADS = 8
C_NOPE = 64
C_ROPE = 32
C_QK_DIM = C_NOPE + C_ROPE
C_VDIM = 64
C_Q_RANK = 384
C_KV_RANK = 256
Q_BLOCK = 128
ROPE_THETA = 10000.0
N_BRANCHES = 3
N_EXPERTS = 16
EXPERT_FF = 1536
CAPACITY_FACTOR = 2
EPS = 1e-6

IN_SIZES = (
    A_WIDTH,
    A_WIDTH,
    A_WIDTH,
    A_WIDTH,
    4 * A_HEADS,
    2 * B_WIDTH,
    C_Q_RANK,
    C_KV_RANK,
    C_ROPE,
    N_BRANCHES * D_MODEL,
)
IN_WIDTH = int(sum(IN_SIZES))
IN_OFFSETS = tuple(int(o) for o in np.cumsum(IN_SIZES)[:-1])

kernel_name = "hybrid_mlstm_conv_mla_ec_encoder"


def _rmsnorm(x, g):
    xf = x.astype(jnp.float32)
    y = xf * lax.rsqrt(jnp.mean(xf * xf, axis=-1, keepdims=True) + EPS)
    return (y * g.astype(jnp.float32)).astype(x.dtype)


def _layernorm(x, g, b):
    xf = x.astype(jnp.float32)
    mu = jnp.mean(xf, axis=-1, keepdims=True)
    xc = xf - mu
    y = xc * lax.rsqrt(jnp.mean(xc * xc, axis=-1, keepdims=True) + EPS)
    return (y * g.astype(jnp.float32) + b.astype(jnp.float32)).astype(x.dtype)


def _mlstm_chunkwise(q, k, v, i_pre, f_pre):
    B, H, S, Dh = q.shape
    nc = S // A_CHUNK
    f32 = jnp.float32

    def chunks(t):
        return jnp.moveaxis(t.astype(f32).reshape(B, H, nc, A_CHUNK, *t.shape[3:]), 2, 0)

    qc = chunks(q)
    kc = chunks(k) * (Dh ** -0.5)
    vc = chunks(v)
    ic = chunks(i_pre)
    lfc = chunks(jax.nn.log_sigmoid(f_pre.astype(f32)))
    tril = jnp.tril(jnp.ones((A_CHUNK, A_CHUNK), dtype=bool))

    def step(carry, inp):
        C, n, m = carry
        qb, kb, vb, ib, lfb = inp
        bcum = jnp.cumsum(lfb, axis=-1)
        dlog = bcum[..., :, None] - bcum[..., None, :] + ib[..., None, :]
        dlog = jnp.where(tril, dlog, -jnp.inf)
        m_inter = bcum + m[..., None]
        m_t = jnp.maximum(m_inter, jnp.max(dlog, axis=-1))
        w_inter = jnp.exp(m_inter - m_t)
        scores = jnp.einsum('bhtd,bhsd->bhts', qb, kb) * jnp.exp(dlog - m_t[..., None])
        num = (w_inter[..., None] * jnp.einsum('bhtd,bhde->bhte', qb, C)
               + jnp.einsum('bhts,bhse->bhte', scores, vb))
        den = w_inter * jnp.einsum('bhtd,bhd->bht', qb, n) + jnp.sum(scores, axis=-1)
        h = num / jnp.maximum(jnp.abs(den), jnp.exp(-m_t))[..., None]
        g_tot = bcum[..., -1]
        a = g_tot[..., None] - bcum + ib
        m_new = jnp.maximum(g_tot + m, jnp.max(a, axis=-1))
        decay = jnp.exp(g_tot + m - m_new)
        wk = jnp.exp(a - m_new[..., None])
        C_new = decay[..., None, None] * C + jnp.einsum('bhs,bhsd,bhse->bhde', wk, kb, vb)
        n_new = decay[..., None] * n + jnp.einsum('bhs,bhsd->bhd', wk, kb)
        return (C_new, n_new, m_new), h

    init = (jnp.zeros((B, H, Dh, Dh), f32), jnp.zeros((B, H, Dh), f32), jnp.zeros((B, H), f32))
    _, h = lax.scan(step, init, (qc, kc, vc, ic, lfc))
    return jnp.moveaxis(h, 0, 2).reshape(B, H, S, Dh)


def _rope_tail(t, cos, sin):
    nope, r = t[..., :C_NOPE], t[..., C_NOPE:]
    r1, r2 = jnp.split(r, 2, axis=-1)
    c = cos[:, None, :].astype(t.dtype)
    s = sin[:, None, :].astype(t.dtype)
    return jnp.concatenate([nope, r1 * c - r2 * s, r2 * c + r1 * s], axis=-1)


def _dense_attention(q, k, v):
    B, H, S, Dq = q.shape
    nq = S // Q_BLOCK
    qb = jnp.moveaxis(q.reshape(B, H, nq, Q_BLOCK, Dq), 2, 0)
    scale = Dq ** -0.5

    def block(qi):
        s = jnp.einsum('bhqd,bhkd->bhqk', qi, k).astype(jnp.float32) * scale
        p = jax.nn.softmax(s, axis=-1).astype(v.dtype)
        return jnp.einsum('bhqk,bhkd->bhqd', p, v)

    o = lax.map(block, qb)
    return jnp.moveaxis(o, 0, 2).reshape(B, H, S, v.shape[-1])


def _mixer(x, cos, sin, norm_g, w_in, b_if, a_norm_g, w_a_out, conv_w, conv_b,
           conv_ln_g, conv_ln_b, w_b_out, cq_norm_g, w_uq, ckv_norm_g, w_ukv,
           q_norm_g, k_norm_g, w_c_out, w_out):
    B, S, _ = x.shape
    xn = _rmsnorm(x, norm_g)
    z = xn @ w_in
    a_q, a_k, a_v, a_o, a_g, b_glu, c_q, c_kv, c_kr, gts = jnp.split(z, IN_OFFSETS, axis=-1)

    def heads(t):
        return t.reshape(B, S, A_HEADS, A_HEAD_DIM).transpose(0, 2, 1, 3)
    q, k, v = heads(a_q), heads(a_k), heads(a_v)
    gate_pre = (a_g + b_if).reshape(B, S, 4, A_HEADS).transpose(2, 0, 3, 1)
    i_fw, f_fw, i_bw, f_bw = gate_pre[0], gate_pre[1], gate_pre[2], gate_pre[3]
    rev = lambda t: jnp.flip(t, axis=2)
    h_fw = _mlstm_chunkwise(q, k, v, i_fw, f_fw)
    h_bw = rev(_mlstm_chunkwise(rev(q), rev(k), rev(v), rev(i_bw), rev(f_bw)))
    h = _rmsnorm(h_fw + h_bw, a_norm_g[:, None, :])
    h = h.transpose(0, 2, 1, 3).reshape(B, S, A_WIDTH) * jax.nn.sigmoid(a_o)
    y_a = h @ w_a_out

    u_a, u_g = jnp.split(b_glu, 2, axis=-1)
    u = u_a * jax.nn.sigmoid(u_g)
    u = lax.conv_general_dilated(
        u, conv_w[:, None, :].astype(u.dtype), window_strides=(1,),
        padding=[(B_KERNEL // 2, B_KERNEL // 2)],
        dimension_numbers=('NWC', 'WIO', 'NWC'),
        feature_group_count=B_WIDTH) + conv_b
    u = jax.nn.silu(_layernorm(u, conv_ln_g, conv_ln_b))
    y_b = u @ w_b_out

    qh = (_rmsnorm(c_q, cq_norm_g) @ w_uq).reshape(B, S, C_HEADS, C_QK_DIM)
    kv = (_rmsnorm(c_kv, ckv_norm_g) @ w_ukv).reshape(B, S, C_HEADS, C_NOPE + C_VDIM)
    k_nope, v_c = kv[..., :C_NOPE], kv[..., C_NOPE:]
    k_r = jnp.broadcast_to(c_kr[:, :, None, :], (B, S, C_HEADS, C_ROPE))
    kh = jnp.concatenate([k_nope, k_r], axis=-1)
    qh = _rope_tail(_rmsnorm(qh, q_norm_g), cos, sin)
    kh = _rope_tail(_rmsnorm(kh, k_norm_g), cos, sin)
    o = _dense_attention(qh.transpose(0, 2, 1, 3), kh.transpose(0, 2, 1, 3),
                         v_c.transpose(0, 2, 1, 3))
    y_c = o.transpose(0, 2, 1, 3).reshape(B, S, C_HEADS * C_VDIM) @ w_c_out

    g = jax.nn.sigmoid(gts).reshape(B, S, N_BRANCHES, D_MODEL)
    merged = g[..., 0, :] * y_a + g[..., 1, :] * y_b + g[..., 2, :] * y_c
    return merged @ w_out


def _expert_choice_ffn(x, norm_g, w_router, w_e_gate, w_e_up, w_e_down):
    B, S, D = x.shape
    xn = _rmsnorm(x, norm_g)
    aff = jax.nn.softmax((xn @ w_router).astype(jnp.float32), axis=-1)
    cap = CAPACITY_FACTOR * S // N_EXPERTS
    gate, idx = lax.top_k(jnp.swapaxes(aff, 1, 2), cap)
    bidx = jnp.arange(B)[:, None, None]
    xg = xn[bidx, idx]
    h = (jax.nn.silu(jnp.einsum('becd,edf->becf', xg, w_e_gate))
         * jnp.einsum('becd,edf->becf', xg, w_e_up))
    yo = jnp.einsum('becf,efd->becd', h, w_e_down) * gate[..., None].astype(xg.dtype)
    return jnp.zeros((B, S, D), yo.dtype).at[bidx, idx].add(yo)


def setup_inputs(seed: int = 0) -> dict:
    key = jax.random.key(seed)
    ks = jax.random.split(key, 32)
    L, D, H = DEPTH, D_MODEL, A_HEADS
    f32 = jnp.float32

    def nrm(k, shape, scale):
        return jax.random.normal(k, shape, f32) * scale

    def gain(k, shape):
        return 1.0 + 0.02 * jax.random.normal(k, shape, f32)

    b_i = 0.1 * jax.random.normal(ks[3], (L, 2, H), f32)
    b_f = 3.0 + 3.0 * jax.random.uniform(ks[4], (L, 2, H), f32)
    b_if = jnp.stack([b_i[:, 0], b_f[:, 0], b_i[:, 1], b_f[:, 1]], axis=1).reshape(L, 4 * H)

    return {
        "x": jax.random.normal(ks[0], (BATCH, SEQ, D), f32),
        "mix_norm_g": gain(ks[1], (L, D)),
        "w_in": nrm(ks[2], (L, D, IN_WIDTH), D ** -0.5),
        "b_if": b_if,
        "a_norm_g": gain(ks[5], (L, H, A_HEAD_DIM)),
        "w_a_out": nrm(ks[6], (L, A_WIDTH, D), A_WIDTH ** -0.5),
        "conv_w": nrm(ks[7], (L, B_KERNEL, B_WIDTH), B_KERNEL ** -0.5),
        "conv_b": 0.02 * jax.random.normal(ks[8], (L, B_WIDTH), f32),
        "conv_ln_g": gain(ks[9], (L, B_WIDTH)),
        "conv_ln_b": 0.02 * jax.random.normal(ks[10], (L, B_WIDTH), f32),
        "w_b_out": nrm(ks[11], (L, B_WIDTH, D), B_WIDTH ** -0.5),
        "cq_norm_g": gain(ks[12], (L, C_Q_RANK)),
        "w_uq": nrm(ks[13], (L, C_Q_RANK, C_HEADS * C_QK_DIM), C_Q_RANK ** -0.5),
        "ckv_norm_g": gain(ks[14], (L, C_KV_RANK)),
        "w_ukv": nrm(ks[15], (L, C_KV_RANK, C_HEADS * (C_NOPE + C_VDIM)), C_KV_RANK ** -0.5),
        "q_norm_g": gain(ks[16], (L, C_QK_DIM)),
        "k_norm_g": gain(ks[17], (L, C_QK_DIM)),
        "w_c_out": nrm(ks[18], (L, C_HEADS * C_VDIM, D), (C_HEADS * C_VDIM) ** -0.5),
        "w_out": nrm(ks[19], (L, D, D), D ** -0.5),
        "ffn_norm_g": gain(ks[20], (L, D)),
        "w_router": nrm(ks[21], (L, D, N_EXPERTS), D ** -0.5),
        "w_e_gate": nrm(ks[22], (L, N_EXPERTS, D, EXPERT_FF), D ** -0.5),
        "w_e_up": nrm(ks[23], (L, N_EXPERTS, D, EXPERT_FF), D ** -0.5),
        "w_e_down": nrm(ks[24], (L, N_EXPERTS, EXPERT_FF, D), EXPERT_FF ** -0.5),
    }


def reference(x, mix_norm_g, w_in, b_if, a_norm_g, w_a_out, conv_w, conv_b, conv_ln_g,
              conv_ln_b, w_b_out, cq_norm_g, w_uq, ckv_norm_g, w_ukv, q_norm_g, k_norm_g,
              w_c_out, w_out, ffn_norm_g, w_router, w_e_gate, w_e_up, w_e_down):
    S = x.shape[1]
    pos = jnp.arange(S, dtype=jnp.float32)
    inv_freq = ROPE_THETA ** (-jnp.arange(0, C_ROPE, 2, dtype=jnp.float32) / C_ROPE)
    ang = pos[:, None] * inv_freq[None, :]
    cos, sin = jnp.cos(ang), jnp.sin(ang)
    for l in range(DEPTH):
        x = x + _mixer(x, cos, sin, mix_norm_g[l], w_in[l], b_if[l], a_norm_g[l], w_a_out[l],
                       conv_w[l], conv_b[l], conv_ln_g[l], conv_ln_b[l], w_b_out[l],
                       cq_norm_g[l], w_uq[l], ckv_norm_g[l], w_ukv[l], q_norm_g[l],
                       k_norm_g[l], w_c_out[l], w_out[l])
        x = x + _expert_choice_ffn(x, ffn_norm_g[l], w_router[l], w_e_gate[l], w_e_up[l],
                                   w_e_down[l])
    return x
```

```python
import math
from contextlib import ExitStack

import numpy as np
import ml_dtypes

import concourse.bass as bass
import concourse.mybir as mybir
from concourse.bass_utils import run_bass_kernel_spmd

F32 = mybir.dt.float32
BF16 = mybir.dt.bfloat16
I32 = mybir.dt.int32
AF = mybir.ActivationFunctionType
ALU = mybir.AluOpType
AX = mybir.AxisListType
NPBF = ml_dtypes.bfloat16

D = 1024
S = 16384
NB = 2
INW = 6832
EPS = 1e-6
NCORES = 8
TPC = S * NB // NCORES

O_AQ, O_AK, O_AV, O_AO, O_AG = 0, 512, 1024, 1536, 2048
O_GLU = 2064
O_CQ = 3088
O_CKV = 3472
O_CKR = 3728
O_GTS = 3760


class Prog:
    RING = 8

    def __init__(self, nc, es):
        self.nc = nc
        self.es = es
        self.ops = []
        self.engs = ["pe", "act", "dve", "pool", "sp"]
        self.csem = None
        self.rings = {}
        self.ccsem = None
        self.ccount = {e: 0 for e in self.engs}
        self.dcount = {e: 0 for e in self.engs}
        self.cccount = 0
        self.nstage = 0

    def cc(self, fn, r=(), w=()):
        self.ops.append(dict(eng="pool", fn=fn, r=tuple(r), w=tuple(w), dma=True, cc=True))

    def op(self, eng, fn, r=(), w=()):
        self.ops.append(dict(eng=eng, fn=fn, r=tuple(r), w=tuple(w), dma=False))

    def dma(self, eng, fn, r=(), w=()):
        self.ops.append(dict(eng=eng, fn=fn, r=tuple(r), w=tuple(w), dma=True))

    def emit(self):
        nc, es = self.nc, self.es
        ops = self.ops
        last_w = {}
        readers = {}
        deps = []
        for i, o in enumerate(ops):
            d = set()
            for k in o["r"]:
                if k in last_w:
                    d.add((last_w[k], "raw"))
            for k in o["w"]:
                if k in last_w:
                    d.add((last_w[k], "waw"))
                for j in readers.get(k, ()):
                    if j != i:
                        d.add((j, "war"))
            for k in o["r"]:
                lst = readers.setdefault(k, [])
                if not o["dma"]:
                    lst[:] = [j for j in lst if ops[j]["dma"] or ops[j]["eng"] != o["eng"]]
                lst.append(i)
            for k in o["w"]:
                last_w[k] = i
                readers[k] = []
            dd = set()
            for j, kind in d:
                p = ops[j]
                if (not p["dma"]) and (not o["dma"]) and p["eng"] == o["eng"]:
                    if o["eng"] == "pe":
                        continue
                    if kind == "war":
                        continue
                dd.add(j)
            deps.append(dd)
        needed = set()
        for dd in deps:
            needed |= dd
        engs = self.engs
        if self.csem is None:
            self.csem = {e: es.enter_context(nc.semaphore("c_" + e)) for e in engs}
            self.ccsem = es.enter_context(nc.semaphore("c_cc"))
        csem = self.csem
        rings = self.rings
        ccount = self.ccount
        dcount = self.dcount
        prev_end = dict(c={e: ccount[e] for e in engs}, d={e: dcount[e] for e in rings}, cc=self.cccount)
        lastc = {}
        for i, o in enumerate(ops):
            if not o["dma"]:
                lastc[o["eng"]] = i
        needed |= set(lastc.values())
        sig = {}
        prewait = {}
        for i, o in enumerate(ops):
            e = o["eng"]
            if o.get("cc"):
                self.cccount += 1
                sig[i] = (self.ccsem, self.cccount, 1)
                if self.cccount > 1:
                    prewait[i] = (self.ccsem, self.cccount - 1)
            elif o["dma"]:
                if e not in rings:
                    rings[e] = [es.enter_context(nc.semaphore("r_%s%d" % (e, k)))
                                for k in range(self.RING)]
                n = dcount[e]
                dcount[e] += 1
                sem = rings[e][n % self.RING]
                sig[i] = (sem, 16 * (n // self.RING + 1), 16)
                if n >= self.RING:
                    prewait[i] = (sem, 16 * (n // self.RING))
            elif i in needed:
                ccount[e] += 1
                sig[i] = (csem[e], ccount[e], 1)
        per = {e: [] for e in engs}
        for i, o in enumerate(ops):
            per[o["eng"]].append(i)
        self.stats = dict(n_ops=len(ops), ccount=ccount, dcount=dcount)

        nstage = self.nstage
        self.nstage += 1

        def run(e, engobj):
            waited = {}
            if nstage > 0:
                for e2 in engs:
                    if prev_end["c"][e2] > 0:
                        engobj.wait_ge(csem[e2], prev_end["c"][e2])
                for e2, n in prev_end["d"].items():
                    for k in range(self.RING):
                        cnt = (n - k + self.RING - 1) // self.RING if n > k else 0
                        if cnt > 0:
                            engobj.wait_ge(rings[e2][k], 16 * cnt)
                if prev_end["cc"] > 0:
                    engobj.wait_ge(self.ccsem, prev_end["cc"])
            for i in per[e]:
                o = ops[i]
                ws = [sig[j][:2] for j in deps[i]]
                if i in prewait:
                    ws.append(prewait[i])
                mx = {}
                for sem, val in ws:
                    key = id(sem)
                    if key not in mx or mx[key][1] < val:
                        mx[key] = (sem, val)
                for key, (sem, val) in mx.items():
                    if waited.get(key, 0) >= val:
                        continue
                    waited[key] = val
                    engobj.wait_ge(sem, val)
                ins = o["fn"](engobj)
                if i in sig:
                    sem, val, inc = sig[i]
                    ins.then_inc(sem, inc)
            if e in rings:
                n = dcount[e]
                for k in range(self.RING):
                    cnt = (n - k + self.RING - 1) // self.RING if n > k else 0
                    if cnt > 0:
                        engobj.wait_ge(rings[e][k], 16 * cnt)

        with nc.Block() as block:
            @block.tensor
            def _(t):
                run("pe", t)

            @block.scalar
            def _(t):
                run("act", t)

            @block.vector
            def _(t):
                run("dve", t)

            @block.gpsimd
            def _(t):
                run("pool", t)

            @block.sync
            def _(t):
                run("sp", t)
        self.ops = []


class Ctx:
    def __init__(self, nc, es, pre=None, tag=""):
        self.nc, self.es = nc, es
        self.n = 0
        self.pre = pre
        self.tag = tag

    def sb(self, shape, dt, name=None):
        self.n += 1
        t = self.es.enter_context(self.nc.sbuf_tensor("%s%s_%d" % (self.tag, name or "t", self.n), list(shape), dt))
        esz = 4 if dt in (F32, I32) else 2
        nbytes = int(np.prod(shape[1:])) * esz
        alloc = (nbytes + 31) // 32 * 32
        if alloc % 64 != 0:
            self.n += 1
            self.es.enter_context(self.nc.sbuf_tensor("%spad_%d" % (self.tag, self.n), [128, 8], F32))
        return t

    def ps(self, shape, dt, name=None):
        self.n += 1
        return self.es.enter_context(self.nc.psum_tensor("%s%s_%d" % (self.tag, name or "p", self.n), list(shape), dt))

    def dram(self, name, shape, dt, kind):
        if self.pre is not None:
            h = self.pre[name]
            assert list(h.shape) == list(shape), (name, h.shape, shape)
            return h
        return self.nc.dram_tensor(name, list(shape), dt, kind=kind)


class Rot:
    def __init__(self, items):
        self.items = items
        self.i = 0

    def next(self):
        it = self.items[self.i % len(self.items)]
        self.i += 1
        return it


class Env:
    def __init__(self):
        self.nc = bass.Bass("TRN2", target_bir_lowering=False)
        self.es = ExitStack()
        self.P = Prog(self.nc, self.es)
        self.nstage = 0


def _begin(env, pre=None):
    if env is None:
        nc = bass.Bass("TRN2", target_bir_lowering=False)
        es = ExitStack()
        return nc, es, Ctx(nc, es), Prog(nc, es)
    env.nstage += 1
    es = ExitStack()
    return env.nc, es, Ctx(env.nc, es, pre=pre, tag="s%d_" % env.nstage), env.P


def run_spmd(nc, in_maps):
    res = run_bass_kernel_spmd(nc, in_maps, core_ids=list(range(NCORES)))
    return res.results


def ACT(P, out, in_, func, r, w, **kw):
    P.op("act", lambda e: e.activation(out=out, in_=in_, func=func, **kw), r, w)


def TS(P, eng, out, in0, s1, s2, op0, op1, r, w):
    if op1 is None:
        P.op(eng, lambda e: e.tensor_scalar(out=out, in0=in0, scalar1=s1, scalar2=None, op0=op0), r, w)
    else:
        P.op(eng, lambda e: e.tensor_scalar(out=out, in0=in0, scalar1=s1, scalar2=s2, op0=op0, op1=op1), r, w)


def TT(P, eng, out, in0, in1, op, r, w):
    P.op(eng, lambda e: e.tensor_tensor(out=out, in0=in0, in1=in1, op=op), r, w)


def STT(P, eng, out, in0, scalar, in1, op0, op1, r, w):
    P.op(eng, lambda e: e.scalar_tensor_tensor(out=out, in0=in0, scalar=scalar, in1=in1, op0=op0, op1=op1), r, w)


def CP(P, eng, out, in_, r, w):
    if eng == "act":
        P.op(eng, lambda e: e.copy(out=out, in_=in_), r, w)
    else:
        P.op(eng, lambda e: e.tensor_copy(out=out, in_=in_), r, w)


def RSUM(P, eng, out, in_, r, w, axis=None):
    ax = axis if axis is not None else AX.X
    P.op(eng, lambda e: e.reduce_sum(out=out, in_=in_, axis=ax), r, w)


def RMAX(P, eng, out, in_, r, w, axis=None):
    ax = axis if axis is not None else AX.X
    P.op(eng, lambda e: e.reduce_max(out=out, in_=in_, axis=ax), r, w)


def MM(P, out, lhsT, rhs, start, stop, r, w):
    P.op("pe", lambda e: e.matmul(out, lhsT, rhs, start=start, stop=stop), r, w)


def TR(P, out, in_, ident, r, w):
    P.op("pe", lambda e: e.transpose(out, in_, ident), r, w)


def DMA(P, eng, out, in_, r, w):
    P.dma(eng, lambda e: e.dma_start(out=out, in_=in_), r, w)


def RECIP(P, eng, out, in_, r, w):
    P.op(eng, lambda e: e.reciprocal(out=out, in_=in_), r, w)


def MEMSET(P, eng, ap, val, w):
    P.op(eng, lambda e: e.memset(ap, val), (), w)


TP = 1024
HALO = 128
NTP = TP + 2 * HALO
NPASS = TPC // TP


def build_stageA(phases=("fm", "tm", "conv", "mla"), env=None, pre=None):
    nc, es, C, P = _begin(env, pre)
    with es:
        xe = C.dram("xe", [TPC + 2 * HALO, D], F32, "ExternalInput")
        w_in = C.dram("w_in", [D, INW], F32, "ExternalInput")
        gmix = C.dram("gmix", [128, 8], F32, "ExternalInput")
        identb = C.dram("identb", [128, 128], BF16, "ExternalInput")
        convp = C.dram("convp", [128, 4, 34], F32, "ExternalInput")
        gcq = C.dram("gcq", [128, 3], F32, "ExternalInput")
        gckv = C.dram("gckv", [128, 2], F32, "ExternalInput")
        w_uq = C.dram("w_uq", [384, 768], F32, "ExternalInput")
        w_ukv = C.dram("w_ukv", [256, 1024], F32, "ExternalInput")
        gqk = C.dram("gqk", [96, 2], F32, "ExternalInput")
        ropeT = C.dram("ropeT", [96, 2, TPC], F32, "ExternalInput")
        rmat = C.dram("rmat", [96, 96], BF16, "ExternalInput")
        onesf = C.dram("onesf", [128, 128], F32, "ExternalInput")

        QT = C.dram("QT", [512, TPC], BF16, "ExternalOutput")
        KT = C.dram("KT", [512, TPC], BF16, "ExternalOutput")
        Kt = C.dram("Kt", [4, TPC, 128], BF16, "ExternalOutput")
        Vt = C.dram("Vt", [4, TPC, 128], BF16, "ExternalOutput")
        OG = C.dram("OG", [4, TPC, 128], BF16, "ExternalOutput")
        G4 = C.dram("G4", [4, TPC, 4], F32, "ExternalOutput")
        GTS = C.dram("GTS", [TPC, 3072], BF16, "ExternalOutput")
        UT = C.dram("UT", [512, TPC], BF16, "ExternalOutput")
        MQ = C.dram("MQ", [8, 96, TPC], BF16, "ExternalOutput")
        MK = C.dram("MK", [8, 96, TPC], BF16, "ExternalOutput")
        MV = C.dram("MV", [8, 128, TPC // 128, 64], BF16, "ExternalOutput")

        w_v = w_in.ap().rearrange("(c p) n -> p c n", p=128)
        xnT = C.sb([128, 8, NTP], BF16, "xnT")
        f32t = Rot([(("f32t", i), C.sb([128, 512], F32, "f32t")) for i in range(4)])
        uT = C.sb([128, 4, NTP], BF16, "uT")
        cacc = [C.sb([128, 512], F32, "cacc") for g in range(4)]
        csq = [C.sb([128, 512], F32, "csq") for g in range(4)]
        cpar = C.sb([128, 4, 34], F32, "cpar")
        rope_sb = C.sb([96, 2, 512], F32, "rope")
        cql = C.sb([128, 3, 512], F32, "cql")
        ckl = C.sb([128, 2, 512], F32, "ckl")
        latsq = C.sb([128, 3, 512], BF16, "latsq")
        cqn = C.sb([128, 3, 512], BF16, "cqn")
        ckn = C.sb([128, 2, 512], BF16, "ckn")
        krt = C.sb([128, 512], F32, "krt")
        hx = Rot([(("hx", i), C.sb([128, 512], F32, "hx")) for i in range(2)])
        hsq = Rot([(("hsq", i), C.sb([128, 512], BF16, "hsq")) for i in range(2)])
        hxg = Rot([(("hxg", i), C.sb([128, 512], BF16, "hxg")) for i in range(2)])
        wuqb = C.sb([128, 3, 768], BF16, "wuqb")
        wukvb = C.sb([128, 2, 1024], BF16, "wukvb")
        wkrp = C.sb([128, 8, 128], BF16, "wkrp")
        gqk_sb = C.sb([96, 2], F32, "gqk")
        rmat_sb = C.sb([96, 96], BF16, "rmat")
        gcq_sb = C.sb([128, 3], F32, "gcqs")
        gckv_sb = C.sb([128, 2], F32, "gckvs")
        ident = C.sb([128, 128], BF16, "ident")
        gm = C.sb([128, 8], F32, "gm")
        onesb = C.sb([128, 128], BF16, "onesb")
        ones32 = C.sb([128, 128], F32, "ones32")
        xin = Rot([(("xin", i), C.sb([128, D], F32, "xin")) for i in range(2)])
        sqj = C.sb([128, D], F32, "sqj")
        xs = Rot([(("xs", i), C.sb([128, D], BF16, "xs")) for i in range(2)])
        stat = Rot([(("stat", i), C.sb([128, 2], F32, "stat")) for i in range(2)])
        wst = Rot([(("wst", i), C.sb([128, 8, 512], F32, "wst")) for i in range(2)])
        wb = Rot([(("wb", i), C.sb([128, 8, 512], BF16, "wb")) for i in range(2)])
        ob = Rot([(("ob", i), C.sb([128, 512], BF16, "ob")) for i in range(4)])
        g4sb = C.sb([128, NTP // 128, 16], F32, "g4sb")
        pacc = Rot([(("pacc", i), C.ps([128, 512], F32, "pacc")) for i in range(5)])
        ptr = Rot([(("ptr", i), C.ps([128, 1024], BF16, "ptr")) for i in range(2)])

        DMA(P, "sp", cpar[:], convp[:, :, :], [], ["cpar"])
        DMA(P, "sp", gqk_sb[:], gqk[:, :], [], ["gqk"])
        DMA(P, "sp", rmat_sb[:], rmat[:, :], [], ["rmat"])
        DMA(P, "sp", gcq_sb[:], gcq[:, :], [], ["gcqs"])
        DMA(P, "sp", gckv_sb[:], gckv[:, :], [], ["gckvs"])
        DMA(P, "sp", gm[:], gmix[:, :], [], ["gm"])
        if "mla" in phases:
            sk0, st0 = wst.items[0]
            st0f = st0[:].rearrange("p a b -> p (a b)")
            DMA(P, "sp", st0f[:, 0:2304].rearrange("p (a b) -> p a b", a=3),
                w_uq.ap().rearrange("(c p) n -> p c n", p=128), [], [sk0])
            for j in range(3):
                TS(P, "dve", wuqb[:, j, :], st0f[:, j * 768:(j + 1) * 768],
                   gcq_sb[:, j:j + 1], None, ALU.mult, None, [sk0, "gcqs"], ["wuqb"])
            sk1, st1 = wst.items[1]
            st1f = st1[:].rearrange("p a b -> p (a b)")
            DMA(P, "sp", st1f[:, 0:2048].rearrange("p (a b) -> p a b", a=2),
                w_ukv.ap().rearrange("(c p) n -> p c n", p=128), [], [sk1])
            for j in range(2):
                src = st1f[:, j * 1024:(j + 1) * 1024].rearrange("p (h x) -> p h x", h=8)
                TS(P, "dve", wukvb[:, j, 0:512].rearrange("p (h x) -> p h x", h=8), src[:, :, 0:64],
                   gckv_sb[:, j:j + 1], None, ALU.mult, None, [sk1, "gckvs"], ["wukvb"])
                TS(P, "dve", wukvb[:, j, 512:1024].rearrange("p (h x) -> p h x", h=8), src[:, :, 64:128],
                   gckv_sb[:, j:j + 1], None, ALU.mult, None, [sk1, "gckvs"], ["wukvb"])
            MEMSET(P, "pool", wkrp[:], 0.0, ["wkrp"])
            sk2_, st2_ = wst.items[0]
            DMA(P, "sp", st2_[:, :, 0:32], w_v[:, :, O_CKR:O_CKR + 32], [], [sk2_])
            for k in range(8):
                TS(P, "dve", wkrp[:, k, 64:96], st2_[:, k, 0:32], gm[:, k:k + 1], None, ALU.mult, None,
                   [sk2_, "gm"], ["wkrp"])
            MEMSET(P, "pool", krt[:], 0.0, ["krt"])
        DMA(P, "sp", ident[:], identb[:, :], [], ["ident"])
        DMA(P, "sp", gm[:], gmix[:, :], [], ["gm"])
        DMA(P, "sp", ones32[:], onesf[:, :], [], ["ones32"])
        CP(P, "dve", onesb[:], ones32[:], ["ones32"], ["onesb"])

        evac_i = [0]

        def load_w(c0, ncols):
            sk, st = wst.next()
            bk, bt = wb.next()
            DMA(P, "sp", st[:, :, 0:ncols], w_v[:, :, c0:c0 + ncols], [], [sk])
            for k in range(8):
                eng = ("dve", "pool")[k % 2]
                TS(P, eng, bt[:, k, 0:ncols], st[:, k, 0:ncols], gm[:, k:k + 1], None, ALU.mult, None,
                   [sk, "gm"], [(bk, k)])
            return [(bk, k) for k in range(8)], bt

        for ps_i in range(NPASS):
            t0 = ps_i * TP
            for t in range(NTP // 128):
                xk, xt = xin.next()
                sk2, stt = stat.next()
                xsk, xst = xs.next()
                pk, pt = ptr.next()
                r0 = t0 + t * 128
                DMA(P, "sp", xt[:], xe[r0:r0 + 128, :], [], [xk])
                ACT(P, sqj[:], xt[:], AF.Square, [xk], ["sqj"])
                RSUM(P, "dve", stt[:, 0:1], sqj[:], ["sqj"], [sk2])
                ACT(P, stt[:, 1:2], stt[:, 0:1], AF.Sqrt, [sk2], [sk2], scale=1.0 / D, bias=EPS)
                RECIP(P, "dve", stt[:, 1:2], stt[:, 1:2], [sk2], [sk2])
                ACT(P, xst[:], xt[:], AF.Copy, [xk, sk2], [xsk], scale=stt[:, 1:2])
                for k in range(8):
                    TR(P, pt[:, k * 128:(k + 1) * 128], xst[:, k * 128:(k + 1) * 128], ident[:],
                       [xsk, "ident"], [pk])
                CP(P, ("dve", "pool")[0], xnT[:, :, t * 128:(t + 1) * 128],
                   pt[:].rearrange("p (k t) -> p k t", k=8), [pk], [("xnT", t)])

            def xk_keys(tok0, ntok):
                return [("xnT", t) for t in range(tok0 // 128, (tok0 + ntok - 1) // 128 + 1)]

            if "fm" in phases:
                for (c0, dst) in ((O_AQ, QT), (O_AK, KT)):
                    wk, wt = load_w(c0, 512)
                    for cb in range(4):
                        for tb in range(TP // 512):
                            tk0 = HALO + tb * 512
                            ak, at = pacc.next()
                            for k in range(8):
                                MM(P, at[:, :], wt[:, k, cb * 128:(cb + 1) * 128], xnT[:, k, tk0:tk0 + 512],
                                   k == 0, k == 7, [wk[k]] + xk_keys(tk0, 512), [ak])
                            okk, ot = ob.next()
                            evac_i[0] += 1
                            CP(P, ("act", "dve")[evac_i[0] % 2], ot[:, :], at[:, :], [ak], [okk])
                            DMA(P, "sp", dst[cb * 128:(cb + 1) * 128, t0 + tb * 512:t0 + (tb + 1) * 512], ot[:, :],
                                [okk], [])

            if "tm" in phases:
                blocks = [(O_AK, Kt, 0, "copy"), (O_AV, Vt, 0, "copy"), (O_AO, OG, 0, "sig")]
                for j in range(6):
                    blocks.append((O_GTS + j * 512, GTS, j * 512, "sig"))
                for (c0, dst, dc0, mode) in blocks:
                    wk, wt = load_w(c0, 512)
                    for t in range(TP // 128):
                        tk0 = HALO + t * 128
                        ak, at = pacc.next()
                        for k in range(8):
                            MM(P, at[:, :], xnT[:, k, tk0:tk0 + 128], wt[:, k, :], k == 0, k == 7,
                               [wk[k]] + xk_keys(tk0, 128), [ak])
                        okk, ot = ob.next()
                        if mode == "sig":
                            ACT(P, ot[:, :], at[:, :], AF.Sigmoid, [ak], [okk])
                        else:
                            evac_i[0] += 1
                            CP(P, ("act", "dve")[evac_i[0] % 2], ot[:, :], at[:, :], [ak], [okk])
                        if dst is GTS:
                            DMA(P, "sp", dst[t0 + t * 128:t0 + (t + 1) * 128, dc0:dc0 + 512], ot[:, :], [okk], [])
                        else:
                            DMA(P, "sp", dst[:, t0 + t * 128:t0 + (t + 1) * 128, :].rearrange("h t d -> t h d"),
                                ot[:, :].rearrange("p (h d) -> p h d", h=4), [okk], [])
                wk, wt = load_w(O_AG, 16)
                for t in range(TP // 128):
                    tk0 = HALO + t * 128
                    ak, at = pacc.next()
                    for k in range(8):
                        MM(P, at[:, 0:16], xnT[:, k, tk0:tk0 + 128], wt[:, k, 0:16], k == 0, k == 7,
                           [wk[k]] + xk_keys(tk0, 128), [ak])
                    CP(P, "dve", g4sb[:, t, :].rearrange("p (h g) -> p h g", h=4), at[:, 0:16].rearrange("p (g h) -> p h g", h=4),
                       [ak], [("g4", t)])
                for h in range(4):
                    DMA(P, "sp", G4[h, t0:t0 + TP, :].rearrange("(t p) g -> p t g", p=128), g4sb[:, 0:TP // 128, h * 4:(h + 1) * 4],
                        [("g4", t) for t in range(TP // 128)], [])

            if "conv" in phases:
                wak, wat = load_w(O_GLU, 512)
                wgk, wgt = load_w(O_GLU + 512, 512)
                blks = [(b0, min(512, NTP - b0)) for b0 in range(0, NTP, 512)]
                for g in range(4):
                    for (b0, bn) in blks:
                        ak, at = pacc.next()
                        gk, gt = pacc.next()
                        for k in range(8):
                            MM(P, at[:, 0:bn], wat[:, k, g * 128:(g + 1) * 128], xnT[:, k, b0:b0 + bn],
                               k == 0, k == 7, [wak[k]] + xk_keys(b0, bn), [ak])
                        for k in range(8):
                            MM(P, gt[:, 0:bn], wgt[:, k, g * 128:(g + 1) * 128], xnT[:, k, b0:b0 + bn],
                               k == 0, k == 7, [wgk[k]] + xk_keys(b0, bn), [gk])
                        fk, ft = f32t.next()
                        ACT(P, ft[:, 0:bn], gt[:, 0:bn], AF.Sigmoid, [gk], [fk])
                        TT(P, "dve", uT[:, g, b0:b0 + bn], at[:, 0:bn], ft[:, 0:bn], ALU.mult, [ak, fk],
                           [("uT", g, b0 // 512)])
                for tb in range(TP // 512):
                    c0 = HALO + tb * 512 - 15
                    ukeys = lambda g: [("uT", g, j) for j in range(c0 // 512, (c0 + 542 - 1) // 512 + 1)]
                    for g in range(4):
                        eng = "dve"
                        ck = ("cacc", g)
                        ca = cacc[g]
                        TS(P, eng, ca[:, :], uT[:, g, c0:c0 + 512], cpar[:, g, 0:1], cpar[:, g, 31:32],
                           ALU.mult, ALU.add, ukeys(g) + ["cpar"], [ck])
                        for k in range(1, 31):
                            STT(P, eng, ca[:, :], uT[:, g, c0 + k:c0 + k + 512], cpar[:, g, k:k + 1], ca[:, :],
                                ALU.mult, ALU.add, ukeys(g) + ["cpar", ck], [ck])
                    mk, mt = pacc.next()
                    for g in range(4):
                        MM(P, mt[:, :], ones32[:, :], cacc[g][:, :], g == 0, g == 3, ["ones32", ("cacc", g)], [mk])
                    for g in range(4):
                        STT(P, "dve", cacc[g][:, :], mt[:, :], -1.0 / 512, cacc[g][:, :], ALU.mult, ALU.add,
                            [mk, ("cacc", g)], [("cacc", g)])
                        ACT(P, csq[g][:, :], cacc[g][:, :], AF.Square, [("cacc", g)], [("csq", g)])
                    vk, vt = pacc.next()
                    for g in range(4):
                        MM(P, vt[:, :], ones32[:, :], csq[g][:, :], g == 0, g == 3, ["ones32", ("csq", g)], [vk])
                    fk, ft = f32t.next()
                    ACT(P, ft[:, :], vt[:, :], AF.Sqrt, [vk], [fk], scale=1.0 / 512, bias=EPS)
                    RECIP(P, "dve", ft[:, :], ft[:, :], [fk], [fk])
                    for g in range(4):
                        TT(P, "dve", csq[g][:, :], cacc[g][:, :], ft[:, :], ALU.mult, [("cacc", g), fk], [("csq", g)])
                        TS(P, "pool", csq[g][:, :], csq[g][:, :], cpar[:, g, 32:33], cpar[:, g, 33:34],
                           ALU.mult, ALU.add, [("csq", g), "cpar"], [("csq", g)])
                        okk, ot = ob.next()
                        ACT(P, ot[:, :], csq[g][:, :], AF.Silu, [("csq", g)], [okk])
                        DMA(P, "sp", UT[g * 128:(g + 1) * 128, t0 + tb * 512:t0 + (tb + 1) * 512], ot[:, :], [okk], [])

            if "mla" in phases:
                wqk, wqt = load_w(O_CQ, 384)
                wkk, wkt = load_w(O_CKV, 288)
                for tb in range(TP // 512):
                    tk0 = HALO + tb * 512
                    g0 = t0 + tb * 512
                    DMA(P, "sp", rope_sb[:, :, :], ropeT[:, :, g0:g0 + 512], [], ["rope"])
                    for (wk_, wt_, nblk, lat, latn, lkey, dim) in ((wqk, wqt, 3, cql, cqn, "cq", 384.0),
                                                                 (wkk, wkt, 2, ckl, ckn, "ckv", 256.0)):
                        for j in range(nblk):
                            ak, at = pacc.next()
                            for k in range(8):
                                MM(P, at[:, :], wt_[:, k, j * 128:(j + 1) * 128], xnT[:, k, tk0:tk0 + 512],
                                   k == 0, k == 7, [wk_[k]] + xk_keys(tk0, 512), [ak])
                            CP(P, "dve", lat[:, j, :], at[:, :], [ak], [(lkey, j)])
                            ACT(P, latsq[:, j, :], lat[:, j, :], AF.Square, [(lkey, j)], [(lkey + "sq", j)])
                        sk_, st_ = pacc.next()
                        for j in range(nblk):
                            MM(P, st_[:, :], onesb[:, :], latsq[:, j, :], j == 0, j == nblk - 1,
                               ["onesb", (lkey + "sq", j)], [sk_])
                        fk, ft = f32t.next()
                        ACT(P, ft[:, :], st_[:, :], AF.Sqrt, [sk_], [fk], scale=1.0 / dim, bias=EPS)
                        RECIP(P, "dve", ft[:, :], ft[:, :], [fk], [fk])
                        for j in range(nblk):
                            if "dbg5" in phases:
                                TT(P, "dve", lat[:, j, :], lat[:, j, :], ft[:, :], ALU.mult,
                                   [(lkey, j), fk], [(lkey, j)])
                                CP(P, "act", latn[:, j, :], lat[:, j, :], [(lkey, j)], [(lkey + "n", j)])
                            else:
                                TT(P, "dve", latn[:, j, :], lat[:, j, :], ft[:, :], ALU.mult,
                                   [(lkey, j), fk], [(lkey + "n", j)])
                    if "nokr" not in phases:
                        ak, at = pacc.next()
                        for k in range(8):
                            MM(P, at[:, :], wkrp[:, k, :], xnT[:, k, tk0:tk0 + 512], k == 0, k == 7,
                               ["wkrp"] + xk_keys(tk0, 512), [ak])
                        CP(P, "act", krt[0:96, :], at[0:96, :], [ak], ["krt"])
                    cqn_keys = [("cqn", j) for j in range(3)]
                    ckn_keys = [("ckvn", j) for j in range(2)]
                    for h in (range(8) if "nomlah" not in phases else []):
                        for which in ("q", "k"):
                            ak, at = pacc.next()
                            xk_, xt_ = hx.next()
                            if which == "q":
                                for j in range(3):
                                    MM(P, at[0:96, :], wuqb[:, j, h * 96:(h + 1) * 96], cqn[:, j, :], j == 0, j == 2,
                                       ["wuqb"] + cqn_keys, [ak])
                                CP(P, "act", xt_[0:96, :], at[0:96, :], [ak], [xk_])
                            else:
                                for j in range(2):
                                    MM(P, at[0:64, :], wukvb[:, j, h * 64:(h + 1) * 64], ckn[:, j, :], j == 0, j == 1,
                                       ["wukvb"] + ckn_keys, [ak])
                                CP(P, "act", xt_[0:64, :], at[0:64, :], [ak], [xk_])
                                CP(P, "pool", xt_[64:96, :], krt[64:96, :], ["krt"], [xk_])
                            sqk, sqt = hsq.next()
                            ACT(P, sqt[0:96, :], xt_[0:96, :], AF.Square, [xk_], [sqk])
                            sk_, st_ = pacc.next()
                            MM(P, st_[0:96, :], onesb[0:96, 0:96], sqt[0:96, :], True, True, ["onesb", sqk], [sk_])
                            fk, ft = f32t.next()
                            ACT(P, ft[0:96, :], st_[0:96, :], AF.Sqrt, [sk_], [fk], scale=1.0 / 96, bias=EPS)
                            RECIP(P, "dve", ft[0:96, :], ft[0:96, :], [fk], [fk])
                            gcol = 0 if which == "q" else 1
                            xgk, xgt = hxg.next()
                            TS(P, "dve", xgt[0:96, :], xt_[0:96, :], gqk_sb[0:96, gcol:gcol + 1], None, ALU.mult, None,
                               [xk_, "gqk"], [xgk])
                            rk, rt = pacc.next()
                            MM(P, rt[0:96, :], rmat_sb[0:96, 0:96], xgt[0:96, :], True, True, ["rmat", xgk], [rk])
                            t1k, t1 = f32t.next()
                            TT(P, "pool", t1[0:96, :], xgt[0:96, :], rope_sb[:, 0, :], ALU.mult, [xgk, "rope"], [t1k])
                            t2k, t2 = f32t.next()
                            TT(P, "dve", t2[0:96, :], rt[0:96, :], rope_sb[:, 1, :], ALU.mult, [rk, "rope"], [t2k])
                            TT(P, "pool", t1[0:96, :], t1[0:96, :], t2[0:96, :], ALU.add, [t1k, t2k], [t1k])
                            okk, ot = ob.next()
                            TT(P, "dve", ot[0:96, :], t1[0:96, :], ft[0:96, :], ALU.mult, [t1k, fk], [okk])
                            dst = MQ if which == "q" else MK
                            DMA(P, "sp", dst[h, :, g0:g0 + 512], ot[0:96, :], [okk], [])
                    if "dupgrp" in phases:
                        for rep in range(2):
                            ak, at = pacc.next()
                            for k in range(8):
                                MM(P, at[:, :], wkt[:, k, 0:128], xnT[:, k, tk0:tk0 + 512],
                                   k == 0, k == 7, [wkk[k]] + xk_keys(tk0, 512), [ak])
                    for t in (range(4 if "v_one" not in phases else 1) if "nomlav" not in phases else []):
                        ak, at = pacc.next()
                        for j in range(2):
                            MM(P, at[:, :], (xnT[:, j, tk0 + t * 128:tk0 + (t + 1) * 128] if "dbg1" in phases else ckn[:, j, t * 128:(t + 1) * 128]),
                               (wkt[:, j, :] if "dbg2" in phases else (wukvb[:, j, 0:512] if "dbg4" in phases else wukvb[:, j, 512:1024])), j == 0, j == 1,
                               ["wukvb"] + ckn_keys, [ak])
                        okk, ot = ob.next()
                        if "v_noevac" in phases:
                            continue
                        CP(P, "dve", ot[:, :], at[:, :], [ak], [okk])
                        if "v_nodma" in phases:
                            continue
                        DMA(P, "sp", MV[:, :, (g0 + t * 128) // 128, :].rearrange("h p d -> p h d"),
                            ot[:, :].rearrange("p (h d) -> p h d", h=8), [okk], [])

        P.emit()
        stats = P.stats
    return nc, stats


def _gain_cols(g, nch):
    return np.ascontiguousarray(g.reshape(nch, 128).T)


def rope_tables():
    pos = np.arange(S, dtype=np.float32)
    inv = (10000.0 ** (-np.arange(0, 32, 2, dtype=np.float32) / np.float32(32))).astype(np.float32)
    ang = (pos[:, None] * inv[None, :]).astype(np.float32)
    c = np.cos(ang.astype(np.float64)).astype(np.float32)
    s = np.sin(ang.astype(np.float64)).astype(np.float32)
    CT = np.ones((96, S), np.float32)
    ST = np.zeros((96, S), np.float32)
    CT[64:80] = c.T
    CT[80:96] = c.T
    ST[64:80] = s.T
    ST[80:96] = s.T
    return CT, ST


def consts():
    R = np.zeros((96, 96), np.float32)
    for j in range(16):
        R[80 + j, 64 + j] = -1.0
        R[64 + j, 80 + j] = 1.0
    return dict(identb=np.eye(128, dtype=np.float32).astype(NPBF), rmat=R.astype(NPBF),
                onesf=np.ones((128, 128), np.float32))


def stageA_inmaps(x, prm, l):
    CT, ST = rope_tables()
    cst = consts()
    convp = np.zeros((128, 4, 34), np.float32)
    cw = prm["conv_w"][l]
    convp[:, :, 0:31] = cw.T.reshape(4, 128, 31).transpose(1, 0, 2)
    convp[:, :, 31] = prm["conv_b"][l].reshape(4, 128).T
    convp[:, :, 32] = prm["conv_ln_g"][l].reshape(4, 128).T
    convp[:, :, 33] = prm["conv_ln_b"][l].reshape(4, 128).T
    maps = []
    for c in range(NCORES):
        b, q = c // 4, c % 4
        s0 = q * TPC
        xe = np.zeros((TPC + 2 * HALO, D), np.float32)
        lo, hi = max(0, s0 - HALO), min(S, s0 + TPC + HALO)
        xe[lo - (s0 - HALO):hi - (s0 - HALO)] = x[b, lo:hi]
        rope = np.stack([CT[:, s0:s0 + TPC], ST[:, s0:s0 + TPC]], axis=1)
        maps.append(dict(
            xe=xe, w_in=prm["w_in"][l], gmix=_gain_cols(prm["mix_norm_g"][l], 8),
            identb=cst["identb"], convp=convp, gcq=_gain_cols(prm["cq_norm_g"][l], 3),
            gckv=_gain_cols(prm["ckv_norm_g"][l], 2), w_uq=prm["w_uq"][l], w_ukv=prm["w_ukv"][l],
            gqk=np.ascontiguousarray(np.stack([prm["q_norm_g"][l], prm["k_norm_g"][l]], axis=1)),
            ropeT=np.ascontiguousarray(rope), rmat=cst["rmat"], onesf=cst["onesf"]))
    return maps


def build_mlstm(nch=S // 128, env=None, pre=None):
    nc, es, C, P = _begin(env, pre)
    ns = nch * 128
    lnscale = math.log(128.0 ** -0.5)
    with es:
        qTd = C.dram("qT", [128, ns], BF16, "ExternalInput")
        ktd = C.dram("kt", [ns, 128], BF16, "ExternalInput")
        vtd = C.dram("vt", [ns, 128], BF16, "ExternalInput")
        g4d = C.dram("g4", [ns, 4], F32, "ExternalInput")
        bifd = C.dram("bif", [128, 4], F32, "ExternalInput")
        ogd = C.dram("og", [ns, 128], BF16, "ExternalInput")
        gAd = C.dram("gA", [128, 128], F32, "ExternalInput")
        cmat = C.dram("cmat", [128, 6, 128], F32, "ExternalInput")
        identbd = C.dram("identb", [128, 128], BF16, "ExternalInput")
        HT = C.dram("HT", [128, ns], BF16, "ExternalOutput")

        qT = C.sb([128, ns], BF16, "qT")
        kt = C.sb([128, nch, 128], BF16, "kt")
        vt = C.sb([128, nch, 129], BF16, "vt")
        hacc = C.sb([128, nch, 128], F32, "hacc")
        g4 = C.sb([128, nch, 4], F32, "g4")
        bif = C.sb([128, 4], F32, "bif")
        nbif = C.sb([128, 4], F32, "nbif")
        gA = C.sb([128, 128], F32, "gA")
        cm = C.sb([128, 6, 128], F32, "cm")
        identb = C.sb([128, 128], BF16, "identb")
        gt = {n: C.sb([128, nch], F32, n) for n in ("lf", "ib", "bcum", "gtot", "biasS", "wint", "wk", "dec", "tmp")}
        Cf = C.sb([128, 129], F32, "Cf")
        Cb = C.sb([128, 129], BF16, "Cb")
        LF = Rot([(("LF", i), C.sb([128, 128], F32, "LF")) for i in range(2)])
        Dm = Rot([(("Dm", i), C.sb([128, 128], F32, "Dm")) for i in range(2)])
        kTc = Rot([(("kTc", i), C.sb([128, 128], BF16, "kTc")) for i in range(2)])
        SD = Rot([(("SD", i), C.sb([128, 128], BF16, "SD")) for i in range(2)])
        isb = Rot([(("isb", i), C.sb([128, 129], F32, "isb")) for i in range(2)])
        num = Rot([(("num", i), C.sb([128, 129], F32, "num")) for i in range(2)])
        dn = Rot([(("dn", i), C.sb([128, 2], F32, "dn")) for i in range(2)])
        Vw = Rot([(("Vw", i), C.sb([128, 129], BF16, "Vw")) for i in range(2)])
        ogt = Rot([(("ogt", i), C.sb([128, 128], BF16, "ogt")) for i in range(2)])
        hn = Rot([(("hn", i), C.sb([128, 128], F32, "hn")) for i in range(2)])
        hb = Rot([(("hb", i), C.sb([128, 128], BF16, "hb")) for i in range(2)])
        hT = Rot([(("hT", i), C.sb([128, 128], BF16, "hT")) for i in range(2)])
        sq = C.sb([128, 128], F32, "sq")
        pA = Rot([(("pA", i), C.ps([128, 512], F32, "pA")) for i in range(6)])
        pB = Rot([(("pB", i), C.ps([128, 1024], BF16, "pB")) for i in range(2)])

        DMA(P, "sp", qT[:, :], qTd[:, :], [], ["qT"])
        DMA(P, "sp", kt[:, :, :], ktd.ap().rearrange("(c p) d -> p c d", p=128), [], ["kt"])
        DMA(P, "sp", vt[:, :, 0:128], vtd.ap().rearrange("(c p) d -> p c d", p=128), [], ["vt"])
        MEMSET(P, "pool", vt[:, :, 128:129], 1.0, ["vt1"])
        DMA(P, "sp", g4[:, :, :], g4d.ap().rearrange("(c p) g -> p c g", p=128), [], ["g4"])
        DMA(P, "sp", bif[:, :], bifd[:, :], [], ["bif"])
        DMA(P, "sp", gA[:, :], gAd[:, :], [], ["gA"])
        DMA(P, "sp", cm[:, :, :], cmat[:, :, :], [], ["cm"])
        DMA(P, "sp", identb[:, :], identbd[:, :], [], ["identb"])
        TS(P, "dve", nbif[:, :], bif[:, :], -1.0, None, ALU.mult, None, ["bif"], ["nbif"])
        ident32 = cm[:, 4, :]
        ones32 = cm[:, 5, :]
        for d in range(2):
            Ud = cm[:, d, :]
            NEGd = cm[:, 2 + d, :]
            ic, fc = 2 * d, 2 * d + 1
            ACT(P, gt["tmp"][:, :], g4[:, :, fc], AF.Exp, ["g4", "nbif"], ["tmp"], scale=-1.0, bias=nbif[:, fc:fc + 1])
            ACT(P, gt["tmp"][:, :], gt["tmp"][:, :], AF.Ln, ["tmp"], ["tmp"], bias=1.0)
            TS(P, "dve", gt["lf"][:, :], gt["tmp"][:, :], -1.0, None, ALU.mult, None, ["tmp"], ["lf"])
            TS(P, "dve", gt["ib"][:, :], g4[:, :, ic], bif[:, ic:ic + 1], lnscale, ALU.add, ALU.add, ["g4", "bif"], ["ib"])
            bk, bp = pA.next()
            MM(P, bp[:, 0:nch], Ud, gt["lf"][:, :], True, True, ["cm", "lf"], [bk])
            CP(P, "dve", gt["bcum"][:, :], bp[:, 0:nch], [bk], ["bcum"])
            gk, gp = pA.next()
            MM(P, gp[:, 0:nch], ones32, gt["lf"][:, :], True, True, ["cm", "lf"], [gk])
            CP(P, "dve", gt["gtot"][:, :], gp[:, 0:nch], [gk], ["gtot"])
            TT(P, "dve", gt["biasS"][:, :], gt["ib"][:, :], gt["bcum"][:, :], ALU.subtract, ["ib", "bcum"], ["biasS"])
            ACT(P, gt["wint"][:, :], gt["bcum"][:, :], AF.Exp, ["bcum"], ["wint"])
            TT(P, "dve", gt["tmp"][:, :], gt["biasS"][:, :], gt["gtot"][:, :], ALU.add, ["biasS", "gtot"], ["tmp"])
            ACT(P, gt["wk"][:, :], gt["tmp"][:, :], AF.Exp, ["tmp"], ["wk"])
            ACT(P, gt["dec"][:, :], gt["gtot"][:, :], AF.Exp, ["gtot"], ["dec"])
            MEMSET(P, "dve", Cf[:, :], 0.0, ["Cf"])
            MEMSET(P, "pool", Cb[:, :], 0.0, ["Cb"])
            order = range(nch) if d == 0 else range(nch - 1, -1, -1)
            for c in order:
                lk, lt = LF.next()
                TS(P, "pool", lt[:, :], ones32, gt["lf"][:, c:c + 1], None, ALU.mult, None, ["cm", "lf"], [lk])
                dk, dp = pA.next()
                MM(P, dp[:, 0:128], lt[:, :], Ud, True, False, [lk, "cm"], [dk])
                MM(P, dp[:, 0:128], ident32, NEGd, False, True, ["cm"], [dk])
                mk_, mt_ = Dm.next()
                ACT(P, mt_[:, :], dp[:, 0:128], AF.Exp, [dk, "biasS"], [mk_], bias=gt["biasS"][:, c:c + 1])
                tk, tp = pB.next()
                TR(P, tp[:, 0:128], kt[:, c, :], identb[:, :], ["kt", "identb"], [tk])
                kck, kct = kTc.next()
                CP(P, "dve", kct[:, :], tp[:, 0:128], [tk], [kck])
                sk, sp_ = pA.next()
                MM(P, sp_[:, 0:128], kct[:, :], qT[:, c * 128:(c + 1) * 128], True, True, [kck, "qT"], [sk])
                sdk, sdt = SD.next()
                TT(P, "dve", sdt[:, :], sp_[:, 0:128], mt_[:, :], ALU.mult, [sk, mk_], [sdk])
                nk_, np_ = pA.next()
                MM(P, np_[:, 0:129], sdt[:, :], vt[:, c, :], True, True, [sdk, "vt", "vt1"], [nk_])
                ik, ip = pA.next()
                MM(P, ip[:, 0:129], qT[:, c * 128:(c + 1) * 128], Cb[:, :], True, True, ["qT", "Cb"], [ik])
                isk, ist = isb.next()
                ACT(P, ist[:, :], ip[:, 0:129], AF.Copy, [ik, "wint"], [isk], scale=gt["wint"][:, c:c + 1])
                nmk, nmt = num.next()
                TT(P, "dve", nmt[:, :], np_[:, 0:129], ist[:, :], ALU.add, [nk_, isk], [nmk])
                dnk, dnt = dn.next()
                ACT(P, dnt[:, 0:1], nmt[:, 128:129], AF.Abs, [nmk], [dnk])
                TS(P, "dve", dnt[:, 0:1], dnt[:, 0:1], 1.0, None, ALU.max, None, [dnk], [dnk])
                RECIP(P, "dve", dnt[:, 1:2], dnt[:, 0:1], [dnk], [dnk])
                if d == 0:
                    TS(P, "dve", hacc[:, c, :], nmt[:, 0:128], dnt[:, 1:2], None, ALU.mult, None, [nmk, dnk], [("hacc", c)])
                else:
                    STT(P, "dve", hacc[:, c, :], nmt[:, 0:128], dnt[:, 1:2], hacc[:, c, :], ALU.mult, ALU.add,
                        [nmk, dnk, ("hacc", c)], [("hacc", c)])
                vwk, vwt = Vw.next()
                TS(P, "pool", vwt[:, :], vt[:, c, :], gt["wk"][:, c:c + 1], None, ALU.mult, None, ["vt", "vt1", "wk"], [vwk])
                ck, cp_ = pA.next()
                MM(P, cp_[:, 0:129], kt[:, c, :], vwt[:, :], True, True, ["kt", vwk], [ck])
                STT(P, "dve", Cf[:, :], Cf[:, :], gt["dec"][:, c:c + 1], cp_[:, 0:129], ALU.mult, ALU.add,
                    ["Cf", "dec", ck], ["Cf"])
                CP(P, "act", Cb[:, :], Cf[:, :], ["Cf"], ["Cb"])
        for c in range(nch):
            ogk, ogt_ = ogt.next()
            DMA(P, "sp", ogt_[:, :], ogd[c * 128:(c + 1) * 128, :], [], [ogk])
            dnk, dnt = dn.next()
            ACT(P, sq[:, :], hacc[:, c, :], AF.Square, [("hacc", c)], ["sq"])
            RSUM(P, "dve", dnt[:, 0:1], sq[:, :], ["sq"], [dnk])
            ACT(P, dnt[:, 1:2], dnt[:, 0:1], AF.Sqrt, [dnk], [dnk], scale=1.0 / 128, bias=EPS)
            RECIP(P, "dve", dnt[:, 1:2], dnt[:, 1:2], [dnk], [dnk])
            hk, ht = hn.next()
            STT(P, "dve", ht[:, :], hacc[:, c, :], dnt[:, 1:2], gA[:, :], ALU.mult, ALU.mult, [("hacc", c), dnk, "gA"], [hk])
            hbk, hbt = hb.next()
            TT(P, "pool", hbt[:, :], ht[:, :], ogt_[:, :], ALU.mult, [hk, ogk], [hbk])
            tk, tp = pB.next()
            TR(P, tp[:, 0:128], hbt[:, :], identb[:, :], [hbk, "identb"], [tk])
            htk, htt = hT.next()
            CP(P, "act", htt[:, :], tp[:, 0:128], [tk], [htk])
            DMA(P, "sp", HT[:, c * 128:(c + 1) * 128], htt[:, :], [htk], [])
        P.emit()
        stats = P.stats
    return nc, stats


def mlstm_consts():
    s_ = np.arange(128)[:, None]
    t_ = np.arange(128)[None, :]
    U = (s_ <= t_).astype(np.float32)
    cm = np.zeros((128, 6, 128), np.float32)
    cm[:, 0] = U
    cm[:, 1] = U.T
    cm[:, 2] = np.where(s_ <= t_, 0.0, -30000.0)
    cm[:, 3] = np.where(s_ >= t_, 0.0, -30000.0)
    cm[:, 4] = np.eye(128)
    cm[:, 5] = 1.0
    return dict(cmat=cm, identb=np.eye(128, dtype=np.float32).astype(NPBF))


def build_merge(ntile=TPC // 128, env=None, pre=None):
    nc, es, C, P = _begin(env, pre)
    nt = ntile * 128
    with es:
        xd = C.dram("x", [nt, D], F32, "ExternalInput")
        srcs = [C.dram(n, [512, nt], BF16, "ExternalInput") for n in ("HT", "UT", "OT")]
        gtsd = C.dram("GTS", [nt, 3072], BF16, "ExternalInput")
        wds = [C.dram(n, [512, D], F32, "ExternalInput") for n in ("w_a", "w_b", "w_c")]
        wod = C.dram("w_o", [D, D], F32, "ExternalInput")
        gfd = C.dram("gffn", [128, 8], F32, "ExternalInput")
        wrd = C.dram("w_r", [D, 16], F32, "ExternalInput")
        identbd = C.dram("identb", [128, 128], BF16, "ExternalInput")
        ident32d = C.dram("ident32", [128, 128], F32, "ExternalInput")
        x1d = C.dram("x1", [nt, D], F32, "ExternalOutput")
        xn2d = C.dram("xn2T", [D, nt], BF16, "ExternalOutput")
        affd = C.dram("aff", [nt, 16], F32, "ExternalOutput")
        affTd = C.dram("affT", [16, nt], F32, "ExternalOutput")
        affTs = C.sb([16, nt], F32, "affTs")

        wbr = [C.sb([128, 4, D], BF16, "wbr") for _ in range(3)]
        wo = C.sb([128, 8, D], BF16, "wo")
        wst = Rot([(("wst", i), C.sb([128, 4, D], F32, "wst")) for i in range(2)])
        wr = C.sb([128, 8, 16], F32, "wr")
        gf = C.sb([128, 8], F32, "gf")
        gfull = C.sb([128, 8, 128], F32, "gfull")
        identb = C.sb([128, 128], BF16, "identb")
        ident32 = C.sb([128, 128], F32, "ident32")
        srct = [Rot([((("src", b), i), C.sb([128, 4, 128], BF16, "src")) for i in range(2)]) for b in range(3)]
        gts = Rot([(("gts", i), C.sb([128, 3072], BF16, "gts")) for i in range(2)])
        xt = Rot([(("xt", i), C.sb([128, D], F32, "xt")) for i in range(2)])
        mg = C.sb([128, D], F32, "mg")
        tmpm = Rot([(("tmpm", i), C.sb([128, 512], F32, "tmpm")) for i in range(2)])
        mgb = C.sb([128, D], BF16, "mgb")
        mT = C.sb([128, 8, 128], BF16, "mT")
        x1 = Rot([(("x1", i), C.sb([128, D], F32, "x1")) for i in range(2)])
        sqj = C.sb([128, D], F32, "sqj")
        xs = C.sb([128, D], F32, "xs")
        st = Rot([(("st", i), C.sb([128, 16], F32, "st")) for i in range(2)])
        xT32 = C.sb([128, 8, 128], F32, "xT32")
        xTb = Rot([(("xTb", i), C.sb([128, 8, 128], BF16, "xTb")) for i in range(2)])
        lg = C.sb([128, 16], F32, "lg")
        ex = C.sb([128, 16], F32, "ex")
        affs = C.sb([128, ntile, 16], F32, "affs")
        pA = Rot([(("pA", i), C.ps([128, 512], F32, "pA")) for i in range(5)])
        pB = C.ps([128, 1024], BF16, "pB")

        DMA(P, "sp", identb[:, :], identbd[:, :], [], ["identb"])
        DMA(P, "sp", ident32[:, :], ident32d[:, :], [], ["ident32"])
        DMA(P, "sp", gf[:, :], gfd[:, :], [], ["gf"])
        DMA(P, "sp", wr[:, :, :], wrd.ap().rearrange("(c p) e -> p c e", p=128), [], ["wr"])
        for b in range(3):
            sk, stg = wst.next()
            DMA(P, "sp", stg[:, :, :], wds[b].ap().rearrange("(c p) n -> p c n", p=128), [], [sk])
            for c in range(4):
                CP(P, ("dve", "pool")[c % 2], wbr[b][:, c, :], stg[:, c, :], [sk], [("wbr", b)])
        for hh in range(2):
            sk, stg = wst.next()
            DMA(P, "sp", stg[:, :, :], wod.ap().rearrange("(c p) n -> p c n", p=128)[:, hh * 4:(hh + 1) * 4, :], [], [sk])
            for c in range(4):
                CP(P, ("dve", "pool")[c % 2], wo[:, hh * 4 + c, :], stg[:, c, :], [sk], ["wo"])
        for k in range(8):
            TS(P, "pool", gfull[:, k, :], ident32[:, :], 0.0, gf[:, k:k + 1], ALU.mult, ALU.add, ["ident32", "gf"], ["gfull"])
        for t in range(ntile):
            r0 = t * 128
            xk, xt_ = xt.next()
            DMA(P, "sp", xt_[:, :], xd[r0:r0 + 128, :], [], [xk])
            gk, gt_ = gts.next()
            DMA(P, "sp", gt_[:, :], gtsd[r0:r0 + 128, :], [], [gk])
            skeys = []
            stiles = []
            for b in range(3):
                k_, t_ = srct[b].next()
                DMA(P, "sp", t_[:, :, :], srcs[b].ap().rearrange("(c p) t -> p c t", p=128)[:, :, r0:r0 + 128], [], [k_])
                skeys.append(k_)
                stiles.append(t_)
            for b in range(3):
                for hf in range(2):
                    ak, at = pA.next()
                    for c in range(4):
                        MM(P, at[:, :], stiles[b][:, c, :], wbr[b][:, c, hf * 512:(hf + 1) * 512], c == 0, c == 3,
                           [skeys[b], ("wbr", b)], [ak])
                    gsl = gt_[:, b * 1024 + hf * 512:b * 1024 + (hf + 1) * 512]
                    if b == 0:
                        TT(P, "dve", mg[:, hf * 512:(hf + 1) * 512], at[:, :], gsl, ALU.mult, [ak, gk], [("mg", hf)])
                    else:
                        tk, tt_ = tmpm.next()
                        TT(P, "dve", tt_[:, :], at[:, :], gsl, ALU.mult, [ak, gk], [tk])
                        TT(P, "pool", mg[:, hf * 512:(hf + 1) * 512], mg[:, hf * 512:(hf + 1) * 512], tt_[:, :], ALU.add,
                           [("mg", hf), tk], [("mg", hf)])
            CP(P, "act", mgb[:, :], mg[:, :], [("mg", 0), ("mg", 1)], ["mgb"])
            for k in range(8):
                TR(P, pB[:, k * 128:(k + 1) * 128], mgb[:, k * 128:(k + 1) * 128], identb[:, :], ["mgb", "identb"], ["pB"])
            CP(P, "dve", mT[:, :, :], pB[:].rearrange("p (k t) -> p k t", k=8), ["pB"], ["mT"])
            x1k, x1t = x1.next()
            for hf in range(2):
                ak, at = pA.next()
                for k in range(8):
                    MM(P, at[:, :], mT[:, k, :], wo[:, k, hf * 512:(hf + 1) * 512], k == 0, k == 7, ["mT", "wo"], [ak])
                TT(P, "dve", x1t[:, hf * 512:(hf + 1) * 512], at[:, :], xt_[:, hf * 512:(hf + 1) * 512], ALU.add,
                   [ak, xk], [(x1k, hf)])
            DMA(P, "sp", x1d[r0:r0 + 128, :], x1t[:, :], [(x1k, 0), (x1k, 1)], [])
            sk_, st_ = st.next()
            ACT(P, sqj[:, :], x1t[:, :], AF.Square, [(x1k, 0), (x1k, 1)], ["sqj"])
            RSUM(P, "dve", st_[:, 0:1], sqj[:, :], ["sqj"], [sk_])
            ACT(P, st_[:, 1:2], st_[:, 0:1], AF.Sqrt, [sk_], [sk_], scale=1.0 / D, bias=EPS)
            RECIP(P, "dve", st_[:, 1:2], st_[:, 1:2], [sk_], [sk_])
            ACT(P, xs[:, :], x1t[:, :], AF.Copy, [(x1k, 0), (x1k, 1), sk_], ["xs"], scale=st_[:, 1:2])
            for hf in range(2):
                ak, at = pA.next()
                for k in range(4):
                    kk = hf * 4 + k
                    TR(P, at[:, k * 128:(k + 1) * 128], xs[:, kk * 128:(kk + 1) * 128], ident32[:, :], ["xs", "ident32"], [ak])
                TT(P, "dve", xT32[:, hf * 4:(hf + 1) * 4, :], at[:].rearrange("p (k t) -> p k t", k=4),
                   gfull[:, hf * 4:(hf + 1) * 4, :], ALU.mult, [ak, "gfull"], [("xT32", hf)])
            xbk, xbt = xTb.next()
            CP(P, "act", xbt[:, :, :], xT32[:, :, :], [("xT32", 0), ("xT32", 1)], [xbk])
            DMA(P, "sp", xn2d.ap().rearrange("(k p) t -> p k t", p=128)[:, :, r0:r0 + 128], xbt[:, :, :], [xbk], [])
            ak, at = pA.next()
            for k in range(8):
                MM(P, at[:, 0:16], xT32[:, k, :], wr[:, k, :], k == 0, k == 7, [("xT32", 0), ("xT32", 1), "wr"], [ak])
            CP(P, "dve", lg[:, :], at[:, 0:16], [ak], ["lg"])
            RMAX(P, "dve", st_[:, 2:3], lg[:, :], ["lg"], [sk_])
            TS(P, "dve", st_[:, 2:3], st_[:, 2:3], -1.0, None, ALU.mult, None, [sk_], [sk_])
            ACT(P, ex[:, :], lg[:, :], AF.Exp, ["lg", sk_], ["ex"], bias=st_[:, 2:3])
            RSUM(P, "dve", st_[:, 3:4], ex[:, :], ["ex"], [sk_])
            RECIP(P, "dve", st_[:, 3:4], st_[:, 3:4], [sk_], [sk_])
            TS(P, "dve", affs[:, t, :], ex[:, :], st_[:, 3:4], None, ALU.mult, None, ["ex", sk_], [("affs", t)])
            ak, at = pA.next()
            TR(P, at[0:16, 0:128], affs[:, t, :], ident32[:, :], [("affs", t), "ident32"], [ak])
            CP(P, "act", affTs[:, r0:r0 + 128], at[0:16, 0:128], [ak], [("affT", t)])
        DMA(P, "sp", affd.ap().rearrange("(t p) e -> p t e", p=128), affs[:, :, :], [("affs", t) for t in range(ntile)], [])
        DMA(P, "sp", affTd[:, :], affTs[:, :], [("affT", t) for t in range(ntile)], [])
        P.emit()
        stats = P.stats
    return nc, stats


def build_thr(ns=S, cap=2 * S // 16, iters=30, env=None, pre=None):
    nc, es, C, P = _begin(env, pre)
    with es:
        affT = C.dram("affT", [16, ns], F32, "ExternalInput")
        thr = C.dram("thr", [16, 2], F32, "ExternalOutput")
        a = C.sb([16, ns], F32, "a")
        junk = C.sb([16, ns], F32, "junk")
        lh = C.sb([16, 2], F32, "lh")
        w = C.sb([16, 8], F32, "w")
        DMA(P, "sp", a[:, :], affT[:, :], [], ["a"])
        MEMSET(P, "dve", lh[:, 0:1], 0.0, ["lh"])
        MEMSET(P, "dve", lh[:, 1:2], 1.0, ["lh"])
        for it in range(iters):
            TT(P, "dve", w[:, 0:1], lh[:, 0:1], lh[:, 1:2], ALU.add, ["lh"], ["w"])
            TS(P, "dve", w[:, 0:1], w[:, 0:1], 0.5, None, ALU.mult, None, ["w"], ["w"])
            P.op("dve", lambda e: e.tensor_scalar(out=junk[:, :], in0=a[:, :], scalar1=w[:, 0:1], scalar2=0.0,
                                                  op0=ALU.is_ge, op1=ALU.add, accum_out=w[:, 1:2]),
                 ["a", "w"], ["junk", "w"])
            TS(P, "dve", w[:, 2:3], w[:, 1:2], float(cap), None, ALU.is_ge, None, ["w"], ["w"])
            TT(P, "dve", w[:, 3:4], w[:, 0:1], lh[:, 0:1], ALU.subtract, ["w", "lh"], ["w"])
            TT(P, "dve", w[:, 4:5], lh[:, 1:2], w[:, 0:1], ALU.subtract, ["w", "lh"], ["w"])
            STT(P, "dve", lh[:, 0:1], w[:, 3:4], w[:, 2:3], lh[:, 0:1], ALU.mult, ALU.add, ["w", "lh"], ["lh"])
            STT(P, "dve", lh[:, 1:2], w[:, 4:5], w[:, 2:3], w[:, 0:1], ALU.mult, ALU.add, ["w", "lh"], ["lh"])
        DMA(P, "sp", thr[:, :], lh[:, :], ["lh"], [])
        P.emit()
        stats = P.stats
    return nc, stats


def build_ffn(nt=TPC, nexp=16, tb=1024, env=None, pre=None):
    nc, es, C, P = _begin(env, pre)
    FF = 1536
    ntile = nt // 128
    nblk = nt // tb
    with es:
        x1d = C.dram("x1", [nt, D], F32, "ExternalInput")
        xnd = C.dram("xn2T", [D, nt], BF16, "ExternalInput")
        affd = C.dram("aff", [nt, 16], F32, "ExternalInput")
        thrd = C.dram("thr_row", [128, 16], F32, "ExternalInput") if not (pre is not None and "thr16" in pre) else None
        wgd = C.dram("wg", [nexp, D, FF], F32, "ExternalInput")
        wud = C.dram("wu", [nexp, D, FF], F32, "ExternalInput")
        wdd = C.dram("wd", [nexp, FF, D], F32, "ExternalInput")
        x2d = C.dram("x2", [nt, D], F32, "ExternalOutput")

        xb = C.sb([128, 8, tb], BF16, "xb")
        acc = C.sb([128, tb // 128, D], F32, "acc")
        wgb = C.sb([128, 8, FF], BF16, "wgb")
        wub = C.sb([128, 8, FF], BF16, "wub")
        wdb = C.sb([128, 12, D], BF16, "wdb")
        stg = Rot([(("stg", i), C.sb([128, 4096], F32, "stg")) for i in range(2)])
        hT = C.sb([128, 12, tb], BF16, "hT")
        sg = Rot([(("sg", i), C.sb([128, 512], F32, "sg")) for i in range(2)])
        affs = C.sb([128, ntile, 16], F32, "affs")
        gw = C.sb([128, ntile, 16], F32, "gw")
        thr = C.sb([128, 16], F32, "thr")
        xo = Rot([(("xo", i), C.sb([128, D], F32, "xo")) for i in range(2)])
        pA = Rot([(("pA", i), C.ps([128, 512], F32, "pA")) for i in range(7)])

        if pre is not None and "thr16" in pre:
            t16d = pre["thr16"]
            i32d = pre["ident32"]
            t16 = C.sb([16, 2], F32, "t16")
            tbc = C.sb([16, 128], F32, "tbc")
            i16 = C.sb([16, 16], F32, "i16")
            DMA(P, "sp", t16[:, :], t16d[:, :], [], ["t16"])
            DMA(P, "sp", i16[:, :], i32d[0:16, 0:16], [], ["i16"])
            MEMSET(P, "dve", tbc[:, :], 1.0, ["tbc"])
            TS(P, "dve", tbc[:, :], tbc[:, :], t16[:, 0:1], None, ALU.mult, None, ["tbc", "t16"], ["tbc"])
            tk_, tp_ = pA.next()
            MM(P, tp_[:, 0:16], tbc[:, :], i16[:, :], True, True, ["tbc", "i16"], [tk_])
            CP(P, "dve", thr[:, :], tp_[:, 0:16], [tk_], ["thr"])
        else:
            DMA(P, "sp", thr[:, :], thrd[:, :], [], ["thr"])
        DMA(P, "sp", affs[:, :, :], affd.ap().rearrange("(t p) e -> p t e", p=128), [], ["affs"])
        for t in range(ntile):
            TT(P, "dve", gw[:, t, :], affs[:, t, :], thr[:, :], ALU.is_ge, ["affs", "thr"], ["gw"])
            TT(P, "dve", gw[:, t, :], gw[:, t, :], affs[:, t, :], ALU.mult, ["gw", "affs"], ["gw"])
        cv = [0]

        def conv(dst, src, r, w):
            eng = ("dve", "pool", "act")[cv[0] % 3]
            cv[0] += 1
            CP(P, eng, dst, src, r, w)

        def load_gu(e, chs=(0, 1, 2)):
            for ch in chs:
                for (wd_, dstb, key) in ((wgd, wgb, "wgb"), (wud, wub, "wub")):
                    sk, st = stg.next()
                    sv = st[:, :].rearrange("p (c f) -> p c f", c=8)
                    DMA(P, "sp", sv, wd_[e].rearrange("(c p) f -> p c f", p=128)[:, :, ch * 512:(ch + 1) * 512], [], [sk])
                    for hh in range(2):
                        conv(dstb[:, hh * 4:(hh + 1) * 4, ch * 512:(ch + 1) * 512], sv[:, hh * 4:(hh + 1) * 4, :], [sk],
                             [(key, ch)])

        def load_d(e):
            for ch in range(3):
                sk, st = stg.next()
                sv = st[:, :].rearrange("p (c n) -> p c n", c=4)
                DMA(P, "sp", sv, wdd[e].rearrange("(c p) n -> p c n", p=128)[:, ch * 4:(ch + 1) * 4, :], [], [sk])
                for hh in range(2):
                    conv(wdb[:, ch * 4 + hh * 2:ch * 4 + hh * 2 + 2, :], sv[:, hh * 2:hh * 2 + 2, :], [sk], ["wdb"])

        first = True
        for blk in range(nblk):
            b0 = blk * tb
            DMA(P, "sp", xb[:, :, :], xnd.ap().rearrange("(k p) t -> p k t", p=128)[:, :, b0:b0 + tb], [], ["xb"])
            for e in range(nexp):
                if first:
                    load_gu(e)
                    load_d(e)
                    first = False
                nxt = (blk * nexp + e + 1)
                for f in range(12):
                    for tq in range(tb // 512):
                        gk, gp = pA.next()
                        uk, up = pA.next()
                        for k in range(8):
                            MM(P, gp[:, :], wgb[:, k, f * 128:(f + 1) * 128], xb[:, k, tq * 512:(tq + 1) * 512],
                               k == 0, k == 7, [("wgb", f // 4), "xb"], [gk])
                        for k in range(8):
                            MM(P, up[:, :], wub[:, k, f * 128:(f + 1) * 128], xb[:, k, tq * 512:(tq + 1) * 512],
                               k == 0, k == 7, [("wub", f // 4), "xb"], [uk])
                        sk, st = sg.next()
                        ACT(P, st[:, :], gp[:, :], AF.Silu, [gk], [sk])
                        TT(P, "dve", hT[:, f, tq * 512:(tq + 1) * 512], up[:, :], st[:, :], ALU.mult, [uk, sk], [("hT", f)])
                    if f % 4 == 3 and nxt < nblk * nexp:
                        load_gu(nxt % nexp, chs=(f // 4,))
                for tt in range(tb // 128):
                    gcol = gw[:, blk * (tb // 128) + tt, e:e + 1]
                    for hf in range(2):
                        yk, yp = pA.next()
                        for f in range(12):
                            MM(P, yp[:, :], hT[:, f, tt * 128:(tt + 1) * 128], wdb[:, f, hf * 512:(hf + 1) * 512],
                               f == 0, f == 11, [("hT", f), "wdb"], [yk])
                        asl = acc[:, tt, hf * 512:(hf + 1) * 512]
                        if e == 0:
                            TS(P, "dve", asl, yp[:, :], gcol, None, ALU.mult, None, [yk, "gw"], [("acc", tt, hf)])
                        else:
                            STT(P, "dve", asl, yp[:, :], gcol, asl, ALU.mult, ALU.add, [yk, "gw", ("acc", tt, hf)],
                                [("acc", tt, hf)])
                if nxt < nblk * nexp:
                    load_d(nxt % nexp)
            for tt in range(tb // 128):
                xk, xt_ = xo.next()
                r0 = b0 + tt * 128
                DMA(P, "sp", xt_[:, :], x1d[r0:r0 + 128, :], [], [xk])
                TT(P, "pool", xt_[:, :], xt_[:, :], acc[:, tt, :], ALU.add, [xk, ("acc", tt, 0), ("acc", tt, 1)], [xk])
                DMA(P, "sp", x2d[r0:r0 + 128, :], xt_[:, :], [xk], [])
        P.emit()
        stats = P.stats
    return nc, stats


def build_attn(nq=TPC, nk=S, nheads=8, env=None, pre=None):
    nc, es, C, P = _begin(env, pre)
    NKT = nk // 128
    NQB = nq // 512
    scale = 96.0 ** -0.5
    with es:
        mq = C.dram("mq", [nheads, 96, nq], BF16, "ExternalInput")
        mk = C.dram("mk", [nheads, 96, nk], BF16, "ExternalInput")
        mv = C.dram("mv", [nheads, 128, NKT * 64], BF16, "ExternalInput")
        esel = C.dram("esel", [65, 64], F32, "ExternalInput")
        OT = C.dram("OT", [nheads * 64, nq], BF16, "ExternalOutput")

        kT = Rot([(("kT", i), C.sb([96, nk], BF16, "kT")) for i in range(2)])
        vv = Rot([(("vv", i), C.sb([128, NKT, 65], BF16, "vv")) for i in range(2)])
        qT = Rot([(("qT", i), C.sb([96, nq], BF16, "qT")) for i in range(2)])
        pT = Rot([(("pT", i), C.sb([128, 512], BF16, "pT")) for i in range(4)])
        osb = C.sb([65, 512], F32, "osb")
        rbc = C.sb([64, 512], F32, "rbc")
        oo = Rot([(("oo", i), C.sb([64, 512], BF16, "oo")) for i in range(2)])
        es_sb = C.sb([65, 64], F32, "esel")
        sps = Rot([(("sps", i), C.ps([128, 512], F32, "sps")) for i in range(4)])
        ops_ = Rot([(("ops", i), C.ps([128, 512], F32, "ops")) for i in range(2)])
        bps = C.ps([128, 512], F32, "bps")
        DMA(P, "sp", es_sb[:], esel[:, :], [], ["esel"])
        for i in range(2):
            MEMSET(P, "pool", vv.items[i][1][:, :, 64:65], 1.0, [("vv1", i)])
        for h in range(nheads):
            kk, kt_ = kT.next()
            vk, vt_ = vv.next()
            qk, qt_ = qT.next()
            DMA(P, "sp", kt_[:, :], mk[h, :, :], [], [kk])
            DMA(P, "sp", vt_[:, :, 0:64], mv[h, :, :].rearrange("p (t d) -> p t d", d=64), [], [vk])
            DMA(P, "sp", qt_[:, :], mq[h, :, :], [], [qk])
            vkeys = [vk, ("vv1", (vv.i - 1) % 2)]
            for qb in range(NQB):
                ok_, ot_ = ops_.next()

                def s_mm(t, kt_=kt_, qt_=qt_, qb=qb, kk=kk, qk=qk):
                    sk, st = sps.next()
                    MM(P, st[:, :], kt_[:, t * 128:(t + 1) * 128], qt_[:, qb * 512:(qb + 1) * 512], True, True,
                       [kk, qk], [sk])
                    return sk, st
                pend = [s_mm(0)]
                if NKT > 1:
                    pend.append(s_mm(1))
                for t in range(NKT):
                    if t + 2 < NKT:
                        pend.append(s_mm(t + 2))
                    sk, st = pend.pop(0)
                    pk, pt = pT.next()
                    ACT(P, pt[:, :], st[:, :], AF.Exp, [sk], [pk], scale=scale)
                    MM(P, ot_[0:65, :], vt_[:, t, :], pt[:, :], t == 0, t == NKT - 1, vkeys + [pk], [ok_])
                CP(P, "dve", osb[:, :], ot_[0:65, :], [ok_], ["osb"])
                MM(P, bps[0:64, :], es_sb[:, :], osb[:, :], True, True, ["esel", "osb"], ["bps"])
                CP(P, "dve", rbc[:, :], bps[0:64, :], ["bps"], ["rbc"])
                RECIP(P, "dve", rbc[:, :], rbc[:, :], ["rbc"], ["rbc"])
                ook, oot = oo.next()
                TT(P, "dve", oot[:, :], osb[0:64, :], rbc[:, :], ALU.mult, ["osb", "rbc"], [ook])
                DMA(P, "sp", OT[h * 64:(h + 1) * 64, qb * 512:(qb + 1) * 512], oot[:, :], [ook], [])
        P.emit()
        stats = P.stats
    return nc, stats


def attn_consts():
    e = np.zeros((65, 64), np.float32)
    e[64, :] = 1.0
    return dict(esel=e)


RG = [[0, 1, 2, 3], [4, 5, 6, 7]]
LAYER_W = [("w_in", [D, INW], F32), ("gmix", [128, 8], F32), ("convp", [128, 4, 34], F32), ("gcq", [128, 3], F32),
           ("gckv", [128, 2], F32), ("w_uq", [384, 768], F32), ("w_ukv", [256, 1024], F32), ("gqk", [96, 2], F32),
           ("bif", [128, 4], F32), ("gA", [128, 128], F32), ("w_a", [512, D], F32), ("w_b", [512, D], F32),
           ("w_c", [512, D], F32), ("w_o", [D, D], F32), ("gffn", [128, 8], F32), ("w_r", [D, 16], F32),
           ("wg", [16, D, 1536], F32), ("wu", [16, D, 1536], F32), ("wd", [16, 1536, D], F32)]
CONSTS = [("identb", [128, 128], BF16), ("ropeT", [96, 2, TPC], F32), ("rmat", [96, 96], BF16), ("onesf", [128, 128], F32),
          ("cmat", [128, 6, 128], F32), ("ident32", [128, 128], F32), ("esel", [65, 64], F32), ("idx", [128, 16], I32)]


def build_fused(stop=None):
    env = Env()
    nc, P = env.nc, env.P
    BYP = ALU.bypass
    CCB = 256 * 1024
    with env.es:
        def DT(name, shape, dt, kind="Internal"):
            return nc.dram_tensor(name, list(shape), dt, kind=kind)

        def allgather(name, src2d, rows, cols, dt, rkeys, wkey):
            esz = 4 if dt in (F32, I32) else 2
            rc = max(1, min(rows, CCB // (cols * esz)))
            assert rows % rc == 0
            g = DT(name, [4 * rows, cols], dt)
            for k in range(rows // rc):
                P.cc(lambda e, k=k: e.collective_compute("AllGather", BYP, replica_groups=RG, ins=[src2d[k * rc:(k + 1) * rc, :]],
                                                         outs=[g[k * 4 * rc:(k + 1) * 4 * rc, :]]), rkeys, [wkey])
            return g, rc

        def rankview(g, rc, r):
            return g.ap().rearrange("(k r x) c -> r k x c", r=4, x=rc)[r]

        ext = {"xe0": DT("xe0", [TPC + 2 * HALO, D], F32, "ExternalInput")}
        for (n, sh, dt) in CONSTS:
            ext[n] = DT(n, sh, dt, "ExternalInput")
        for l in range(2):
            for (n, sh, dt) in LAYER_W:
                ext["%s_%d" % (n, l)] = DT("%s_%d" % (n, l), sh, dt, "ExternalInput")
        out = DT("out", [TPC, D], F32, "ExternalOutput")
        xe = ext["xe0"]
        x_own = None
        for l in range(2):
            W = {n: ext["%s_%d" % (n, l)] for (n, _, _) in LAYER_W}
            L = lambda n, sh, dt: DT("%s_L%d" % (n, l), sh, dt)
            A = dict(QT=L("QT", [512, TPC], BF16), KT=L("KT", [512, TPC], BF16), Kt=L("Kt", [4, TPC, 128], BF16),
                     Vt=L("Vt", [4, TPC, 128], BF16), OG=L("OG", [4, TPC, 128], BF16), G4=L("G4", [4, TPC, 4], F32),
                     GTS=L("GTS", [TPC, 3072], BF16), UT=L("UT", [512, TPC], BF16), MQ=L("MQ", [8, 96, TPC], BF16),
                     MK=L("MK", [8, 96, TPC], BF16), MV=L("MV", [8, 128, TPC // 128, 64], BF16))
            preA = dict(xe=xe, w_in=W["w_in"], gmix=W["gmix"], identb=ext["identb"], convp=W["convp"], gcq=W["gcq"],
                        gckv=W["gckv"], w_uq=W["w_uq"], w_ukv=W["w_ukv"], gqk=W["gqk"], ropeT=ext["ropeT"],
                        rmat=ext["rmat"], onesf=ext["onesf"], **A)
            build_stageA(env=env, pre=preA)
            nc_, es_, C_, _ = _begin(env)
            with es_:
                idx = C_.sb([128, 16], I32, "idx")
                DMA(P, "sp", idx[:, :], ext["idx"][:, :], [], ["idx"])
                gQT, rcQ = allgather("gQT_L%d" % l, A["QT"].ap(), 512, TPC, BF16, [], ("g", 0))
                gKt, rcK = allgather("gKt_L%d" % l, A["Kt"].ap().rearrange("h t d -> (h t) d"), 4 * TPC, 128, BF16, [], ("g", 1))
                gVt, _ = allgather("gVt_L%d" % l, A["Vt"].ap().rearrange("h t d -> (h t) d"), 4 * TPC, 128, BF16, [], ("g", 2))
                gOG, _ = allgather("gOG_L%d" % l, A["OG"].ap().rearrange("h t d -> (h t) d"), 4 * TPC, 128, BF16, [], ("g", 3))
                gG4, rcG = allgather("gG4_L%d" % l, A["G4"].ap().rearrange("h t g -> (h t) g"), 4 * TPC, 4, F32, [], ("g", 4))
                gMK, rcMK = allgather("gMK_L%d" % l, A["MK"].ap().rearrange("h f t -> (h f) t"), 768, TPC, BF16, [], ("g", 5))
                gMV, rcMV = allgather("gMV_L%d" % l, A["MV"].ap().rearrange("h p t d -> (h p) (t d)"), 1024, 2048, BF16, [], ("g", 6))
                assert (rcQ, rcK, rcG, rcMK, rcMV) == (32, 1024, 4 * TPC, 32, 64), (rcQ, rcK, rcG, rcMK, rcMV)
                qT_s = L("qT_s", [128, S], BF16)
                kt_s = L("kt_s", [S, 128], BF16)
                vt_s = L("vt_s", [S, 128], BF16)
                og_s = L("og_s", [S, 128], BF16)
                g4_s = L("g4_s", [S, 4], F32)
                mk_s = L("mk_s", [8, 96, S], BF16)
                mv_s = L("mv_s", [8, 128, (S // 128) * 64], BF16)
                stb = Rot([(("stb", i), C_.sb([128, 16384], BF16, "stb")) for i in range(2)])
                stf = C_.sb([128, 512], F32, "stf")

                def gather(dst_ap, src_ap, col, rkeys, wkeys, tile_ap, tkey):
                    P.dma("pool", lambda e: e.indirect_dma_start(
                        out=tile_ap, out_offset=None, in_=src_ap,
                        in_offset=bass.IndirectOffsetOnAxis(ap=idx[:, col:col + 1], axis=0)), ["idx"] + rkeys, [tkey])
                    DMA(P, "sp", dst_ap, tile_ap, [tkey], wkeys)
                for i in range(4):
                    tk_, tt_ = stb.next()
                    gather(qT_s[:, i * TPC:(i + 1) * TPC], gQT[:, :], i, [("g", 0)], [("qT_s", i)], tt_[:, 0:TPC], tk_)
                for j, (gsrc, dst) in enumerate(((gKt, kt_s), (gVt, vt_s), (gOG, og_s))):
                    tk_, tt_ = stb.next()
                    gather(dst.ap().rearrange("(c p) d -> c (p d)", p=128),
                           gsrc.ap().rearrange("(c p) d -> c (p d)", p=128), 4, [("g", 1 + j)], [("tm_s", j)], tt_[:, :], tk_)
                gather(g4_s.ap().rearrange("(c p) d -> c (p d)", p=128),
                       gG4.ap().rearrange("(c p) d -> c (p d)", p=128), 11, [("g", 4)], [("tm_s", 3)], stf[:, :], "stf")
                for i in range(4):
                    DMA(P, "sp", mk_s.ap().rearrange("h f t -> (h f) t")[:, i * TPC:(i + 1) * TPC].rearrange("(k x) t -> k x t", x=rcMK),
                        rankview(gMK, rcMK, i), [("g", 5)], [("mk_s", i)])
                    DMA(P, "sp", mv_s.ap().rearrange("h p x -> (h p) x")[:, i * 2048:(i + 1) * 2048].rearrange("(k x) c -> k x c", x=rcMV),
                        rankview(gMV, rcMV, i), [("g", 6)], [("mv_s", i)])
                P.emit()
            if stop == "x1":
                return nc
            HT = L("HT", [128, S], BF16)
            build_mlstm(env=env, pre=dict(qT=qT_s, kt=kt_s, vt=vt_s, g4=g4_s, bif=W["bif"], og=og_s, gA=W["gA"],
                                          cmat=ext["cmat"], identb=ext["identb"], HT=HT))
            if stop == "m":
                return nc
            OT = L("OT", [512, TPC], BF16)
            build_attn(env=env, pre=dict(mq=A["MQ"], mk=mk_s, mv=mv_s, esel=ext["esel"], OT=OT))
            if stop == "t":
                return nc
            nc_, es_, C_, _ = _begin(env)
            with es_:
                idx = C_.sb([128, 16], I32, "idx")
                DMA(P, "sp", idx[:, :], ext["idx"][:, :], [], ["idx"])
                gHT, rcH = allgather("gHT_L%d" % l, HT.ap(), 128, S, BF16, [], "gHT")
                assert rcH == 8
                HT_own = L("HT_own", [512, TPC], BF16)
                src = gHT.ap().rearrange("r (i t) -> (r i) t", i=4)
                stb = Rot([(("stb", i), C_.sb([128, TPC], BF16, "stb")) for i in range(2)])
                for h in range(4):
                    tk_, tt_ = stb.next()
                    P.dma("pool", lambda e, h=h, tt_=tt_: e.indirect_dma_start(
                        out=tt_[:, :], out_offset=None, in_=src,
                        in_offset=bass.IndirectOffsetOnAxis(ap=idx[:, 5 + h:6 + h], axis=0)), ["idx", "gHT"], [tk_])
                    DMA(P, "sp", HT_own[h * 128:(h + 1) * 128, :], tt_[:, :], [tk_], [("HT_own", h)])
                P.emit()
            if stop == "x2":
                return nc
            x1 = L("x1", [TPC, D], F32)
            xn2T = L("xn2T", [D, TPC], BF16)
            aff = L("aff", [TPC, 16], F32)
            affT = L("affT", [16, TPC], F32)
            if l == 0:
                xin = L("xin", [TPC, D], F32)
                nc_, es_, C_, _ = _begin(env)
                with es_:
                    DMA(P, "sp", xin[:, :], ext["xe0"][HALO:HALO + TPC, :], [], ["xin"])
                    P.emit()
            else:
                xin = x_own
            build_merge(env=env, pre=dict(x=xin, HT=HT_own, UT=A["UT"], OT=OT, GTS=A["GTS"], w_a=W["w_a"], w_b=W["w_b"],
                                          w_c=W["w_c"], w_o=W["w_o"], gffn=W["gffn"], w_r=W["w_r"], identb=ext["identb"],
                                          ident32=ext["ident32"], x1=x1, xn2T=xn2T, aff=aff, affT=affT))
            if stop == "c1":
                return nc
            affT_s = L("affT_s", [16, S], F32)
            nc_, es_, C_, _ = _begin(env)
            with es_:
                gAf, rcA = allgather("gAf_L%d" % l, affT.ap(), 16, TPC, F32, [], "gAf")
                assert rcA == 16
                for i in range(4):
                    DMA(P, "sp", affT_s[:, i * TPC:(i + 1) * TPC], gAf[i * 16:(i + 1) * 16, :], ["gAf"], [("affT_s", i)])
                P.emit()
            thr = L("thr", [16, 2], F32)
            build_thr(env=env, pre=dict(affT=affT_s, thr=thr))
            if stop == "h":
                return nc
            x2 = out if l == 1 else L("x2", [TPC, D], F32)
            build_ffn(env=env, pre=dict(x1=x1, xn2T=xn2T, aff=aff, thr16=thr, ident32=ext["ident32"], wg=W["wg"], wu=W["wu"],
                                        wd=W["wd"], x2=x2))
            if l == 0:
                xe1 = L("xe1", [TPC + 2 * HALO, D], F32)
                nc_, es_, C_, _ = _begin(env)
                with es_:
                    idx = C_.sb([128, 16], I32, "idx")
                    zt = C_.sb([128, D], F32, "zt")
                    DMA(P, "sp", idx[:, :], ext["idx"][:, :], [], ["idx"])
                    MEMSET(P, "dve", zt[:, :], 0.0, ["zt"])
                    edges = L("edges", [384, D], F32)
                    DMA(P, "sp", edges[0:128, :], x2[0:128, :], [], ["edges"])
                    DMA(P, "sp", edges[128:256, :], x2[TPC - 128:TPC, :], [], ["edges"])
                    DMA(P, "sp", edges[256:384, :], zt[:, :], ["zt"], ["edges"])
                    DMA(P, "sp", xe1[HALO:HALO + TPC, :], x2[:, :], [], ["xe1m"])
                    gE, rcE = allgather("gE_L%d" % l, edges.ap(), 384, D, F32, ["edges"], "gE")
                    assert rcE == 64
                    hl = C_.sb([128, D], F32, "hl")
                    hr = C_.sb([128, D], F32, "hr")
                    P.dma("pool", lambda e: e.indirect_dma_start(
                        out=hl[:, :], out_offset=None, in_=gE[:, :],
                        in_offset=bass.IndirectOffsetOnAxis(ap=idx[:, 9:10], axis=0)), ["idx", "gE"], ["hl"])
                    P.dma("pool", lambda e: e.indirect_dma_start(
                        out=hr[:, :], out_offset=None, in_=gE[:, :],
                        in_offset=bass.IndirectOffsetOnAxis(ap=idx[:, 10:11], axis=0)), ["idx", "gE"], ["hr"])
                    DMA(P, "sp", xe1[0:HALO, :], hl[:, :], ["hl"], ["xe1l"])
                    DMA(P, "sp", xe1[HALO + TPC:, :], hr[:, :], ["hr"], ["xe1r"])
                    P.emit()
                xe = xe1
                x_own = x2
    return nc


def _grow(x, rc, r):
    return (x // rc) * (4 * rc) + r * rc + (x % rc)


def fused_idx(c):
    r = c % 4
    p = np.arange(128)
    idx = np.zeros((128, 16), np.int32)
    for i in range(4):
        idx[:, i] = _grow(r * 128 + p, 32, i)
    y = r * 32 + (p % 32)
    idx[:, 4] = _grow(y, 8, p // 32)
    idx[:, 11] = (p // 32) * 128 + y
    for h in range(4):
        idx[:, 5 + h] = _grow(p, 8, h) * 4 + r
    idx[:, 9] = _grow(128 + p, 64, r - 1) if r > 0 else _grow(256 + p, 64, r)
    idx[:, 10] = _grow(p, 64, r + 1) if r < 3 else _grow(256 + p, 64, r)
    return idx


def kernel(**inputs):
    prm = {k: np.asarray(v) for k, v in inputs.items()}
    x = np.ascontiguousarray(prm["x"], dtype=np.float32)
    nc = build_fused()
    CT, ST = rope_tables()
    cst = consts()
    mc = mlstm_consts()
    ac = attn_consts()
    i32 = np.eye(128, dtype=np.float32)
    lay = []
    for l in range(2):
        convp = np.zeros((128, 4, 34), np.float32)
        convp[:, :, 0:31] = prm["conv_w"][l].T.reshape(4, 128, 31).transpose(1, 0, 2)
        convp[:, :, 31] = prm["conv_b"][l].reshape(4, 128).T
        convp[:, :, 32] = prm["conv_ln_g"][l].reshape(4, 128).T
        convp[:, :, 33] = prm["conv_ln_b"][l].reshape(4, 128).T
        lay.append(dict(w_in=prm["w_in"][l], gmix=_gain_cols(prm["mix_norm_g"][l], 8), convp=convp,
                        gcq=_gain_cols(prm["cq_norm_g"][l], 3), gckv=_gain_cols(prm["ckv_norm_g"][l], 2),
                        w_uq=prm["w_uq"][l], w_ukv=prm["w_ukv"][l],
                        gqk=np.ascontiguousarray(np.stack([prm["q_norm_g"][l], prm["k_norm_g"][l]], axis=1)),
                        w_a=prm["w_a_out"][l], w_b=prm["w_b_out"][l], w_c=prm["w_c_out"][l], w_o=prm["w_out"][l],
                        gffn=_gain_cols(prm["ffn_norm_g"][l], 8), w_r=prm["w_router"][l],
                        wg=prm["w_e_gate"][l], wu=prm["w_e_up"][l], wd=prm["w_e_down"][l]))
    maps = []
    for c in range(NCORES):
        b, r = c // 4, c % 4
        s0 = r * TPC
        xe = np.zeros((TPC + 2 * HALO, D), np.float32)
        lo, hi = max(0, s0 - HALO), min(S, s0 + TPC + HALO)
        xe[lo - (s0 - HALO):hi - (s0 - HALO)] = x[b, lo:hi]
        m = dict(xe0=xe, identb=cst["identb"], ropeT=np.ascontiguousarray(np.stack([CT[:, s0:s0 + TPC], ST[:, s0:s0 + TPC]], axis=1)),
                 rmat=cst["rmat"], onesf=cst["onesf"], cmat=mc["cmat"], ident32=i32, esel=ac["esel"], idx=fused_idx(c))
        cols = [r, 4 + r, 8 + r, 12 + r]
        for l in range(2):
            for k_, v_ in lay[l].items():
                m["%s_%d" % (k_, l)] = v_
            m["bif_%d" % l] = np.ascontiguousarray(np.broadcast_to(prm["b_if"][l][cols], (128, 4)))
            m["gA_%d" % l] = np.ascontiguousarray(np.broadcast_to(prm["a_norm_g"][l][r], (128, 128)))
        maps.append(m)
    res = run_spmd(nc, maps)
    out = np.empty_like(x)
    for c in range(NCORES):
        b, r = c // 4, c % 4
        out[b, r * TPC:(r + 1) * TPC] = np.asarray(res[c]["out"])
    return out
```

```python
import math
from contextlib import ExitStack

import numpy as np
import ml_dtypes

import concourse.bass as bass
import concourse.mybir as mybir
from concourse.bass_utils import run_bass_kernel_spmd

F32 = mybir.dt.float32
BF16 = mybir.dt.bfloat16
I32 = mybir.dt.int32
AF = mybir.ActivationFunctionType
ALU = mybir.AluOpType
AX = mybir.AxisListType
NPBF = ml_dtypes.bfloat16

D = 1024
S = 16384
NB = 2
INW = 6832
EPS = 1e-6
NCORES = 8
TPC = S * NB // NCORES

O_AQ, O_AK, O_AV, O_AO, O_AG = 0, 512, 1024, 1536, 2048
O_GLU = 2064
O_CQ = 3088
O_CKV = 3472
O_CKR = 3728
O_GTS = 3760


class Prog:
    RING = 8

    def __init__(self, nc, es):
        self.nc = nc
        self.es = es
        self.ops = []
        self.engs = ["pe", "act", "dve", "pool", "sp"]
        self.csem = None
        self.rings = {}
        self.ccsem = None
        self.ccount = {e: 0 for e in self.engs}
        self.dcount = {e: 0 for e in self.engs}
        self.cccount = 0
        self.nstage = 0

    def cc(self, fn, r=(), w=()):
        self.ops.append(dict(eng="pool", fn=fn, r=tuple(r), w=tuple(w), dma=True, cc=True))

    def op(self, eng, fn, r=(), w=()):
        self.ops.append(dict(eng=eng, fn=fn, r=tuple(r), w=tuple(w), dma=False))

    def dma(self, eng, fn, r=(), w=()):
        self.ops.append(dict(eng=eng, fn=fn, r=tuple(r), w=tuple(w), dma=True))

    def emit(self):
        nc, es = self.nc, self.es
        ops = self.ops
        last_w = {}
        readers = {}
        deps = []
        for i, o in enumerate(ops):
            d = set()
            for k in o["r"]:
                if k in last_w:
                    d.add((last_w[k], "raw"))
            for k in o["w"]:
                if k in last_w:
                    d.add((last_w[k], "waw"))
                for j in readers.get(k, ()):
                    if j != i:
                        d.add((j, "war"))
            for k in o["r"]:
                lst = readers.setdefault(k, [])
                if not o["dma"]:
                    lst[:] = [j for j in lst if ops[j]["dma"] or ops[j]["eng"] != o["eng"]]
                lst.append(i)
            for k in o["w"]:
                last_w[k] = i
                readers[k] = []
            dd = set()
            for j, kind in d:
                p = ops[j]
                if (not p["dma"]) and (not o["dma"]) and p["eng"] == o["eng"]:
                    if o["eng"] == "pe":
                        continue
                    if kind == "war":
                        continue
                dd.add(j)
            deps.append(dd)
        needed = set()
        for dd in deps:
            needed |= dd
        engs = self.engs
        if self.csem is None:
            self.csem = {e: es.enter_context(nc.semaphore("c_" + e)) for e in engs}
            self.ccsem = es.enter_context(nc.semaphore("c_cc"))
        csem = self.csem
        rings = self.rings
        ccount = self.ccount
        dcount = self.dcount
        prev_end = dict(c={e: ccount[e] for e in engs}, d={e: dcount[e] for e in rings}, cc=self.cccount)
        lastc = {}
        for i, o in enumerate(ops):
            if not o["dma"]:
                lastc[o["eng"]] = i
        needed |= set(lastc.values())
        sig = {}
        prewait = {}
        for i, o in enumerate(ops):
            e = o["eng"]
            if o.get("cc"):
                self.cccount += 1
                sig[i] = (self.ccsem, self.cccount, 1)
                if self.cccount > 1:
                    prewait[i] = (self.ccsem, self.cccount - 1)
            elif o["dma"]:
                if e not in rings:
                    rings[e] = [es.enter_context(nc.semaphore("r_%s%d" % (e, k)))
                                for k in range(self.RING)]
                n = dcount[e]
                dcount[e] += 1
                sem = rings[e][n % self.RING]
                sig[i] = (sem, 16 * (n // self.RING + 1), 16)
                if n >= self.RING:
                    prewait[i] = (sem, 16 * (n // self.RING))
            elif i in needed:
                ccount[e] += 1
                sig[i] = (csem[e], ccount[e], 1)
        per = {e: [] for e in engs}
        for i, o in enumerate(ops):
            per[o["eng"]].append(i)
        self.stats = dict(n_ops=len(ops), ccount=ccount, dcount=dcount)

        nstage = self.nstage
        self.nstage += 1

        def run(e, engobj):
            waited = {}
            if nstage > 0:
                for e2 in engs:
                    if prev_end["c"][e2] > 0:
                        engobj.wait_ge(csem[e2], prev_end["c"][e2])
                for e2, n in prev_end["d"].items():
                    for k in range(self.RING):
                        cnt = (n - k + self.RING - 1) // self.RING if n > k else 0
                        if cnt > 0:
                            engobj.wait_ge(rings[e2][k], 16 * cnt)
                if prev_end["cc"] > 0:
                    engobj.wait_ge(self.ccsem, prev_end["cc"])
            for i in per[e]:
                o = ops[i]
                ws = [sig[j][:2] for j in deps[i]]
                if i in prewait:
                    ws.append(prewait[i])
                mx = {}
                for sem, val in ws:
                    key = id(sem)
                    if key not in mx or mx[key][1] < val:
                        mx[key] = (sem, val)
                for key, (sem, val) in mx.items():
                    if waited.get(key, 0) >= val:
                        continue
                    waited[key] = val
                    engobj.wait_ge(sem, val)
                ins = o["fn"](engobj)
                if i in sig:
                    sem, val, inc = sig[i]
                    ins.then_inc(sem, inc)
            if e in rings:
                n = dcount[e]
                for k in range(self.RING):
                    cnt = (n - k + self.RING - 1) // self.RING if n > k else 0
                    if cnt > 0:
                        engobj.wait_ge(rings[e][k], 16 * cnt)

        with nc.Block() as block:
            @block.tensor
            def _(t):
                run("pe", t)

            @block.scalar
            def _(t):
                run("act", t)

            @block.vector
            def _(t):
                run("dve", t)

            @block.gpsimd
            def _(t):
                run("pool", t)

            @block.sync
            def _(t):
                run("sp", t)
        self.ops = []


class Ctx:
    def __init__(self, nc, es, pre=None, tag=""):
        self.nc, self.es = nc, es
        self.n = 0
        self.pre = pre
        self.tag = tag

    def sb(self, shape, dt, name=None):
        self.n += 1
        t = self.es.enter_context(self.nc.sbuf_tensor("%s%s_%d" % (self.tag, name or "t", self.n), list(shape), dt))
        esz = 4 if dt in (F32, I32) else 2
        nbytes = int(np.prod(shape[1:])) * esz
        alloc = (nbytes + 31) // 32 * 32
        if alloc % 64 != 0:
            self.n += 1
            self.es.enter_context(self.nc.sbuf_tensor("%spad_%d" % (self.tag, self.n), [128, 8], F32))
        return t

    def ps(self, shape, dt, name=None):
        self.n += 1
        return self.es.enter_context(self.nc.psum_tensor("%s%s_%d" % (self.tag, name or "p", self.n), list(shape), dt))

    def dram(self, name, shape, dt, kind):
        if self.pre is not None:
            h = self.pre[name]
            assert list(h.shape) == list(shape), (name, h.shape, shape)
            return h
        return self.nc.dram_tensor(name, list(shape), dt, kind=kind)


class Rot:
    def __init__(self, items):
        self.items = items
        self.i = 0

    def next(self):
        it = self.items[self.i % len(self.items)]
        self.i += 1
        return it


class Env:
    def __init__(self):
        self.nc = bass.Bass("TRN2", target_bir_lowering=False)
        self.es = ExitStack()
        self.P = Prog(self.nc, self.es)
        self.nstage = 0


def _begin(env, pre=None):
    if env is None:
        nc = bass.Bass("TRN2", target_bir_lowering=False)
        es = ExitStack()
        return nc, es, Ctx(nc, es), Prog(nc, es)
    env.nstage += 1
    es = ExitStack()
    return env.nc, es, Ctx(env.nc, es, pre=pre, tag="s%d_" % env.nstage), env.P


def run_spmd(nc, in_maps):
    res = run_bass_kernel_spmd(nc, in_maps, core_ids=list(range(NCORES)))
    return res.results


def ACT(P, out, in_, func, r, w, **kw):
    P.op("act", lambda e: e.activation(out=out, in_=in_, func=func, **kw), r, w)


def TS(P, eng, out, in0, s1, s2, op0, op1, r, w):
    if op1 is None:
        P.op(eng, lambda e: e.tensor_scalar(out=out, in0=in0, scalar1=s1, scalar2=None, op0=op0), r, w)
    else:
        P.op(eng, lambda e: e.tensor_scalar(out=out, in0=in0, scalar1=s1, scalar2=s2, op0=op0, op1=op1), r, w)


def TT(P, eng, out, in0, in1, op, r, w):
    P.op(eng, lambda e: e.tensor_tensor(out=out, in0=in0, in1=in1, op=op), r, w)


def STT(P, eng, out, in0, scalar, in1, op0, op1, r, w):
    P.op(eng, lambda e: e.scalar_tensor_tensor(out=out, in0=in0, scalar=scalar, in1=in1, op0=op0, op1=op1), r, w)


def CP(P, eng, out, in_, r, w):
    if eng == "act":
        P.op(eng, lambda e: e.copy(out=out, in_=in_), r, w)
    else:
        P.op(eng, lambda e: e.tensor_copy(out=out, in_=in_), r, w)


def RSUM(P, eng, out, in_, r, w, axis=None):
    ax = axis if axis is not None else AX.X
    P.op(eng, lambda e: e.reduce_sum(out=out, in_=in_, axis=ax), r, w)


def RMAX(P, eng, out, in_, r, w, axis=None):
    ax = axis if axis is not None else AX.X
    P.op(eng, lambda e: e.reduce_max(out=out, in_=in_, axis=ax), r, w)


def MM(P, out, lhsT, rhs, start, stop, r, w):
    P.op("pe", lambda e: e.matmul(out, lhsT, rhs, start=start, stop=stop), r, w)


def TR(P, out, in_, ident, r, w):
    P.op("pe", lambda e: e.transpose(out, in_, ident), r, w)


def DMA(P, eng, out, in_, r, w):
    P.dma(eng, lambda e: e.dma_start(out=out, in_=in_), r, w)


def RECIP(P, eng, out, in_, r, w):
    P.op(eng, lambda e: e.reciprocal(out=out, in_=in_), r, w)


def MEMSET(P, eng, ap, val, w):
    P.op(eng, lambda e: e.memset(ap, val), (), w)


TP = 1024
HALO = 128
NTP = TP + 2 * HALO
NPASS = TPC // TP


def build_stageA(phases=("fm", "tm", "conv", "mla"), env=None, pre=None):
    nc, es, C, P = _begin(env, pre)
    with es:
        xe = C.dram("xe", [TPC + 2 * HALO, D], F32, "ExternalInput")
        w_in = C.dram("w_in", [D, INW], F32, "ExternalInput")
        gmix = C.dram("gmix", [128, 8], F32, "ExternalInput")
        identb = C.dram("identb", [128, 128], BF16, "ExternalInput")
        convp = C.dram("convp", [128, 4, 34], F32, "ExternalInput")
        gcq = C.dram("gcq", [128, 3], F32, "ExternalInput")
        gckv = C.dram("gckv", [128, 2], F32, "ExternalInput")
        w_uq = C.dram("w_uq", [384, 768], F32, "ExternalInput")
        w_ukv = C.dram("w_ukv", [256, 1024], F32, "ExternalInput")
        gqk = C.dram("gqk", [96, 2], F32, "ExternalInput")
        ropeT = C.dram("ropeT", [96, 2, TPC], F32, "ExternalInput")
        rmat = C.dram("rmat", [96, 96], BF16, "ExternalInput")
        onesf = C.dram("onesf", [128, 128], F32, "ExternalInput")

        QT = C.dram("QT", [512, TPC], BF16, "ExternalOutput")
        KT = C.dram("KT", [512, TPC], BF16, "ExternalOutput")
        Kt = C.dram("Kt", [4, TPC, 128], BF16, "ExternalOutput")
        Vt = C.dram("Vt", [4, TPC, 128], BF16, "ExternalOutput")
        OG = C.dram("OG", [4, TPC, 128], BF16, "ExternalOutput")
        G4 = C.dram("G4", [4, TPC, 4], F32, "ExternalOutput")
        GTS = C.dram("GTS", [TPC, 3072], BF16, "ExternalOutput")
        UT = C.dram("UT", [512, TPC], BF16, "ExternalOutput")
        MQ = C.dram("MQ", [8, 96, TPC], BF16, "ExternalOutput")
        MK = C.dram("MK", [8, 96, TPC], BF16, "ExternalOutput")
        MV = C.dram("MV", [8, 128, TPC // 128, 64], BF16, "ExternalOutput")

        w_v = w_in.ap().rearrange("(c p) n -> p c n", p=128)
        xnT = C.sb([128, 8, NTP], BF16, "xnT")
        f32t = Rot([(("f32t", i), C.sb([128, 512], F32, "f32t")) for i in range(4)])
        uT = C.sb([128, 4, NTP], BF16, "uT")
        cacc = [C.sb([128, 512], F32, "cacc") for g in range(4)]
        csq = [C.sb([128, 512], F32, "csq") for g in range(4)]
        cpar = C.sb([128, 4, 34], F32, "cpar")
        rope_sb = C.sb([96, 2, 512], F32, "rope")
        cql = C.sb([128, 3, 512], F32, "cql")
        ckl = C.sb([128, 2, 512], F32, "ckl")
        latsq = C.sb([128, 3, 512], BF16, "latsq")
        cqn = C.sb([128, 3, 512], BF16, "cqn")
        ckn = C.sb([128, 2, 512], BF16, "ckn")
        krt = C.sb([128, 512], F32, "krt")
        hx = Rot([(("hx", i), C.sb([128, 512], F32, "hx")) for i in range(2)])
        hsq = Rot([(("hsq", i), C.sb([128, 512], BF16, "hsq")) for i in range(2)])
        hxg = Rot([(("hxg", i), C.sb([128, 512], BF16, "hxg")) for i in range(2)])
        wuqb = C.sb([128, 3, 768], BF16, "wuqb")
        wukvb = C.sb([128, 2, 1024], BF16, "wukvb")
        wkrp = C.sb([128, 8, 128], BF16, "wkrp")
        gqk_sb = C.sb([96, 2], F32, "gqk")
        rmat_sb = C.sb([96, 96], BF16, "rmat")
        gcq_sb = C.sb([128, 3], F32, "gcqs")
        gckv_sb = C.sb([128, 2], F32, "gckvs")
        ident = C.sb([128, 128], BF16, "ident")
        gm = C.sb([128, 8], F32, "gm")
        onesb = C.sb([128, 128], BF16, "onesb")
        ones32 = C.sb([128, 128], F32, "ones32")
        xin = Rot([(("xin", i), C.sb([128, D], F32, "xin")) for i in range(2)])
        sqj = C.sb([128, D], F32, "sqj")
        xs = Rot([(("xs", i), C.sb([128, D], BF16, "xs")) for i in range(2)])
        stat = Rot([(("stat", i), C.sb([128, 2], F32, "stat")) for i in range(2)])
        wst = Rot([(("wst", i), C.sb([128, 8, 512], F32, "wst")) for i in range(3)])
        wb = Rot([(("wb", i), C.sb([128, 8, 512], BF16, "wb")) for i in range(3)])
        ob = Rot([(("ob", i), C.sb([128, 512], BF16, "ob")) for i in range(4)])
        g4sb = C.sb([128, NTP // 128, 16], F32, "g4sb")
        pacc = Rot([(("pacc", i), C.ps([128, 512], F32, "pacc")) for i in range(5)])
        ptr = Rot([(("ptr", i), C.ps([128, 1024], BF16, "ptr")) for i in range(2)])

        DMA(P, "sp", cpar[:], convp[:, :, :], [], ["cpar"])
        DMA(P, "sp", gqk_sb[:], gqk[:, :], [], ["gqk"])
        DMA(P, "sp", rmat_sb[:], rmat[:, :], [], ["rmat"])
        DMA(P, "sp", gcq_sb[:], gcq[:, :], [], ["gcqs"])
        DMA(P, "sp", gckv_sb[:], gckv[:, :], [], ["gckvs"])
        DMA(P, "sp", gm[:], gmix[:, :], [], ["gm"])
        if "mla" in phases:
            sk0, st0 = wst.items[0]
            st0f = st0[:].rearrange("p a b -> p (a b)")
            DMA(P, "sp", st0f[:, 0:2304].rearrange("p (a b) -> p a b", a=3),
                w_uq.ap().rearrange("(c p) n -> p c n", p=128), [], [sk0])
            for j in range(3):
                TS(P, "dve", wuqb[:, j, :], st0f[:, j * 768:(j + 1) * 768],
                   gcq_sb[:, j:j + 1], None, ALU.mult, None, [sk0, "gcqs"], ["wuqb"])
            sk1, st1 = wst.items[1]
            st1f = st1[:].rearrange("p a b -> p (a b)")
            DMA(P, "sp", st1f[:, 0:2048].rearrange("p (a b) -> p a b", a=2),
                w_ukv.ap().rearrange("(c p) n -> p c n", p=128), [], [sk1])
            for j in range(2):
                src = st1f[:, j * 1024:(j + 1) * 1024].rearrange("p (h x) -> p h x", h=8)
                TS(P, "dve", wukvb[:, j, 0:512].rearrange("p (h x) -> p h x", h=8), src[:, :, 0:64],
                   gckv_sb[:, j:j + 1], None, ALU.mult, None, [sk1, "gckvs"], ["wukvb"])
                TS(P, "dve", wukvb[:, j, 512:1024].rearrange("p (h x) -> p h x", h=8), src[:, :, 64:128],
                   gckv_sb[:, j:j + 1], None, ALU.mult, None, [sk1, "gckvs"], ["wukvb"])
            MEMSET(P, "pool", wkrp[:], 0.0, ["wkrp"])
            sk2_, st2_ = wst.items[0]
            DMA(P, "sp", st2_[:, :, 0:32], w_v[:, :, O_CKR:O_CKR + 32], [], [sk2_])
            for k in range(8):
                TS(P, "dve", wkrp[:, k, 64:96], st2_[:, k, 0:32], gm[:, k:k + 1], None, ALU.mult, None,
                   [sk2_, "gm"], ["wkrp"])
            MEMSET(P, "pool", krt[:], 0.0, ["krt"])
        DMA(P, "sp", ident[:], identb[:, :], [], ["ident"])
        DMA(P, "sp", gm[:], gmix[:, :], [], ["gm"])
        DMA(P, "sp", ones32[:], onesf[:, :], [], ["ones32"])
        CP(P, "dve", onesb[:], ones32[:], ["ones32"], ["onesb"])

        evac_i = [0]

        def load_w_impl(c0, ncols):
            sk, st = wst.next()
            bk, bt = wb.next()
            DMA(P, "sp", st[:, :, 0:ncols], w_v[:, :, c0:c0 + ncols], [], [sk])
            for k in range(8):
                eng = ("dve", "pool")[k % 2]
                TS(P, eng, bt[:, k, 0:ncols], st[:, k, 0:ncols], gm[:, k:k + 1], None, ALU.mult, None,
                   [sk, "gm"], [(bk, k)])
            return [(bk, k) for k in range(8)], bt

        wlist = []
        for _ps in range(NPASS):
            if "fm" in phases:
                wlist += [(O_AQ, 512), (O_AK, 512)]
            if "tm" in phases:
                wlist += [(O_AK, 512), (O_AV, 512), (O_AO, 512)] + [(O_GTS + j * 512, 512) for j in range(6)] + [(O_AG, 16)]
            if "conv" in phases:
                wlist += [(O_GLU, 512), (O_GLU + 512, 512)]
            if "mla" in phases:
                wlist += [(O_CQ, 384), (O_CKV, 288)]
        issued = []

        def nextw(c0, ncols):
            if not issued:
                issued.append((wlist[0], load_w_impl(*wlist.pop(0))))
            req, cur = issued.pop(0)
            assert req == (c0, ncols), (req, c0, ncols)
            if wlist:
                issued.append((wlist[0], load_w_impl(*wlist.pop(0))))
            return cur

        for ps_i in range(NPASS):
            t0 = ps_i * TP
            for t in range(NTP // 128):
                xk, xt = xin.next()
                sk2, stt = stat.next()
                xsk, xst = xs.next()
                pk, pt = ptr.next()
                r0 = t0 + t * 128
                DMA(P, "sp", xt[:], xe[r0:r0 + 128, :], [], [xk])
                ACT(P, sqj[:], xt[:], AF.Square, [xk], ["sqj"])
                RSUM(P, "dve", stt[:, 0:1], sqj[:], ["sqj"], [sk2])
                ACT(P, stt[:, 1:2], stt[:, 0:1], AF.Sqrt, [sk2], [sk2], scale=1.0 / D, bias=EPS)
                RECIP(P, "dve", stt[:, 1:2], stt[:, 1:2], [sk2], [sk2])
                ACT(P, xst[:], xt[:], AF.Copy, [xk, sk2], [xsk], scale=stt[:, 1:2])
                for k in range(8):
                    TR(P, pt[:, k * 128:(k + 1) * 128], xst[:, k * 128:(k + 1) * 128], ident[:],
                       [xsk, "ident"], [pk])
                CP(P, ("dve", "pool")[0], xnT[:, :, t * 128:(t + 1) * 128],
                   pt[:].rearrange("p (k t) -> p k t", k=8), [pk], [("xnT", t)])

            def xk_keys(tok0, ntok):
                return [("xnT", t) for t in range(tok0 // 128, (tok0 + ntok - 1) // 128 + 1)]

            if "fm" in phases:
                for (c0, dst) in ((O_AQ, QT), (O_AK, KT)):
                    wk, wt = nextw(c0, 512)
                    for cb in range(4):
                        for tb in range(TP // 512):
                            tk0 = HALO + tb * 512
                            ak, at = pacc.next()
                            for k in range(8):
                                MM(P, at[:, :], wt[:, k, cb * 128:(cb + 1) * 128], xnT[:, k, tk0:tk0 + 512],
                                   k == 0, k == 7, [wk[k]] + xk_keys(tk0, 512), [ak])
                            okk, ot = ob.next()
                            evac_i[0] += 1
                            CP(P, ("act", "dve")[evac_i[0] % 2], ot[:, :], at[:, :], [ak], [okk])
                            DMA(P, "sp", dst[cb * 128:(cb + 1) * 128, t0 + tb * 512:t0 + (tb + 1) * 512], ot[:, :],
                                [okk], [])

            if "tm" in phases:
                blocks = [(O_AK, Kt, 0, "copy"), (O_AV, Vt, 0, "copy"), (O_AO, OG, 0, "sig")]
                for j in range(6):
                    blocks.append((O_GTS + j * 512, GTS, j * 512, "sig"))
                for (c0, dst, dc0, mode) in blocks:
                    wk, wt = nextw(c0, 512)
                    for t in range(TP // 128):
                        tk0 = HALO + t * 128
                        ak, at = pacc.next()
                        for k in range(8):
                            MM(P, at[:, :], xnT[:, k, tk0:tk0 + 128], wt[:, k, :], k == 0, k == 7,
                               [wk[k]] + xk_keys(tk0, 128), [ak])
                        okk, ot = ob.next()
                        if mode == "sig":
                            ACT(P, ot[:, :], at[:, :], AF.Sigmoid, [ak], [okk])
                        else:
                            evac_i[0] += 1
                            CP(P, ("act", "dve")[evac_i[0] % 2], ot[:, :], at[:, :], [ak], [okk])
                        if dst is GTS:
                            DMA(P, "sp", dst[t0 + t * 128:t0 + (t + 1) * 128, dc0:dc0 + 512], ot[:, :], [okk], [])
                        else:
                            DMA(P, "sp", dst[:, t0 + t * 128:t0 + (t + 1) * 128, :].rearrange("h t d -> t h d"),
                                ot[:, :].rearrange("p (h d) -> p h d", h=4), [okk], [])
                wk, wt = nextw(O_AG, 16)
                for t in range(TP // 128):
                    tk0 = HALO + t * 128
                    ak, at = pacc.next()
                    for k in range(8):
                        MM(P, at[:, 0:16], xnT[:, k, tk0:tk0 + 128], wt[:, k, 0:16], k == 0, k == 7,
                           [wk[k]] + xk_keys(tk0, 128), [ak])
                    CP(P, "dve", g4sb[:, t, :].rearrange("p (h g) -> p h g", h=4), at[:, 0:16].rearrange("p (g h) -> p h g", h=4),
                       [ak], [("g4", t)])
                for h in range(4):
                    DMA(P, "sp", G4[h, t0:t0 + TP, :].rearrange("(t p) g -> p t g", p=128), g4sb[:, 0:TP // 128, h * 4:(h + 1) * 4],
                        [("g4", t) for t in range(TP // 128)], [])

            if "conv" in phases:
                wak, wat = nextw(O_GLU, 512)
                wgk, wgt = nextw(O_GLU + 512, 512)
                blks = [(b0, min(512, NTP - b0)) for b0 in range(0, NTP, 512)]
                for g in range(4):
                    for (b0, bn) in blks:
                        ak, at = pacc.next()
                        gk, gt = pacc.next()
                        for k in range(8):
                            MM(P, at[:, 0:bn], wat[:, k, g * 128:(g + 1) * 128], xnT[:, k, b0:b0 + bn],
                               k == 0, k == 7, [wak[k]] + xk_keys(b0, bn), [ak])
                        for k in range(8):
                            MM(P, gt[:, 0:bn], wgt[:, k, g * 128:(g + 1) * 128], xnT[:, k, b0:b0 + bn],
                               k == 0, k == 7, [wgk[k]] + xk_keys(b0, bn), [gk])
                        fk, ft = f32t.next()
                        ACT(P, ft[:, 0:bn], gt[:, 0:bn], AF.Sigmoid, [gk], [fk])
                        TT(P, "dve", uT[:, g, b0:b0 + bn], at[:, 0:bn], ft[:, 0:bn], ALU.mult, [ak, fk],
                           [("uT", g, b0 // 512)])
                for tb in range(TP // 512):
                    c0 = HALO + tb * 512 - 15
                    ukeys = lambda g: [("uT", g, j) for j in range(c0 // 512, (c0 + 542 - 1) // 512 + 1)]
                    for g in range(4):
                        eng = "dve"
                        ck = ("cacc", g)
                        ca = cacc[g]
                        TS(P, eng, ca[:, :], uT[:, g, c0:c0 + 512], cpar[:, g, 0:1], cpar[:, g, 31:32],
                           ALU.mult, ALU.add, ukeys(g) + ["cpar"], [ck])
                        for k in range(1, 31):
                            STT(P, eng, ca[:, :], uT[:, g, c0 + k:c0 + k + 512], cpar[:, g, k:k + 1], ca[:, :],
                                ALU.mult, ALU.add, ukeys(g) + ["cpar", ck], [ck])
                    mk, mt = pacc.next()
                    for g in range(4):
                        MM(P, mt[:, :], ones32[:, :], cacc[g][:, :], g == 0, g == 3, ["ones32", ("cacc", g)], [mk])
                    for g in range(4):
                        STT(P, "dve", cacc[g][:, :], mt[:, :], -1.0 / 512, cacc[g][:, :], ALU.mult, ALU.add,
                            [mk, ("cacc", g)], [("cacc", g)])
                        ACT(P, csq[g][:, :], cacc[g][:, :], AF.Square, [("cacc", g)], [("csq", g)])
                    vk, vt = pacc.next()
                    for g in range(4):
                        MM(P, vt[:, :], ones32[:, :], csq[g][:, :], g == 0, g == 3, ["ones32", ("csq", g)], [vk])
                    fk, ft = f32t.next()
                    ACT(P, ft[:, :], vt[:, :], AF.Sqrt, [vk], [fk], scale=1.0 / 512, bias=EPS)
                    RECIP(P, "dve", ft[:, :], ft[:, :], [fk], [fk])
                    for g in range(4):
                        TT(P, "dve", csq[g][:, :], cacc[g][:, :], ft[:, :], ALU.mult, [("cacc", g), fk], [("csq", g)])
                        TS(P, "pool", csq[g][:, :], csq[g][:, :], cpar[:, g, 32:33], cpar[:, g, 33:34],
                           ALU.mult, ALU.add, [("csq", g), "cpar"], [("csq", g)])
                        okk, ot = ob.next()
                        ACT(P, ot[:, :], csq[g][:, :], AF.Silu, [("csq", g)], [okk])
                        DMA(P, "sp", UT[g * 128:(g + 1) * 128, t0 + tb * 512:t0 + (tb + 1) * 512], ot[:, :], [okk], [])

            if "mla" in phases:
                wqk, wqt = nextw(O_CQ, 384)
                wkk, wkt = nextw(O_CKV, 288)
                for tb in range(TP // 512):
                    tk0 = HALO + tb * 512
                    g0 = t0 + tb * 512
                    DMA(P, "sp", rope_sb[:, :, :], ropeT[:, :, g0:g0 + 512], [], ["rope"])
                    for (wk_, wt_, nblk, lat, latn, lkey, dim) in ((wqk, wqt, 3, cql, cqn, "cq", 384.0),
                                                                 (wkk, wkt, 2, ckl, ckn, "ckv", 256.0)):
                        for j in range(nblk):
                            ak, at = pacc.next()
                            for k in range(8):
                                MM(P, at[:, :], wt_[:, k, j * 128:(j + 1) * 128], xnT[:, k, tk0:tk0 + 512],
                                   k == 0, k == 7, [wk_[k]] + xk_keys(tk0, 512), [ak])
                            CP(P, "dve", lat[:, j, :], at[:, :], [ak], [(lkey, j)])
                            ACT(P, latsq[:, j, :], lat[:, j, :], AF.Square, [(lkey, j)], [(lkey + "sq", j)])
                        sk_, st_ = pacc.next()
                        for j in range(nblk):
                            MM(P, st_[:, :], onesb[:, :], latsq[:, j, :], j == 0, j == nblk - 1,
                               ["onesb", (lkey + "sq", j)], [sk_])
                        fk, ft = f32t.next()
                        ACT(P, ft[:, :], st_[:, :], AF.Sqrt, [sk_], [fk], scale=1.0 / dim, bias=EPS)
                        RECIP(P, "dve", ft[:, :], ft[:, :], [fk], [fk])
                        for j in range(nblk):
                            if "dbg5" in phases:
                                TT(P, "dve", lat[:, j, :], lat[:, j, :], ft[:, :], ALU.mult,
                                   [(lkey, j), fk], [(lkey, j)])
                                CP(P, "act", latn[:, j, :], lat[:, j, :], [(lkey, j)], [(lkey + "n", j)])
                            else:
                                TT(P, "dve", latn[:, j, :], lat[:, j, :], ft[:, :], ALU.mult,
                                   [(lkey, j), fk], [(lkey + "n", j)])
                    if "nokr" not in phases:
                        ak, at = pacc.next()
                        for k in range(8):
                            MM(P, at[:, :], wkrp[:, k, :], xnT[:, k, tk0:tk0 + 512], k == 0, k == 7,
                               ["wkrp"] + xk_keys(tk0, 512), [ak])
                        CP(P, "act", krt[0:96, :], at[0:96, :], [ak], ["krt"])
                    cqn_keys = [("cqn", j) for j in range(3)]
                    ckn_keys = [("ckvn", j) for j in range(2)]
                    for h in (range(8) if "nomlah" not in phases else []):
                        for which in ("q", "k"):
                            ak, at = pacc.next()
                            xk_, xt_ = hx.next()
                            if which == "q":
                                for j in range(3):
                                    MM(P, at[0:96, :], wuqb[:, j, h * 96:(h + 1) * 96], cqn[:, j, :], j == 0, j == 2,
                                       ["wuqb"] + cqn_keys, [ak])
                                CP(P, "act", xt_[0:96, :], at[0:96, :], [ak], [xk_])
                            else:
                                for j in range(2):
                                    MM(P, at[0:64, :], wukvb[:, j, h * 64:(h + 1) * 64], ckn[:, j, :], j == 0, j == 1,
                                       ["wukvb"] + ckn_keys, [ak])
                                CP(P, "act", xt_[0:64, :], at[0:64, :], [ak], [xk_])
                                CP(P, "pool", xt_[64:96, :], krt[64:96, :], ["krt"], [xk_])
                            sqk, sqt = hsq.next()
                            ACT(P, sqt[0:96, :], xt_[0:96, :], AF.Square, [xk_], [sqk])
                            sk_, st_ = pacc.next()
                            MM(P, st_[0:96, :], onesb[0:96, 0:96], sqt[0:96, :], True, True, ["onesb", sqk], [sk_])
                            fk, ft = f32t.next()
                            ACT(P, ft[0:96, :], st_[0:96, :], AF.Sqrt, [sk_], [fk], scale=1.0 / 96, bias=EPS)
                            RECIP(P, "dve", ft[0:96, :], ft[0:96, :], [fk], [fk])
                            gcol = 0 if which == "q" else 1
                            xgk, xgt = hxg.next()
                            TS(P, "dve", xgt[0:96, :], xt_[0:96, :], gqk_sb[0:96, gcol:gcol + 1], None, ALU.mult, None,
                               [xk_, "gqk"], [xgk])
                            rk, rt = pacc.next()
                            MM(P, rt[0:96, :], rmat_sb[0:96, 0:96], xgt[0:96, :], True, True, ["rmat", xgk], [rk])
                            t1k, t1 = f32t.next()
                            TT(P, "pool", t1[0:96, :], xgt[0:96, :], rope_sb[:, 0, :], ALU.mult, [xgk, "rope"], [t1k])
                            t2k, t2 = f32t.next()
                            TT(P, "dve", t2[0:96, :], rt[0:96, :], rope_sb[:, 1, :], ALU.mult, [rk, "rope"], [t2k])
                            TT(P, "pool", t1[0:96, :], t1[0:96, :], t2[0:96, :], ALU.add, [t1k, t2k], [t1k])
                            okk, ot = ob.next()
                            TT(P, "dve", ot[0:96, :], t1[0:96, :], ft[0:96, :], ALU.mult, [t1k, fk], [okk])
                            dst = MQ if which == "q" else MK
                            DMA(P, "sp", dst[h, :, g0:g0 + 512], ot[0:96, :], [okk], [])
                    if "dupgrp" in phases:
                        for rep in range(2):
                            ak, at = pacc.next()
                            for k in range(8):
                                MM(P, at[:, :], wkt[:, k, 0:128], xnT[:, k, tk0:tk0 + 512],
                                   k == 0, k == 7, [wkk[k]] + xk_keys(tk0, 512), [ak])
                    for t in (range(4 if "v_one" not in phases else 1) if "nomlav" not in phases else []):
                        ak, at = pacc.next()
                        for j in range(2):
                            MM(P, at[:, :], (xnT[:, j, tk0 + t * 128:tk0 + (t + 1) * 128] if "dbg1" in phases else ckn[:, j, t * 128:(t + 1) * 128]),
                               (wkt[:, j, :] if "dbg2" in phases else (wukvb[:, j, 0:512] if "dbg4" in phases else wukvb[:, j, 512:1024])), j == 0, j == 1,
                               ["wukvb"] + ckn_keys, [ak])
                        okk, ot = ob.next()
                        if "v_noevac" in phases:
                            continue
                        CP(P, "dve", ot[:, :], at[:, :], [ak], [okk])
                        if "v_nodma" in phases:
                            continue
                        DMA(P, "sp", MV[:, :, (g0 + t * 128) // 128, :].rearrange("h p d -> p h d"),
                            ot[:, :].rearrange("p (h d) -> p h d", h=8), [okk], [])

        P.emit()
        stats = P.stats
    return nc, stats


def _gain_cols(g, nch):
    return np.ascontiguousarray(g.reshape(nch, 128).T)


def rope_tables():
    pos = np.arange(S, dtype=np.float32)
    inv = (10000.0 ** (-np.arange(0, 32, 2, dtype=np.float32) / np.float32(32))).astype(np.float32)
    ang = (pos[:, None] * inv[None, :]).astype(np.float32)
    c = np.cos(ang.astype(np.float64)).astype(np.float32)
    s = np.sin(ang.astype(np.float64)).astype(np.float32)
    CT = np.ones((96, S), np.float32)
    ST = np.zeros((96, S), np.float32)
    CT[64:80] = c.T
    CT[80:96] = c.T
    ST[64:80] = s.T
    ST[80:96] = s.T
    return CT, ST


def consts():
    R = np.zeros((96, 96), np.float32)
    for j in range(16):
        R[80 + j, 64 + j] = -1.0
        R[64 + j, 80 + j] = 1.0
    return dict(identb=np.eye(128, dtype=np.float32).astype(NPBF), rmat=R.astype(NPBF),
                onesf=np.ones((128, 128), np.float32))


def stageA_inmaps(x, prm, l):
    CT, ST = rope_tables()
    cst = consts()
    convp = np.zeros((128, 4, 34), np.float32)
    cw = prm["conv_w"][l]
    convp[:, :, 0:31] = cw.T.reshape(4, 128, 31).transpose(1, 0, 2)
    convp[:, :, 31] = prm["conv_b"][l].reshape(4, 128).T
    convp[:, :, 32] = prm["conv_ln_g"][l].reshape(4, 128).T
    convp[:, :, 33] = prm["conv_ln_b"][l].reshape(4, 128).T
    maps = []
    for c in range(NCORES):
        b, q = c // 4, c % 4
        s0 = q * TPC
        xe = np.zeros((TPC + 2 * HALO, D), np.float32)
        lo, hi = max(0, s0 - HALO), min(S, s0 + TPC + HALO)
        xe[lo - (s0 - HALO):hi - (s0 - HALO)] = x[b, lo:hi]
        rope = np.stack([CT[:, s0:s0 + TPC], ST[:, s0:s0 + TPC]], axis=1)
        maps.append(dict(
            xe=xe, w_in=prm["w_in"][l], gmix=_gain_cols(prm["mix_norm_g"][l], 8),
            identb=cst["identb"], convp=convp, gcq=_gain_cols(prm["cq_norm_g"][l], 3),
            gckv=_gain_cols(prm["ckv_norm_g"][l], 2), w_uq=prm["w_uq"][l], w_ukv=prm["w_ukv"][l],
            gqk=np.ascontiguousarray(np.stack([prm["q_norm_g"][l], prm["k_norm_g"][l]], axis=1)),
            ropeT=np.ascontiguousarray(rope), rmat=cst["rmat"], onesf=cst["onesf"]))
    return maps


def build_mlstm(nch=S // 128, env=None, pre=None):
    nc, es, C, P = _begin(env, pre)
    ns = nch * 128
    lnscale = math.log(128.0 ** -0.5)
    with es:
        qTd = C.dram("qT", [128, ns], BF16, "ExternalInput")
        ktd = C.dram("kt", [ns, 128], BF16, "ExternalInput")
        vtd = C.dram("vt", [ns, 128], BF16, "ExternalInput")
        g4d = C.dram("g4", [ns, 4], F32, "ExternalInput")
        bifd = C.dram("bif", [128, 4], F32, "ExternalInput")
        ogd = C.dram("og", [ns, 128], BF16, "ExternalInput")
        gAd = C.dram("gA", [128, 128], F32, "ExternalInput")
        cmat = C.dram("cmat", [128, 6, 128], F32, "ExternalInput")
        identbd = C.dram("identb", [128, 128], BF16, "ExternalInput")
        HT = C.dram("HT", [128, ns], BF16, "ExternalOutput")

        qT = C.sb([128, ns], BF16, "qT")
        kt = C.sb([128, nch, 128], BF16, "kt")
        vt = C.sb([128, nch, 129], BF16, "vt")
        hacc = C.sb([128, nch, 128], F32, "hacc")
        g4 = C.sb([128, nch, 4], F32, "g4")
        bif = C.sb([128, 4], F32, "bif")
        nbif = C.sb([128, 4], F32, "nbif")
        gA = C.sb([128, 128], F32, "gA")
        cm = C.sb([128, 6, 128], F32, "cm")
        identb = C.sb([128, 128], BF16, "identb")
        gt = {n: C.sb([128, nch], F32, n) for n in ("lf", "ib", "bcum", "gtot", "biasS", "wint", "wk", "dec", "tmp")}
        Cf = C.sb([128, 129], F32, "Cf")
        Cb = C.sb([128, 129], BF16, "Cb")
        LF = Rot([(("LF", i), C.sb([128, 128], F32, "LF")) for i in range(2)])
        Dm = Rot([(("Dm", i), C.sb([128, 128], F32, "Dm")) for i in range(2)])
        kTc = Rot([(("kTc", i), C.sb([128, 128], BF16, "kTc")) for i in range(2)])
        SD = Rot([(("SD", i), C.sb([128, 128], BF16, "SD")) for i in range(2)])
        isb = Rot([(("isb", i), C.sb([128, 129], F32, "isb")) for i in range(2)])
        num = Rot([(("num", i), C.sb([128, 129], F32, "num")) for i in range(2)])
        dn = Rot([(("dn", i), C.sb([128, 2], F32, "dn")) for i in range(2)])
        Vw = Rot([(("Vw", i), C.sb([128, 129], BF16, "Vw")) for i in range(2)])
        ogt = Rot([(("ogt", i), C.sb([128, 128], BF16, "ogt")) for i in range(2)])
        hn = Rot([(("hn", i), C.sb([128, 128], F32, "hn")) for i in range(2)])
        hb = Rot([(("hb", i), C.sb([128, 128], BF16, "hb")) for i in range(2)])
        hT = Rot([(("hT", i), C.sb([128, 128], BF16, "hT")) for i in range(2)])
        sq = C.sb([128, 128], F32, "sq")
        pA = Rot([(("pA", i), C.ps([128, 512], F32, "pA")) for i in range(6)])
        pB = Rot([(("pB", i), C.ps([128, 1024], BF16, "pB")) for i in range(2)])

        DMA(P, "sp", qT[:, :], qTd[:, :], [], ["qT"])
        DMA(P, "sp", kt[:, :, :], ktd.ap().rearrange("(c p) d -> p c d", p=128), [], ["kt"])
        DMA(P, "sp", vt[:, :, 0:128], vtd.ap().rearrange("(c p) d -> p c d", p=128), [], ["vt"])
        MEMSET(P, "pool", vt[:, :, 128:129], 1.0, ["vt1"])
        DMA(P, "sp", g4[:, :, :], g4d.ap().rearrange("(c p) g -> p c g", p=128), [], ["g4"])
        DMA(P, "sp", bif[:, :], bifd[:, :], [], ["bif"])
        DMA(P, "sp", gA[:, :], gAd[:, :], [], ["gA"])
        DMA(P, "sp", cm[:, :, :], cmat[:, :, :], [], ["cm"])
        DMA(P, "sp", identb[:, :], identbd[:, :], [], ["identb"])
        TS(P, "dve", nbif[:, :], bif[:, :], -1.0, None, ALU.mult, None, ["bif"], ["nbif"])
        ident32 = cm[:, 4, :]
        ones32 = cm[:, 5, :]
        for d in range(2):
            Ud = cm[:, d, :]
            NEGd = cm[:, 2 + d, :]
            ic, fc = 2 * d, 2 * d + 1
            ACT(P, gt["tmp"][:, :], g4[:, :, fc], AF.Exp, ["g4", "nbif"], ["tmp"], scale=-1.0, bias=nbif[:, fc:fc + 1])
            ACT(P, gt["tmp"][:, :], gt["tmp"][:, :], AF.Ln, ["tmp"], ["tmp"], bias=1.0)
            TS(P, "dve", gt["lf"][:, :], gt["tmp"][:, :], -1.0, None, ALU.mult, None, ["tmp"], ["lf"])
            TS(P, "dve", gt["ib"][:, :], g4[:, :, ic], bif[:, ic:ic + 1], lnscale, ALU.add, ALU.add, ["g4", "bif"], ["ib"])
            bk, bp = pA.next()
            MM(P, bp[:, 0:nch], Ud, gt["lf"][:, :], True, True, ["cm", "lf"], [bk])
            CP(P, "dve", gt["bcum"][:, :], bp[:, 0:nch], [bk], ["bcum"])
            gk, gp = pA.next()
            MM(P, gp[:, 0:nch], ones32, gt["lf"][:, :], True, True, ["cm", "lf"], [gk])
            CP(P, "dve", gt["gtot"][:, :], gp[:, 0:nch], [gk], ["gtot"])
            TT(P, "dve", gt["biasS"][:, :], gt["ib"][:, :], gt["bcum"][:, :], ALU.subtract, ["ib", "bcum"], ["biasS"])
            ACT(P, gt["wint"][:, :], gt["bcum"][:, :], AF.Exp, ["bcum"], ["wint"])
            TT(P, "dve", gt["tmp"][:, :], gt["biasS"][:, :], gt["gtot"][:, :], ALU.add, ["biasS", "gtot"], ["tmp"])
            ACT(P, gt["wk"][:, :], gt["tmp"][:, :], AF.Exp, ["tmp"], ["wk"])
            ACT(P, gt["dec"][:, :], gt["gtot"][:, :], AF.Exp, ["gtot"], ["dec"])
            MEMSET(P, "dve", Cf[:, :], 0.0, ["Cf"])
            MEMSET(P, "pool", Cb[:, :], 0.0, ["Cb"])
            order = range(nch) if d == 0 else range(nch - 1, -1, -1)
            for c in order:
                lk, lt = LF.next()
                TS(P, "pool", lt[:, :], ones32, gt["lf"][:, c:c + 1], None, ALU.mult, None, ["cm", "lf"], [lk])
                dk, dp = pA.next()
                MM(P, dp[:, 0:128], lt[:, :], Ud, True, False, [lk, "cm"], [dk])
                MM(P, dp[:, 0:128], ident32, NEGd, False, True, ["cm"], [dk])
                mk_, mt_ = Dm.next()
                ACT(P, mt_[:, :], dp[:, 0:128], AF.Exp, [dk, "biasS"], [mk_], bias=gt["biasS"][:, c:c + 1])
                tk, tp = pB.next()
                TR(P, tp[:, 0:128], kt[:, c, :], identb[:, :], ["kt", "identb"], [tk])
                kck, kct = kTc.next()
                CP(P, "dve", kct[:, :], tp[:, 0:128], [tk], [kck])
                sk, sp_ = pA.next()
                MM(P, sp_[:, 0:128], kct[:, :], qT[:, c * 128:(c + 1) * 128], True, True, [kck, "qT"], [sk])
                sdk, sdt = SD.next()
                TT(P, "dve", sdt[:, :], sp_[:, 0:128], mt_[:, :], ALU.mult, [sk, mk_], [sdk])
                nk_, np_ = pA.next()
                MM(P, np_[:, 0:129], sdt[:, :], vt[:, c, :], True, True, [sdk, "vt", "vt1"], [nk_])
                ik, ip = pA.next()
                MM(P, ip[:, 0:129], qT[:, c * 128:(c + 1) * 128], Cb[:, :], True, True, ["qT", "Cb"], [ik])
                isk, ist = isb.next()
                ACT(P, ist[:, :], ip[:, 0:129], AF.Copy, [ik, "wint"], [isk], scale=gt["wint"][:, c:c + 1])
                nmk, nmt = num.next()
                TT(P, "dve", nmt[:, :], np_[:, 0:129], ist[:, :], ALU.add, [nk_, isk], [nmk])
                dnk, dnt = dn.next()
                ACT(P, dnt[:, 0:1], nmt[:, 128:129], AF.Abs, [nmk], [dnk])
                TS(P, "dve", dnt[:, 0:1], dnt[:, 0:1], 1.0, None, ALU.max, None, [dnk], [dnk])
                RECIP(P, "dve", dnt[:, 1:2], dnt[:, 0:1], [dnk], [dnk])
                if d == 0:
                    TS(P, "dve", hacc[:, c, :], nmt[:, 0:128], dnt[:, 1:2], None, ALU.mult, None, [nmk, dnk], [("hacc", c)])
                else:
                    STT(P, "dve", hacc[:, c, :], nmt[:, 0:128], dnt[:, 1:2], hacc[:, c, :], ALU.mult, ALU.add,
                        [nmk, dnk, ("hacc", c)], [("hacc", c)])
                vwk, vwt = Vw.next()
                TS(P, "pool", vwt[:, :], vt[:, c, :], gt["wk"][:, c:c + 1], None, ALU.mult, None, ["vt", "vt1", "wk"], [vwk])
                ck, cp_ = pA.next()
                MM(P, cp_[:, 0:129], kt[:, c, :], vwt[:, :], True, True, ["kt", vwk], [ck])
                STT(P, "dve", Cf[:, :], Cf[:, :], gt["dec"][:, c:c + 1], cp_[:, 0:129], ALU.mult, ALU.add,
                    ["Cf", "dec", ck], ["Cf"])
                CP(P, "act", Cb[:, :], Cf[:, :], ["Cf"], ["Cb"])
        for c in range(nch):
            ogk, ogt_ = ogt.next()
            DMA(P, "sp", ogt_[:, :], ogd[c * 128:(c + 1) * 128, :], [], [ogk])
            dnk, dnt = dn.next()
            ACT(P, sq[:, :], hacc[:, c, :], AF.Square, [("hacc", c)], ["sq"])
            RSUM(P, "dve", dnt[:, 0:1], sq[:, :], ["sq"], [dnk])
            ACT(P, dnt[:, 1:2], dnt[:, 0:1], AF.Sqrt, [dnk], [dnk], scale=1.0 / 128, bias=EPS)
            RECIP(P, "dve", dnt[:, 1:2], dnt[:, 1:2], [dnk], [dnk])
            hk, ht = hn.next()
            STT(P, "dve", ht[:, :], hacc[:, c, :], dnt[:, 1:2], gA[:, :], ALU.mult, ALU.mult, [("hacc", c), dnk, "gA"], [hk])
            hbk, hbt = hb.next()
            TT(P, "pool", hbt[:, :], ht[:, :], ogt_[:, :], ALU.mult, [hk, ogk], [hbk])
            tk, tp = pB.next()
            TR(P, tp[:, 0:128], hbt[:, :], identb[:, :], [hbk, "identb"], [tk])
            htk, htt = hT.next()
            CP(P, "act", htt[:, :], tp[:, 0:128], [tk], [htk])
            DMA(P, "sp", HT[:, c * 128:(c + 1) * 128], htt[:, :], [htk], [])
        P.emit()
        stats = P.stats
    return nc, stats


def mlstm_consts():
    s_ = np.arange(128)[:, None]
    t_ = np.arange(128)[None, :]
    U = (s_ <= t_).astype(np.float32)
    cm = np.zeros((128, 6, 128), np.float32)
    cm[:, 0] = U
    cm[:, 1] = U.T
    cm[:, 2] = np.where(s_ <= t_, 0.0, -30000.0)
    cm[:, 3] = np.where(s_ >= t_, 0.0, -30000.0)
    cm[:, 4] = np.eye(128)
    cm[:, 5] = 1.0
    return dict(cmat=cm, identb=np.eye(128, dtype=np.float32).astype(NPBF))


def build_merge(ntile=TPC // 128, env=None, pre=None):
    nc, es, C, P = _begin(env, pre)
    nt = ntile * 128
    with es:
        xd = C.dram("x", [nt, D], F32, "ExternalInput")
        srcs = [C.dram(n, [512, nt], BF16, "ExternalInput") for n in ("HT", "UT", "OT")]
        gtsd = C.dram("GTS", [nt, 3072], BF16, "ExternalInput")
        wds = [C.dram(n, [512, D], F32, "ExternalInput") for n in ("w_a", "w_b", "w_c")]
        wod = C.dram("w_o", [D, D], F32, "ExternalInput")
        gfd = C.dram("gffn", [128, 8], F32, "ExternalInput")
        wrd = C.dram("w_r", [D, 16], F32, "ExternalInput")
        identbd = C.dram("identb", [128, 128], BF16, "ExternalInput")
        ident32d = C.dram("ident32", [128, 128], F32, "ExternalInput")
        x1d = C.dram("x1", [nt, D], F32, "ExternalOutput")
        xn2d = C.dram("xn2T", [D, nt], BF16, "ExternalOutput")
        affd = C.dram("aff", [nt, 16], F32, "ExternalOutput")
        affTd = C.dram("affT", [16, nt], F32, "ExternalOutput")
        affTs = C.sb([16, nt], F32, "affTs")

        wbr = [C.sb([128, 4, D], BF16, "wbr") for _ in range(3)]
        wo = C.sb([128, 8, D], BF16, "wo")
        wst = Rot([(("wst", i), C.sb([128, 4, D], F32, "wst")) for i in range(2)])
        wr = C.sb([128, 8, 16], F32, "wr")
        gf = C.sb([128, 8], F32, "gf")
        gfull = C.sb([128, 8, 128], F32, "gfull")
        identb = C.sb([128, 128], BF16, "identb")
        ident32 = C.sb([128, 128], F32, "ident32")
        srct = [Rot([((("src", b), i), C.sb([128, 4, 128], BF16, "src")) for i in range(2)]) for b in range(3)]
        gts = Rot([(("gts", i), C.sb([128, 3072], BF16, "gts")) for i in range(2)])
        xt = Rot([(("xt", i), C.sb([128, D], F32, "xt")) for i in range(2)])
        mg = C.sb([128, D], F32, "mg")
        tmpm = Rot([(("tmpm", i), C.sb([128, 512], F32, "tmpm")) for i in range(2)])
        mgb = C.sb([128, D], BF16, "mgb")
        mT = C.sb([128, 8, 128], BF16, "mT")
        x1 = Rot([(("x1", i), C.sb([128, D], F32, "x1")) for i in range(2)])
        sqj = C.sb([128, D], F32, "sqj")
        xs = C.sb([128, D], F32, "xs")
        st = Rot([(("st", i), C.sb([128, 16], F32, "st")) for i in range(2)])
        xT32 = C.sb([128, 8, 128], F32, "xT32")
        xTb = Rot([(("xTb", i), C.sb([128, 8, 128], BF16, "xTb")) for i in range(2)])
        lg = C.sb([128, 16], F32, "lg")
        ex = C.sb([128, 16], F32, "ex")
        affs = C.sb([128, ntile, 16], F32, "affs")
        pA = Rot([(("pA", i), C.ps([128, 512], F32, "pA")) for i in range(5)])
        pB = C.ps([128, 1024], BF16, "pB")

        DMA(P, "sp", identb[:, :], identbd[:, :], [], ["identb"])
        DMA(P, "sp", ident32[:, :], ident32d[:, :], [], ["ident32"])
        DMA(P, "sp", gf[:, :], gfd[:, :], [], ["gf"])
        DMA(P, "sp", wr[:, :, :], wrd.ap().rearrange("(c p) e -> p c e", p=128), [], ["wr"])
        for b in range(3):
            sk, stg = wst.next()
            DMA(P, "sp", stg[:, :, :], wds[b].ap().rearrange("(c p) n -> p c n", p=128), [], [sk])
            for c in range(4):
                CP(P, ("dve", "pool")[c % 2], wbr[b][:, c, :], stg[:, c, :], [sk], [("wbr", b)])
        for hh in range(2):
            sk, stg = wst.next()
            DMA(P, "sp", stg[:, :, :], wod.ap().rearrange("(c p) n -> p c n", p=128)[:, hh * 4:(hh + 1) * 4, :], [], [sk])
            for c in range(4):
                CP(P, ("dve", "pool")[c % 2], wo[:, hh * 4 + c, :], stg[:, c, :], [sk], ["wo"])
        for k in range(8):
            TS(P, "pool", gfull[:, k, :], ident32[:, :], 0.0, gf[:, k:k + 1], ALU.mult, ALU.add, ["ident32", "gf"], ["gfull"])
        for t in range(ntile):
            r0 = t * 128
            xk, xt_ = xt.next()
            DMA(P, "sp", xt_[:, :], xd[r0:r0 + 128, :], [], [xk])
            gk, gt_ = gts.next()
            DMA(P, "sp", gt_[:, :], gtsd[r0:r0 + 128, :], [], [gk])
            skeys = []
            stiles = []
            for b in range(3):
                k_, t_ = srct[b].next()
                DMA(P, "sp", t_[:, :, :], srcs[b].ap().rearrange("(c p) t -> p c t", p=128)[:, :, r0:r0 + 128], [], [k_])
                skeys.append(k_)
                stiles.append(t_)
            for b in range(3):
                for hf in range(2):
                    ak, at = pA.next()
                    for c in range(4):
                        MM(P, at[:, :], stiles[b][:, c, :], wbr[b][:, c, hf * 512:(hf + 1) * 512], c == 0, c == 3,
                           [skeys[b], ("wbr", b)], [ak])
                    gsl = gt_[:, b * 1024 + hf * 512:b * 1024 + (hf + 1) * 512]
                    if b == 0:
                        TT(P, "dve", mg[:, hf * 512:(hf + 1) * 512], at[:, :], gsl, ALU.mult, [ak, gk], [("mg", hf)])
                    else:
                        tk, tt_ = tmpm.next()
                        TT(P, "dve", tt_[:, :], at[:, :], gsl, ALU.mult, [ak, gk], [tk])
                        TT(P, "pool", mg[:, hf * 512:(hf + 1) * 512], mg[:, hf * 512:(hf + 1) * 512], tt_[:, :], ALU.add,
                           [("mg", hf), tk], [("mg", hf)])
            CP(P, "act", mgb[:, :], mg[:, :], [("mg", 0), ("mg", 1)], ["mgb"])
            for k in range(8):
                TR(P, pB[:, k * 128:(k + 1) * 128], mgb[:, k * 128:(k + 1) * 128], identb[:, :], ["mgb", "identb"], ["pB"])
            CP(P, "dve", mT[:, :, :], pB[:].rearrange("p (k t) -> p k t", k=8), ["pB"], ["mT"])
            x1k, x1t = x1.next()
            for hf in range(2):
                ak, at = pA.next()
                for k in range(8):
                    MM(P, at[:, :], mT[:, k, :], wo[:, k, hf * 512:(hf + 1) * 512], k == 0, k == 7, ["mT", "wo"], [ak])
                TT(P, "dve", x1t[:, hf * 512:(hf + 1) * 512], at[:, :], xt_[:, hf * 512:(hf + 1) * 512], ALU.add,
                   [ak, xk], [(x1k, hf)])
            DMA(P, "sp", x1d[r0:r0 + 128, :], x1t[:, :], [(x1k, 0), (x1k, 1)], [])
            sk_, st_ = st.next()
            ACT(P, sqj[:, :], x1t[:, :], AF.Square, [(x1k, 0), (x1k, 1)], ["sqj"])
            RSUM(P, "dve", st_[:, 0:1], sqj[:, :], ["sqj"], [sk_])
            ACT(P, st_[:, 1:2], st_[:, 0:1], AF.Sqrt, [sk_], [sk_], scale=1.0 / D, bias=EPS)
            RECIP(P, "dve", st_[:, 1:2], st_[:, 1:2], [sk_], [sk_])
            ACT(P, xs[:, :], x1t[:, :], AF.Copy, [(x1k, 0), (x1k, 1), sk_], ["xs"], scale=st_[:, 1:2])
            for hf in range(2):
                ak, at = pA.next()
                for k in range(4):
                    kk = hf * 4 + k
                    TR(P, at[:, k * 128:(k + 1) * 128], xs[:, kk * 128:(kk + 1) * 128], ident32[:, :], ["xs", "ident32"], [ak])
                TT(P, "dve", xT32[:, hf * 4:(hf + 1) * 4, :], at[:].rearrange("p (k t) -> p k t", k=4),
                   gfull[:, hf * 4:(hf + 1) * 4, :], ALU.mult, [ak, "gfull"], [("xT32", hf)])
            xbk, xbt = xTb.next()
            CP(P, "act", xbt[:, :, :], xT32[:, :, :], [("xT32", 0), ("xT32", 1)], [xbk])
            DMA(P, "sp", xn2d.ap().rearrange("(k p) t -> p k t", p=128)[:, :, r0:r0 + 128], xbt[:, :, :], [xbk], [])
            ak, at = pA.next()
            for k in range(8):
                MM(P, at[:, 0:16], xT32[:, k, :], wr[:, k, :], k == 0, k == 7, [("xT32", 0), ("xT32", 1), "wr"], [ak])
            CP(P, "dve", lg[:, :], at[:, 0:16], [ak], ["lg"])
            RMAX(P, "dve", st_[:, 2:3], lg[:, :], ["lg"], [sk_])
            TS(P, "dve", st_[:, 2:3], st_[:, 2:3], -1.0, None, ALU.mult, None, [sk_], [sk_])
            ACT(P, ex[:, :], lg[:, :], AF.Exp, ["lg", sk_], ["ex"], bias=st_[:, 2:3])
            RSUM(P, "dve", st_[:, 3:4], ex[:, :], ["ex"], [sk_])
            RECIP(P, "dve", st_[:, 3:4], st_[:, 3:4], [sk_], [sk_])
            TS(P, "dve", affs[:, t, :], ex[:, :], st_[:, 3:4], None, ALU.mult, None, ["ex", sk_], [("affs", t)])
            ak, at = pA.next()
            TR(P, at[0:16, 0:128], affs[:, t, :], ident32[:, :], [("affs", t), "ident32"], [ak])
            CP(P, "act", affTs[:, r0:r0 + 128], at[0:16, 0:128], [ak], [("affT", t)])
        DMA(P, "sp", affd.ap().rearrange("(t p) e -> p t e", p=128), affs[:, :, :], [("affs", t) for t in range(ntile)], [])
        DMA(P, "sp", affTd[:, :], affTs[:, :], [("affT", t) for t in range(ntile)], [])
        P.emit()
        stats = P.stats
    return nc, stats


def build_thr(ns=S, cap=2 * S // 16, iters=30, env=None, pre=None):
    nc, es, C, P = _begin(env, pre)
    with es:
        affT = C.dram("affT", [16, ns], F32, "ExternalInput")
        thr = C.dram("thr", [16, 2], F32, "ExternalOutput")
        a = C.sb([16, ns], F32, "a")
        junk = C.sb([16, ns], F32, "junk")
        lh = C.sb([16, 2], F32, "lh")
        w = C.sb([16, 8], F32, "w")
        DMA(P, "sp", a[:, :], affT[:, :], [], ["a"])
        MEMSET(P, "dve", lh[:, 0:1], 0.0, ["lh"])
        MEMSET(P, "dve", lh[:, 1:2], 1.0, ["lh"])
        for it in range(iters):
            TT(P, "dve", w[:, 0:1], lh[:, 0:1], lh[:, 1:2], ALU.add, ["lh"], ["w"])
            TS(P, "dve", w[:, 0:1], w[:, 0:1], 0.5, None, ALU.mult, None, ["w"], ["w"])
            P.op("dve", lambda e: e.tensor_scalar(out=junk[:, :], in0=a[:, :], scalar1=w[:, 0:1], scalar2=0.0,
                                                  op0=ALU.is_ge, op1=ALU.add, accum_out=w[:, 1:2]),
                 ["a", "w"], ["junk", "w"])
            TS(P, "dve", w[:, 2:3], w[:, 1:2], float(cap), None, ALU.is_ge, None, ["w"], ["w"])
            TT(P, "dve", w[:, 3:4], w[:, 0:1], lh[:, 0:1], ALU.subtract, ["w", "lh"], ["w"])
            TT(P, "dve", w[:, 4:5], lh[:, 1:2], w[:, 0:1], ALU.subtract, ["w", "lh"], ["w"])
            STT(P, "dve", lh[:, 0:1], w[:, 3:4], w[:, 2:3], lh[:, 0:1], ALU.mult, ALU.add, ["w", "lh"], ["lh"])
            STT(P, "dve", lh[:, 1:2], w[:, 4:5], w[:, 2:3], w[:, 0:1], ALU.mult, ALU.add, ["w", "lh"], ["lh"])
        DMA(P, "sp", thr[:, :], lh[:, :], ["lh"], [])
        P.emit()
        stats = P.stats
    return nc, stats


def build_ffn(nt=TPC, nexp=16, tb=1024, env=None, pre=None):
    nc, es, C, P = _begin(env, pre)
    FF = 1536
    ntile = nt // 128
    nblk = nt // tb
    with es:
        x1d = C.dram("x1", [nt, D], F32, "ExternalInput")
        xnd = C.dram("xn2T", [D, nt], BF16, "ExternalInput")
        affd = C.dram("aff", [nt, 16], F32, "ExternalInput")
        thrd = C.dram("thr_row", [128, 16], F32, "ExternalInput") if not (pre is not None and "thr16" in pre) else None
        wgd = C.dram("wg", [nexp, D, FF], F32, "ExternalInput")
        wud = C.dram("wu", [nexp, D, FF], F32, "ExternalInput")
        wdd = C.dram("wd", [nexp, FF, D], F32, "ExternalInput")
        x2d = C.dram("x2", [nt, D], F32, "ExternalOutput")

        xb = C.sb([128, 8, tb], BF16, "xb")
        acc = C.sb([128, tb // 128, D], F32, "acc")
        wgb = C.sb([128, 8, FF], BF16, "wgb")
        wub = C.sb([128, 8, FF], BF16, "wub")
        wdb = C.sb([128, 12, D], BF16, "wdb")
        stg = Rot([(("stg", i), C.sb([128, 4096], F32, "stg")) for i in range(2)])
        hT = C.sb([128, 12, tb], BF16, "hT")
        sg = Rot([(("sg", i), C.sb([128, 512], F32, "sg")) for i in range(2)])
        affs = C.sb([128, ntile, 16], F32, "affs")
        gw = C.sb([128, ntile, 16], F32, "gw")
        thr = C.sb([128, 16], F32, "thr")
        xo = Rot([(("xo", i), C.sb([128, D], F32, "xo")) for i in range(2)])
        pA = Rot([(("pA", i), C.ps([128, 512], F32, "pA")) for i in range(7)])

        if pre is not None and "thr16" in pre:
            t16d = pre["thr16"]
            i32d = pre["ident32"]
            t16 = C.sb([16, 2], F32, "t16")
            tbc = C.sb([16, 128], F32, "tbc")
            i16 = C.sb([16, 16], F32, "i16")
            DMA(P, "sp", t16[:, :], t16d[:, :], [], ["t16"])
            DMA(P, "sp", i16[:, :], i32d[0:16, 0:16], [], ["i16"])
            MEMSET(P, "dve", tbc[:, :], 1.0, ["tbc"])
            TS(P, "dve", tbc[:, :], tbc[:, :], t16[:, 0:1], None, ALU.mult, None, ["tbc", "t16"], ["tbc"])
            tk_, tp_ = pA.next()
            MM(P, tp_[:, 0:16], tbc[:, :], i16[:, :], True, True, ["tbc", "i16"], [tk_])
            CP(P, "dve", thr[:, :], tp_[:, 0:16], [tk_], ["thr"])
        else:
            DMA(P, "sp", thr[:, :], thrd[:, :], [], ["thr"])
        DMA(P, "sp", affs[:, :, :], affd.ap().rearrange("(t p) e -> p t e", p=128), [], ["affs"])
        for t in range(ntile):
            TT(P, "dve", gw[:, t, :], affs[:, t, :], thr[:, :], ALU.is_ge, ["affs", "thr"], ["gw"])
            TT(P, "dve", gw[:, t, :], gw[:, t, :], affs[:, t, :], ALU.mult, ["gw", "affs"], ["gw"])
        cv = [0]

        def conv(dst, src, r, w):
            eng = ("dve", "pool", "act")[cv[0] % 3]
            cv[0] += 1
            CP(P, eng, dst, src, r, w)

        def load_gu(e, chs=(0, 1, 2)):
            for ch in chs:
                for (wd_, dstb, key) in ((wgd, wgb, "wgb"), (wud, wub, "wub")):
                    sk, st = stg.next()
                    sv = st[:, :].rearrange("p (c f) -> p c f", c=8)
                    DMA(P, "sp", sv, wd_[e].rearrange("(c p) f -> p c f", p=128)[:, :, ch * 512:(ch + 1) * 512], [], [sk])
                    for hh in range(2):
                        conv(dstb[:, hh * 4:(hh + 1) * 4, ch * 512:(ch + 1) * 512], sv[:, hh * 4:(hh + 1) * 4, :], [sk],
                             [(key, ch)])

        def load_d(e):
            for ch in range(3):
                sk, st = stg.next()
                sv = st[:, :].rearrange("p (c n) -> p c n", c=4)
                DMA(P, "sp", sv, wdd[e].rearrange("(c p) n -> p c n", p=128)[:, ch * 4:(ch + 1) * 4, :], [], [sk])
                for hh in range(2):
                    conv(wdb[:, ch * 4 + hh * 2:ch * 4 + hh * 2 + 2, :], sv[:, hh * 2:hh * 2 + 2, :], [sk], ["wdb"])

        first = True
        for blk in range(nblk):
            b0 = blk * tb
            DMA(P, "sp", xb[:, :, :], xnd.ap().rearrange("(k p) t -> p k t", p=128)[:, :, b0:b0 + tb], [], ["xb"])
            for e in range(nexp):
                if first:
                    load_gu(e)
                    load_d(e)
                    first = False
                nxt = (blk * nexp + e + 1)
                for f in range(12):
                    for tq in range(tb // 512):
                        gk, gp = pA.next()
                        uk, up = pA.next()
                        for k in range(8):
                            MM(P, gp[:, :], wgb[:, k, f * 128:(f + 1) * 128], xb[:, k, tq * 512:(tq + 1) * 512],
                               k == 0, k == 7, [("wgb", f // 4), "xb"], [gk])
                        for k in range(8):
                            MM(P, up[:, :], wub[:, k, f * 128:(f + 1) * 128], xb[:, k, tq * 512:(tq + 1) * 512],
                               k == 0, k == 7, [("wub", f // 4), "xb"], [uk])
                        sk, st = sg.next()
                        ACT(P, st[:, :], gp[:, :], AF.Silu, [gk], [sk])
                        TT(P, "dve", hT[:, f, tq * 512:(tq + 1) * 512], up[:, :], st[:, :], ALU.mult, [uk, sk], [("hT", f)])
                    if f % 4 == 3 and nxt < nblk * nexp:
                        load_gu(nxt % nexp, chs=(f // 4,))
                for tt in range(tb // 128):
                    gcol = gw[:, blk * (tb // 128) + tt, e:e + 1]
                    for hf in range(2):
                        yk, yp = pA.next()
                        for f in range(12):
                            MM(P, yp[:, :], hT[:, f, tt * 128:(tt + 1) * 128], wdb[:, f, hf * 512:(hf + 1) * 512],
                               f == 0, f == 11, [("hT", f), "wdb"], [yk])
                        asl = acc[:, tt, hf * 512:(hf + 1) * 512]
                        if e == 0:
                            TS(P, "dve", asl, yp[:, :], gcol, None, ALU.mult, None, [yk, "gw"], [("acc", tt, hf)])
                        else:
                            STT(P, "dve", asl, yp[:, :], gcol, asl, ALU.mult, ALU.add, [yk, "gw", ("acc", tt, hf)],
                                [("acc", tt, hf)])
                if nxt < nblk * nexp:
                    load_d(nxt % nexp)
            for tt in range(tb // 128):
                xk, xt_ = xo.next()
                r0 = b0 + tt * 128
                DMA(P, "sp", xt_[:, :], x1d[r0:r0 + 128, :], [], [xk])
                TT(P, "pool", xt_[:, :], xt_[:, :], acc[:, tt, :], ALU.add, [xk, ("acc", tt, 0), ("acc", tt, 1)], [xk])
                DMA(P, "sp", x2d[r0:r0 + 128, :], xt_[:, :], [xk], [])
        P.emit()
        stats = P.stats
    return nc, stats


def build_attn(nq=TPC, nk=S, nheads=8, env=None, pre=None):
    nc, es, C, P = _begin(env, pre)
    NKT = nk // 128
    NQB = nq // 512
    scale = 96.0 ** -0.5
    with es:
        mq = C.dram("mq", [nheads, 96, nq], BF16, "ExternalInput")
        mk = C.dram("mk", [nheads, 96, nk], BF16, "ExternalInput")
        mv = C.dram("mv", [nheads, 128, NKT * 64], BF16, "ExternalInput")
        esel = C.dram("esel", [65, 64], F32, "ExternalInput")
        OT = C.dram("OT", [nheads * 64, nq], BF16, "ExternalOutput")

        kT = Rot([(("kT", i), C.sb([96, nk], BF16, "kT")) for i in range(2)])
        vv = Rot([(("vv", i), C.sb([128, NKT, 65], BF16, "vv")) for i in range(2)])
        qT = Rot([(("qT", i), C.sb([96, nq], BF16, "qT")) for i in range(2)])
        pT = Rot([(("pT", i), C.sb([128, 512], BF16, "pT")) for i in range(4)])
        osb = C.sb([65, 512], F32, "osb")
        rbc = C.sb([64, 512], F32, "rbc")
        oo = Rot([(("oo", i), C.sb([64, 512], BF16, "oo")) for i in range(2)])
        es_sb = C.sb([65, 64], F32, "esel")
        sps = Rot([(("sps", i), C.ps([128, 512], F32, "sps")) for i in range(4)])
        ops_ = Rot([(("ops", i), C.ps([128, 512], F32, "ops")) for i in range(2)])
        bps = C.ps([128, 512], F32, "bps")
        DMA(P, "sp", es_sb[:], esel[:, :], [], ["esel"])
        for i in range(2):
            MEMSET(P, "pool", vv.items[i][1][:, :, 64:65], 1.0, [("vv1", i)])
        for h in range(nheads):
            kk, kt_ = kT.next()
            vk, vt_ = vv.next()
            qk, qt_ = qT.next()
            DMA(P, "sp", kt_[:, :], mk[h, :, :], [], [kk])
            DMA(P, "sp", vt_[:, :, 0:64], mv[h, :, :].rearrange("p (t d) -> p t d", d=64), [], [vk])
            DMA(P, "sp", qt_[:, :], mq[h, :, :], [], [qk])
            vkeys = [vk, ("vv1", (vv.i - 1) % 2)]
            for qb in range(NQB):
                ok_, ot_ = ops_.next()

                def s_mm(t, kt_=kt_, qt_=qt_, qb=qb, kk=kk, qk=qk):
                    sk, st = sps.next()
                    MM(P, st[:, :], kt_[:, t * 128:(t + 1) * 128], qt_[:, qb * 512:(qb + 1) * 512], True, True,
                       [kk, qk], [sk])
                    return sk, st
                pend = [s_mm(0)]
                if NKT > 1:
                    pend.append(s_mm(1))
                for t in range(NKT):
                    if t + 2 < NKT:
                        pend.append(s_mm(t + 2))
                    sk, st = pend.pop(0)
                    pk, pt = pT.next()
                    ACT(P, pt[:, :], st[:, :], AF.Exp, [sk], [pk], scale=scale)
                    MM(P, ot_[0:65, :], vt_[:, t, :], pt[:, :], t == 0, t == NKT - 1, vkeys + [pk], [ok_])
                CP(P, "dve", osb[:, :], ot_[0:65, :], [ok_], ["osb"])
                MM(P, bps[0:64, :], es_sb[:, :], osb[:, :], True, True, ["esel", "osb"], ["bps"])
                CP(P, "dve", rbc[:, :], bps[0:64, :], ["bps"], ["rbc"])
                RECIP(P, "dve", rbc[:, :], rbc[:, :], ["rbc"], ["rbc"])
                ook, oot = oo.next()
                TT(P, "dve", oot[:, :], osb[0:64, :], rbc[:, :], ALU.mult, ["osb", "rbc"], [ook])
                DMA(P, "sp", OT[h * 64:(h + 1) * 64, qb * 512:(qb + 1) * 512], oot[:, :], [ook], [])
        P.emit()
        stats = P.stats
    return nc, stats


def attn_consts():
    e = np.zeros((65, 64), np.float32)
    e[64, :] = 1.0
    return dict(esel=e)


RG = [[0, 1, 2, 3], [4, 5, 6, 7]]
LAYER_W = [("w_in", [D, INW], F32), ("gmix", [128, 8], F32), ("convp", [128, 4, 34], F32), ("gcq", [128, 3], F32),
           ("gckv", [128, 2], F32), ("w_uq", [384, 768], F32), ("w_ukv", [256, 1024], F32), ("gqk", [96, 2], F32),
           ("bif", [128, 4], F32), ("gA", [128, 128], F32), ("w_a", [512, D], F32), ("w_b", [512, D], F32),
           ("w_c", [512, D], F32), ("w_o", [D, D], F32), ("gffn", [128, 8], F32), ("w_r", [D, 16], F32),
           ("wg", [16, D, 1536], F32), ("wu", [16, D, 1536], F32), ("wd", [16, 1536, D], F32)]
CONSTS = [("identb", [128, 128], BF16), ("ropeT", [96, 2, TPC], F32), ("rmat", [96, 96], BF16), ("onesf", [128, 128], F32),
          ("cmat", [128, 6, 128], F32), ("ident32", [128, 128], F32), ("esel", [65, 64], F32), ("idx", [128, 16], I32)]


def build_fused(stop=None):
    env = Env()
    nc, P = env.nc, env.P
    BYP = ALU.bypass
    CCB = 256 * 1024
    with env.es:
        def DT(name, shape, dt, kind="Internal"):
            return nc.dram_tensor(name, list(shape), dt, kind=kind)

        def allgather(name, src2d, rows, cols, dt, rkeys, wkey):
            esz = 4 if dt in (F32, I32) else 2
            rc = max(1, min(rows, CCB // (cols * esz)))
            assert rows % rc == 0
            g = DT(name, [4 * rows, cols], dt)
            for k in range(rows // rc):
                P.cc(lambda e, k=k: e.collective_compute("AllGather", BYP, replica_groups=RG, ins=[src2d[k * rc:(k + 1) * rc, :]],
                                                         outs=[g[k * 4 * rc:(k + 1) * 4 * rc, :]]), rkeys, [wkey])
            return g, rc

        def rankview(g, rc, r):
            return g.ap().rearrange("(k r x) c -> r k x c", r=4, x=rc)[r]

        ext = {"xe0": DT("xe0", [TPC + 2 * HALO, D], F32, "ExternalInput")}
        for (n, sh, dt) in CONSTS:
            ext[n] = DT(n, sh, dt, "ExternalInput")
        for l in range(2):
            for (n, sh, dt) in LAYER_W:
                ext["%s_%d" % (n, l)] = DT("%s_%d" % (n, l), sh, dt, "ExternalInput")
        out = DT("out", [TPC, D], F32, "ExternalOutput")
        xe = ext["xe0"]
        x_own = None
        for l in range(2):
            W = {n: ext["%s_%d" % (n, l)] for (n, _, _) in LAYER_W}
            L = lambda n, sh, dt: DT("%s_L%d" % (n, l), sh, dt)
            A = dict(QT=L("QT", [512, TPC], BF16), KT=L("KT", [512, TPC], BF16), Kt=L("Kt", [4, TPC, 128], BF16),
                     Vt=L("Vt", [4, TPC, 128], BF16), OG=L("OG", [4, TPC, 128], BF16), G4=L("G4", [4, TPC, 4], F32),
                     GTS=L("GTS", [TPC, 3072], BF16), UT=L("UT", [512, TPC], BF16), MQ=L("MQ", [8, 96, TPC], BF16),
                     MK=L("MK", [8, 96, TPC], BF16), MV=L("MV", [8, 128, TPC // 128, 64], BF16))
            preA = dict(xe=xe, w_in=W["w_in"], gmix=W["gmix"], identb=ext["identb"], convp=W["convp"], gcq=W["gcq"],
                        gckv=W["gckv"], w_uq=W["w_uq"], w_ukv=W["w_ukv"], gqk=W["gqk"], ropeT=ext["ropeT"],
                        rmat=ext["rmat"], onesf=ext["onesf"], **A)
            build_stageA(env=env, pre=preA)
            nc_, es_, C_, _ = _begin(env)
            with es_:
                idx = C_.sb([128, 16], I32, "idx")
                DMA(P, "sp", idx[:, :], ext["idx"][:, :], [], ["idx"])
                gQT, rcQ = allgather("gQT_L%d" % l, A["QT"].ap(), 512, TPC, BF16, [], ("g", 0))
                gKt, rcK = allgather("gKt_L%d" % l, A["Kt"].ap().rearrange("h t d -> (h t) d"), 4 * TPC, 128, BF16, [], ("g", 1))
                gVt, _ = allgather("gVt_L%d" % l, A["Vt"].ap().rearrange("h t d -> (h t) d"), 4 * TPC, 128, BF16, [], ("g", 2))
                gOG, _ = allgather("gOG_L%d" % l, A["OG"].ap().rearrange("h t d -> (h t) d"), 4 * TPC, 128, BF16, [], ("g", 3))
                gG4, rcG = allgather("gG4_L%d" % l, A["G4"].ap().rearrange("h t g -> (h t) g"), 4 * TPC, 4, F32, [], ("g", 4))
                gMK, rcMK = allgather("gMK_L%d" % l, A["MK"].ap().rearrange("h f t -> (h f) t"), 768, TPC, BF16, [], ("g", 5))
                gMV, rcMV = allgather("gMV_L%d" % l, A["MV"].ap().rearrange("h p t d -> (h p) (t d)"), 1024, 2048, BF16, [], ("g", 6))
                assert (rcQ, rcK, rcG, rcMK, rcMV) == (32, 1024, 4 * TPC, 32, 64), (rcQ, rcK, rcG, rcMK, rcMV)
                qT_s = L("qT_s", [128, S], BF16)
                kt_s = L("kt_s", [S, 128], BF16)
                vt_s = L("vt_s", [S, 128], BF16)
                og_s = L("og_s", [S, 128], BF16)
                g4_s = L("g4_s", [S, 4], F32)
                mk_s = L("mk_s", [8, 96, S], BF16)
                mv_s = L("mv_s", [8, 128, (S // 128) * 64], BF16)
                stb = Rot([(("stb", i), C_.sb([128, 16384], BF16, "stb")) for i in range(2)])
                stf = C_.sb([128, 512], F32, "stf")

                def gather(dst_ap, src_ap, col, rkeys, wkeys, tile_ap, tkey):
                    P.dma("pool", lambda e: e.indirect_dma_start(
                        out=tile_ap, out_offset=None, in_=src_ap,
                        in_offset=bass.IndirectOffsetOnAxis(ap=idx[:, col:col + 1], axis=0)), ["idx"] + rkeys, [tkey])
                    DMA(P, "sp", dst_ap, tile_ap, [tkey], wkeys)
                for i in range(4):
                    tk_, tt_ = stb.next()
                    gather(qT_s[:, i * TPC:(i + 1) * TPC], gQT[:, :], i, [("g", 0)], [("qT_s", i)], tt_[:, 0:TPC], tk_)
                for j, (gsrc, dst) in enumerate(((gKt, kt_s), (gVt, vt_s), (gOG, og_s))):
                    tk_, tt_ = stb.next()
                    gather(dst.ap().rearrange("(c p) d -> c (p d)", p=128),
                           gsrc.ap().rearrange("(c p) d -> c (p d)", p=128), 4, [("g", 1 + j)], [("tm_s", j)], tt_[:, :], tk_)
                gather(g4_s.ap().rearrange("(c p) d -> c (p d)", p=128),
                       gG4.ap().rearrange("(c p) d -> c (p d)", p=128), 11, [("g", 4)], [("tm_s", 3)], stf[:, :], "stf")
                for i in range(4):
                    DMA(P, "sp", mk_s.ap().rearrange("h f t -> (h f) t")[:, i * TPC:(i + 1) * TPC].rearrange("(k x) t -> k x t", x=rcMK),
                        rankview(gMK, rcMK, i), [("g", 5)], [("mk_s", i)])
                    DMA(P, "sp", mv_s.ap().rearrange("h p x -> (h p) x")[:, i * 2048:(i + 1) * 2048].rearrange("(k x) c -> k x c", x=rcMV),
                        rankview(gMV, rcMV, i), [("g", 6)], [("mv_s", i)])
                P.emit()
            if stop == "x1":
                return nc
            HT = L("HT", [128, S], BF16)
            build_mlstm(env=env, pre=dict(qT=qT_s, kt=kt_s, vt=vt_s, g4=g4_s, bif=W["bif"], og=og_s, gA=W["gA"],
                                          cmat=ext["cmat"], identb=ext["identb"], HT=HT))
            if stop == "m":
                return nc
            OT = L("OT", [512, TPC], BF16)
            build_attn(env=env, pre=dict(mq=A["MQ"], mk=mk_s, mv=mv_s, esel=ext["esel"], OT=OT))
            if stop == "t":
                return nc
            nc_, es_, C_, _ = _begin(env)
            with es_:
                idx = C_.sb([128, 16], I32, "idx")
                DMA(P, "sp", idx[:, :], ext["idx"][:, :], [], ["idx"])
                gHT, rcH = allgather("gHT_L%d" % l, HT.ap(), 128, S, BF16, [], "gHT")
                assert rcH == 8
                HT_own = L("HT_own", [512, TPC], BF16)
                src = gHT.ap().rearrange("r (i t) -> (r i) t", i=4)
                stb = Rot([(("stb", i), C_.sb([128, TPC], BF16, "stb")) for i in range(2)])
                for h in range(4):
                    tk_, tt_ = stb.next()
                    P.dma("pool", lambda e, h=h, tt_=tt_: e.indirect_dma_start(
                        out=tt_[:, :], out_offset=None, in_=src,
                        in_offset=bass.IndirectOffsetOnAxis(ap=idx[:, 5 + h:6 + h], axis=0)), ["idx", "gHT"], [tk_])
                    DMA(P, "sp", HT_own[h * 128:(h + 1) * 128, :], tt_[:, :], [tk_], [("HT_own", h)])
                P.emit()
            if stop == "x2":
                return nc
            x1 = L("x1", [TPC, D], F32)
            xn2T = L("xn2T", [D, TPC], BF16)
            aff = L("aff", [TPC, 16], F32)
            affT = L("affT", [16, TPC], F32)
            if l == 0:
                xin = L("xin", [TPC, D], F32)
                nc_, es_, C_, _ = _begin(env)
                with es_:
                    DMA(P, "sp", xin[:, :], ext["xe0"][HALO:HALO + TPC, :], [], ["xin"])
                    P.emit()
            else:
                xin = x_own
            build_merge(env=env, pre=dict(x=xin, HT=HT_own, UT=A["UT"], OT=OT, GTS=A["GTS"], w_a=W["w_a"], w_b=W["w_b"],
                                          w_c=W["w_c"], w_o=W["w_o"], gffn=W["gffn"], w_r=W["w_r"], identb=ext["identb"],
                                          ident32=ext["ident32"], x1=x1, xn2T=xn2T, aff=aff, affT=affT))
            if stop == "c1":
                return nc
            affT_s = L("affT_s", [16, S], F32)
            nc_, es_, C_, _ = _begin(env)
            with es_:
                gAf, rcA = allgather("gAf_L%d" % l, affT.ap(), 16, TPC, F32, [], "gAf")
                assert rcA == 16
                for i in range(4):
                    DMA(P, "sp", affT_s[:, i * TPC:(i + 1) * TPC], gAf[i * 16:(i + 1) * 16, :], ["gAf"], [("affT_s", i)])
                P.emit()
            thr = L("thr", [16, 2], F32)
            build_thr(env=env, pre=dict(affT=affT_s, thr=thr))
            if stop == "h":
                return nc
            x2 = out if l == 1 else L("x2", [TPC, D], F32)
            build_ffn(env=env, pre=dict(x1=x1, xn2T=xn2T, aff=aff, thr16=thr, ident32=ext["ident32"], wg=W["wg"], wu=W["wu"],
                                        wd=W["wd"], x2=x2))
            if l == 0:
                xe1 = L("xe1", [TPC + 2 * HALO, D], F32)
                nc_, es_, C_, _ = _begin(env)
                with es_:
                    idx = C_.sb([128, 16], I32, "idx")
                    zt = C_.sb([128, D], F32, "zt")
                    DMA(P, "sp", idx[:, :], ext["idx"][:, :], [], ["idx"])
                    MEMSET(P, "dve", zt[:, :], 0.0, ["zt"])
                    edges = L("edges", [384, D], F32)
                    DMA(P, "sp", edges[0:128, :], x2[0:128, :], [], ["edges"])
                    DMA(P, "sp", edges[128:256, :], x2[TPC - 128:TPC, :], [], ["edges"])
                    DMA(P, "sp", edges[256:384, :], zt[:, :], ["zt"], ["edges"])
                    DMA(P, "sp", xe1[HALO:HALO + TPC, :], x2[:, :], [], ["xe1m"])
                    gE, rcE = allgather("gE_L%d" % l, edges.ap(), 384, D, F32, ["edges"], "gE")
                    assert rcE == 64
                    hl = C_.sb([128, D], F32, "hl")
                    hr = C_.sb([128, D], F32, "hr")
                    P.dma("pool", lambda e: e.indirect_dma_start(
                        out=hl[:, :], out_offset=None, in_=gE[:, :],
                        in_offset=bass.IndirectOffsetOnAxis(ap=idx[:, 9:10], axis=0)), ["idx", "gE"], ["hl"])
                    P.dma("pool", lambda e: e.indirect_dma_start(
                        out=hr[:, :], out_offset=None, in_=gE[:, :],
                        in_offset=bass.IndirectOffsetOnAxis(ap=idx[:, 10:11], axis=0)), ["idx", "gE"], ["hr"])
                    DMA(P, "sp", xe1[0:HALO, :], hl[:, :], ["hl"], ["xe1l"])
                    DMA(P, "sp", xe1[HALO + TPC:, :], hr[:, :], ["hr"], ["xe1r"])
                    P.emit()
                xe = xe1
                x_own = x2
    return nc


def _grow(x, rc, r):
    return (x // rc) * (4 * rc) + r * rc + (x % rc)


def fused_idx(c):
    r = c % 4
    p = np.arange(128)
    idx = np.zeros((128, 16), np.int32)
    for i in range(4):
        idx[:, i] = _grow(r * 128 + p, 32, i)
    y = r * 32 + (p % 32)
    idx[:, 4] = _grow(y, 8, p // 32)
    idx[:, 11] = (p // 32) * 128 + y
    for h in range(4):
        idx[:, 5 + h] = _grow(p, 8, h) * 4 + r
    idx[:, 9] = _grow(128 + p, 64, r - 1) if r > 0 else _grow(256 + p, 64, r)
    idx[:, 10] = _grow(p, 64, r + 1) if r < 3 else _grow(256 + p, 64, r)
    return idx


def kernel(**inputs):
    prm = {k: np.asarray(v) for k, v in inputs.items()}
    x = np.ascontiguousarray(prm["x"], dtype=np.float32)
    nc = build_fused()
    CT, ST = rope_tables()
    cst = consts()
    mc = mlstm_consts()
    ac = attn_consts()
    i32 = np.eye(128, dtype=np.float32)
    lay = []
    for l in range(2):
        convp = np.zeros((128, 4, 34), np.float32)
        convp[:, :, 0:31] = prm["conv_w"][l].T.reshape(4, 128, 31).transpose(1, 0, 2)
        convp[:, :, 31] = prm["conv_b"][l].reshape(4, 128).T
        convp[:, :, 32] = prm["conv_ln_g"][l].reshape(4, 128).T
        convp[:, :, 33] = prm["conv_ln_b"][l].reshape(4, 128).T
        lay.append(dict(w_in=prm["w_in"][l], gmix=_gain_cols(prm["mix_norm_g"][l], 8), convp=convp,
                        gcq=_gain_cols(prm["cq_norm_g"][l], 3), gckv=_gain_cols(prm["ckv_norm_g"][l], 2),
                        w_uq=prm["w_uq"][l], w_ukv=prm["w_ukv"][l],
                        gqk=np.ascontiguousarray(np.stack([prm["q_norm_g"][l], prm["k_norm_g"][l]], axis=1)),
                        w_a=prm["w_a_out"][l], w_b=prm["w_b_out"][l], w_c=prm["w_c_out"][l], w_o=prm["w_out"][l],
                        gffn=_gain_cols(prm["ffn_norm_g"][l], 8), w_r=prm["w_router"][l],
                        wg=prm["w_e_gate"][l], wu=prm["w_e_up"][l], wd=prm["w_e_down"][l]))
    maps = []
    for c in range(NCORES):
        b, r = c // 4, c % 4
        s0 = r * TPC
        xe = np.zeros((TPC + 2 * HALO, D), np.float32)
        lo, hi = max(0, s0 - HALO), min(S, s0 + TPC + HALO)
        xe[lo - (s0 - HALO):hi - (s0 - HALO)] = x[b, lo:hi]
        m = dict(xe0=xe, identb=cst["identb"], ropeT=np.ascontiguousarray(np.stack([CT[:, s0:s0 + TPC], ST[:, s0:s0 + TPC]], axis=1)),
                 rmat=cst["rmat"], onesf=cst["onesf"], cmat=mc["cmat"], ident32=i32, esel=ac["esel"], idx=fused_idx(c))
        cols = [r, 4 + r, 8 + r, 12 + r]
        for l in range(2):
            for k_, v_ in lay[l].items():
                m["%s_%d" % (k_, l)] = v_
            m["bif_%d" % l] = np.ascontiguousarray(np.broadcast_to(prm["b_if"][l][cols], (128, 4)))
            m["gA_%d" % l] = np.ascontiguousarray(np.broadcast_to(prm["a_norm_g"][l][r], (128, 128)))
        maps.append(m)
    res = run_spmd(nc, maps)
    out = np.empty_like(x)
    for c in range(NCORES):
        b, r = c // 4, c % 4
        out[b, r * TPC:(r + 1) * TPC] = np.asarray(res[c]["out"])
    return out
```

```python
import math
from contextlib import ExitStack

import numpy as np
import ml_dtypes

import concourse.bass as bass
import concourse.mybir as mybir
from concourse.bass_utils import run_bass_kernel_spmd

F32 = mybir.dt.float32
BF16 = mybir.dt.bfloat16
I32 = mybir.dt.int32
AF = mybir.ActivationFunctionType
ALU = mybir.AluOpType
AX = mybir.AxisListType
NPBF = ml_dtypes.bfloat16

D = 1024
S = 16384
NB = 2
INW = 6832
EPS = 1e-6
NCORES = 8
TPC = S * NB // NCORES

O_AQ, O_AK, O_AV, O_AO, O_AG = 0, 512, 1024, 1536, 2048
O_GLU = 2064
O_CQ = 3088
O_CKV = 3472
O_CKR = 3728
O_GTS = 3760


class Prog:
    RING = 8

    def __init__(self, nc, es):
        self.nc = nc
        self.es = es
        self.ops = []
        self.engs = ["pe", "act", "dve", "pool", "sp"]
        self.csem = None
        self.rings = {}
        self.ccsem = None
        self.ccount = {e: 0 for e in self.engs}
        self.dcount = {e: 0 for e in self.engs}
        self.cccount = 0
        self.nstage = 0

    def cc(self, fn, r=(), w=()):
        self.ops.append(dict(eng="pool", fn=fn, r=tuple(r), w=tuple(w), dma=True, cc=True))

    def op(self, eng, fn, r=(), w=()):
        self.ops.append(dict(eng=eng, fn=fn, r=tuple(r), w=tuple(w), dma=False))

    def dma(self, eng, fn, r=(), w=()):
        self.ops.append(dict(eng=eng, fn=fn, r=tuple(r), w=tuple(w), dma=True))

    def emit(self):
        nc, es = self.nc, self.es
        ops = self.ops
        last_w = {}
        readers = {}
        deps = []
        for i, o in enumerate(ops):
            d = set()
            for k in o["r"]:
                if k in last_w:
                    d.add((last_w[k], "raw"))
            for k in o["w"]:
                if k in last_w:
                    d.add((last_w[k], "waw"))
                for j in readers.get(k, ()):
                    if j != i:
                        d.add((j, "war"))
            for k in o["r"]:
                lst = readers.setdefault(k, [])
                if not o["dma"]:
                    lst[:] = [j for j in lst if ops[j]["dma"] or ops[j]["eng"] != o["eng"]]
                lst.append(i)
            for k in o["w"]:
                last_w[k] = i
                readers[k] = []
            dd = set()
            for j, kind in d:
                p = ops[j]
                if (not p["dma"]) and (not o["dma"]) and p["eng"] == o["eng"]:
                    if o["eng"] == "pe":
                        continue
                    if kind == "war":
                        continue
                dd.add(j)
            deps.append(dd)
        needed = set()
        for dd in deps:
            needed |= dd
        engs = self.engs
        if self.csem is None:
            self.csem = {e: es.enter_context(nc.semaphore("c_" + e)) for e in engs}
            self.ccsem = es.enter_context(nc.semaphore("c_cc"))
        csem = self.csem
        rings = self.rings
        ccount = self.ccount
        dcount = self.dcount
        prev_end = dict(c={e: ccount[e] for e in engs}, d={e: dcount[e] for e in rings}, cc=self.cccount)
        lastc = {}
        for i, o in enumerate(ops):
            if not o["dma"]:
                lastc[o["eng"]] = i
        needed |= set(lastc.values())
        sig = {}
        prewait = {}
        for i, o in enumerate(ops):
            e = o["eng"]
            if o.get("cc"):
                self.cccount += 1
                sig[i] = (self.ccsem, self.cccount, 1)
                if self.cccount > 1:
                    prewait[i] = (self.ccsem, self.cccount - 1)
            elif o["dma"]:
                if e not in rings:
                    rings[e] = [es.enter_context(nc.semaphore("r_%s%d" % (e, k)))
                                for k in range(self.RING)]
                n = dcount[e]
                dcount[e] += 1
                sem = rings[e][n % self.RING]
                sig[i] = (sem, 16 * (n // self.RING + 1), 16)
                if n >= self.RING:
                    prewait[i] = (sem, 16 * (n // self.RING))
            elif i in needed:
                ccount[e] += 1
                sig[i] = (csem[e], ccount[e], 1)
        per = {e: [] for e in engs}
        for i, o in enumerate(ops):
            per[o["eng"]].append(i)
        self.stats = dict(n_ops=len(ops), ccount=ccount, dcount=dcount)

        nstage = self.nstage
        self.nstage += 1

        def run(e, engobj):
            waited = {}
            if nstage > 0:
                for e2 in engs:
                    if prev_end["c"][e2] > 0:
                        engobj.wait_ge(csem[e2], prev_end["c"][e2])
                for e2, n in prev_end["d"].items():
                    for k in range(self.RING):
                        cnt = (n - k + self.RING - 1) // self.RING if n > k else 0
                        if cnt > 0:
                            engobj.wait_ge(rings[e2][k], 16 * cnt)
                if prev_end["cc"] > 0:
                    engobj.wait_ge(self.ccsem, prev_end["cc"])
            for i in per[e]:
                o = ops[i]
                ws = [sig[j][:2] for j in deps[i]]
                if i in prewait:
                    ws.append(prewait[i])
                mx = {}
                for sem, val in ws:
                    key = id(sem)
                    if key not in mx or mx[key][1] < val:
                        mx[key] = (sem, val)
                for key, (sem, val) in mx.items():
                    if waited.get(key, 0) >= val:
                        continue
                    waited[key] = val
                    engobj.wait_ge(sem, val)
                ins = o["fn"](engobj)
                if i in sig:
                    sem, val, inc = sig[i]
                    ins.then_inc(sem, inc)
            if e in rings:
                n = dcount[e]
                for k in range(self.RING):
                    cnt = (n - k + self.RING - 1) // self.RING if n > k else 0
                    if cnt > 0:
                        engobj.wait_ge(rings[e][k], 16 * cnt)

        with nc.Block() as block:
            @block.tensor
            def _(t):
                run("pe", t)

            @block.scalar
            def _(t):
                run("act", t)

            @block.vector
            def _(t):
                run("dve", t)

            @block.gpsimd
            def _(t):
                run("pool", t)

            @block.sync
            def _(t):
                run("sp", t)
        self.ops = []


class Ctx:
    def __init__(self, nc, es, pre=None, tag=""):
        self.nc, self.es = nc, es
        self.n = 0
        self.pre = pre
        self.tag = tag

    def sb(self, shape, dt, name=None):
        self.n += 1
        t = self.es.enter_context(self.nc.sbuf_tensor("%s%s_%d" % (self.tag, name or "t", self.n), list(shape), dt))
        esz = 4 if dt in (F32, I32) else 2
        nbytes = int(np.prod(shape[1:])) * esz
        alloc = (nbytes + 31) // 32 * 32
        if alloc % 64 != 0:
            self.n += 1
            self.es.enter_context(self.nc.sbuf_tensor("%spad_%d" % (self.tag, self.n), [128, 8], F32))
        return t

    def ps(self, shape, dt, name=None):
        self.n += 1
        return self.es.enter_context(self.nc.psum_tensor("%s%s_%d" % (self.tag, name or "p", self.n), list(shape), dt))

    def dram(self, name, shape, dt, kind):
        if self.pre is not None:
            h = self.pre[name]
            assert list(h.shape) == list(shape), (name, h.shape, shape)
            return h
        return self.nc.dram_tensor(name, list(shape), dt, kind=kind)


class Rot:
    def __init__(self, items):
        self.items = items
        self.i = 0

    def next(self):
        it = self.items[self.i % len(self.items)]
        self.i += 1
        return it


class Env:
    def __init__(self):
        self.nc = bass.Bass("TRN2", target_bir_lowering=False)
        self.es = ExitStack()
        self.P = Prog(self.nc, self.es)
        self.nstage = 0


def _begin(env, pre=None):
    if env is None:
        nc = bass.Bass("TRN2", target_bir_lowering=False)
        es = ExitStack()
        return nc, es, Ctx(nc, es), Prog(nc, es)
    env.nstage += 1
    es = ExitStack()
    return env.nc, es, Ctx(env.nc, es, pre=pre, tag="s%d_" % env.nstage), env.P


def run_spmd(nc, in_maps):
    res = run_bass_kernel_spmd(nc, in_maps, core_ids=list(range(NCORES)))
    return res.results


def ACT(P, out, in_, func, r, w, **kw):
    P.op("act", lambda e: e.activation(out=out, in_=in_, func=func, **kw), r, w)


def TS(P, eng, out, in0, s1, s2, op0, op1, r, w):
    if op1 is None:
        P.op(eng, lambda e: e.tensor_scalar(out=out, in0=in0, scalar1=s1, scalar2=None, op0=op0), r, w)
    else:
        P.op(eng, lambda e: e.tensor_scalar(out=out, in0=in0, scalar1=s1, scalar2=s2, op0=op0, op1=op1), r, w)


def TT(P, eng, out, in0, in1, op, r, w):
    P.op(eng, lambda e: e.tensor_tensor(out=out, in0=in0, in1=in1, op=op), r, w)


def STT(P, eng, out, in0, scalar, in1, op0, op1, r, w):
    P.op(eng, lambda e: e.scalar_tensor_tensor(out=out, in0=in0, scalar=scalar, in1=in1, op0=op0, op1=op1), r, w)


def CP(P, eng, out, in_, r, w):
    if eng == "act":
        P.op(eng, lambda e: e.copy(out=out, in_=in_), r, w)
    else:
        P.op(eng, lambda e: e.tensor_copy(out=out, in_=in_), r, w)


def RSUM(P, eng, out, in_, r, w, axis=None):
    ax = axis if axis is not None else AX.X
    P.op(eng, lambda e: e.reduce_sum(out=out, in_=in_, axis=ax), r, w)


def RMAX(P, eng, out, in_, r, w, axis=None):
    ax = axis if axis is not None else AX.X
    P.op(eng, lambda e: e.reduce_max(out=out, in_=in_, axis=ax), r, w)


def MM(P, out, lhsT, rhs, start, stop, r, w):
    P.op("pe", lambda e: e.matmul(out, lhsT, rhs, start=start, stop=stop), r, w)


def TR(P, out, in_, ident, r, w):
    P.op("pe", lambda e: e.transpose(out, in_, ident), r, w)


def DMA(P, eng, out, in_, r, w):
    P.dma(eng, lambda e: e.dma_start(out=out, in_=in_), r, w)


def RECIP(P, eng, out, in_, r, w):
    P.op(eng, lambda e: e.reciprocal(out=out, in_=in_), r, w)


def MEMSET(P, eng, ap, val, w):
    P.op(eng, lambda e: e.memset(ap, val), (), w)


TP = 1024
HALO = 128
NTP = TP + 2 * HALO
NPASS = TPC // TP


def build_stageA(phases=("fm", "tm", "conv", "mla"), env=None, pre=None):
    nc, es, C, P = _begin(env, pre)
    with es:
        xe = C.dram("xe", [TPC + 2 * HALO, D], F32, "ExternalInput")
        w_in = C.dram("w_in", [D, INW], F32, "ExternalInput")
        gmix = C.dram("gmix", [128, 8], F32, "ExternalInput")
        identb = C.dram("identb", [128, 128], BF16, "ExternalInput")
        convp = C.dram("convp", [128, 4, 34], F32, "ExternalInput")
        gcq = C.dram("gcq", [128, 3], F32, "ExternalInput")
        gckv = C.dram("gckv", [128, 2], F32, "ExternalInput")
        w_uq = C.dram("w_uq", [384, 768], F32, "ExternalInput")
        w_ukv = C.dram("w_ukv", [256, 1024], F32, "ExternalInput")
        gqk = C.dram("gqk", [96, 2], F32, "ExternalInput")
        ropeT = C.dram("ropeT", [96, 2, TPC], F32, "ExternalInput")
        rmat = C.dram("rmat", [96, 96], BF16, "ExternalInput")
        onesf = C.dram("onesf", [128, 128], F32, "ExternalInput")

        QT = C.dram("QT", [512, TPC], BF16, "ExternalOutput")
        KT = C.dram("KT", [512, TPC], BF16, "ExternalOutput")
        Kt = C.dram("Kt", [4, TPC, 128], BF16, "ExternalOutput")
        Vt = C.dram("Vt", [4, TPC, 128], BF16, "ExternalOutput")
        OG = C.dram("OG", [4, TPC, 128], BF16, "ExternalOutput")
        G4 = C.dram("G4", [4, TPC, 4], F32, "ExternalOutput")
        GTS = C.dram("GTS", [TPC, 3072], BF16, "ExternalOutput")
        UT = C.dram("UT", [512, TPC], BF16, "ExternalOutput")
        MQ = C.dram("MQ", [8, 96, TPC], BF16, "ExternalOutput")
        MK = C.dram("MK", [8, 96, TPC], BF16, "ExternalOutput")
        MV = C.dram("MV", [8, 128, TPC // 128, 64], BF16, "ExternalOutput")

        w_v = w_in.ap().rearrange("(c p) n -> p c n", p=128)
        xnT = C.sb([128, 8, NTP], BF16, "xnT")
        f32t = Rot([(("f32t", i), C.sb([128, 512], F32, "f32t")) for i in range(4)])
        uT = C.sb([128, 4, NTP], BF16, "uT")
        cacc = [C.sb([128, 512], F32, "cacc") for g in range(4)]
        csq = [C.sb([128, 512], F32, "csq") for g in range(4)]
        cpar = C.sb([128, 4, 34], F32, "cpar")
        rope_sb = C.sb([96, 2, 512], F32, "rope")
        cql = C.sb([128, 3, 512], F32, "cql")
        ckl = C.sb([128, 2, 512], F32, "ckl")
        latsq = C.sb([128, 3, 512], BF16, "latsq")
        cqn = C.sb([128, 3, 512], BF16, "cqn")
        ckn = C.sb([128, 2, 512], BF16, "ckn")
        krt = C.sb([128, 512], F32, "krt")
        hx = Rot([(("hx", i), C.sb([128, 512], F32, "hx")) for i in range(2)])
        hsq = Rot([(("hsq", i), C.sb([128, 512], BF16, "hsq")) for i in range(2)])
        hxg = Rot([(("hxg", i), C.sb([128, 512], BF16, "hxg")) for i in range(2)])
        wuqb = C.sb([128, 3, 768], BF16, "wuqb")
        wukvb = C.sb([128, 2, 1024], BF16, "wukvb")
        wkrp = C.sb([128, 8, 128], BF16, "wkrp")
        gqk_sb = C.sb([96, 2], F32, "gqk")
        rmat_sb = C.sb([96, 96], BF16, "rmat")
        gcq_sb = C.sb([128, 3], F32, "gcqs")
        gckv_sb = C.sb([128, 2], F32, "gckvs")
        ident = C.sb([128, 128], BF16, "ident")
        gm = C.sb([128, 8], F32, "gm")
        onesb = C.sb([128, 128], BF16, "onesb")
        ones32 = C.sb([128, 128], F32, "ones32")
        xin = Rot([(("xin", i), C.sb([128, D], F32, "xin")) for i in range(2)])
        sqj = C.sb([128, D], F32, "sqj")
        xs = Rot([(("xs", i), C.sb([128, D], BF16, "xs")) for i in range(2)])
        stat = Rot([(("stat", i), C.sb([128, 2], F32, "stat")) for i in range(2)])
        wst = Rot([(("wst", i), C.sb([128, 8, 512], F32, "wst")) for i in range(3)])
        wb = Rot([(("wb", i), C.sb([128, 8, 512], BF16, "wb")) for i in range(3)])
        ob = Rot([(("ob", i), C.sb([128, 512], BF16, "ob")) for i in range(4)])
        g4sb = C.sb([128, NTP // 128, 16], F32, "g4sb")
        pacc = Rot([(("pacc", i), C.ps([128, 512], F32, "pacc")) for i in range(5)])
        ptr = Rot([(("ptr", i), C.ps([128, 1024], BF16, "ptr")) for i in range(2)])

        DMA(P, "sp", cpar[:], convp[:, :, :], [], ["cpar"])
        DMA(P, "sp", gqk_sb[:], gqk[:, :], [], ["gqk"])
        DMA(P, "sp", rmat_sb[:], rmat[:, :], [], ["rmat"])
        DMA(P, "sp", gcq_sb[:], gcq[:, :], [], ["gcqs"])
        DMA(P, "sp", gckv_sb[:], gckv[:, :], [], ["gckvs"])
        DMA(P, "sp", gm[:], gmix[:, :], [], ["gm"])
        if "mla" in phases:
            sk0, st0 = wst.items[0]
            st0f = st0[:].rearrange("p a b -> p (a b)")
            DMA(P, "sp", st0f[:, 0:2304].rearrange("p (a b) -> p a b", a=3),
                w_uq.ap().rearrange("(c p) n -> p c n", p=128), [], [sk0])
            for j in range(3):
                TS(P, "dve", wuqb[:, j, :], st0f[:, j * 768:(j + 1) * 768],
                   gcq_sb[:, j:j + 1], None, ALU.mult, None, [sk0, "gcqs"], ["wuqb"])
            sk1, st1 = wst.items[1]
            st1f = st1[:].rearrange("p a b -> p (a b)")
            DMA(P, "sp", st1f[:, 0:2048].rearrange("p (a b) -> p a b", a=2),
                w_ukv.ap().rearrange("(c p) n -> p c n", p=128), [], [sk1])
            for j in range(2):
                src = st1f[:, j * 1024:(j + 1) * 1024].rearrange("p (h x) -> p h x", h=8)
                TS(P, "dve", wukvb[:, j, 0:512].rearrange("p (h x) -> p h x", h=8), src[:, :, 0:64],
                   gckv_sb[:, j:j + 1], None, ALU.mult, None, [sk1, "gckvs"], ["wukvb"])
                TS(P, "dve", wukvb[:, j, 512:1024].rearrange("p (h x) -> p h x", h=8), src[:, :, 64:128],
                   gckv_sb[:, j:j + 1], None, ALU.mult, None, [sk1, "gckvs"], ["wukvb"])
            MEMSET(P, "pool", wkrp[:], 0.0, ["wkrp"])
            sk2_, st2_ = wst.items[0]
            DMA(P, "sp", st2_[:, :, 0:32], w_v[:, :, O_CKR:O_CKR + 32], [], [sk2_])
            for k in range(8):
                TS(P, "dve", wkrp[:, k, 64:96], st2_[:, k, 0:32], gm[:, k:k + 1], None, ALU.mult, None,
                   [sk2_, "gm"], ["wkrp"])
            MEMSET(P, "pool", krt[:], 0.0, ["krt"])
        DMA(P, "sp", ident[:], identb[:, :], [], ["ident"])
        DMA(P, "sp", gm[:], gmix[:, :], [], ["gm"])
        DMA(P, "sp", ones32[:], onesf[:, :], [], ["ones32"])
        CP(P, "dve", onesb[:], ones32[:], ["ones32"], ["onesb"])

        evac_i = [0]

        def load_w_impl(c0, ncols):
            sk, st = wst.next()
            bk, bt = wb.next()
            DMA(P, "sp", st[:, :, 0:ncols], w_v[:, :, c0:c0 + ncols], [], [sk])
            for k in range(8):
                ACT(P, bt[:, k, 0:ncols], st[:, k, 0:ncols], AF.Copy, [sk, "gm"], [(bk, k)], scale=gm[:, k:k + 1])
            return [(bk, k) for k in range(8)], bt

        wlist = []
        for _ps in range(NPASS):
            if "fm" in phases:
                wlist += [(O_AQ, 512), (O_AK, 512)]
            if "tm" in phases:
                wlist += [(O_AK, 512), (O_AV, 512), (O_AO, 512)] + [(O_GTS + j * 512, 512) for j in range(6)] + [(O_AG, 16)]
            if "conv" in phases:
                wlist += [(O_GLU, 512), (O_GLU + 512, 512)]
            if "mla" in phases:
                wlist += [(O_CQ, 384), (O_CKV, 288)]
        issued = []

        def nextw(c0, ncols):
            if not issued:
                issued.append((wlist[0], load_w_impl(*wlist.pop(0))))
            req, cur = issued.pop(0)
            assert req == (c0, ncols), (req, c0, ncols)
            if wlist:
                issued.append((wlist[0], load_w_impl(*wlist.pop(0))))
            return cur

        for ps_i in range(NPASS):
            t0 = ps_i * TP
            for t in range(NTP // 128):
                xk, xt = xin.next()
                sk2, stt = stat.next()
                xsk, xst = xs.next()
                pk, pt = ptr.next()
                r0 = t0 + t * 128
                DMA(P, "sp", xt[:], xe[r0:r0 + 128, :], [], [xk])
                ACT(P, sqj[:], xt[:], AF.Square, [xk], ["sqj"])
                RSUM(P, "dve", stt[:, 0:1], sqj[:], ["sqj"], [sk2])
                ACT(P, stt[:, 1:2], stt[:, 0:1], AF.Sqrt, [sk2], [sk2], scale=1.0 / D, bias=EPS)
                RECIP(P, "dve", stt[:, 1:2], stt[:, 1:2], [sk2], [sk2])
                ACT(P, xst[:], xt[:], AF.Copy, [xk, sk2], [xsk], scale=stt[:, 1:2])
                for k in range(8):
                    TR(P, pt[:, k * 128:(k + 1) * 128], xst[:, k * 128:(k + 1) * 128], ident[:],
                       [xsk, "ident"], [pk])
                CP(P, ("dve", "pool")[0], xnT[:, :, t * 128:(t + 1) * 128],
                   pt[:].rearrange("p (k t) -> p k t", k=8), [pk], [("xnT", t)])

            def xk_keys(tok0, ntok):
                return [("xnT", t) for t in range(tok0 // 128, (tok0 + ntok - 1) // 128 + 1)]

            if "fm" in phases:
                for (c0, dst) in ((O_AQ, QT), (O_AK, KT)):
                    wk, wt = nextw(c0, 512)
                    for cb in range(4):
                        for tb in range(TP // 512):
                            tk0 = HALO + tb * 512
                            ak, at = pacc.next()
                            for k in range(8):
                                MM(P, at[:, :], wt[:, k, cb * 128:(cb + 1) * 128], xnT[:, k, tk0:tk0 + 512],
                                   k == 0, k == 7, [wk[k]] + xk_keys(tk0, 512), [ak])
                            okk, ot = ob.next()
                            evac_i[0] += 1
                            CP(P, ("act", "dve")[evac_i[0] % 2], ot[:, :], at[:, :], [ak], [okk])
                            DMA(P, "sp", dst[cb * 128:(cb + 1) * 128, t0 + tb * 512:t0 + (tb + 1) * 512], ot[:, :],
                                [okk], [])

            if "tm" in phases:
                blocks = [(O_AK, Kt, 0, "copy"), (O_AV, Vt, 0, "copy"), (O_AO, OG, 0, "sig")]
                for j in range(6):
                    blocks.append((O_GTS + j * 512, GTS, j * 512, "sig"))
                for (c0, dst, dc0, mode) in blocks:
                    wk, wt = nextw(c0, 512)
                    for t in range(TP // 128):
                        tk0 = HALO + t * 128
                        ak, at = pacc.next()
                        for k in range(8):
                            MM(P, at[:, :], xnT[:, k, tk0:tk0 + 128], wt[:, k, :], k == 0, k == 7,
                               [wk[k]] + xk_keys(tk0, 128), [ak])
                        okk, ot = ob.next()
                        if mode == "sig":
                            ACT(P, ot[:, :], at[:, :], AF.Sigmoid, [ak], [okk])
                        else:
                            evac_i[0] += 1
                            CP(P, ("act", "dve")[evac_i[0] % 2], ot[:, :], at[:, :], [ak], [okk])
                        if dst is GTS:
                            DMA(P, "sp", dst[t0 + t * 128:t0 + (t + 1) * 128, dc0:dc0 + 512], ot[:, :], [okk], [])
                        else:
                            DMA(P, "sp", dst[:, t0 + t * 128:t0 + (t + 1) * 128, :].rearrange("h t d -> t h d"),
                                ot[:, :].rearrange("p (h d) -> p h d", h=4), [okk], [])
                wk, wt = nextw(O_AG, 16)
                for t in range(TP // 128):
                    tk0 = HALO + t * 128
                    ak, at = pacc.next()
                    for k in range(8):
                        MM(P, at[:, 0:16], xnT[:, k, tk0:tk0 + 128], wt[:, k, 0:16], k == 0, k == 7,
                           [wk[k]] + xk_keys(tk0, 128), [ak])
                    CP(P, "dve", g4sb[:, t, :].rearrange("p (h g) -> p h g", h=4), at[:, 0:16].rearrange("p (g h) -> p h g", h=4),
                       [ak], [("g4", t)])
                for h in range(4):
                    DMA(P, "sp", G4[h, t0:t0 + TP, :].rearrange("(t p) g -> p t g", p=128), g4sb[:, 0:TP // 128, h * 4:(h + 1) * 4],
                        [("g4", t) for t in range(TP // 128)], [])

            if "conv" in phases:
                wak, wat = nextw(O_GLU, 512)
                wgk, wgt = nextw(O_GLU + 512, 512)
                blks = [(b0, min(512, NTP - b0)) for b0 in range(0, NTP, 512)]
                for g in range(4):
                    for (b0, bn) in blks:
                        ak, at = pacc.next()
                        gk, gt = pacc.next()
                        for k in range(8):
                            MM(P, at[:, 0:bn], wat[:, k, g * 128:(g + 1) * 128], xnT[:, k, b0:b0 + bn],
                               k == 0, k == 7, [wak[k]] + xk_keys(b0, bn), [ak])
                        for k in range(8):
                            MM(P, gt[:, 0:bn], wgt[:, k, g * 128:(g + 1) * 128], xnT[:, k, b0:b0 + bn],
                               k == 0, k == 7, [wgk[k]] + xk_keys(b0, bn), [gk])
                        fk, ft = f32t.next()
                        ACT(P, ft[:, 0:bn], gt[:, 0:bn], AF.Sigmoid, [gk], [fk])
                        TT(P, "dve", uT[:, g, b0:b0 + bn], at[:, 0:bn], ft[:, 0:bn], ALU.mult, [ak, fk],
                           [("uT", g, b0 // 512)])
                for tb in range(TP // 512):
                    c0 = HALO + tb * 512 - 15
                    ukeys = lambda g: [("uT", g, j) for j in range(c0 // 512, (c0 + 542 - 1) // 512 + 1)]
                    for g in range(4):
                        eng = "dve"
                        ck = ("cacc", g)
                        ca = cacc[g]
                        TS(P, eng, ca[:, :], uT[:, g, c0:c0 + 512], cpar[:, g, 0:1], cpar[:, g, 31:32],
                           ALU.mult, ALU.add, ukeys(g) + ["cpar"], [ck])
                        for k in range(1, 31):
                            STT(P, eng, ca[:, :], uT[:, g, c0 + k:c0 + k + 512], cpar[:, g, k:k + 1], ca[:, :],
                                ALU.mult, ALU.add, ukeys(g) + ["cpar", ck], [ck])
                    mk, mt = pacc.next()
                    for g in range(4):
                        MM(P, mt[:, :], ones32[:, :], cacc[g][:, :], g == 0, g == 3, ["ones32", ("cacc", g)], [mk])
                    for g in range(4):
                        STT(P, "dve", cacc[g][:, :], mt[:, :], -1.0 / 512, cacc[g][:, :], ALU.mult, ALU.add,
                            [mk, ("cacc", g)], [("cacc", g)])
                        ACT(P, csq[g][:, :], cacc[g][:, :], AF.Square, [("cacc", g)], [("csq", g)])
                    vk, vt = pacc.next()
                    for g in range(4):
                        MM(P, vt[:, :], ones32[:, :], csq[g][:, :], g == 0, g == 3, ["ones32", ("csq", g)], [vk])
                    fk, ft = f32t.next()
                    ACT(P, ft[:, :], vt[:, :], AF.Sqrt, [vk], [fk], scale=1.0 / 512, bias=EPS)
                    RECIP(P, "dve", ft[:, :], ft[:, :], [fk], [fk])
                    for g in range(4):
                        TT(P, "dve", csq[g][:, :], cacc[g][:, :], ft[:, :], ALU.mult, [("cacc", g), fk], [("csq", g)])
                        TS(P, "pool", csq[g][:, :], csq[g][:, :], cpar[:, g, 32:33], cpar[:, g, 33:34],
                           ALU.mult, ALU.add, [("csq", g), "cpar"], [("csq", g)])
                        okk, ot = ob.next()
                        ACT(P, ot[:, :], csq[g][:, :], AF.Silu, [("csq", g)], [okk])
                        DMA(P, "sp", UT[g * 128:(g + 1) * 128, t0 + tb * 512:t0 + (tb + 1) * 512], ot[:, :], [okk], [])

            if "mla" in phases:
                wqk, wqt = nextw(O_CQ, 384)
                wkk, wkt = nextw(O_CKV, 288)
                for tb in range(TP // 512):
                    tk0 = HALO + tb * 512
                    g0 = t0 + tb * 512
                    DMA(P, "sp", rope_sb[:, :, :], ropeT[:, :, g0:g0 + 512], [], ["rope"])
                    for (wk_, wt_, nblk, lat, latn, lkey, dim) in ((wqk, wqt, 3, cql, cqn, "cq", 384.0),
                                                                 (wkk, wkt, 2, ckl, ckn, "ckv", 256.0)):
                        for j in range(nblk):
                            ak, at = pacc.next()
                            for k in range(8):
                                MM(P, at[:, :], wt_[:, k, j * 128:(j + 1) * 128], xnT[:, k, tk0:tk0 + 512],
                                   k == 0, k == 7, [wk_[k]] + xk_keys(tk0, 512), [ak])
                            CP(P, "dve", lat[:, j, :], at[:, :], [ak], [(lkey, j)])
                            ACT(P, latsq[:, j, :], lat[:, j, :], AF.Square, [(lkey, j)], [(lkey + "sq", j)])
                        sk_, st_ = pacc.next()
                        for j in range(nblk):
                            MM(P, st_[:, :], onesb[:, :], latsq[:, j, :], j == 0, j == nblk - 1,
                               ["onesb", (lkey + "sq", j)], [sk_])
                        fk, ft = f32t.next()
                        ACT(P, ft[:, :], st_[:, :], AF.Sqrt, [sk_], [fk], scale=1.0 / dim, bias=EPS)
                        RECIP(P, "dve", ft[:, :], ft[:, :], [fk], [fk])
                        for j in range(nblk):
                            if "dbg5" in phases:
                                TT(P, "dve", lat[:, j, :], lat[:, j, :], ft[:, :], ALU.mult,
                                   [(lkey, j), fk], [(lkey, j)])
                                CP(P, "act", latn[:, j, :], lat[:, j, :], [(lkey, j)], [(lkey + "n", j)])
                            else:
                                TT(P, "dve", latn[:, j, :], lat[:, j, :], ft[:, :], ALU.mult,
                                   [(lkey, j), fk], [(lkey + "n", j)])
                    if "nokr" not in phases:
                        ak, at = pacc.next()
                        for k in range(8):
                            MM(P, at[:, :], wkrp[:, k, :], xnT[:, k, tk0:tk0 + 512], k == 0, k == 7,
                               ["wkrp"] + xk_keys(tk0, 512), [ak])
                        CP(P, "act", krt[0:96, :], at[0:96, :], [ak], ["krt"])
                    cqn_keys = [("cqn", j) for j in range(3)]
                    ckn_keys = [("ckvn", j) for j in range(2)]
                    for h in (range(8) if "nomlah" not in phases else []):
                        for which in ("q", "k"):
                            ak, at = pacc.next()
                            xk_, xt_ = hx.next()
                            if which == "q":
                                for j in range(3):
                                    MM(P, at[0:96, :], wuqb[:, j, h * 96:(h + 1) * 96], cqn[:, j, :], j == 0, j == 2,
                                       ["wuqb"] + cqn_keys, [ak])
                                CP(P, "act", xt_[0:96, :], at[0:96, :], [ak], [xk_])
                            else:
                                for j in range(2):
                                    MM(P, at[0:64, :], wukvb[:, j, h * 64:(h + 1) * 64], ckn[:, j, :], j == 0, j == 1,
                                       ["wukvb"] + ckn_keys, [ak])
                                CP(P, "act", xt_[0:64, :], at[0:64, :], [ak], [xk_])
                                CP(P, "pool", xt_[64:96, :], krt[64:96, :], ["krt"], [xk_])
                            sqk, sqt = hsq.next()
                            ACT(P, sqt[0:96, :], xt_[0:96, :], AF.Square, [xk_], [sqk])
                            sk_, st_ = pacc.next()
                            MM(P, st_[0:96, :], onesb[0:96, 0:96], sqt[0:96, :], True, True, ["onesb", sqk], [sk_])
                            fk, ft = f32t.next()
                            ACT(P, ft[0:96, :], st_[0:96, :], AF.Sqrt, [sk_], [fk], scale=1.0 / 96, bias=EPS)
                            RECIP(P, "dve", ft[0:96, :], ft[0:96, :], [fk], [fk])
                            gcol = 0 if which == "q" else 1
                            xgk, xgt = hxg.next()
                            TS(P, "dve", xgt[0:96, :], xt_[0:96, :], gqk_sb[0:96, gcol:gcol + 1], None, ALU.mult, None,
                               [xk_, "gqk"], [xgk])
                            rk, rt = pacc.next()
                            MM(P, rt[0:96, :], rmat_sb[0:96, 0:96], xgt[0:96, :], True, True, ["rmat", xgk], [rk])
                            t1k, t1 = f32t.next()
                            TT(P, "pool", t1[0:96, :], xgt[0:96, :], rope_sb[:, 0, :], ALU.mult, [xgk, "rope"], [t1k])
                            t2k, t2 = f32t.next()
                            TT(P, "dve", t2[0:96, :], rt[0:96, :], rope_sb[:, 1, :], ALU.mult, [rk, "rope"], [t2k])
                            TT(P, "pool", t1[0:96, :], t1[0:96, :], t2[0:96, :], ALU.add, [t1k, t2k], [t1k])
                            okk, ot = ob.next()
                            TT(P, "dve", ot[0:96, :], t1[0:96, :], ft[0:96, :], ALU.mult, [t1k, fk], [okk])
                            dst = MQ if which == "q" else MK
                            DMA(P, "sp", dst[h, :, g0:g0 + 512], ot[0:96, :], [okk], [])
                    if "dupgrp" in phases:
                        for rep in range(2):
                            ak, at = pacc.next()
                            for k in range(8):
                                MM(P, at[:, :], wkt[:, k, 0:128], xnT[:, k, tk0:tk0 + 512],
                                   k == 0, k == 7, [wkk[k]] + xk_keys(tk0, 512), [ak])
                    for t in (range(4 if "v_one" not in phases else 1) if "nomlav" not in phases else []):
                        ak, at = pacc.next()
                        for j in range(2):
                            MM(P, at[:, :], (xnT[:, j, tk0 + t * 128:tk0 + (t + 1) * 128] if "dbg1" in phases else ckn[:, j, t * 128:(t + 1) * 128]),
                               (wkt[:, j, :] if "dbg2" in phases else (wukvb[:, j, 0:512] if "dbg4" in phases else wukvb[:, j, 512:1024])), j == 0, j == 1,
                               ["wukvb"] + ckn_keys, [ak])
                        okk, ot = ob.next()
                        if "v_noevac" in phases:
                            continue
                        CP(P, "dve", ot[:, :], at[:, :], [ak], [okk])
                        if "v_nodma" in phases:
                            continue
                        DMA(P, "sp", MV[:, :, (g0 + t * 128) // 128, :].rearrange("h p d -> p h d"),
                            ot[:, :].rearrange("p (h d) -> p h d", h=8), [okk], [])

        P.emit()
        stats = P.stats
    return nc, stats


def _gain_cols(g, nch):
    return np.ascontiguousarray(g.reshape(nch, 128).T)


def rope_tables():
    pos = np.arange(S, dtype=np.float32)
    inv = (10000.0 ** (-np.arange(0, 32, 2, dtype=np.float32) / np.float32(32))).astype(np.float32)
    ang = (pos[:, None] * inv[None, :]).astype(np.float32)
    c = np.cos(ang.astype(np.float64)).astype(np.float32)
    s = np.sin(ang.astype(np.float64)).astype(np.float32)
    CT = np.ones((96, S), np.float32)
    ST = np.zeros((96, S), np.float32)
    CT[64:80] = c.T
    CT[80:96] = c.T
    ST[64:80] = s.T
    ST[80:96] = s.T
    return CT, ST


def consts():
    R = np.zeros((96, 96), np.float32)
    for j in range(16):
        R[80 + j, 64 + j] = -1.0
        R[64 + j, 80 + j] = 1.0
    return dict(identb=np.eye(128, dtype=np.float32).astype(NPBF), rmat=R.astype(NPBF),
                onesf=np.ones((128, 128), np.float32))


def stageA_inmaps(x, prm, l):
    CT, ST = rope_tables()
    cst = consts()
    convp = np.zeros((128, 4, 34), np.float32)
    cw = prm["conv_w"][l]
    convp[:, :, 0:31] = cw.T.reshape(4, 128, 31).transpose(1, 0, 2)
    convp[:, :, 31] = prm["conv_b"][l].reshape(4, 128).T
    convp[:, :, 32] = prm["conv_ln_g"][l].reshape(4, 128).T
    convp[:, :, 33] = prm["conv_ln_b"][l].reshape(4, 128).T
    maps = []
    for c in range(NCORES):
        b, q = c // 4, c % 4
        s0 = q * TPC
        xe = np.zeros((TPC + 2 * HALO, D), np.float32)
        lo, hi = max(0, s0 - HALO), min(S, s0 + TPC + HALO)
        xe[lo - (s0 - HALO):hi - (s0 - HALO)] = x[b, lo:hi]
        rope = np.stack([CT[:, s0:s0 + TPC], ST[:, s0:s0 + TPC]], axis=1)
        maps.append(dict(
            xe=xe, w_in=prm["w_in"][l], gmix=_gain_cols(prm["mix_norm_g"][l], 8),
            identb=cst["identb"], convp=convp, gcq=_gain_cols(prm["cq_norm_g"][l], 3),
            gckv=_gain_cols(prm["ckv_norm_g"][l], 2), w_uq=prm["w_uq"][l], w_ukv=prm["w_ukv"][l],
            gqk=np.ascontiguousarray(np.stack([prm["q_norm_g"][l], prm["k_norm_g"][l]], axis=1)),
            ropeT=np.ascontiguousarray(rope), rmat=cst["rmat"], onesf=cst["onesf"]))
    return maps


def build_mlstm(nch=S // 128, env=None, pre=None):
    nc, es, C, P = _begin(env, pre)
    ns = nch * 128
    lnscale = math.log(128.0 ** -0.5)
    with es:
        qTd = C.dram("qT", [128, ns], BF16, "ExternalInput")
        ktd = C.dram("kt", [ns, 128], BF16, "ExternalInput")
        vtd = C.dram("vt", [ns, 128], BF16, "ExternalInput")
        g4d = C.dram("g4", [ns, 4], F32, "ExternalInput")
        bifd = C.dram("bif", [128, 4], F32, "ExternalInput")
        ogd = C.dram("og", [ns, 128], BF16, "ExternalInput")
        gAd = C.dram("gA", [128, 128], F32, "ExternalInput")
        cmat = C.dram("cmat", [128, 6, 128], F32, "ExternalInput")
        identbd = C.dram("identb", [128, 128], BF16, "ExternalInput")
        HT = C.dram("HT", [128, ns], BF16, "ExternalOutput")

        qT = C.sb([128, ns], BF16, "qT")
        kt = C.sb([128, nch, 128], BF16, "kt")
        vt = C.sb([128, nch, 129], BF16, "vt")
        hacc = C.sb([128, nch, 128], F32, "hacc")
        g4 = C.sb([128, nch, 4], F32, "g4")
        bif = C.sb([128, 4], F32, "bif")
        nbif = C.sb([128, 4], F32, "nbif")
        gA = C.sb([128, 128], F32, "gA")
        cm = C.sb([128, 6, 128], F32, "cm")
        identb = C.sb([128, 128], BF16, "identb")
        gt = {n: C.sb([128, nch], F32, n) for n in ("lf", "ib", "bcum", "gtot", "biasS", "wint", "wk", "dec", "tmp")}
        Cf = C.sb([128, 129], F32, "Cf")
        Cb = C.sb([128, 129], BF16, "Cb")
        LF = Rot([(("LF", i), C.sb([128, 128], F32, "LF")) for i in range(2)])
        Dm = Rot([(("Dm", i), C.sb([128, 128], F32, "Dm")) for i in range(2)])
        kTc = Rot([(("kTc", i), C.sb([128, 128], BF16, "kTc")) for i in range(2)])
        SD = Rot([(("SD", i), C.sb([128, 128], BF16, "SD")) for i in range(2)])
        isb = Rot([(("isb", i), C.sb([128, 129], F32, "isb")) for i in range(2)])
        num = Rot([(("num", i), C.sb([128, 129], F32, "num")) for i in range(2)])
        dn = Rot([(("dn", i), C.sb([128, 2], F32, "dn")) for i in range(2)])
        Vw = Rot([(("Vw", i), C.sb([128, 129], BF16, "Vw")) for i in range(2)])
        ogt = Rot([(("ogt", i), C.sb([128, 128], BF16, "ogt")) for i in range(2)])
        hn = Rot([(("hn", i), C.sb([128, 128], F32, "hn")) for i in range(2)])
        hb = Rot([(("hb", i), C.sb([128, 128], BF16, "hb")) for i in range(2)])
        hT = Rot([(("hT", i), C.sb([128, 128], BF16, "hT")) for i in range(2)])
        sq = C.sb([128, 128], F32, "sq")
        pA = Rot([(("pA", i), C.ps([128, 512], F32, "pA")) for i in range(6)])
        pB = Rot([(("pB", i), C.ps([128, 1024], BF16, "pB")) for i in range(2)])

        DMA(P, "sp", qT[:, :], qTd[:, :], [], ["qT"])
        DMA(P, "sp", kt[:, :, :], ktd.ap().rearrange("(c p) d -> p c d", p=128), [], ["kt"])
        DMA(P, "sp", vt[:, :, 0:128], vtd.ap().rearrange("(c p) d -> p c d", p=128), [], ["vt"])
        MEMSET(P, "pool", vt[:, :, 128:129], 1.0, ["vt1"])
        DMA(P, "sp", g4[:, :, :], g4d.ap().rearrange("(c p) g -> p c g", p=128), [], ["g4"])
        DMA(P, "sp", bif[:, :], bifd[:, :], [], ["bif"])
        DMA(P, "sp", gA[:, :], gAd[:, :], [], ["gA"])
        DMA(P, "sp", cm[:, :, :], cmat[:, :, :], [], ["cm"])
        DMA(P, "sp", identb[:, :], identbd[:, :], [], ["identb"])
        TS(P, "dve", nbif[:, :], bif[:, :], -1.0, None, ALU.mult, None, ["bif"], ["nbif"])
        ident32 = cm[:, 4, :]
        ones32 = cm[:, 5, :]
        for d in range(2):
            Ud = cm[:, d, :]
            NEGd = cm[:, 2 + d, :]
            ic, fc = 2 * d, 2 * d + 1
            ACT(P, gt["tmp"][:, :], g4[:, :, fc], AF.Exp, ["g4", "nbif"], ["tmp"], scale=-1.0, bias=nbif[:, fc:fc + 1])
            ACT(P, gt["tmp"][:, :], gt["tmp"][:, :], AF.Ln, ["tmp"], ["tmp"], bias=1.0)
            TS(P, "dve", gt["lf"][:, :], gt["tmp"][:, :], -1.0, None, ALU.mult, None, ["tmp"], ["lf"])
            TS(P, "dve", gt["ib"][:, :], g4[:, :, ic], bif[:, ic:ic + 1], lnscale, ALU.add, ALU.add, ["g4", "bif"], ["ib"])
            bk, bp = pA.next()
            MM(P, bp[:, 0:nch], Ud, gt["lf"][:, :], True, True, ["cm", "lf"], [bk])
            CP(P, "dve", gt["bcum"][:, :], bp[:, 0:nch], [bk], ["bcum"])
            gk, gp = pA.next()
            MM(P, gp[:, 0:nch], ones32, gt["lf"][:, :], True, True, ["cm", "lf"], [gk])
            CP(P, "dve", gt["gtot"][:, :], gp[:, 0:nch], [gk], ["gtot"])
            TT(P, "dve", gt["biasS"][:, :], gt["ib"][:, :], gt["bcum"][:, :], ALU.subtract, ["ib", "bcum"], ["biasS"])
            ACT(P, gt["wint"][:, :], gt["bcum"][:, :], AF.Exp, ["bcum"], ["wint"])
            TT(P, "dve", gt["tmp"][:, :], gt["biasS"][:, :], gt["gtot"][:, :], ALU.add, ["biasS", "gtot"], ["tmp"])
            ACT(P, gt["wk"][:, :], gt["tmp"][:, :], AF.Exp, ["tmp"], ["wk"])
            ACT(P, gt["dec"][:, :], gt["gtot"][:, :], AF.Exp, ["gtot"], ["dec"])
            MEMSET(P, "dve", Cf[:, :], 0.0, ["Cf"])
            MEMSET(P, "pool", Cb[:, :], 0.0, ["Cb"])
            order = range(nch) if d == 0 else range(nch - 1, -1, -1)
            for c in order:
                lk, lt = LF.next()
                TS(P, "pool", lt[:, :], ones32, gt["lf"][:, c:c + 1], None, ALU.mult, None, ["cm", "lf"], [lk])
                dk, dp = pA.next()
                MM(P, dp[:, 0:128], lt[:, :], Ud, True, False, [lk, "cm"], [dk])
                MM(P, dp[:, 0:128], ident32, NEGd, False, True, ["cm"], [dk])
                mk_, mt_ = Dm.next()
                ACT(P, mt_[:, :], dp[:, 0:128], AF.Exp, [dk, "biasS"], [mk_], bias=gt["biasS"][:, c:c + 1])
                tk, tp = pB.next()
                TR(P, tp[:, 0:128], kt[:, c, :], identb[:, :], ["kt", "identb"], [tk])
                kck, kct = kTc.next()
                CP(P, "dve", kct[:, :], tp[:, 0:128], [tk], [kck])
                sk, sp_ = pA.next()
                MM(P, sp_[:, 0:128], kct[:, :], qT[:, c * 128:(c + 1) * 128], True, True, [kck, "qT"], [sk])
                sdk, sdt = SD.next()
                TT(P, "dve", sdt[:, :], sp_[:, 0:128], mt_[:, :], ALU.mult, [sk, mk_], [sdk])
                nk_, np_ = pA.next()
                MM(P, np_[:, 0:129], sdt[:, :], vt[:, c, :], True, True, [sdk, "vt", "vt1"], [nk_])
                ik, ip = pA.next()
                MM(P, ip[:, 0:129], qT[:, c * 128:(c + 1) * 128], Cb[:, :], True, True, ["qT", "Cb"], [ik])
                isk, ist = isb.next()
                ACT(P, ist[:, :], ip[:, 0:129], AF.Copy, [ik, "wint"], [isk], scale=gt["wint"][:, c:c + 1])
                nmk, nmt = num.next()
                TT(P, "dve", nmt[:, :], np_[:, 0:129], ist[:, :], ALU.add, [nk_, isk], [nmk])
                dnk, dnt = dn.next()
                ACT(P, dnt[:, 0:1], nmt[:, 128:129], AF.Abs, [nmk], [dnk])
                TS(P, "dve", dnt[:, 0:1], dnt[:, 0:1], 1.0, None, ALU.max, None, [dnk], [dnk])
                RECIP(P, "dve", dnt[:, 1:2], dnt[:, 0:1], [dnk], [dnk])
                if d == 0:
                    TS(P, "dve", hacc[:, c, :], nmt[:, 0:128], dnt[:, 1:2], None, ALU.mult, None, [nmk, dnk], [("hacc", c)])
                else:
                    STT(P, "dve", hacc[:, c, :], nmt[:, 0:128], dnt[:, 1:2], hacc[:, c, :], ALU.mult, ALU.add,
                        [nmk, dnk, ("hacc", c)], [("hacc", c)])
                vwk, vwt = Vw.next()
                TS(P, "pool", vwt[:, :], vt[:, c, :], gt["wk"][:, c:c + 1], None, ALU.mult, None, ["vt", "vt1", "wk"], [vwk])
                ck, cp_ = pA.next()
                MM(P, cp_[:, 0:129], kt[:, c, :], vwt[:, :], True, True, ["kt", vwk], [ck])
                STT(P, "dve", Cf[:, :], Cf[:, :], gt["dec"][:, c:c + 1], cp_[:, 0:129], ALU.mult, ALU.add,
                    ["Cf", "dec", ck], ["Cf"])
                CP(P, "act", Cb[:, :], Cf[:, :], ["Cf"], ["Cb"])
        for c in range(nch):
            ogk, ogt_ = ogt.next()
            DMA(P, "sp", ogt_[:, :], ogd[c * 128:(c + 1) * 128, :], [], [ogk])
            dnk, dnt = dn.next()
            ACT(P, sq[:, :], hacc[:, c, :], AF.Square, [("hacc", c)], ["sq"])
            RSUM(P, "dve", dnt[:, 0:1], sq[:, :], ["sq"], [dnk])
            ACT(P, dnt[:, 1:2], dnt[:, 0:1], AF.Sqrt, [dnk], [dnk], scale=1.0 / 128, bias=EPS)
            RECIP(P, "dve", dnt[:, 1:2], dnt[:, 1:2], [dnk], [dnk])
            hk, ht = hn.next()
            STT(P, "dve", ht[:, :], hacc[:, c, :], dnt[:, 1:2], gA[:, :], ALU.mult, ALU.mult, [("hacc", c), dnk, "gA"], [hk])
            hbk, hbt = hb.next()
            TT(P, "pool", hbt[:, :], ht[:, :], ogt_[:, :], ALU.mult, [hk, ogk], [hbk])
            tk, tp = pB.next()
            TR(P, tp[:, 0:128], hbt[:, :], identb[:, :], [hbk, "identb"], [tk])
            htk, htt = hT.next()
            CP(P, "act", htt[:, :], tp[:, 0:128], [tk], [htk])
            DMA(P, "sp", HT[:, c * 128:(c + 1) * 128], htt[:, :], [htk], [])
        P.emit()
        stats = P.stats
    return nc, stats


def mlstm_consts():
    s_ = np.arange(128)[:, None]
    t_ = np.arange(128)[None, :]
    U = (s_ <= t_).astype(np.float32)
    cm = np.zeros((128, 6, 128), np.float32)
    cm[:, 0] = U
    cm[:, 1] = U.T
    cm[:, 2] = np.where(s_ <= t_, 0.0, -30000.0)
    cm[:, 3] = np.where(s_ >= t_, 0.0, -30000.0)
    cm[:, 4] = np.eye(128)
    cm[:, 5] = 1.0
    return dict(cmat=cm, identb=np.eye(128, dtype=np.float32).astype(NPBF))


def build_merge(ntile=TPC // 128, env=None, pre=None):
    nc, es, C, P = _begin(env, pre)
    nt = ntile * 128
    with es:
        xd = C.dram("x", [nt, D], F32, "ExternalInput")
        srcs = [C.dram(n, [512, nt], BF16, "ExternalInput") for n in ("HT", "UT", "OT")]
        gtsd = C.dram("GTS", [nt, 3072], BF16, "ExternalInput")
        wds = [C.dram(n, [512, D], F32, "ExternalInput") for n in ("w_a", "w_b", "w_c")]
        wod = C.dram("w_o", [D, D], F32, "ExternalInput")
        gfd = C.dram("gffn", [128, 8], F32, "ExternalInput")
        wrd = C.dram("w_r", [D, 16], F32, "ExternalInput")
        identbd = C.dram("identb", [128, 128], BF16, "ExternalInput")
        ident32d = C.dram("ident32", [128, 128], F32, "ExternalInput")
        x1d = C.dram("x1", [nt, D], F32, "ExternalOutput")
        xn2d = C.dram("xn2T", [D, nt], BF16, "ExternalOutput")
        affd = C.dram("aff", [nt, 16], F32, "ExternalOutput")
        affTd = C.dram("affT", [16, nt], F32, "ExternalOutput")
        affTs = C.sb([16, nt], F32, "affTs")

        wbr = [C.sb([128, 4, D], BF16, "wbr") for _ in range(3)]
        wo = C.sb([128, 8, D], BF16, "wo")
        wst = Rot([(("wst", i), C.sb([128, 4, D], F32, "wst")) for i in range(2)])
        wr = C.sb([128, 8, 16], F32, "wr")
        gf = C.sb([128, 8], F32, "gf")
        gfull = C.sb([128, 8, 128], F32, "gfull")
        identb = C.sb([128, 128], BF16, "identb")
        ident32 = C.sb([128, 128], F32, "ident32")
        srct = [Rot([((("src", b), i), C.sb([128, 4, 128], BF16, "src")) for i in range(2)]) for b in range(3)]
        gts = Rot([(("gts", i), C.sb([128, 3072], BF16, "gts")) for i in range(2)])
        xt = Rot([(("xt", i), C.sb([128, D], F32, "xt")) for i in range(2)])
        mg = C.sb([128, D], F32, "mg")
        tmpm = Rot([(("tmpm", i), C.sb([128, 512], F32, "tmpm")) for i in range(2)])
        mgb = C.sb([128, D], BF16, "mgb")
        mT = C.sb([128, 8, 128], BF16, "mT")
        x1 = Rot([(("x1", i), C.sb([128, D], F32, "x1")) for i in range(2)])
        sqj = C.sb([128, D], F32, "sqj")
        xs = C.sb([128, D], F32, "xs")
        st = Rot([(("st", i), C.sb([128, 16], F32, "st")) for i in range(2)])
        xT32 = C.sb([128, 8, 128], F32, "xT32")
        xTb = Rot([(("xTb", i), C.sb([128, 8, 128], BF16, "xTb")) for i in range(2)])
        lg = C.sb([128, 16], F32, "lg")
        ex = C.sb([128, 16], F32, "ex")
        affs = C.sb([128, ntile, 16], F32, "affs")
        pA = Rot([(("pA", i), C.ps([128, 512], F32, "pA")) for i in range(5)])
        pB = C.ps([128, 1024], BF16, "pB")

        DMA(P, "sp", identb[:, :], identbd[:, :], [], ["identb"])
        DMA(P, "sp", ident32[:, :], ident32d[:, :], [], ["ident32"])
        DMA(P, "sp", gf[:, :], gfd[:, :], [], ["gf"])
        DMA(P, "sp", wr[:, :, :], wrd.ap().rearrange("(c p) e -> p c e", p=128), [], ["wr"])
        for b in range(3):
            sk, stg = wst.next()
            DMA(P, "sp", stg[:, :, :], wds[b].ap().rearrange("(c p) n -> p c n", p=128), [], [sk])
            for c in range(4):
                CP(P, ("dve", "pool")[c % 2], wbr[b][:, c, :], stg[:, c, :], [sk], [("wbr", b)])
        for hh in range(2):
            sk, stg = wst.next()
            DMA(P, "sp", stg[:, :, :], wod.ap().rearrange("(c p) n -> p c n", p=128)[:, hh * 4:(hh + 1) * 4, :], [], [sk])
            for c in range(4):
                CP(P, ("dve", "pool")[c % 2], wo[:, hh * 4 + c, :], stg[:, c, :], [sk], ["wo"])
        for k in range(8):
            TS(P, "pool", gfull[:, k, :], ident32[:, :], 0.0, gf[:, k:k + 1], ALU.mult, ALU.add, ["ident32", "gf"], ["gfull"])
        for t in range(ntile):
            r0 = t * 128
            xk, xt_ = xt.next()
            DMA(P, "sp", xt_[:, :], xd[r0:r0 + 128, :], [], [xk])
            gk, gt_ = gts.next()
            DMA(P, "sp", gt_[:, :], gtsd[r0:r0 + 128, :], [], [gk])
            skeys = []
            stiles = []
            for b in range(3):
                k_, t_ = srct[b].next()
                DMA(P, "sp", t_[:, :, :], srcs[b].ap().rearrange("(c p) t -> p c t", p=128)[:, :, r0:r0 + 128], [], [k_])
                skeys.append(k_)
                stiles.append(t_)
            for b in range(3):
                for hf in range(2):
                    ak, at = pA.next()
                    for c in range(4):
                        MM(P, at[:, :], stiles[b][:, c, :], wbr[b][:, c, hf * 512:(hf + 1) * 512], c == 0, c == 3,
                           [skeys[b], ("wbr", b)], [ak])
                    gsl = gt_[:, b * 1024 + hf * 512:b * 1024 + (hf + 1) * 512]
                    if b == 0:
                        TT(P, "dve", mg[:, hf * 512:(hf + 1) * 512], at[:, :], gsl, ALU.mult, [ak, gk], [("mg", hf)])
                    else:
                        tk, tt_ = tmpm.next()
                        TT(P, "dve", tt_[:, :], at[:, :], gsl, ALU.mult, [ak, gk], [tk])
                        TT(P, "pool", mg[:, hf * 512:(hf + 1) * 512], mg[:, hf * 512:(hf + 1) * 512], tt_[:, :], ALU.add,
                           [("mg", hf), tk], [("mg", hf)])
            CP(P, "act", mgb[:, :], mg[:, :], [("mg", 0), ("mg", 1)], ["mgb"])
            for k in range(8):
                TR(P, pB[:, k * 128:(k + 1) * 128], mgb[:, k * 128:(k + 1) * 128], identb[:, :], ["mgb", "identb"], ["pB"])
            CP(P, "dve", mT[:, :, :], pB[:].rearrange("p (k t) -> p k t", k=8), ["pB"], ["mT"])
            x1k, x1t = x1.next()
            for hf in range(2):
                ak, at = pA.next()
                for k in range(8):
                    MM(P, at[:, :], mT[:, k, :], wo[:, k, hf * 512:(hf + 1) * 512], k == 0, k == 7, ["mT", "wo"], [ak])
                TT(P, "dve", x1t[:, hf * 512:(hf + 1) * 512], at[:, :], xt_[:, hf * 512:(hf + 1) * 512], ALU.add,
                   [ak, xk], [(x1k, hf)])
            DMA(P, "sp", x1d[r0:r0 + 128, :], x1t[:, :], [(x1k, 0), (x1k, 1)], [])
            sk_, st_ = st.next()
            ACT(P, sqj[:, :], x1t[:, :], AF.Square, [(x1k, 0), (x1k, 1)], ["sqj"])
            RSUM(P, "dve", st_[:, 0:1], sqj[:, :], ["sqj"], [sk_])
            ACT(P, st_[:, 1:2], st_[:, 0:1], AF.Sqrt, [sk_], [sk_], scale=1.0 / D, bias=EPS)
            RECIP(P, "dve", st_[:, 1:2], st_[:, 1:2], [sk_], [sk_])
            ACT(P, xs[:, :], x1t[:, :], AF.Copy, [(x1k, 0), (x1k, 1), sk_], ["xs"], scale=st_[:, 1:2])
            for hf in range(2):
                ak, at = pA.next()
                for k in range(4):
                    kk = hf * 4 + k
                    TR(P, at[:, k * 128:(k + 1) * 128], xs[:, kk * 128:(kk + 1) * 128], ident32[:, :], ["xs", "ident32"], [ak])
                TT(P, "dve", xT32[:, hf * 4:(hf + 1) * 4, :], at[:].rearrange("p (k t) -> p k t", k=4),
                   gfull[:, hf * 4:(hf + 1) * 4, :], ALU.mult, [ak, "gfull"], [("xT32", hf)])
            xbk, xbt = xTb.next()
            CP(P, "act", xbt[:, :, :], xT32[:, :, :], [("xT32", 0), ("xT32", 1)], [xbk])
            DMA(P, "sp", xn2d.ap().rearrange("(k p) t -> p k t", p=128)[:, :, r0:r0 + 128], xbt[:, :, :], [xbk], [])
            ak, at = pA.next()
            for k in range(8):
                MM(P, at[:, 0:16], xT32[:, k, :], wr[:, k, :], k == 0, k == 7, [("xT32", 0), ("xT32", 1), "wr"], [ak])
            CP(P, "dve", lg[:, :], at[:, 0:16], [ak], ["lg"])
            RMAX(P, "dve", st_[:, 2:3], lg[:, :], ["lg"], [sk_])
            TS(P, "dve", st_[:, 2:3], st_[:, 2:3], -1.0, None, ALU.mult, None, [sk_], [sk_])
            ACT(P, ex[:, :], lg[:, :], AF.Exp, ["lg", sk_], ["ex"], bias=st_[:, 2:3])
            RSUM(P, "dve", st_[:, 3:4], ex[:, :], ["ex"], [sk_])
            RECIP(P, "dve", st_[:, 3:4], st_[:, 3:4], [sk_], [sk_])
            TS(P, "dve", affs[:, t, :], ex[:, :], st_[:, 3:4], None, ALU.mult, None, ["ex", sk_], [("affs", t)])
            ak, at = pA.next()
            TR(P, at[0:16, 0:128], affs[:, t, :], ident32[:, :], [("affs", t), "ident32"], [ak])
            CP(P, "act", affTs[:, r0:r0 + 128], at[0:16, 0:128], [ak], [("affT", t)])
        DMA(P, "sp", affd.ap().rearrange("(t p) e -> p t e", p=128), affs[:, :, :], [("affs", t) for t in range(ntile)], [])
        DMA(P, "sp", affTd[:, :], affTs[:, :], [("affT", t) for t in range(ntile)], [])
        P.emit()
        stats = P.stats
    return nc, stats


def build_thr(ns=S, cap=2 * S // 16, iters=30, env=None, pre=None):
    nc, es, C, P = _begin(env, pre)
    with es:
        affT = C.dram("affT", [16, ns], F32, "ExternalInput")
        thr = C.dram("thr", [16, 2], F32, "ExternalOutput")
        a = C.sb([16, ns], F32, "a")
        junk = C.sb([16, ns], F32, "junk")
        lh = C.sb([16, 2], F32, "lh")
        w = C.sb([16, 8], F32, "w")
        DMA(P, "sp", a[:, :], affT[:, :], [], ["a"])
        MEMSET(P, "dve", lh[:, 0:1], 0.0, ["lh"])
        MEMSET(P, "dve", lh[:, 1:2], 1.0, ["lh"])
        for it in range(iters):
            TT(P, "dve", w[:, 0:1], lh[:, 0:1], lh[:, 1:2], ALU.add, ["lh"], ["w"])
            TS(P, "dve", w[:, 0:1], w[:, 0:1], 0.5, None, ALU.mult, None, ["w"], ["w"])
            P.op("dve", lambda e: e.tensor_scalar(out=junk[:, :], in0=a[:, :], scalar1=w[:, 0:1], scalar2=0.0,
                                                  op0=ALU.is_ge, op1=ALU.add, accum_out=w[:, 1:2]),
                 ["a", "w"], ["junk", "w"])
            TS(P, "dve", w[:, 2:3], w[:, 1:2], float(cap), None, ALU.is_ge, None, ["w"], ["w"])
            TT(P, "dve", w[:, 3:4], w[:, 0:1], lh[:, 0:1], ALU.subtract, ["w", "lh"], ["w"])
            TT(P, "dve", w[:, 4:5], lh[:, 1:2], w[:, 0:1], ALU.subtract, ["w", "lh"], ["w"])
            STT(P, "dve", lh[:, 0:1], w[:, 3:4], w[:, 2:3], lh[:, 0:1], ALU.mult, ALU.add, ["w", "lh"], ["lh"])
            STT(P, "dve", lh[:, 1:2], w[:, 4:5], w[:, 2:3], w[:, 0:1], ALU.mult, ALU.add, ["w", "lh"], ["lh"])
        DMA(P, "sp", thr[:, :], lh[:, :], ["lh"], [])
        P.emit()
        stats = P.stats
    return nc, stats


def build_ffn(nt=TPC, nexp=16, tb=1024, env=None, pre=None):
    nc, es, C, P = _begin(env, pre)
    FF = 1536
    ntile = nt // 128
    nblk = nt // tb
    with es:
        x1d = C.dram("x1", [nt, D], F32, "ExternalInput")
        xnd = C.dram("xn2T", [D, nt], BF16, "ExternalInput")
        affd = C.dram("aff", [nt, 16], F32, "ExternalInput")
        thrd = C.dram("thr_row", [128, 16], F32, "ExternalInput") if not (pre is not None and "thr16" in pre) else None
        wgd = C.dram("wg", [nexp, D, FF], F32, "ExternalInput")
        wud = C.dram("wu", [nexp, D, FF], F32, "ExternalInput")
        wdd = C.dram("wd", [nexp, FF, D], F32, "ExternalInput")
        x2d = C.dram("x2", [nt, D], F32, "ExternalOutput")

        xb = C.sb([128, 8, tb], BF16, "xb")
        acc = C.sb([128, tb // 128, D], F32, "acc")
        wgb = C.sb([128, 8, FF], BF16, "wgb")
        wub = C.sb([128, 8, FF], BF16, "wub")
        wdb = C.sb([128, 12, D], BF16, "wdb")
        stg = Rot([(("stg", i), C.sb([128, 4096], F32, "stg")) for i in range(2)])
        hT = C.sb([128, 12, tb], BF16, "hT")
        sg = Rot([(("sg", i), C.sb([128, 512], F32, "sg")) for i in range(2)])
        affs = C.sb([128, ntile, 16], F32, "affs")
        gw = C.sb([128, ntile, 16], F32, "gw")
        thr = C.sb([128, 16], F32, "thr")
        xo = Rot([(("xo", i), C.sb([128, D], F32, "xo")) for i in range(2)])
        pA = Rot([(("pA", i), C.ps([128, 512], F32, "pA")) for i in range(7)])

        if pre is not None and "thr16" in pre:
            t16d = pre["thr16"]
            i32d = pre["ident32"]
            t16 = C.sb([16, 2], F32, "t16")
            tbc = C.sb([16, 128], F32, "tbc")
            i16 = C.sb([16, 16], F32, "i16")
            DMA(P, "sp", t16[:, :], t16d[:, :], [], ["t16"])
            DMA(P, "sp", i16[:, :], i32d[0:16, 0:16], [], ["i16"])
            MEMSET(P, "dve", tbc[:, :], 1.0, ["tbc"])
            TS(P, "dve", tbc[:, :], tbc[:, :], t16[:, 0:1], None, ALU.mult, None, ["tbc", "t16"], ["tbc"])
            tk_, tp_ = pA.next()
            MM(P, tp_[:, 0:16], tbc[:, :], i16[:, :], True, True, ["tbc", "i16"], [tk_])
            CP(P, "dve", thr[:, :], tp_[:, 0:16], [tk_], ["thr"])
        else:
            DMA(P, "sp", thr[:, :], thrd[:, :], [], ["thr"])
        DMA(P, "sp", affs[:, :, :], affd.ap().rearrange("(t p) e -> p t e", p=128), [], ["affs"])
        for t in range(ntile):
            TT(P, "dve", gw[:, t, :], affs[:, t, :], thr[:, :], ALU.is_ge, ["affs", "thr"], ["gw"])
            TT(P, "dve", gw[:, t, :], gw[:, t, :], affs[:, t, :], ALU.mult, ["gw", "affs"], ["gw"])
        cv = [0]

        def conv(dst, src, r, w):
            eng = ("dve", "pool", "act")[cv[0] % 3]
            cv[0] += 1
            CP(P, eng, dst, src, r, w)

        def load_gu(e, chs=(0, 1, 2)):
            for ch in chs:
                for (wd_, dstb, key) in ((wgd, wgb, "wgb"), (wud, wub, "wub")):
                    sk, st = stg.next()
                    sv = st[:, :].rearrange("p (c f) -> p c f", c=8)
                    DMA(P, "sp", sv, wd_[e].rearrange("(c p) f -> p c f", p=128)[:, :, ch * 512:(ch + 1) * 512], [], [sk])
                    for hh in range(2):
                        conv(dstb[:, hh * 4:(hh + 1) * 4, ch * 512:(ch + 1) * 512], sv[:, hh * 4:(hh + 1) * 4, :], [sk],
                             [(key, ch)])

        def load_d(e):
            for ch in range(3):
                sk, st = stg.next()
                sv = st[:, :].rearrange("p (c n) -> p c n", c=4)
                DMA(P, "sp", sv, wdd[e].rearrange("(c p) n -> p c n", p=128)[:, ch * 4:(ch + 1) * 4, :], [], [sk])
                for hh in range(2):
                    conv(wdb[:, ch * 4 + hh * 2:ch * 4 + hh * 2 + 2, :], sv[:, hh * 2:hh * 2 + 2, :], [sk], ["wdb"])

        first = True
        for blk in range(nblk):
            b0 = blk * tb
            DMA(P, "sp", xb[:, :, :], xnd.ap().rearrange("(k p) t -> p k t", p=128)[:, :, b0:b0 + tb], [], ["xb"])
            for e in range(nexp):
                if first:
                    load_gu(e)
                    load_d(e)
                    first = False
                nxt = (blk * nexp + e + 1)
                for f in range(12):
                    for tq in range(tb // 512):
                        gk, gp = pA.next()
                        uk, up = pA.next()
                        for k in range(8):
                            MM(P, gp[:, :], wgb[:, k, f * 128:(f + 1) * 128], xb[:, k, tq * 512:(tq + 1) * 512],
                               k == 0, k == 7, [("wgb", f // 4), "xb"], [gk])
                        for k in range(8):
                            MM(P, up[:, :], wub[:, k, f * 128:(f + 1) * 128], xb[:, k, tq * 512:(tq + 1) * 512],
                               k == 0, k == 7, [("wub", f // 4), "xb"], [uk])
                        sk, st = sg.next()
                        ACT(P, st[:, :], gp[:, :], AF.Silu, [gk], [sk])
                        TT(P, "dve", hT[:, f, tq * 512:(tq + 1) * 512], up[:, :], st[:, :], ALU.mult, [uk, sk], [("hT", f)])
                    if f % 4 == 3 and nxt < nblk * nexp:
                        load_gu(nxt % nexp, chs=(f // 4,))
                for tt in range(tb // 128):
                    gcol = gw[:, blk * (tb // 128) + tt, e:e + 1]
                    for hf in range(2):
                        yk, yp = pA.next()
                        for f in range(12):
                            MM(P, yp[:, :], hT[:, f, tt * 128:(tt + 1) * 128], wdb[:, f, hf * 512:(hf + 1) * 512],
                               f == 0, f == 11, [("hT", f), "wdb"], [yk])
                        asl = acc[:, tt, hf * 512:(hf + 1) * 512]
                        if e == 0:
                            TS(P, "dve", asl, yp[:, :], gcol, None, ALU.mult, None, [yk, "gw"], [("acc", tt, hf)])
                        else:
                            STT(P, "dve", asl, yp[:, :], gcol, asl, ALU.mult, ALU.add, [yk, "gw", ("acc", tt, hf)],
                                [("acc", tt, hf)])
                if nxt < nblk * nexp:
                    load_d(nxt % nexp)
            for tt in range(tb // 128):
                xk, xt_ = xo.next()
                r0 = b0 + tt * 128
                DMA(P, "sp", xt_[:, :], x1d[r0:r0 + 128, :], [], [xk])
                TT(P, "pool", xt_[:, :], xt_[:, :], acc[:, tt, :], ALU.add, [xk, ("acc", tt, 0), ("acc", tt, 1)], [xk])
                DMA(P, "sp", x2d[r0:r0 + 128, :], xt_[:, :], [xk], [])
        P.emit()
        stats = P.stats
    return nc, stats


def build_attn(nq=TPC, nk=S, nheads=8, env=None, pre=None):
    nc, es, C, P = _begin(env, pre)
    NKT = nk // 128
    NQB = nq // 512
    scale = 96.0 ** -0.5
    with es:
        mq = C.dram("mq", [nheads, 96, nq], BF16, "ExternalInput")
        mk = C.dram("mk", [nheads, 96, nk], BF16, "ExternalInput")
        mv = C.dram("mv", [nheads, 128, NKT * 64], BF16, "ExternalInput")
        esel = C.dram("esel", [65, 64], F32, "ExternalInput")
        OT = C.dram("OT", [nheads * 64, nq], BF16, "ExternalOutput")

        kT = Rot([(("kT", i), C.sb([96, nk], BF16, "kT")) for i in range(2)])
        vv = Rot([(("vv", i), C.sb([128, NKT, 65], BF16, "vv")) for i in range(2)])
        qT = Rot([(("qT", i), C.sb([96, nq], BF16, "qT")) for i in range(2)])
        pT = Rot([(("pT", i), C.sb([128, 512], BF16, "pT")) for i in range(4)])
        osb = C.sb([65, 512], F32, "osb")
        rbc = C.sb([64, 512], F32, "rbc")
        oo = Rot([(("oo", i), C.sb([64, 512], BF16, "oo")) for i in range(2)])
        es_sb = C.sb([65, 64], F32, "esel")
        sps = Rot([(("sps", i), C.ps([128, 512], F32, "sps")) for i in range(4)])
        ops_ = Rot([(("ops", i), C.ps([128, 512], F32, "ops")) for i in range(2)])
        bps = C.ps([128, 512], F32, "bps")
        DMA(P, "sp", es_sb[:], esel[:, :], [], ["esel"])
        for i in range(2):
            MEMSET(P, "pool", vv.items[i][1][:, :, 64:65], 1.0, [("vv1", i)])
        for h in range(nheads):
            kk, kt_ = kT.next()
            vk, vt_ = vv.next()
            qk, qt_ = qT.next()
            DMA(P, "sp", kt_[:, :], mk[h, :, :], [], [kk])
            DMA(P, "sp", vt_[:, :, 0:64], mv[h, :, :].rearrange("p (t d) -> p t d", d=64), [], [vk])
            DMA(P, "sp", qt_[:, :], mq[h, :, :], [], [qk])
            vkeys = [vk, ("vv1", (vv.i - 1) % 2)]
            for qb in range(NQB):
                ok_, ot_ = ops_.next()

                def s_mm(t, kt_=kt_, qt_=qt_, qb=qb, kk=kk, qk=qk):
                    sk, st = sps.next()
                    MM(P, st[:, :], kt_[:, t * 128:(t + 1) * 128], qt_[:, qb * 512:(qb + 1) * 512], True, True,
                       [kk, qk], [sk])
                    return sk, st
                pend = [s_mm(0)]
                if NKT > 1:
                    pend.append(s_mm(1))
                for t in range(NKT):
                    if t + 2 < NKT:
                        pend.append(s_mm(t + 2))
                    sk, st = pend.pop(0)
                    pk, pt = pT.next()
                    ACT(P, pt[:, :], st[:, :], AF.Exp, [sk], [pk], scale=scale)
                    MM(P, ot_[0:65, :], vt_[:, t, :], pt[:, :], t == 0, t == NKT - 1, vkeys + [pk], [ok_])
                CP(P, "dve", osb[:, :], ot_[0:65, :], [ok_], ["osb"])
                MM(P, bps[0:64, :], es_sb[:, :], osb[:, :], True, True, ["esel", "osb"], ["bps"])
                CP(P, "dve", rbc[:, :], bps[0:64, :], ["bps"], ["rbc"])
                RECIP(P, "dve", rbc[:, :], rbc[:, :], ["rbc"], ["rbc"])
                ook, oot = oo.next()
                TT(P, "dve", oot[:, :], osb[0:64, :], rbc[:, :], ALU.mult, ["osb", "rbc"], [ook])
                DMA(P, "sp", OT[h * 64:(h + 1) * 64, qb * 512:(qb + 1) * 512], oot[:, :], [ook], [])
        P.emit()
        stats = P.stats
    return nc, stats


def attn_consts():
    e = np.zeros((65, 64), np.float32)
    e[64, :] = 1.0
    return dict(esel=e)


RG = [[0, 1, 2, 3], [4, 5, 6, 7]]
LAYER_W = [("w_in", [D, INW], F32), ("gmix", [128, 8], F32), ("convp", [128, 4, 34], F32), ("gcq", [128, 3], F32),
           ("gckv", [128, 2], F32), ("w_uq", [384, 768], F32), ("w_ukv", [256, 1024], F32), ("gqk", [96, 2], F32),
           ("bif", [128, 4], F32), ("gA", [128, 128], F32), ("w_a", [512, D], F32), ("w_b", [512, D], F32),
           ("w_c", [512, D], F32), ("w_o", [D, D], F32), ("gffn", [128, 8], F32), ("w_r", [D, 16], F32),
           ("wg", [16, D, 1536], F32), ("wu", [16, D, 1536], F32), ("wd", [16, 1536, D], F32)]
CONSTS = [("identb", [128, 128], BF16), ("ropeT", [96, 2, TPC], F32), ("rmat", [96, 96], BF16), ("onesf", [128, 128], F32),
          ("cmat", [128, 6, 128], F32), ("ident32", [128, 128], F32), ("esel", [65, 64], F32), ("idx", [128, 16], I32)]


def build_fused(stop=None):
    env = Env()
    nc, P = env.nc, env.P
    BYP = ALU.bypass
    CCB = 256 * 1024
    with env.es:
        def DT(name, shape, dt, kind="Internal"):
            return nc.dram_tensor(name, list(shape), dt, kind=kind)

        def allgather(name, src2d, rows, cols, dt, rkeys, wkey):
            esz = 4 if dt in (F32, I32) else 2
            rc = max(1, min(rows, CCB // (cols * esz)))
            assert rows % rc == 0
            g = DT(name, [4 * rows, cols], dt)
            for k in range(rows // rc):
                P.cc(lambda e, k=k: e.collective_compute("AllGather", BYP, replica_groups=RG, ins=[src2d[k * rc:(k + 1) * rc, :]],
                                                         outs=[g[k * 4 * rc:(k + 1) * 4 * rc, :]]), rkeys, [wkey])
            return g, rc

        def rankview(g, rc, r):
            return g.ap().rearrange("(k r x) c -> r k x c", r=4, x=rc)[r]

        ext = {"xe0": DT("xe0", [TPC + 2 * HALO, D], F32, "ExternalInput")}
        for (n, sh, dt) in CONSTS:
            ext[n] = DT(n, sh, dt, "ExternalInput")
        for l in range(2):
            for (n, sh, dt) in LAYER_W:
                ext["%s_%d" % (n, l)] = DT("%s_%d" % (n, l), sh, dt, "ExternalInput")
        out = DT("out", [TPC, D], F32, "ExternalOutput")
        xe = ext["xe0"]
        x_own = None
        for l in range(2):
            W = {n: ext["%s_%d" % (n, l)] for (n, _, _) in LAYER_W}
            L = lambda n, sh, dt: DT("%s_L%d" % (n, l), sh, dt)
            A = dict(QT=L("QT", [512, TPC], BF16), KT=L("KT", [512, TPC], BF16), Kt=L("Kt", [4, TPC, 128], BF16),
                     Vt=L("Vt", [4, TPC, 128], BF16), OG=L("OG", [4, TPC, 128], BF16), G4=L("G4", [4, TPC, 4], F32),
                     GTS=L("GTS", [TPC, 3072], BF16), UT=L("UT", [512, TPC], BF16), MQ=L("MQ", [8, 96, TPC], BF16),
                     MK=L("MK", [8, 96, TPC], BF16), MV=L("MV", [8, 128, TPC // 128, 64], BF16))
            preA = dict(xe=xe, w_in=W["w_in"], gmix=W["gmix"], identb=ext["identb"], convp=W["convp"], gcq=W["gcq"],
                        gckv=W["gckv"], w_uq=W["w_uq"], w_ukv=W["w_ukv"], gqk=W["gqk"], ropeT=ext["ropeT"],
                        rmat=ext["rmat"], onesf=ext["onesf"], **A)
            build_stageA(env=env, pre=preA)
            nc_, es_, C_, _ = _begin(env)
            with es_:
                idx = C_.sb([128, 16], I32, "idx")
                DMA(P, "sp", idx[:, :], ext["idx"][:, :], [], ["idx"])
                gQT, rcQ = allgather("gQT_L%d" % l, A["QT"].ap(), 512, TPC, BF16, [], ("g", 0))
                gKt, rcK = allgather("gKt_L%d" % l, A["Kt"].ap().rearrange("h t d -> (h t) d"), 4 * TPC, 128, BF16, [], ("g", 1))
                gVt, _ = allgather("gVt_L%d" % l, A["Vt"].ap().rearrange("h t d -> (h t) d"), 4 * TPC, 128, BF16, [], ("g", 2))
                gOG, _ = allgather("gOG_L%d" % l, A["OG"].ap().rearrange("h t d -> (h t) d"), 4 * TPC, 128, BF16, [], ("g", 3))
                gG4, rcG = allgather("gG4_L%d" % l, A["G4"].ap().rearrange("h t g -> (h t) g"), 4 * TPC, 4, F32, [], ("g", 4))
                gMK, rcMK = allgather("gMK_L%d" % l, A["MK"].ap().rearrange("h f t -> (h f) t"), 768, TPC, BF16, [], ("g", 5))
                gMV, rcMV = allgather("gMV_L%d" % l, A["MV"].ap().rearrange("h p t d -> (h p) (t d)"), 1024, 2048, BF16, [], ("g", 6))
                assert (rcQ, rcK, rcG, rcMK, rcMV) == (32, 1024, 4 * TPC, 32, 64), (rcQ, rcK, rcG, rcMK, rcMV)
                qT_s = L("qT_s", [128, S], BF16)
                kt_s = L("kt_s", [S, 128], BF16)
                vt_s = L("vt_s", [S, 128], BF16)
                og_s = L("og_s", [S, 128], BF16)
                g4_s = L("g4_s", [S, 4], F32)
                mk_s = L("mk_s", [8, 96, S], BF16)
                mv_s = L("mv_s", [8, 128, (S // 128) * 64], BF16)
                stb = Rot([(("stb", i), C_.sb([128, 16384], BF16, "stb")) for i in range(2)])
                stf = C_.sb([128, 512], F32, "stf")

                def gather(dst_ap, src_ap, col, rkeys, wkeys, tile_ap, tkey):
                    P.dma("pool", lambda e: e.indirect_dma_start(
                        out=tile_ap, out_offset=None, in_=src_ap,
                        in_offset=bass.IndirectOffsetOnAxis(ap=idx[:, col:col + 1], axis=0)), ["idx"] + rkeys, [tkey])
                    DMA(P, "sp", dst_ap, tile_ap, [tkey], wkeys)
                for i in range(4):
                    tk_, tt_ = stb.next()
                    gather(qT_s[:, i * TPC:(i + 1) * TPC], gQT[:, :], i, [("g", 0)], [("qT_s", i)], tt_[:, 0:TPC], tk_)
                for j, (gsrc, dst) in enumerate(((gKt, kt_s), (gVt, vt_s), (gOG, og_s))):
                    tk_, tt_ = stb.next()
                    gather(dst.ap().rearrange("(c p) d -> c (p d)", p=128),
                           gsrc.ap().rearrange("(c p) d -> c (p d)", p=128), 4, [("g", 1 + j)], [("tm_s", j)], tt_[:, :], tk_)
                gather(g4_s.ap().rearrange("(c p) d -> c (p d)", p=128),
                       gG4.ap().rearrange("(c p) d -> c (p d)", p=128), 11, [("g", 4)], [("tm_s", 3)], stf[:, :], "stf")
                for i in range(4):
                    DMA(P, "sp", mk_s.ap().rearrange("h f t -> (h f) t")[:, i * TPC:(i + 1) * TPC].rearrange("(k x) t -> k x t", x=rcMK),
                        rankview(gMK, rcMK, i), [("g", 5)], [("mk_s", i)])
                    DMA(P, "sp", mv_s.ap().rearrange("h p x -> (h p) x")[:, i * 2048:(i + 1) * 2048].rearrange("(k x) c -> k x c", x=rcMV),
                        rankview(gMV, rcMV, i), [("g", 6)], [("mv_s", i)])
                P.emit()
            if stop == "x1":
                return nc
            HT = L("HT", [128, S], BF16)
            build_mlstm(env=env, pre=dict(qT=qT_s, kt=kt_s, vt=vt_s, g4=g4_s, bif=W["bif"], og=og_s, gA=W["gA"],
                                          cmat=ext["cmat"], identb=ext["identb"], HT=HT))
            if stop == "m":
                return nc
            OT = L("OT", [512, TPC], BF16)
            build_attn(env=env, pre=dict(mq=A["MQ"], mk=mk_s, mv=mv_s, esel=ext["esel"], OT=OT))
            if stop == "t":
                return nc
            nc_, es_, C_, _ = _begin(env)
            with es_:
                idx = C_.sb([128, 16], I32, "idx")
                DMA(P, "sp", idx[:, :], ext["idx"][:, :], [], ["idx"])
                gHT, rcH = allgather("gHT_L%d" % l, HT.ap(), 128, S, BF16, [], "gHT")
                assert rcH == 8
                HT_own = L("HT_own", [512, TPC], BF16)
                src = gHT.ap().rearrange("r (i t) -> (r i) t", i=4)
                stb = Rot([(("stb", i), C_.sb([128, TPC], BF16, "stb")) for i in range(2)])
                for h in range(4):
                    tk_, tt_ = stb.next()
                    P.dma("pool", lambda e, h=h, tt_=tt_: e.indirect_dma_start(
                        out=tt_[:, :], out_offset=None, in_=src,
                        in_offset=bass.IndirectOffsetOnAxis(ap=idx[:, 5 + h:6 + h], axis=0)), ["idx", "gHT"], [tk_])
                    DMA(P, "sp", HT_own[h * 128:(h + 1) * 128, :], tt_[:, :], [tk_], [("HT_own", h)])
                P.emit()
            if stop == "x2":
                return nc
            x1 = L("x1", [TPC, D], F32)
            xn2T = L("xn2T", [D, TPC], BF16)
            aff = L("aff", [TPC, 16], F32)
            affT = L("affT", [16, TPC], F32)
            if l == 0:
                xin = L("xin", [TPC, D], F32)
                nc_, es_, C_, _ = _begin(env)
                with es_:
                    DMA(P, "sp", xin[:, :], ext["xe0"][HALO:HALO + TPC, :], [], ["xin"])
                    P.emit()
            else:
                xin = x_own
            build_merge(env=env, pre=dict(x=xin, HT=HT_own, UT=A["UT"], OT=OT, GTS=A["GTS"], w_a=W["w_a"], w_b=W["w_b"],
                                          w_c=W["w_c"], w_o=W["w_o"], gffn=W["gffn"], w_r=W["w_r"], identb=ext["identb"],
                                          ident32=ext["ident32"], x1=x1, xn2T=xn2T, aff=aff, affT=affT))
            if stop == "c1":
                return nc
            affT_s = L("affT_s", [16, S], F32)
            nc_, es_, C_, _ = _begin(env)
            with es_:
                gAf, rcA = allgather("gAf_L%d" % l, affT.ap(), 16, TPC, F32, [], "gAf")
                assert rcA == 16
                for i in range(4):
                    DMA(P, "sp", affT_s[:, i * TPC:(i + 1) * TPC], gAf[i * 16:(i + 1) * 16, :], ["gAf"], [("affT_s", i)])
                P.emit()
            thr = L("thr", [16, 2], F32)
            build_thr(env=env, pre=dict(affT=affT_s, thr=thr))
            if stop == "h":
                return nc
            x2 = out if l == 1 else L("x2", [TPC, D], F32)
            build_ffn(env=env, pre=dict(x1=x1, xn2T=xn2T, aff=aff, thr16=thr, ident32=ext["ident32"], wg=W["wg"], wu=W["wu"],
                                        wd=W["wd"], x2=x2))
            if l == 0:
                xe1 = L("xe1", [TPC + 2 * HALO, D], F32)
                nc_, es_, C_, _ = _begin(env)
                with es_:
                    idx = C_.sb([128, 16], I32, "idx")
                    zt = C_.sb([128, D], F32, "zt")
                    DMA(P, "sp", idx[:, :], ext["idx"][:, :], [], ["idx"])
                    MEMSET(P, "dve", zt[:, :], 0.0, ["zt"])
                    edges = L("edges", [384, D], F32)
                    DMA(P, "sp", edges[0:128, :], x2[0:128, :], [], ["edges"])
                    DMA(P, "sp", edges[128:256, :], x2[TPC - 128:TPC, :], [], ["edges"])
                    DMA(P, "sp", edges[256:384, :], zt[:, :], ["zt"], ["edges"])
                    DMA(P, "sp", xe1[HALO:HALO + TPC, :], x2[:, :], [], ["xe1m"])
                    gE, rcE = allgather("gE_L%d" % l, edges.ap(), 384, D, F32, ["edges"], "gE")
                    assert rcE == 64
                    hl = C_.sb([128, D], F32, "hl")
                    hr = C_.sb([128, D], F32, "hr")
                    P.dma("pool", lambda e: e.indirect_dma_start(
                        out=hl[:, :], out_offset=None, in_=gE[:, :],
                        in_offset=bass.IndirectOffsetOnAxis(ap=idx[:, 9:10], axis=0)), ["idx", "gE"], ["hl"])
                    P.dma("pool", lambda e: e.indirect_dma_start(
                        out=hr[:, :], out_offset=None, in_=gE[:, :],
                        in_offset=bass.IndirectOffsetOnAxis(ap=idx[:, 10:11], axis=0)), ["idx", "gE"], ["hr"])
                    DMA(P, "sp", xe1[0:HALO, :], hl[:, :], ["hl"], ["xe1l"])
                    DMA(P, "sp", xe1[HALO + TPC:, :], hr[:, :], ["hr"], ["xe1r"])
                    P.emit()
                xe = xe1
                x_own = x2
    return nc


def _grow(x, rc, r):
    return (x // rc) * (4 * rc) + r * rc + (x % rc)


def fused_idx(c):
    r = c % 4
    p = np.arange(128)
    idx = np.zeros((128, 16), np.int32)
    for i in range(4):
        idx[:, i] = _grow(r * 128 + p, 32, i)
    y = r * 32 + (p % 32)
    idx[:, 4] = _grow(y, 8, p // 32)
    idx[:, 11] = (p // 32) * 128 + y
    for h in range(4):
        idx[:, 5 + h] = _grow(p, 8, h) * 4 + r
    idx[:, 9] = _grow(128 + p, 64, r - 1) if r > 0 else _grow(256 + p, 64, r)
    idx[:, 10] = _grow(p, 64, r + 1) if r < 3 else _grow(256 + p, 64, r)
    return idx


def kernel(**inputs):
    prm = {k: np.asarray(v) for k, v in inputs.items()}
    x = np.ascontiguousarray(prm["x"], dtype=np.float32)
    nc = build_fused()
    CT, ST = rope_tables()
    cst = consts()
    mc = mlstm_consts()
    ac = attn_consts()
    i32 = np.eye(128, dtype=np.float32)
    lay = []
    for l in range(2):
        convp = np.zeros((128, 4, 34), np.float32)
        convp[:, :, 0:31] = prm["conv_w"][l].T.reshape(4, 128, 31).transpose(1, 0, 2)
        convp[:, :, 31] = prm["conv_b"][l].reshape(4, 128).T
        convp[:, :, 32] = prm["conv_ln_g"][l].reshape(4, 128).T
        convp[:, :, 33] = prm["conv_ln_b"][l].reshape(4, 128).T
        lay.append(dict(w_in=prm["w_in"][l], gmix=_gain_cols(prm["mix_norm_g"][l], 8), convp=convp,
                        gcq=_gain_cols(prm["cq_norm_g"][l], 3), gckv=_gain_cols(prm["ckv_norm_g"][l], 2),
                        w_uq=prm["w_uq"][l], w_ukv=prm["w_ukv"][l],
                        gqk=np.ascontiguousarray(np.stack([prm["q_norm_g"][l], prm["k_norm_g"][l]], axis=1)),
                        w_a=prm["w_a_out"][l], w_b=prm["w_b_out"][l], w_c=prm["w_c_out"][l], w_o=prm["w_out"][l],
                        gffn=_gain_cols(prm["ffn_norm_g"][l], 8), w_r=prm["w_router"][l],
                        wg=prm["w_e_gate"][l], wu=prm["w_e_up"][l], wd=prm["w_e_down"][l]))
    maps = []
    for c in range(NCORES):
        b, r = c // 4, c % 4
        s0 = r * TPC
        xe = np.zeros((TPC + 2 * HALO, D), np.float32)
        lo, hi = max(0, s0 - HALO), min(S, s0 + TPC + HALO)
        xe[lo - (s0 - HALO):hi - (s0 - HALO)] = x[b, lo:hi]
        m = dict(xe0=xe, identb=cst["identb"], ropeT=np.ascontiguousarray(np.stack([CT[:, s0:s0 + TPC], ST[:, s0:s0 + TPC]], axis=1)),
                 rmat=cst["rmat"], onesf=cst["onesf"], cmat=mc["cmat"], ident32=i32, esel=ac["esel"], idx=fused_idx(c))
        cols = [r, 4 + r, 8 + r, 12 + r]
        for l in range(2):
            for k_, v_ in lay[l].items():
                m["%s_%d" % (k_, l)] = v_
            m["bif_%d" % l] = np.ascontiguousarray(np.broadcast_to(prm["b_if"][l][cols], (128, 4)))
            m["gA_%d" % l] = np.ascontiguousarray(np.broadcast_to(prm["a_norm_g"][l][r], (128, 128)))
        maps.append(m)
    res = run_spmd(nc, maps)
    out = np.empty_like(x)
    for c in range(NCORES):
        b, r = c // 4, c % 4
        out[b, r * TPC:(r + 1) * TPC] = np.asarray(res[c]["out"])
    return out
```

```python
import math
from contextlib import ExitStack

import numpy as np
import ml_dtypes

import concourse.bass as bass
import concourse.mybir as mybir
from concourse.bass_utils import run_bass_kernel_spmd

F32 = mybir.dt.float32
BF16 = mybir.dt.bfloat16
I32 = mybir.dt.int32
AF = mybir.ActivationFunctionType
ALU = mybir.AluOpType
AX = mybir.AxisListType
NPBF = ml_dtypes.bfloat16

D = 1024
S = 16384
NB = 2
INW = 6832
EPS = 1e-6
NCORES = 8
TPC = S * NB // NCORES

O_AQ, O_AK, O_AV, O_AO, O_AG = 0, 512, 1024, 1536, 2048
O_GLU = 2064
O_CQ = 3088
O_CKV = 3472
O_CKR = 3728
O_GTS = 3760


class Prog:
    RING = 8

    def __init__(self, nc, es):
        self.nc = nc
        self.es = es
        self.ops = []
        self.engs = ["pe", "act", "dve", "pool", "sp"]
        self.csem = None
        self.rings = {}
        self.ccsem = None
        self.ccount = {e: 0 for e in self.engs}
        self.dcount = {e: 0 for e in self.engs}
        self.cccount = 0
        self.nstage = 0

    def cc(self, fn, r=(), w=()):
        self.ops.append(dict(eng="pool", fn=fn, r=tuple(r), w=tuple(w), dma=True, cc=True))

    def op(self, eng, fn, r=(), w=()):
        self.ops.append(dict(eng=eng, fn=fn, r=tuple(r), w=tuple(w), dma=False))

    def dma(self, eng, fn, r=(), w=()):
        self.ops.append(dict(eng=eng, fn=fn, r=tuple(r), w=tuple(w), dma=True))

    def emit(self):
        nc, es = self.nc, self.es
        ops = self.ops
        last_w = {}
        readers = {}
        deps = []
        for i, o in enumerate(ops):
            d = set()
            for k in o["r"]:
                if k in last_w:
                    d.add((last_w[k], "raw"))
            for k in o["w"]:
                if k in last_w:
                    d.add((last_w[k], "waw"))
                for j in readers.get(k, ()):
                    if j != i:
                        d.add((j, "war"))
            for k in o["r"]:
                lst = readers.setdefault(k, [])
                if not o["dma"]:
                    lst[:] = [j for j in lst if ops[j]["dma"] or ops[j]["eng"] != o["eng"]]
                lst.append(i)
            for k in o["w"]:
                last_w[k] = i
                readers[k] = []
            dd = set()
            for j, kind in d:
                p = ops[j]
                if (not p["dma"]) and (not o["dma"]) and p["eng"] == o["eng"]:
                    if o["eng"] == "pe":
                        continue
                    if kind == "war":
                        continue
                dd.add(j)
            deps.append(dd)
        needed = set()
        for dd in deps:
            needed |= dd
        engs = self.engs
        if self.csem is None:
            self.csem = {e: es.enter_context(nc.semaphore("c_" + e)) for e in engs}
            self.ccsem = es.enter_context(nc.semaphore("c_cc"))
        csem = self.csem
        rings = self.rings
        ccount = self.ccount
        dcount = self.dcount
        prev_end = dict(c={e: ccount[e] for e in engs}, d={e: dcount[e] for e in rings}, cc=self.cccount)
        lastc = {}
        for i, o in enumerate(ops):
            if not o["dma"]:
                lastc[o["eng"]] = i
        needed |= set(lastc.values())
        sig = {}
        prewait = {}
        for i, o in enumerate(ops):
            e = o["eng"]
            if o.get("cc"):
                self.cccount += 1
                sig[i] = (self.ccsem, self.cccount, 1)
                if self.cccount > 1:
                    prewait[i] = (self.ccsem, self.cccount - 1)
            elif o["dma"]:
                if e not in rings:
                    rings[e] = [es.enter_context(nc.semaphore("r_%s%d" % (e, k)))
                                for k in range(self.RING)]
                n = dcount[e]
                dcount[e] += 1
                sem = rings[e][n % self.RING]
                sig[i] = (sem, 16 * (n // self.RING + 1), 16)
                if n >= self.RING:
                    prewait[i] = (sem, 16 * (n // self.RING))
            elif i in needed:
                ccount[e] += 1
                sig[i] = (csem[e], ccount[e], 1)
        per = {e: [] for e in engs}
        for i, o in enumerate(ops):
            per[o["eng"]].append(i)
        self.stats = dict(n_ops=len(ops), ccount=ccount, dcount=dcount)

        nstage = self.nstage
        self.nstage += 1

        def run(e, engobj):
            waited = {}
            if nstage > 0:
                for e2 in engs:
                    if prev_end["c"][e2] > 0:
                        engobj.wait_ge(csem[e2], prev_end["c"][e2])
                for e2, n in prev_end["d"].items():
                    for k in range(self.RING):
                        cnt = (n - k + self.RING - 1) // self.RING if n > k else 0
                        if cnt > 0:
                            engobj.wait_ge(rings[e2][k], 16 * cnt)
                if prev_end["cc"] > 0:
                    engobj.wait_ge(self.ccsem, prev_end["cc"])
            for i in per[e]:
                o = ops[i]
                ws = [sig[j][:2] for j in deps[i]]
                if i in prewait:
                    ws.append(prewait[i])
                mx = {}
                for sem, val in ws:
                    key = id(sem)
                    if key not in mx or mx[key][1] < val:
                        mx[key] = (sem, val)
                for key, (sem, val) in mx.items():
                    if waited.get(key, 0) >= val:
                        continue
                    waited[key] = val
                    engobj.wait_ge(sem, val)
                ins = o["fn"](engobj)
                if i in sig:
                    sem, val, inc = sig[i]
                    ins.then_inc(sem, inc)
            if e in rings:
                n = dcount[e]
                for k in range(self.RING):
                    cnt = (n - k + self.RING - 1) // self.RING if n > k else 0
                    if cnt > 0:
                        engobj.wait_ge(rings[e][k], 16 * cnt)

        with nc.Block() as block:
            @block.tensor
            def _(t):
                run("pe", t)

            @block.scalar
            def _(t):
                run("act", t)

            @block.vector
            def _(t):
                run("dve", t)

            @block.gpsimd
            def _(t):
                run("pool", t)

            @block.sync
            def _(t):
                run("sp", t)
        self.ops = []


class Ctx:
    def __init__(self, nc, es, pre=None, tag=""):
        self.nc, self.es = nc, es
        self.n = 0
        self.pre = pre
        self.tag = tag

    def sb(self, shape, dt, name=None):
        self.n += 1
        t = self.es.enter_context(self.nc.sbuf_tensor("%s%s_%d" % (self.tag, name or "t", self.n), list(shape), dt))
        esz = 4 if dt in (F32, I32) else 2
        nbytes = int(np.prod(shape[1:])) * esz
        alloc = (nbytes + 31) // 32 * 32
        if alloc % 64 != 0:
            self.n += 1
            self.es.enter_context(self.nc.sbuf_tensor("%spad_%d" % (self.tag, self.n), [128, 8], F32))
        return t

    def ps(self, shape, dt, name=None):
        self.n += 1
        return self.es.enter_context(self.nc.psum_tensor("%s%s_%d" % (self.tag, name or "p", self.n), list(shape), dt))

    def dram(self, name, shape, dt, kind):
        if self.pre is not None:
            h = self.pre[name]
            assert list(h.shape) == list(shape), (name, h.shape, shape)
            return h
        return self.nc.dram_tensor(name, list(shape), dt, kind=kind)


class Rot:
    def __init__(self, items):
        self.items = items
        self.i = 0

    def next(self):
        it = self.items[self.i % len(self.items)]
        self.i += 1
        return it


class Env:
    def __init__(self):
        self.nc = bass.Bass("TRN2", target_bir_lowering=False)
        self.es = ExitStack()
        self.P = Prog(self.nc, self.es)
        self.nstage = 0


def _begin(env, pre=None):
    if env is None:
        nc = bass.Bass("TRN2", target_bir_lowering=False)
        es = ExitStack()
        return nc, es, Ctx(nc, es), Prog(nc, es)
    env.nstage += 1
    es = ExitStack()
    return env.nc, es, Ctx(env.nc, es, pre=pre, tag="s%d_" % env.nstage), env.P


def run_spmd(nc, in_maps):
    res = run_bass_kernel_spmd(nc, in_maps, core_ids=list(range(NCORES)))
    return res.results


def ACT(P, out, in_, func, r, w, **kw):
    P.op("act", lambda e: e.activation(out=out, in_=in_, func=func, **kw), r, w)


def TS(P, eng, out, in0, s1, s2, op0, op1, r, w):
    if op1 is None:
        P.op(eng, lambda e: e.tensor_scalar(out=out, in0=in0, scalar1=s1, scalar2=None, op0=op0), r, w)
    else:
        P.op(eng, lambda e: e.tensor_scalar(out=out, in0=in0, scalar1=s1, scalar2=s2, op0=op0, op1=op1), r, w)


def TT(P, eng, out, in0, in1, op, r, w):
    P.op(eng, lambda e: e.tensor_tensor(out=out, in0=in0, in1=in1, op=op), r, w)


def STT(P, eng, out, in0, scalar, in1, op0, op1, r, w):
    P.op(eng, lambda e: e.scalar_tensor_tensor(out=out, in0=in0, scalar=scalar, in1=in1, op0=op0, op1=op1), r, w)


def CP(P, eng, out, in_, r, w):
    if eng == "act":
        P.op(eng, lambda e: e.copy(out=out, in_=in_), r, w)
    else:
        P.op(eng, lambda e: e.tensor_copy(out=out, in_=in_), r, w)


def RSUM(P, eng, out, in_, r, w, axis=None):
    ax = axis if axis is not None else AX.X
    P.op(eng, lambda e: e.reduce_sum(out=out, in_=in_, axis=ax), r, w)


def RMAX(P, eng, out, in_, r, w, axis=None):
    ax = axis if axis is not None else AX.X
    P.op(eng, lambda e: e.reduce_max(out=out, in_=in_, axis=ax), r, w)


def MM(P, out, lhsT, rhs, start, stop, r, w):
    P.op("pe", lambda e: e.matmul(out, lhsT, rhs, start=start, stop=stop), r, w)


def TR(P, out, in_, ident, r, w):
    P.op("pe", lambda e: e.transpose(out, in_, ident), r, w)


def DMA(P, eng, out, in_, r, w):
    P.dma(eng, lambda e: e.dma_start(out=out, in_=in_), r, w)


def RECIP(P, eng, out, in_, r, w):
    P.op(eng, lambda e: e.reciprocal(out=out, in_=in_), r, w)


def MEMSET(P, eng, ap, val, w):
    P.op(eng, lambda e: e.memset(ap, val), (), w)


TP = 1024
HALO = 128
NTP = TP + 2 * HALO
NPASS = TPC // TP


def build_stageA(phases=("fm", "tm", "conv", "mla"), env=None, pre=None):
    nc, es, C, P = _begin(env, pre)
    with es:
        xe = C.dram("xe", [TPC + 2 * HALO, D], F32, "ExternalInput")
        w_in = C.dram("w_in", [D, INW], F32, "ExternalInput")
        gmix = C.dram("gmix", [128, 8], F32, "ExternalInput")
        identb = C.dram("identb", [128, 128], BF16, "ExternalInput")
        convp = C.dram("convp", [128, 4, 34], F32, "ExternalInput")
        gcq = C.dram("gcq", [128, 3], F32, "ExternalInput")
        gckv = C.dram("gckv", [128, 2], F32, "ExternalInput")
        w_uq = C.dram("w_uq", [384, 768], F32, "ExternalInput")
        w_ukv = C.dram("w_ukv", [256, 1024], F32, "ExternalInput")
        gqk = C.dram("gqk", [96, 2], F32, "ExternalInput")
        ropeT = C.dram("ropeT", [96, 2, TPC], F32, "ExternalInput")
        rmat = C.dram("rmat", [96, 96], BF16, "ExternalInput")
        onesf = C.dram("onesf", [128, 128], F32, "ExternalInput")

        QT = C.dram("QT", [512, TPC], BF16, "ExternalOutput")
        KT = C.dram("KT", [512, TPC], BF16, "ExternalOutput")
        Kt = C.dram("Kt", [4, TPC, 128], BF16, "ExternalOutput")
        Vt = C.dram("Vt", [4, TPC, 128], BF16, "ExternalOutput")
        OG = C.dram("OG", [4, TPC, 128], BF16, "ExternalOutput")
        G4 = C.dram("G4", [4, TPC, 4], F32, "ExternalOutput")
        GTS = C.dram("GTS", [TPC, 3072], BF16, "ExternalOutput")
        UT = C.dram("UT", [512, TPC], BF16, "ExternalOutput")
        MQ = C.dram("MQ", [8, 96, TPC], BF16, "ExternalOutput")
        MK = C.dram("MK", [8, 96, TPC], BF16, "ExternalOutput")
        MV = C.dram("MV", [8, 128, TPC // 128, 64], BF16, "ExternalOutput")

        w_v = w_in.ap().rearrange("(c p) n -> p c n", p=128)
        xnT = C.sb([128, 8, NTP], BF16, "xnT")
        f32t = Rot([(("f32t", i), C.sb([128, 512], F32, "f32t")) for i in range(4)])
        uT = C.sb([128, 4, NTP], BF16, "uT")
        cacc = [C.sb([128, 512], F32, "cacc") for g in range(4)]
        csq = [C.sb([128, 512], F32, "csq") for g in range(4)]
        cpar = C.sb([128, 4, 34], F32, "cpar")
        rope_sb = C.sb([96, 2, 512], F32, "rope")
        cql = C.sb([128, 3, 512], F32, "cql")
        ckl = C.sb([128, 2, 512], F32, "ckl")
        latsq = C.sb([128, 3, 512], BF16, "latsq")
        cqn = C.sb([128, 3, 512], BF16, "cqn")
        ckn = C.sb([128, 2, 512], BF16, "ckn")
        krt = C.sb([128, 512], F32, "krt")
        hx = Rot([(("hx", i), C.sb([128, 512], F32, "hx")) for i in range(2)])
        hsq = Rot([(("hsq", i), C.sb([128, 512], BF16, "hsq")) for i in range(2)])
        hxg = Rot([(("hxg", i), C.sb([128, 512], BF16, "hxg")) for i in range(2)])
        wuqb = C.sb([128, 3, 768], BF16, "wuqb")
        wukvb = C.sb([128, 2, 1024], BF16, "wukvb")
        wkrp = C.sb([128, 8, 128], BF16, "wkrp")
        gqk_sb = C.sb([96, 2], F32, "gqk")
        rmat_sb = C.sb([96, 96], BF16, "rmat")
        gcq_sb = C.sb([128, 3], F32, "gcqs")
        gckv_sb = C.sb([128, 2], F32, "gckvs")
        ident = C.sb([128, 128], BF16, "ident")
        gm = C.sb([128, 8], F32, "gm")
        onesb = C.sb([128, 128], BF16, "onesb")
        ones32 = C.sb([128, 128], F32, "ones32")
        xin = Rot([(("xin", i), C.sb([128, D], F32, "xin")) for i in range(2)])
        sqj = C.sb([128, D], F32, "sqj")
        xs = Rot([(("xs", i), C.sb([128, D], BF16, "xs")) for i in range(2)])
        stat = Rot([(("stat", i), C.sb([128, 2], F32, "stat")) for i in range(2)])
        wst = Rot([(("wst", i), C.sb([128, 8, 512], F32, "wst")) for i in range(3)])
        wb = Rot([(("wb", i), C.sb([128, 8, 512], BF16, "wb")) for i in range(3)])
        ob = Rot([(("ob", i), C.sb([128, 512], BF16, "ob")) for i in range(4)])
        g4sb = C.sb([128, NTP // 128, 16], F32, "g4sb")
        pacc = Rot([(("pacc", i), C.ps([128, 512], F32, "pacc")) for i in range(5)])
        ptr = Rot([(("ptr", i), C.ps([128, 1024], BF16, "ptr")) for i in range(2)])

        DMA(P, "sp", cpar[:], convp[:, :, :], [], ["cpar"])
        DMA(P, "sp", gqk_sb[:], gqk[:, :], [], ["gqk"])
        DMA(P, "sp", rmat_sb[:], rmat[:, :], [], ["rmat"])
        DMA(P, "sp", gcq_sb[:], gcq[:, :], [], ["gcqs"])
        DMA(P, "sp", gckv_sb[:], gckv[:, :], [], ["gckvs"])
        DMA(P, "sp", gm[:], gmix[:, :], [], ["gm"])
        if "mla" in phases:
            sk0, st0 = wst.items[0]
            st0f = st0[:].rearrange("p a b -> p (a b)")
            DMA(P, "sp", st0f[:, 0:2304].rearrange("p (a b) -> p a b", a=3),
                w_uq.ap().rearrange("(c p) n -> p c n", p=128), [], [sk0])
            for j in range(3):
                TS(P, "dve", wuqb[:, j, :], st0f[:, j * 768:(j + 1) * 768],
                   gcq_sb[:, j:j + 1], None, ALU.mult, None, [sk0, "gcqs"], ["wuqb"])
            sk1, st1 = wst.items[1]
            st1f = st1[:].rearrange("p a b -> p (a b)")
            DMA(P, "sp", st1f[:, 0:2048].rearrange("p (a b) -> p a b", a=2),
                w_ukv.ap().rearrange("(c p) n -> p c n", p=128), [], [sk1])
            for j in range(2):
                src = st1f[:, j * 1024:(j + 1) * 1024].rearrange("p (h x) -> p h x", h=8)
                TS(P, "dve", wukvb[:, j, 0:512].rearrange("p (h x) -> p h x", h=8), src[:, :, 0:64],
                   gckv_sb[:, j:j + 1], None, ALU.mult, None, [sk1, "gckvs"], ["wukvb"])
                TS(P, "dve", wukvb[:, j, 512:1024].rearrange("p (h x) -> p h x", h=8), src[:, :, 64:128],
                   gckv_sb[:, j:j + 1], None, ALU.mult, None, [sk1, "gckvs"], ["wukvb"])
            MEMSET(P, "pool", wkrp[:], 0.0, ["wkrp"])
            sk2_, st2_ = wst.items[0]
            DMA(P, "sp", st2_[:, :, 0:32], w_v[:, :, O_CKR:O_CKR + 32], [], [sk2_])
            for k in range(8):
                TS(P, "dve", wkrp[:, k, 64:96], st2_[:, k, 0:32], gm[:, k:k + 1], None, ALU.mult, None,
                   [sk2_, "gm"], ["wkrp"])
            MEMSET(P, "pool", krt[:], 0.0, ["krt"])
        DMA(P, "sp", ident[:], identb[:, :], [], ["ident"])
        DMA(P, "sp", gm[:], gmix[:, :], [], ["gm"])
        DMA(P, "sp", ones32[:], onesf[:, :], [], ["ones32"])
        CP(P, "dve", onesb[:], ones32[:], ["ones32"], ["onesb"])

        evac_i = [0]

        def load_w_impl(c0, ncols):
            sk, st = wst.next()
            bk, bt = wb.next()
            DMA(P, "sp", st[:, :, 0:ncols], w_v[:, :, c0:c0 + ncols], [], [sk])
            for k in range(8):
                ACT(P, bt[:, k, 0:ncols], st[:, k, 0:ncols], AF.Copy, [sk, "gm"], [(bk, k)], scale=gm[:, k:k + 1])
            return [(bk, k) for k in range(8)], bt

        wlist = []
        for _ps in range(NPASS):
            if "fm" in phases:
                wlist += [(O_AQ, 512), (O_AK, 512)]
            if "tm" in phases:
                wlist += [(O_AK, 512), (O_AV, 512), (O_AO, 512)] + [(O_GTS + j * 512, 512) for j in range(6)] + [(O_AG, 16)]
            if "conv" in phases:
                wlist += [(O_GLU, 512), (O_GLU + 512, 512)]
            if "mla" in phases:
                wlist += [(O_CQ, 384), (O_CKV, 288)]
        issued = []

        def nextw(c0, ncols):
            if not issued:
                issued.append((wlist[0], load_w_impl(*wlist.pop(0))))
            req, cur = issued.pop(0)
            assert req == (c0, ncols), (req, c0, ncols)
            if wlist:
                issued.append((wlist[0], load_w_impl(*wlist.pop(0))))
            return cur

        for ps_i in range(NPASS):
            t0 = ps_i * TP
            for t in range(NTP // 128):
                xk, xt = xin.next()
                sk2, stt = stat.next()
                xsk, xst = xs.next()
                pk, pt = ptr.next()
                r0 = t0 + t * 128
                DMA(P, "sp", xt[:], xe[r0:r0 + 128, :], [], [xk])
                ACT(P, sqj[:], xt[:], AF.Square, [xk], ["sqj"])
                RSUM(P, "dve", stt[:, 0:1], sqj[:], ["sqj"], [sk2])
                ACT(P, stt[:, 1:2], stt[:, 0:1], AF.Sqrt, [sk2], [sk2], scale=1.0 / D, bias=EPS)
                RECIP(P, "dve", stt[:, 1:2], stt[:, 1:2], [sk2], [sk2])
                ACT(P, xst[:], xt[:], AF.Copy, [xk, sk2], [xsk], scale=stt[:, 1:2])
                for k in range(8):
                    TR(P, pt[:, k * 128:(k + 1) * 128], xst[:, k * 128:(k + 1) * 128], ident[:],
                       [xsk, "ident"], [pk])
                CP(P, ("dve", "pool")[0], xnT[:, :, t * 128:(t + 1) * 128],
                   pt[:].rearrange("p (k t) -> p k t", k=8), [pk], [("xnT", t)])

            def xk_keys(tok0, ntok):
                return [("xnT", t) for t in range(tok0 // 128, (tok0 + ntok - 1) // 128 + 1)]

            if "fm" in phases:
                for (c0, dst) in ((O_AQ, QT), (O_AK, KT)):
                    wk, wt = nextw(c0, 512)
                    for cb in range(4):
                        for tb in range(TP // 512):
                            tk0 = HALO + tb * 512
                            ak, at = pacc.next()
                            for k in range(8):
                                MM(P, at[:, :], wt[:, k, cb * 128:(cb + 1) * 128], xnT[:, k, tk0:tk0 + 512],
                                   k == 0, k == 7, [wk[k]] + xk_keys(tk0, 512), [ak])
                            okk, ot = ob.next()
                            evac_i[0] += 1
                            CP(P, ("act", "dve")[evac_i[0] % 2], ot[:, :], at[:, :], [ak], [okk])
                            DMA(P, "sp", dst[cb * 128:(cb + 1) * 128, t0 + tb * 512:t0 + (tb + 1) * 512], ot[:, :],
                                [okk], [])

            if "tm" in phases:
                blocks = [(O_AK, Kt, 0, "copy"), (O_AV, Vt, 0, "copy"), (O_AO, OG, 0, "sig")]
                for j in range(6):
                    blocks.append((O_GTS + j * 512, GTS, j * 512, "sig"))
                for (c0, dst, dc0, mode) in blocks:
                    wk, wt = nextw(c0, 512)
                    for t in range(TP // 128):
                        tk0 = HALO + t * 128
                        ak, at = pacc.next()
                        for k in range(8):
                            MM(P, at[:, :], xnT[:, k, tk0:tk0 + 128], wt[:, k, :], k == 0, k == 7,
                               [wk[k]] + xk_keys(tk0, 128), [ak])
                        okk, ot = ob.next()
                        if mode == "sig":
                            ACT(P, ot[:, :], at[:, :], AF.Sigmoid, [ak], [okk])
                        else:
                            evac_i[0] += 1
                            CP(P, ("act", "dve")[evac_i[0] % 2], ot[:, :], at[:, :], [ak], [okk])
                        if dst is GTS:
                            DMA(P, "sp", dst[t0 + t * 128:t0 + (t + 1) * 128, dc0:dc0 + 512], ot[:, :], [okk], [])
                        else:
                            DMA(P, "sp", dst[:, t0 + t * 128:t0 + (t + 1) * 128, :].rearrange("h t d -> t h d"),
                                ot[:, :].rearrange("p (h d) -> p h d", h=4), [okk], [])
                wk, wt = nextw(O_AG, 16)
                for t in range(TP // 128):
                    tk0 = HALO + t * 128
                    ak, at = pacc.next()
                    for k in range(8):
                        MM(P, at[:, 0:16], xnT[:, k, tk0:tk0 + 128], wt[:, k, 0:16], k == 0, k == 7,
                           [wk[k]] + xk_keys(tk0, 128), [ak])
                    CP(P, "dve", g4sb[:, t, :].rearrange("p (h g) -> p h g", h=4), at[:, 0:16].rearrange("p (g h) -> p h g", h=4),
                       [ak], [("g4", t)])
                for h in range(4):
                    DMA(P, "sp", G4[h, t0:t0 + TP, :].rearrange("(t p) g -> p t g", p=128), g4sb[:, 0:TP // 128, h * 4:(h + 1) * 4],
                        [("g4", t) for t in range(TP // 128)], [])

            if "conv" in phases:
                wak, wat = nextw(O_GLU, 512)
                wgk, wgt = nextw(O_GLU + 512, 512)
                blks = [(b0, min(512, NTP - b0)) for b0 in range(0, NTP, 512)]
                for g in range(4):
                    for (b0, bn) in blks:
                        ak, at = pacc.next()
                        gk, gt = pacc.next()
                        for k in range(8):
                            MM(P, at[:, 0:bn], wat[:, k, g * 128:(g + 1) * 128], xnT[:, k, b0:b0 + bn],
                               k == 0, k == 7, [wak[k]] + xk_keys(b0, bn), [ak])
                        for k in range(8):
                            MM(P, gt[:, 0:bn], wgt[:, k, g * 128:(g + 1) * 128], xnT[:, k, b0:b0 + bn],
                               k == 0, k == 7, [wgk[k]] + xk_keys(b0, bn), [gk])
                        fk, ft = f32t.next()
                        ACT(P, ft[:, 0:bn], gt[:, 0:bn], AF.Sigmoid, [gk], [fk])
                        TT(P, "dve", uT[:, g, b0:b0 + bn], at[:, 0:bn], ft[:, 0:bn], ALU.mult, [ak, fk],
                           [("uT", g, b0 // 512)])
                for tb in range(TP // 512):
                    c0 = HALO + tb * 512 - 15
                    ukeys = lambda g: [("uT", g, j) for j in range(c0 // 512, (c0 + 542 - 1) // 512 + 1)]
                    for g in range(4):
                        eng = "dve"
                        ck = ("cacc", g)
                        ca = cacc[g]
                        TS(P, eng, ca[:, :], uT[:, g, c0:c0 + 512], cpar[:, g, 0:1], cpar[:, g, 31:32],
                           ALU.mult, ALU.add, ukeys(g) + ["cpar"], [ck])
                        for k in range(1, 31):
                            STT(P, eng, ca[:, :], uT[:, g, c0 + k:c0 + k + 512], cpar[:, g, k:k + 1], ca[:, :],
                                ALU.mult, ALU.add, ukeys(g) + ["cpar", ck], [ck])
                    mk, mt = pacc.next()
                    for g in range(4):
                        MM(P, mt[:, :], ones32[:, :], cacc[g][:, :], g == 0, g == 3, ["ones32", ("cacc", g)], [mk])
                    for g in range(4):
                        STT(P, "dve", cacc[g][:, :], mt[:, :], -1.0 / 512, cacc[g][:, :], ALU.mult, ALU.add,
                            [mk, ("cacc", g)], [("cacc", g)])
                        ACT(P, csq[g][:, :], cacc[g][:, :], AF.Square, [("cacc", g)], [("csq", g)])
                    vk, vt = pacc.next()
                    for g in range(4):
                        MM(P, vt[:, :], ones32[:, :], csq[g][:, :], g == 0, g == 3, ["ones32", ("csq", g)], [vk])
                    fk, ft = f32t.next()
                    ACT(P, ft[:, :], vt[:, :], AF.Sqrt, [vk], [fk], scale=1.0 / 512, bias=EPS)
                    RECIP(P, "dve", ft[:, :], ft[:, :], [fk], [fk])
                    for g in range(4):
                        TT(P, "dve", csq[g][:, :], cacc[g][:, :], ft[:, :], ALU.mult, [("cacc", g), fk], [("csq", g)])
                        TS(P, "pool", csq[g][:, :], csq[g][:, :], cpar[:, g, 32:33], cpar[:, g, 33:34],
                           ALU.mult, ALU.add, [("csq", g), "cpar"], [("csq", g)])
                        okk, ot = ob.next()
                        ACT(P, ot[:, :], csq[g][:, :], AF.Silu, [("csq", g)], [okk])
                        DMA(P, "sp", UT[g * 128:(g + 1) * 128, t0 + tb * 512:t0 + (tb + 1) * 512], ot[:, :], [okk], [])

            if "mla" in phases:
                wqk, wqt = nextw(O_CQ, 384)
                wkk, wkt = nextw(O_CKV, 288)
                for tb in range(TP // 512):
                    tk0 = HALO + tb * 512
                    g0 = t0 + tb * 512
                    DMA(P, "sp", rope_sb[:, :, :], ropeT[:, :, g0:g0 + 512], [], ["rope"])
                    for (wk_, wt_, nblk, lat, latn, lkey, dim) in ((wqk, wqt, 3, cql, cqn, "cq", 384.0),
                                                                 (wkk, wkt, 2, ckl, ckn, "ckv", 256.0)):
                        for j in range(nblk):
                            ak, at = pacc.next()
                            for k in range(8):
                                MM(P, at[:, :], wt_[:, k, j * 128:(j + 1) * 128], xnT[:, k, tk0:tk0 + 512],
                                   k == 0, k == 7, [wk_[k]] + xk_keys(tk0, 512), [ak])
                            CP(P, "dve", lat[:, j, :], at[:, :], [ak], [(lkey, j)])
                            ACT(P, latsq[:, j, :], lat[:, j, :], AF.Square, [(lkey, j)], [(lkey + "sq", j)])
                        sk_, st_ = pacc.next()
                        for j in range(nblk):
                            MM(P, st_[:, :], onesb[:, :], latsq[:, j, :], j == 0, j == nblk - 1,
                               ["onesb", (lkey + "sq", j)], [sk_])
                        fk, ft = f32t.next()
                        ACT(P, ft[:, :], st_[:, :], AF.Sqrt, [sk_], [fk], scale=1.0 / dim, bias=EPS)
                        RECIP(P, "dve", ft[:, :], ft[:, :], [fk], [fk])
                        for j in range(nblk):
                            if "dbg5" in phases:
                                TT(P, "dve", lat[:, j, :], lat[:, j, :], ft[:, :], ALU.mult,
                                   [(lkey, j), fk], [(lkey, j)])
                                CP(P, "act", latn[:, j, :], lat[:, j, :], [(lkey, j)], [(lkey + "n", j)])
                            else:
                                TT(P, "dve", latn[:, j, :], lat[:, j, :], ft[:, :], ALU.mult,
                                   [(lkey, j), fk], [(lkey + "n", j)])
                    if "nokr" not in phases:
                        ak, at = pacc.next()
                        for k in range(8):
                            MM(P, at[:, :], wkrp[:, k, :], xnT[:, k, tk0:tk0 + 512], k == 0, k == 7,
                               ["wkrp"] + xk_keys(tk0, 512), [ak])
                        CP(P, "act", krt[0:96, :], at[0:96, :], [ak], ["krt"])
                    cqn_keys = [("cqn", j) for j in range(3)]
                    ckn_keys = [("ckvn", j) for j in range(2)]
                    for h in (range(8) if "nomlah" not in phases else []):
                        for which in ("q", "k"):
                            ak, at = pacc.next()
                            xk_, xt_ = hx.next()
                            if which == "q":
                                for j in range(3):
                                    MM(P, at[0:96, :], wuqb[:, j, h * 96:(h + 1) * 96], cqn[:, j, :], j == 0, j == 2,
                                       ["wuqb"] + cqn_keys, [ak])
                                CP(P, "act", xt_[0:96, :], at[0:96, :], [ak], [xk_])
                            else:
                                for j in range(2):
                                    MM(P, at[0:64, :], wukvb[:, j, h * 64:(h + 1) * 64], ckn[:, j, :], j == 0, j == 1,
                                       ["wukvb"] + ckn_keys, [ak])
                                CP(P, "act", xt_[0:64, :], at[0:64, :], [ak], [xk_])
                                CP(P, "pool", xt_[64:96, :], krt[64:96, :], ["krt"], [xk_])
                            sqk, sqt = hsq.next()
                            ACT(P, sqt[0:96, :], xt_[0:96, :], AF.Square, [xk_], [sqk])
                            sk_, st_ = pacc.next()
                            MM(P, st_[0:96, :], onesb[0:96, 0:96], sqt[0:96, :], True, True, ["onesb", sqk], [sk_])
                            fk, ft = f32t.next()
                            ACT(P, ft[0:96, :], st_[0:96, :], AF.Sqrt, [sk_], [fk], scale=1.0 / 96, bias=EPS)
                            RECIP(P, "dve", ft[0:96, :], ft[0:96, :], [fk], [fk])
                            gcol = 0 if which == "q" else 1
                            xgk, xgt = hxg.next()
                            TS(P, "dve", xgt[0:96, :], xt_[0:96, :], gqk_sb[0:96, gcol:gcol + 1], None, ALU.mult, None,
                               [xk_, "gqk"], [xgk])
                            rk, rt = pacc.next()
                            MM(P, rt[0:96, :], rmat_sb[0:96, 0:96], xgt[0:96, :], True, True, ["rmat", xgk], [rk])
                            t1k, t1 = f32t.next()
                            TT(P, "pool", t1[0:96, :], xgt[0:96, :], rope_sb[:, 0, :], ALU.mult, [xgk, "rope"], [t1k])
                            t2k, t2 = f32t.next()
                            TT(P, "dve", t2[0:96, :], rt[0:96, :], rope_sb[:, 1, :], ALU.mult, [rk, "rope"], [t2k])
                            TT(P, "pool", t1[0:96, :], t1[0:96, :], t2[0:96, :], ALU.add, [t1k, t2k], [t1k])
                            okk, ot = ob.next()
                            TT(P, "dve", ot[0:96, :], t1[0:96, :], ft[0:96, :], ALU.mult, [t1k, fk], [okk])
                            dst = MQ if which == "q" else MK
                            DMA(P, "sp", dst[h, :, g0:g0 + 512], ot[0:96, :], [okk], [])
                    if "dupgrp" in phases:
                        for rep in range(2):
                            ak, at = pacc.next()
                            for k in range(8):
                                MM(P, at[:, :], wkt[:, k, 0:128], xnT[:, k, tk0:tk0 + 512],
                                   k == 0, k == 7, [wkk[k]] + xk_keys(tk0, 512), [ak])
                    for t in (range(4 if "v_one" not in phases else 1) if "nomlav" not in phases else []):
                        ak, at = pacc.next()
                        for j in range(2):
                            MM(P, at[:, :], (xnT[:, j, tk0 + t * 128:tk0 + (t + 1) * 128] if "dbg1" in phases else ckn[:, j, t * 128:(t + 1) * 128]),
                               (wkt[:, j, :] if "dbg2" in phases else (wukvb[:, j, 0:512] if "dbg4" in phases else wukvb[:, j, 512:1024])), j == 0, j == 1,
                               ["wukvb"] + ckn_keys, [ak])
                        okk, ot = ob.next()
                        if "v_noevac" in phases:
                            continue
                        CP(P, "dve", ot[:, :], at[:, :], [ak], [okk])
                        if "v_nodma" in phases:
                            continue
                        DMA(P, "sp", MV[:, :, (g0 + t * 128) // 128, :].rearrange("h p d -> p h d"),
                            ot[:, :].rearrange("p (h d) -> p h d", h=8), [okk], [])

        P.emit()
        stats = P.stats
    return nc, stats


def _gain_cols(g, nch):
    return np.ascontiguousarray(g.reshape(nch, 128).T)


def rope_tables():
    pos = np.arange(S, dtype=np.float32)
    inv = (10000.0 ** (-np.arange(0, 32, 2, dtype=np.float32) / np.float32(32))).astype(np.float32)
    ang = (pos[:, None] * inv[None, :]).astype(np.float32)
    c = np.cos(ang.astype(np.float64)).astype(np.float32)
    s = np.sin(ang.astype(np.float64)).astype(np.float32)
    CT = np.ones((96, S), np.float32)
    ST = np.zeros((96, S), np.float32)
    CT[64:80] = c.T
    CT[80:96] = c.T
    ST[64:80] = s.T
    ST[80:96] = s.T
    return CT, ST


def consts():
    R = np.zeros((96, 96), np.float32)
    for j in range(16):
        R[80 + j, 64 + j] = -1.0
        R[64 + j, 80 + j] = 1.0
    return dict(identb=np.eye(128, dtype=np.float32).astype(NPBF), rmat=R.astype(NPBF),
                onesf=np.ones((128, 128), np.float32))


def stageA_inmaps(x, prm, l):
    CT, ST = rope_tables()
    cst = consts()
    convp = np.zeros((128, 4, 34), np.float32)
    cw = prm["conv_w"][l]
    convp[:, :, 0:31] = cw.T.reshape(4, 128, 31).transpose(1, 0, 2)
    convp[:, :, 31] = prm["conv_b"][l].reshape(4, 128).T
    convp[:, :, 32] = prm["conv_ln_g"][l].reshape(4, 128).T
    convp[:, :, 33] = prm["conv_ln_b"][l].reshape(4, 128).T
    maps = []
    for c in range(NCORES):
        b, q = c // 4, c % 4
        s0 = q * TPC
        xe = np.zeros((TPC + 2 * HALO, D), np.float32)
        lo, hi = max(0, s0 - HALO), min(S, s0 + TPC + HALO)
        xe[lo - (s0 - HALO):hi - (s0 - HALO)] = x[b, lo:hi]
        rope = np.stack([CT[:, s0:s0 + TPC], ST[:, s0:s0 + TPC]], axis=1)
        maps.append(dict(
            xe=xe, w_in=prm["w_in"][l], gmix=_gain_cols(prm["mix_norm_g"][l], 8),
            identb=cst["identb"], convp=convp, gcq=_gain_cols(prm["cq_norm_g"][l], 3),
            gckv=_gain_cols(prm["ckv_norm_g"][l], 2), w_uq=prm["w_uq"][l], w_ukv=prm["w_ukv"][l],
            gqk=np.ascontiguousarray(np.stack([prm["q_norm_g"][l], prm["k_norm_g"][l]], axis=1)),
            ropeT=np.ascontiguousarray(rope), rmat=cst["rmat"], onesf=cst["onesf"]))
    return maps


def build_mlstm(nch=S // 128, env=None, pre=None):
    nc, es, C, P = _begin(env, pre)
    ns = nch * 128
    lnscale = math.log(128.0 ** -0.5)
    with es:
        qTd = C.dram("qT", [128, ns], BF16, "ExternalInput")
        ktd = C.dram("kt", [ns, 128], BF16, "ExternalInput")
        vtd = C.dram("vt", [ns, 128], BF16, "ExternalInput")
        g4d = C.dram("g4", [ns, 4], F32, "ExternalInput")
        bifd = C.dram("bif", [128, 4], F32, "ExternalInput")
        ogd = C.dram("og", [ns, 128], BF16, "ExternalInput")
        gAd = C.dram("gA", [128, 128], F32, "ExternalInput")
        cmat = C.dram("cmat", [128, 6, 128], F32, "ExternalInput")
        identbd = C.dram("identb", [128, 128], BF16, "ExternalInput")
        HT = C.dram("HT", [128, ns], BF16, "ExternalOutput")

        qT = C.sb([128, ns], BF16, "qT")
        kt = C.sb([128, nch, 128], BF16, "kt")
        vt = C.sb([128, nch, 129], BF16, "vt")
        hacc = C.sb([128, nch, 128], F32, "hacc")
        g4 = C.sb([128, nch, 4], F32, "g4")
        bif = C.sb([128, 4], F32, "bif")
        nbif = C.sb([128, 4], F32, "nbif")
        gA = C.sb([128, 128], F32, "gA")
        cm = C.sb([128, 6, 128], F32, "cm")
        identb = C.sb([128, 128], BF16, "identb")
        gt = {n: C.sb([128, nch], F32, n) for n in ("lf", "ib", "bcum", "gtot", "biasS", "wint", "wk", "dec", "tmp")}
        Cf = C.sb([128, 129], F32, "Cf")
        Cb = C.sb([128, 129], BF16, "Cb")
        LF = Rot([(("LF", i), C.sb([128, 128], F32, "LF")) for i in range(2)])
        Dm = Rot([(("Dm", i), C.sb([128, 128], F32, "Dm")) for i in range(2)])
        kTc = Rot([(("kTc", i), C.sb([128, 128], BF16, "kTc")) for i in range(2)])
        SD = Rot([(("SD", i), C.sb([128, 128], BF16, "SD")) for i in range(2)])
        isb = Rot([(("isb", i), C.sb([128, 129], F32, "isb")) for i in range(2)])
        num = Rot([(("num", i), C.sb([128, 129], F32, "num")) for i in range(2)])
        dn = Rot([(("dn", i), C.sb([128, 2], F32, "dn")) for i in range(2)])
        Vw = Rot([(("Vw", i), C.sb([128, 129], BF16, "Vw")) for i in range(2)])
        ogt = Rot([(("ogt", i), C.sb([128, 128], BF16, "ogt")) for i in range(2)])
        hn = Rot([(("hn", i), C.sb([128, 128], F32, "hn")) for i in range(2)])
        hb = Rot([(("hb", i), C.sb([128, 128], BF16, "hb")) for i in range(2)])
        hT = Rot([(("hT", i), C.sb([128, 128], BF16, "hT")) for i in range(2)])
        sq = C.sb([128, 128], F32, "sq")
        pA = Rot([(("pA", i), C.ps([128, 512], F32, "pA")) for i in range(6)])
        pB = Rot([(("pB", i), C.ps([128, 1024], BF16, "pB")) for i in range(2)])

        DMA(P, "sp", qT[:, :], qTd[:, :], [], ["qT"])
        DMA(P, "sp", kt[:, :, :], ktd.ap().rearrange("(c p) d -> p c d", p=128), [], ["kt"])
        DMA(P, "sp", vt[:, :, 0:128], vtd.ap().rearrange("(c p) d -> p c d", p=128), [], ["vt"])
        MEMSET(P, "pool", vt[:, :, 128:129], 1.0, ["vt1"])
        DMA(P, "sp", g4[:, :, :], g4d.ap().rearrange("(c p) g -> p c g", p=128), [], ["g4"])
        DMA(P, "sp", bif[:, :], bifd[:, :], [], ["bif"])
        DMA(P, "sp", gA[:, :], gAd[:, :], [], ["gA"])
        DMA(P, "sp", cm[:, :, :], cmat[:, :, :], [], ["cm"])
        DMA(P, "sp", identb[:, :], identbd[:, :], [], ["identb"])
        TS(P, "dve", nbif[:, :], bif[:, :], -1.0, None, ALU.mult, None, ["bif"], ["nbif"])
        ident32 = cm[:, 4, :]
        ones32 = cm[:, 5, :]
        for d in range(2):
            Ud = cm[:, d, :]
            NEGd = cm[:, 2 + d, :]
            ic, fc = 2 * d, 2 * d + 1
            ACT(P, gt["tmp"][:, :], g4[:, :, fc], AF.Exp, ["g4", "nbif"], ["tmp"], scale=-1.0, bias=nbif[:, fc:fc + 1])
            ACT(P, gt["tmp"][:, :], gt["tmp"][:, :], AF.Ln, ["tmp"], ["tmp"], bias=1.0)
            TS(P, "dve", gt["lf"][:, :], gt["tmp"][:, :], -1.0, None, ALU.mult, None, ["tmp"], ["lf"])
            TS(P, "dve", gt["ib"][:, :], g4[:, :, ic], bif[:, ic:ic + 1], lnscale, ALU.add, ALU.add, ["g4", "bif"], ["ib"])
            bk, bp = pA.next()
            MM(P, bp[:, 0:nch], Ud, gt["lf"][:, :], True, True, ["cm", "lf"], [bk])
            CP(P, "dve", gt["bcum"][:, :], bp[:, 0:nch], [bk], ["bcum"])
            gk, gp = pA.next()
            MM(P, gp[:, 0:nch], ones32, gt["lf"][:, :], True, True, ["cm", "lf"], [gk])
            CP(P, "dve", gt["gtot"][:, :], gp[:, 0:nch], [gk], ["gtot"])
            TT(P, "dve", gt["biasS"][:, :], gt["ib"][:, :], gt["bcum"][:, :], ALU.subtract, ["ib", "bcum"], ["biasS"])
            ACT(P, gt["wint"][:, :], gt["bcum"][:, :], AF.Exp, ["bcum"], ["wint"])
            TT(P, "dve", gt["tmp"][:, :], gt["biasS"][:, :], gt["gtot"][:, :], ALU.add, ["biasS", "gtot"], ["tmp"])
            ACT(P, gt["wk"][:, :], gt["tmp"][:, :], AF.Exp, ["tmp"], ["wk"])
            ACT(P, gt["dec"][:, :], gt["gtot"][:, :], AF.Exp, ["gtot"], ["dec"])
            MEMSET(P, "dve", Cf[:, :], 0.0, ["Cf"])
            MEMSET(P, "pool", Cb[:, :], 0.0, ["Cb"])
            order = range(nch) if d == 0 else range(nch - 1, -1, -1)
            for c in order:
                lk, lt = LF.next()
                ACT(P, lt[:, :], ones32, AF.Copy, ["cm", "lf"], [lk], scale=gt["lf"][:, c:c + 1])
                dk, dp = pA.next()
                MM(P, dp[:, 0:128], lt[:, :], Ud, True, False, [lk, "cm"], [dk])
                MM(P, dp[:, 0:128], ident32, NEGd, False, True, ["cm"], [dk])
                mk_, mt_ = Dm.next()
                ACT(P, mt_[:, :], dp[:, 0:128], AF.Exp, [dk, "biasS"], [mk_], bias=gt["biasS"][:, c:c + 1])
                tk, tp = pB.next()
                TR(P, tp[:, 0:128], kt[:, c, :], identb[:, :], ["kt", "identb"], [tk])
                kck, kct = kTc.next()
                CP(P, "dve", kct[:, :], tp[:, 0:128], [tk], [kck])
                sk, sp_ = pA.next()
                MM(P, sp_[:, 0:128], kct[:, :], qT[:, c * 128:(c + 1) * 128], True, True, [kck, "qT"], [sk])
                sdk, sdt = SD.next()
                TT(P, "dve", sdt[:, :], sp_[:, 0:128], mt_[:, :], ALU.mult, [sk, mk_], [sdk])
                nk_, np_ = pA.next()
                MM(P, np_[:, 0:129], sdt[:, :], vt[:, c, :], True, True, [sdk, "vt", "vt1"], [nk_])
                ik, ip = pA.next()
                MM(P, ip[:, 0:129], qT[:, c * 128:(c + 1) * 128], Cb[:, :], True, True, ["qT", "Cb"], [ik])
                isk, ist = isb.next()
                ACT(P, ist[:, :], ip[:, 0:129], AF.Copy, [ik, "wint"], [isk], scale=gt["wint"][:, c:c + 1])
                nmk, nmt = num.next()
                TT(P, "dve", nmt[:, :], np_[:, 0:129], ist[:, :], ALU.add, [nk_, isk], [nmk])
                dnk, dnt = dn.next()
                ACT(P, dnt[:, 0:1], nmt[:, 128:129], AF.Abs, [nmk], [dnk])
                TS(P, "dve", dnt[:, 0:1], dnt[:, 0:1], 1.0, None, ALU.max, None, [dnk], [dnk])
                RECIP(P, "dve", dnt[:, 1:2], dnt[:, 0:1], [dnk], [dnk])
                if d == 0:
                    TS(P, "dve", hacc[:, c, :], nmt[:, 0:128], dnt[:, 1:2], None, ALU.mult, None, [nmk, dnk], [("hacc", c)])
                else:
                    STT(P, "dve", hacc[:, c, :], nmt[:, 0:128], dnt[:, 1:2], hacc[:, c, :], ALU.mult, ALU.add,
                        [nmk, dnk, ("hacc", c)], [("hacc", c)])
                vwk, vwt = Vw.next()
                TS(P, "pool", vwt[:, :], vt[:, c, :], gt["wk"][:, c:c + 1], None, ALU.mult, None, ["vt", "vt1", "wk"], [vwk])
                ck, cp_ = pA.next()
                MM(P, cp_[:, 0:129], kt[:, c, :], vwt[:, :], True, True, ["kt", vwk], [ck])
                STT(P, "dve", Cf[:, :], Cf[:, :], gt["dec"][:, c:c + 1], cp_[:, 0:129], ALU.mult, ALU.add,
                    ["Cf", "dec", ck], ["Cf"])
                CP(P, "act", Cb[:, :], Cf[:, :], ["Cf"], ["Cb"])
        for c in range(nch):
            ogk, ogt_ = ogt.next()
            DMA(P, "sp", ogt_[:, :], ogd[c * 128:(c + 1) * 128, :], [], [ogk])
            dnk, dnt = dn.next()
            ACT(P, sq[:, :], hacc[:, c, :], AF.Square, [("hacc", c)], ["sq"])
            RSUM(P, "dve", dnt[:, 0:1], sq[:, :], ["sq"], [dnk])
            ACT(P, dnt[:, 1:2], dnt[:, 0:1], AF.Sqrt, [dnk], [dnk], scale=1.0 / 128, bias=EPS)
            RECIP(P, "dve", dnt[:, 1:2], dnt[:, 1:2], [dnk], [dnk])
            hk, ht = hn.next()
            STT(P, "dve", ht[:, :], hacc[:, c, :], dnt[:, 1:2], gA[:, :], ALU.mult, ALU.mult, [("hacc", c), dnk, "gA"], [hk])
            hbk, hbt = hb.next()
            TT(P, "pool", hbt[:, :], ht[:, :], ogt_[:, :], ALU.mult, [hk, ogk], [hbk])
            tk, tp = pB.next()
            TR(P, tp[:, 0:128], hbt[:, :], identb[:, :], [hbk, "identb"], [tk])
            htk, htt = hT.next()
            CP(P, "act", htt[:, :], tp[:, 0:128], [tk], [htk])
            DMA(P, "sp", HT[:, c * 128:(c + 1) * 128], htt[:, :], [htk], [])
        P.emit()
        stats = P.stats
    return nc, stats


def mlstm_consts():
    s_ = np.arange(128)[:, None]
    t_ = np.arange(128)[None, :]
    U = (s_ <= t_).astype(np.float32)
    cm = np.zeros((128, 6, 128), np.float32)
    cm[:, 0] = U
    cm[:, 1] = U.T
    cm[:, 2] = np.where(s_ <= t_, 0.0, -30000.0)
    cm[:, 3] = np.where(s_ >= t_, 0.0, -30000.0)
    cm[:, 4] = np.eye(128)
    cm[:, 5] = 1.0
    return dict(cmat=cm, identb=np.eye(128, dtype=np.float32).astype(NPBF))


def build_merge(ntile=TPC // 128, env=None, pre=None):
    nc, es, C, P = _begin(env, pre)
    nt = ntile * 128
    with es:
        xd = C.dram("x", [nt, D], F32, "ExternalInput")
        srcs = [C.dram(n, [512, nt], BF16, "ExternalInput") for n in ("HT", "UT", "OT")]
        gtsd = C.dram("GTS", [nt, 3072], BF16, "ExternalInput")
        wds = [C.dram(n, [512, D], F32, "ExternalInput") for n in ("w_a", "w_b", "w_c")]
        wod = C.dram("w_o", [D, D], F32, "ExternalInput")
        gfd = C.dram("gffn", [128, 8], F32, "ExternalInput")
        wrd = C.dram("w_r", [D, 16], F32, "ExternalInput")
        identbd = C.dram("identb", [128, 128], BF16, "ExternalInput")
        ident32d = C.dram("ident32", [128, 128], F32, "ExternalInput")
        x1d = C.dram("x1", [nt, D], F32, "ExternalOutput")
        xn2d = C.dram("xn2T", [D, nt], BF16, "ExternalOutput")
        affd = C.dram("aff", [nt, 16], F32, "ExternalOutput")
        affTd = C.dram("affT", [16, nt], F32, "ExternalOutput")
        affTs = C.sb([16, nt], F32, "affTs")

        wbr = [C.sb([128, 4, D], BF16, "wbr") for _ in range(3)]
        wo = C.sb([128, 8, D], BF16, "wo")
        wst = Rot([(("wst", i), C.sb([128, 4, D], F32, "wst")) for i in range(2)])
        wr = C.sb([128, 8, 16], F32, "wr")
        gf = C.sb([128, 8], F32, "gf")
        gfull = C.sb([128, 8, 128], F32, "gfull")
        identb = C.sb([128, 128], BF16, "identb")
        ident32 = C.sb([128, 128], F32, "ident32")
        srct = [Rot([((("src", b), i), C.sb([128, 4, 128], BF16, "src")) for i in range(2)]) for b in range(3)]
        gts = Rot([(("gts", i), C.sb([128, 3072], BF16, "gts")) for i in range(2)])
        xt = Rot([(("xt", i), C.sb([128, D], F32, "xt")) for i in range(2)])
        mg = C.sb([128, D], F32, "mg")
        tmpm = Rot([(("tmpm", i), C.sb([128, 512], F32, "tmpm")) for i in range(2)])
        mgb = C.sb([128, D], BF16, "mgb")
        mT = C.sb([128, 8, 128], BF16, "mT")
        x1 = Rot([(("x1", i), C.sb([128, D], F32, "x1")) for i in range(2)])
        sqj = C.sb([128, D], F32, "sqj")
        xs = C.sb([128, D], F32, "xs")
        st = Rot([(("st", i), C.sb([128, 16], F32, "st")) for i in range(2)])
        xT32 = C.sb([128, 8, 128], F32, "xT32")
        xTb = Rot([(("xTb", i), C.sb([128, 8, 128], BF16, "xTb")) for i in range(2)])
        lg = C.sb([128, 16], F32, "lg")
        ex = C.sb([128, 16], F32, "ex")
        affs = C.sb([128, ntile, 16], F32, "affs")
        pA = Rot([(("pA", i), C.ps([128, 512], F32, "pA")) for i in range(5)])
        pB = C.ps([128, 1024], BF16, "pB")

        DMA(P, "sp", identb[:, :], identbd[:, :], [], ["identb"])
        DMA(P, "sp", ident32[:, :], ident32d[:, :], [], ["ident32"])
        DMA(P, "sp", gf[:, :], gfd[:, :], [], ["gf"])
        DMA(P, "sp", wr[:, :, :], wrd.ap().rearrange("(c p) e -> p c e", p=128), [], ["wr"])
        for b in range(3):
            sk, stg = wst.next()
            DMA(P, "sp", stg[:, :, :], wds[b].ap().rearrange("(c p) n -> p c n", p=128), [], [sk])
            for c in range(4):
                CP(P, ("dve", "pool")[c % 2], wbr[b][:, c, :], stg[:, c, :], [sk], [("wbr", b)])
        for hh in range(2):
            sk, stg = wst.next()
            DMA(P, "sp", stg[:, :, :], wod.ap().rearrange("(c p) n -> p c n", p=128)[:, hh * 4:(hh + 1) * 4, :], [], [sk])
            for c in range(4):
                CP(P, ("dve", "pool")[c % 2], wo[:, hh * 4 + c, :], stg[:, c, :], [sk], ["wo"])
        for k in range(8):
            TS(P, "pool", gfull[:, k, :], ident32[:, :], 0.0, gf[:, k:k + 1], ALU.mult, ALU.add, ["ident32", "gf"], ["gfull"])
        for t in range(ntile):
            r0 = t * 128
            xk, xt_ = xt.next()
            DMA(P, "sp", xt_[:, :], xd[r0:r0 + 128, :], [], [xk])
            gk, gt_ = gts.next()
            DMA(P, "sp", gt_[:, :], gtsd[r0:r0 + 128, :], [], [gk])
            skeys = []
            stiles = []
            for b in range(3):
                k_, t_ = srct[b].next()
                DMA(P, "sp", t_[:, :, :], srcs[b].ap().rearrange("(c p) t -> p c t", p=128)[:, :, r0:r0 + 128], [], [k_])
                skeys.append(k_)
                stiles.append(t_)
            for b in range(3):
                for hf in range(2):
                    ak, at = pA.next()
                    for c in range(4):
                        MM(P, at[:, :], stiles[b][:, c, :], wbr[b][:, c, hf * 512:(hf + 1) * 512], c == 0, c == 3,
                           [skeys[b], ("wbr", b)], [ak])
                    gsl = gt_[:, b * 1024 + hf * 512:b * 1024 + (hf + 1) * 512]
                    if b == 0:
                        TT(P, "dve", mg[:, hf * 512:(hf + 1) * 512], at[:, :], gsl, ALU.mult, [ak, gk], [("mg", hf)])
                    else:
                        tk, tt_ = tmpm.next()
                        TT(P, "dve", tt_[:, :], at[:, :], gsl, ALU.mult, [ak, gk], [tk])
                        TT(P, "pool", mg[:, hf * 512:(hf + 1) * 512], mg[:, hf * 512:(hf + 1) * 512], tt_[:, :], ALU.add,
                           [("mg", hf), tk], [("mg", hf)])
            CP(P, "act", mgb[:, :], mg[:, :], [("mg", 0), ("mg", 1)], ["mgb"])
            for k in range(8):
                TR(P, pB[:, k * 128:(k + 1) * 128], mgb[:, k * 128:(k + 1) * 128], identb[:, :], ["mgb", "identb"], ["pB"])
            CP(P, "dve", mT[:, :, :], pB[:].rearrange("p (k t) -> p k t", k=8), ["pB"], ["mT"])
            x1k, x1t = x1.next()
            for hf in range(2):
                ak, at = pA.next()
                for k in range(8):
                    MM(P, at[:, :], mT[:, k, :], wo[:, k, hf * 512:(hf + 1) * 512], k == 0, k == 7, ["mT", "wo"], [ak])
                TT(P, "dve", x1t[:, hf * 512:(hf + 1) * 512], at[:, :], xt_[:, hf * 512:(hf + 1) * 512], ALU.add,
                   [ak, xk], [(x1k, hf)])
            DMA(P, "sp", x1d[r0:r0 + 128, :], x1t[:, :], [(x1k, 0), (x1k, 1)], [])
            sk_, st_ = st.next()
            ACT(P, sqj[:, :], x1t[:, :], AF.Square, [(x1k, 0), (x1k, 1)], ["sqj"])
            RSUM(P, "dve", st_[:, 0:1], sqj[:, :], ["sqj"], [sk_])
            ACT(P, st_[:, 1:2], st_[:, 0:1], AF.Sqrt, [sk_], [sk_], scale=1.0 / D, bias=EPS)
            RECIP(P, "dve", st_[:, 1:2], st_[:, 1:2], [sk_], [sk_])
            ACT(P, xs[:, :], x1t[:, :], AF.Copy, [(x1k, 0), (x1k, 1), sk_], ["xs"], scale=st_[:, 1:2])
            for hf in range(2):
                ak, at = pA.next()
                for k in range(4):
                    kk = hf * 4 + k
                    TR(P, at[:, k * 128:(k + 1) * 128], xs[:, kk * 128:(kk + 1) * 128], ident32[:, :], ["xs", "ident32"], [ak])
                TT(P, "dve", xT32[:, hf * 4:(hf + 1) * 4, :], at[:].rearrange("p (k t) -> p k t", k=4),
                   gfull[:, hf * 4:(hf + 1) * 4, :], ALU.mult, [ak, "gfull"], [("xT32", hf)])
            xbk, xbt = xTb.next()
            CP(P, "act", xbt[:, :, :], xT32[:, :, :], [("xT32", 0), ("xT32", 1)], [xbk])
            DMA(P, "sp", xn2d.ap().rearrange("(k p) t -> p k t", p=128)[:, :, r0:r0 + 128], xbt[:, :, :], [xbk], [])
            ak, at = pA.next()
            for k in range(8):
                MM(P, at[:, 0:16], xT32[:, k, :], wr[:, k, :], k == 0, k == 7, [("xT32", 0), ("xT32", 1), "wr"], [ak])
            CP(P, "dve", lg[:, :], at[:, 0:16], [ak], ["lg"])
            RMAX(P, "dve", st_[:, 2:3], lg[:, :], ["lg"], [sk_])
            TS(P, "dve", st_[:, 2:3], st_[:, 2:3], -1.0, None, ALU.mult, None, [sk_], [sk_])
            ACT(P, ex[:, :], lg[:, :], AF.Exp, ["lg", sk_], ["ex"], bias=st_[:, 2:3])
            RSUM(P, "dve", st_[:, 3:4], ex[:, :], ["ex"], [sk_])
            RECIP(P, "dve", st_[:, 3:4], st_[:, 3:4], [sk_], [sk_])
            TS(P, "dve", affs[:, t, :], ex[:, :], st_[:, 3:4], None, ALU.mult, None, ["ex", sk_], [("affs", t)])
            ak, at = pA.next()
            TR(P, at[0:16, 0:128], affs[:, t, :], ident32[:, :], [("affs", t), "ident32"], [ak])
            CP(P, "act", affTs[:, r0:r0 + 128], at[0:16, 0:128], [ak], [("affT", t)])
        DMA(P, "sp", affd.ap().rearrange("(t p) e -> p t e", p=128), affs[:, :, :], [("affs", t) for t in range(ntile)], [])
        DMA(P, "sp", affTd[:, :], affTs[:, :], [("affT", t) for t in range(ntile)], [])
        P.emit()
        stats = P.stats
    return nc, stats


def build_thr(ns=S, cap=2 * S // 16, iters=30, env=None, pre=None):
    nc, es, C, P = _begin(env, pre)
    with es:
        affT = C.dram("affT", [16, ns], F32, "ExternalInput")
        thr = C.dram("thr", [16, 2], F32, "ExternalOutput")
        a = C.sb([16, ns], F32, "a")
        junk = C.sb([16, ns], F32, "junk")
        lh = C.sb([16, 2], F32, "lh")
        w = C.sb([16, 8], F32, "w")
        DMA(P, "sp", a[:, :], affT[:, :], [], ["a"])
        MEMSET(P, "dve", lh[:, 0:1], 0.0, ["lh"])
        MEMSET(P, "dve", lh[:, 1:2], 1.0, ["lh"])
        for it in range(iters):
            TT(P, "dve", w[:, 0:1], lh[:, 0:1], lh[:, 1:2], ALU.add, ["lh"], ["w"])
            TS(P, "dve", w[:, 0:1], w[:, 0:1], 0.5, None, ALU.mult, None, ["w"], ["w"])
            P.op("dve", lambda e: e.tensor_scalar(out=junk[:, :], in0=a[:, :], scalar1=w[:, 0:1], scalar2=0.0,
                                                  op0=ALU.is_ge, op1=ALU.add, accum_out=w[:, 1:2]),
                 ["a", "w"], ["junk", "w"])
            TS(P, "dve", w[:, 2:3], w[:, 1:2], float(cap), None, ALU.is_ge, None, ["w"], ["w"])
            TT(P, "dve", w[:, 3:4], w[:, 0:1], lh[:, 0:1], ALU.subtract, ["w", "lh"], ["w"])
            TT(P, "dve", w[:, 4:5], lh[:, 1:2], w[:, 0:1], ALU.subtract, ["w", "lh"], ["w"])
            STT(P, "dve", lh[:, 0:1], w[:, 3:4], w[:, 2:3], lh[:, 0:1], ALU.mult, ALU.add, ["w", "lh"], ["lh"])
            STT(P, "dve", lh[:, 1:2], w[:, 4:5], w[:, 2:3], w[:, 0:1], ALU.mult, ALU.add, ["w", "lh"], ["lh"])
        DMA(P, "sp", thr[:, :], lh[:, :], ["lh"], [])
        P.emit()
        stats = P.stats
    return nc, stats


def build_ffn(nt=TPC, nexp=16, tb=1024, env=None, pre=None):
    nc, es, C, P = _begin(env, pre)
    FF = 1536
    ntile = nt // 128
    nblk = nt // tb
    with es:
        x1d = C.dram("x1", [nt, D], F32, "ExternalInput")
        xnd = C.dram("xn2T", [D, nt], BF16, "ExternalInput")
        affd = C.dram("aff", [nt, 16], F32, "ExternalInput")
        thrd = C.dram("thr_row", [128, 16], F32, "ExternalInput") if not (pre is not None and "thr16" in pre) else None
        wgd = C.dram("wg", [nexp, D, FF], F32, "ExternalInput")
        wud = C.dram("wu", [nexp, D, FF], F32, "ExternalInput")
        wdd = C.dram("wd", [nexp, FF, D], F32, "ExternalInput")
        x2d = C.dram("x2", [nt, D], F32, "ExternalOutput")

        xb = C.sb([128, 8, tb], BF16, "xb")
        acc = C.sb([128, tb // 128, D], F32, "acc")
        wgb = C.sb([128, 8, FF], BF16, "wgb")
        wub = C.sb([128, 8, FF], BF16, "wub")
        wdb = C.sb([128, 12, D], BF16, "wdb")
        stg = Rot([(("stg", i), C.sb([128, 4096], F32, "stg")) for i in range(2)])
        hT = C.sb([128, 12, tb], BF16, "hT")
        sg = Rot([(("sg", i), C.sb([128, 512], F32, "sg")) for i in range(2)])
        affs = C.sb([128, ntile, 16], F32, "affs")
        gw = C.sb([128, ntile, 16], F32, "gw")
        thr = C.sb([128, 16], F32, "thr")
        xo = Rot([(("xo", i), C.sb([128, D], F32, "xo")) for i in range(2)])
        pA = Rot([(("pA", i), C.ps([128, 512], F32, "pA")) for i in range(7)])

        if pre is not None and "thr16" in pre:
            t16d = pre["thr16"]
            i32d = pre["ident32"]
            t16 = C.sb([16, 2], F32, "t16")
            tbc = C.sb([16, 128], F32, "tbc")
            i16 = C.sb([16, 16], F32, "i16")
            DMA(P, "sp", t16[:, :], t16d[:, :], [], ["t16"])
            DMA(P, "sp", i16[:, :], i32d[0:16, 0:16], [], ["i16"])
            MEMSET(P, "dve", tbc[:, :], 1.0, ["tbc"])
            TS(P, "dve", tbc[:, :], tbc[:, :], t16[:, 0:1], None, ALU.mult, None, ["tbc", "t16"], ["tbc"])
            tk_, tp_ = pA.next()
            MM(P, tp_[:, 0:16], tbc[:, :], i16[:, :], True, True, ["tbc", "i16"], [tk_])
            CP(P, "dve", thr[:, :], tp_[:, 0:16], [tk_], ["thr"])
        else:
            DMA(P, "sp", thr[:, :], thrd[:, :], [], ["thr"])
        DMA(P, "sp", affs[:, :, :], affd.ap().rearrange("(t p) e -> p t e", p=128), [], ["affs"])
        for t in range(ntile):
            TT(P, "dve", gw[:, t, :], affs[:, t, :], thr[:, :], ALU.is_ge, ["affs", "thr"], ["gw"])
            TT(P, "dve", gw[:, t, :], gw[:, t, :], affs[:, t, :], ALU.mult, ["gw", "affs"], ["gw"])
        cv = [0]

        def conv(dst, src, r, w):
            eng = ("act", "dve", "act")[cv[0] % 3]
            cv[0] += 1
            CP(P, eng, dst, src, r, w)

        def load_gu(e, chs=(0, 1, 2)):
            for ch in chs:
                for (wd_, dstb, key) in ((wgd, wgb, "wgb"), (wud, wub, "wub")):
                    sk, st = stg.next()
                    sv = st[:, :].rearrange("p (c f) -> p c f", c=8)
                    DMA(P, "sp", sv, wd_[e].rearrange("(c p) f -> p c f", p=128)[:, :, ch * 512:(ch + 1) * 512], [], [sk])
                    for hh in range(2):
                        conv(dstb[:, hh * 4:(hh + 1) * 4, ch * 512:(ch + 1) * 512], sv[:, hh * 4:(hh + 1) * 4, :], [sk],
                             [(key, ch)])

        def load_d(e):
            for ch in range(3):
                sk, st = stg.next()
                sv = st[:, :].rearrange("p (c n) -> p c n", c=4)
                DMA(P, "sp", sv, wdd[e].rearrange("(c p) n -> p c n", p=128)[:, ch * 4:(ch + 1) * 4, :], [], [sk])
                for hh in range(2):
                    conv(wdb[:, ch * 4 + hh * 2:ch * 4 + hh * 2 + 2, :], sv[:, hh * 2:hh * 2 + 2, :], [sk], ["wdb"])

        first = True
        for blk in range(nblk):
            b0 = blk * tb
            DMA(P, "sp", xb[:, :, :], xnd.ap().rearrange("(k p) t -> p k t", p=128)[:, :, b0:b0 + tb], [], ["xb"])
            for e in range(nexp):
                if first:
                    load_gu(e)
                    load_d(e)
                    first = False
                nxt = (blk * nexp + e + 1)
                for f in range(12):
                    for tq in range(tb // 512):
                        gk, gp = pA.next()
                        uk, up = pA.next()
                        for k in range(8):
                            MM(P, gp[:, :], wgb[:, k, f * 128:(f + 1) * 128], xb[:, k, tq * 512:(tq + 1) * 512],
                               k == 0, k == 7, [("wgb", f // 4), "xb"], [gk])
                        for k in range(8):
                            MM(P, up[:, :], wub[:, k, f * 128:(f + 1) * 128], xb[:, k, tq * 512:(tq + 1) * 512],
                               k == 0, k == 7, [("wub", f // 4), "xb"], [uk])
                        sk, st = sg.next()
                        ACT(P, st[:, :], gp[:, :], AF.Silu, [gk], [sk])
                        TT(P, "dve", hT[:, f, tq * 512:(tq + 1) * 512], up[:, :], st[:, :], ALU.mult, [uk, sk], [("hT", f)])
                    if f % 4 == 3 and nxt < nblk * nexp:
                        load_gu(nxt % nexp, chs=(f // 4,))
                for tt in range(tb // 128):
                    gcol = gw[:, blk * (tb // 128) + tt, e:e + 1]
                    for hf in range(2):
                        yk, yp = pA.next()
                        for f in range(12):
                            MM(P, yp[:, :], hT[:, f, tt * 128:(tt + 1) * 128], wdb[:, f, hf * 512:(hf + 1) * 512],
                               f == 0, f == 11, [("hT", f), "wdb"], [yk])
                        asl = acc[:, tt, hf * 512:(hf + 1) * 512]
                        if e == 0:
                            TS(P, "dve", asl, yp[:, :], gcol, None, ALU.mult, None, [yk, "gw"], [("acc", tt, hf)])
                        else:
                            STT(P, "dve", asl, yp[:, :], gcol, asl, ALU.mult, ALU.add, [yk, "gw", ("acc", tt, hf)],
                                [("acc", tt, hf)])
                if nxt < nblk * nexp:
                    load_d(nxt % nexp)
            for tt in range(tb // 128):
                xk, xt_ = xo.next()
                r0 = b0 + tt * 128
                DMA(P, "sp", xt_[:, :], x1d[r0:r0 + 128, :], [], [xk])
                TT(P, "pool", xt_[:, :], xt_[:, :], acc[:, tt, :], ALU.add, [xk, ("acc", tt, 0), ("acc", tt, 1)], [xk])
                DMA(P, "sp", x2d[r0:r0 + 128, :], xt_[:, :], [xk], [])
        P.emit()
        stats = P.stats
    return nc, stats


def build_attn(nq=TPC, nk=S, nheads=8, env=None, pre=None):
    nc, es, C, P = _begin(env, pre)
    NKT = nk // 128
    NQB = nq // 512
    scale = 96.0 ** -0.5
    with es:
        mq = C.dram("mq", [nheads, 96, nq], BF16, "ExternalInput")
        mk = C.dram("mk", [nheads, 96, nk], BF16, "ExternalInput")
        mv = C.dram("mv", [nheads, 128, NKT * 64], BF16, "ExternalInput")
        esel = C.dram("esel", [65, 64], F32, "ExternalInput")
        OT = C.dram("OT", [nheads * 64, nq], BF16, "ExternalOutput")

        kT = Rot([(("kT", i), C.sb([96, nk], BF16, "kT")) for i in range(2)])
        vv = Rot([(("vv", i), C.sb([128, NKT, 65], BF16, "vv")) for i in range(2)])
        qT = Rot([(("qT", i), C.sb([96, nq], BF16, "qT")) for i in range(2)])
        pT = Rot([(("pT", i), C.sb([128, 512], BF16, "pT")) for i in range(4)])
        osb = C.sb([65, 512], F32, "osb")
        rbc = C.sb([64, 512], F32, "rbc")
        oo = Rot([(("oo", i), C.sb([64, 512], BF16, "oo")) for i in range(2)])
        es_sb = C.sb([65, 64], F32, "esel")
        sps = Rot([(("sps", i), C.ps([128, 512], F32, "sps")) for i in range(4)])
        ops_ = Rot([(("ops", i), C.ps([128, 512], F32, "ops")) for i in range(2)])
        bps = C.ps([128, 512], F32, "bps")
        DMA(P, "sp", es_sb[:], esel[:, :], [], ["esel"])
        for i in range(2):
            MEMSET(P, "pool", vv.items[i][1][:, :, 64:65], 1.0, [("vv1", i)])
        for h in range(nheads):
            kk, kt_ = kT.next()
            vk, vt_ = vv.next()
            qk, qt_ = qT.next()
            DMA(P, "sp", kt_[:, :], mk[h, :, :], [], [kk])
            DMA(P, "sp", vt_[:, :, 0:64], mv[h, :, :].rearrange("p (t d) -> p t d", d=64), [], [vk])
            DMA(P, "sp", qt_[:, :], mq[h, :, :], [], [qk])
            vkeys = [vk, ("vv1", (vv.i - 1) % 2)]
            for qb in range(NQB):
                ok_, ot_ = ops_.next()

                def s_mm(t, kt_=kt_, qt_=qt_, qb=qb, kk=kk, qk=qk):
                    sk, st = sps.next()
                    MM(P, st[:, :], kt_[:, t * 128:(t + 1) * 128], qt_[:, qb * 512:(qb + 1) * 512], True, True,
                       [kk, qk], [sk])
                    return sk, st
                pend = [s_mm(0)]
                if NKT > 1:
                    pend.append(s_mm(1))
                for t in range(NKT):
                    if t + 2 < NKT:
                        pend.append(s_mm(t + 2))
                    sk, st = pend.pop(0)
                    pk, pt = pT.next()
                    ACT(P, pt[:, :], st[:, :], AF.Exp, [sk], [pk], scale=scale)
                    MM(P, ot_[0:65, :], vt_[:, t, :], pt[:, :], t == 0, t == NKT - 1, vkeys + [pk], [ok_])
                CP(P, "dve", osb[:, :], ot_[0:65, :], [ok_], ["osb"])
                MM(P, bps[0:64, :], es_sb[:, :], osb[:, :], True, True, ["esel", "osb"], ["bps"])
                CP(P, "dve", rbc[:, :], bps[0:64, :], ["bps"], ["rbc"])
                RECIP(P, "dve", rbc[:, :], rbc[:, :], ["rbc"], ["rbc"])
                ook, oot = oo.next()
                TT(P, "dve", oot[:, :], osb[0:64, :], rbc[:, :], ALU.mult, ["osb", "rbc"], [ook])
                DMA(P, "sp", OT[h * 64:(h + 1) * 64, qb * 512:(qb + 1) * 512], oot[:, :], [ook], [])
        P.emit()
        stats = P.stats
    return nc, stats


def attn_consts():
    e = np.zeros((65, 64), np.float32)
    e[64, :] = 1.0
    return dict(esel=e)


RG = [[0, 1, 2, 3], [4, 5, 6, 7]]
LAYER_W = [("w_in", [D, INW], F32), ("gmix", [128, 8], F32), ("convp", [128, 4, 34], F32), ("gcq", [128, 3], F32),
           ("gckv", [128, 2], F32), ("w_uq", [384, 768], F32), ("w_ukv", [256, 1024], F32), ("gqk", [96, 2], F32),
           ("bif", [128, 4], F32), ("gA", [128, 128], F32), ("w_a", [512, D], F32), ("w_b", [512, D], F32),
           ("w_c", [512, D], F32), ("w_o", [D, D], F32), ("gffn", [128, 8], F32), ("w_r", [D, 16], F32),
           ("wg", [16, D, 1536], F32), ("wu", [16, D, 1536], F32), ("wd", [16, 1536, D], F32)]
CONSTS = [("identb", [128, 128], BF16), ("ropeT", [96, 2, TPC], F32), ("rmat", [96, 96], BF16), ("onesf", [128, 128], F32),
          ("cmat", [128, 6, 128], F32), ("ident32", [128, 128], F32), ("esel", [65, 64], F32), ("idx", [128, 16], I32)]


def build_fused(stop=None):
    env = Env()
    nc, P = env.nc, env.P
    BYP = ALU.bypass
    CCB = 256 * 1024
    with env.es:
        def DT(name, shape, dt, kind="Internal"):
            return nc.dram_tensor(name, list(shape), dt, kind=kind)

        def allgather(name, src2d, rows, cols, dt, rkeys, wkey):
            esz = 4 if dt in (F32, I32) else 2
            rc = max(1, min(rows, CCB // (cols * esz)))
            assert rows % rc == 0
            g = DT(name, [4 * rows, cols], dt)
            for k in range(rows // rc):
                P.cc(lambda e, k=k: e.collective_compute("AllGather", BYP, replica_groups=RG, ins=[src2d[k * rc:(k + 1) * rc, :]],
                                                         outs=[g[k * 4 * rc:(k + 1) * 4 * rc, :]]), rkeys, [wkey])
            return g, rc

        def rankview(g, rc, r):
            return g.ap().rearrange("(k r x) c -> r k x c", r=4, x=rc)[r]

        ext = {"xe0": DT("xe0", [TPC + 2 * HALO, D], F32, "ExternalInput")}
        for (n, sh, dt) in CONSTS:
            ext[n] = DT(n, sh, dt, "ExternalInput")
        for l in range(2):
            for (n, sh, dt) in LAYER_W:
                ext["%s_%d" % (n, l)] = DT("%s_%d" % (n, l), sh, dt, "ExternalInput")
        out = DT("out", [TPC, D], F32, "ExternalOutput")
        xe = ext["xe0"]
        x_own = None
        for l in range(2):
            W = {n: ext["%s_%d" % (n, l)] for (n, _, _) in LAYER_W}
            L = lambda n, sh, dt: DT("%s_L%d" % (n, l), sh, dt)
            A = dict(QT=L("QT", [512, TPC], BF16), KT=L("KT", [512, TPC], BF16), Kt=L("Kt", [4, TPC, 128], BF16),
                     Vt=L("Vt", [4, TPC, 128], BF16), OG=L("OG", [4, TPC, 128], BF16), G4=L("G4", [4, TPC, 4], F32),
                     GTS=L("GTS", [TPC, 3072], BF16), UT=L("UT", [512, TPC], BF16), MQ=L("MQ", [8, 96, TPC], BF16),
                     MK=L("MK", [8, 96, TPC], BF16), MV=L("MV", [8, 128, TPC // 128, 64], BF16))
            preA = dict(xe=xe, w_in=W["w_in"], gmix=W["gmix"], identb=ext["identb"], convp=W["convp"], gcq=W["gcq"],
                        gckv=W["gckv"], w_uq=W["w_uq"], w_ukv=W["w_ukv"], gqk=W["gqk"], ropeT=ext["ropeT"],
                        rmat=ext["rmat"], onesf=ext["onesf"], **A)
            build_stageA(env=env, pre=preA)
            nc_, es_, C_, _ = _begin(env)
            with es_:
                idx = C_.sb([128, 16], I32, "idx")
                DMA(P, "sp", idx[:, :], ext["idx"][:, :], [], ["idx"])
                gQT, rcQ = allgather("gQT_L%d" % l, A["QT"].ap(), 512, TPC, BF16, [], ("g", 0))
                gKt, rcK = allgather("gKt_L%d" % l, A["Kt"].ap().rearrange("h t d -> (h t) d"), 4 * TPC, 128, BF16, [], ("g", 1))
                gVt, _ = allgather("gVt_L%d" % l, A["Vt"].ap().rearrange("h t d -> (h t) d"), 4 * TPC, 128, BF16, [], ("g", 2))
                gOG, _ = allgather("gOG_L%d" % l, A["OG"].ap().rearrange("h t d -> (h t) d"), 4 * TPC, 128, BF16, [], ("g", 3))
                gG4, rcG = allgather("gG4_L%d" % l, A["G4"].ap().rearrange("h t g -> (h t) g"), 4 * TPC, 4, F32, [], ("g", 4))
                gMK, rcMK = allgather("gMK_L%d" % l, A["MK"].ap().rearrange("h f t -> (h f) t"), 768, TPC, BF16, [], ("g", 5))
                gMV, rcMV = allgather("gMV_L%d" % l, A["MV"].ap().rearrange("h p t d -> (h p) (t d)"), 1024, 2048, BF16, [], ("g", 6))
                assert (rcQ, rcK, rcG, rcMK, rcMV) == (32, 1024, 4 * TPC, 32, 64), (rcQ, rcK, rcG, rcMK, rcMV)
                qT_s = L("qT_s", [128, S], BF16)
                kt_s = L("kt_s", [S, 128], BF16)
                vt_s = L("vt_s", [S, 128], BF16)
                og_s = L("og_s", [S, 128], BF16)
                g4_s = L("g4_s", [S, 4], F32)
                mk_s = L("mk_s", [8, 96, S], BF16)
                mv_s = L("mv_s", [8, 128, (S // 128) * 64], BF16)
                stb = Rot([(("stb", i), C_.sb([128, 16384], BF16, "stb")) for i in range(2)])
                stf = C_.sb([128, 512], F32, "stf")

                def gather(dst_ap, src_ap, col, rkeys, wkeys, tile_ap, tkey):
                    P.dma("pool", lambda e: e.indirect_dma_start(
                        out=tile_ap, out_offset=None, in_=src_ap,
                        in_offset=bass.IndirectOffsetOnAxis(ap=idx[:, col:col + 1], axis=0)), ["idx"] + rkeys, [tkey])
                    DMA(P, "sp", dst_ap, tile_ap, [tkey], wkeys)
                for i in range(4):
                    tk_, tt_ = stb.next()
                    gather(qT_s[:, i * TPC:(i + 1) * TPC], gQT[:, :], i, [("g", 0)], [("qT_s", i)], tt_[:, 0:TPC], tk_)
                for j, (gsrc, dst) in enumerate(((gKt, kt_s), (gVt, vt_s), (gOG, og_s))):
                    tk_, tt_ = stb.next()
                    gather(dst.ap().rearrange("(c p) d -> c (p d)", p=128),
                           gsrc.ap().rearrange("(c p) d -> c (p d)", p=128), 4, [("g", 1 + j)], [("tm_s", j)], tt_[:, :], tk_)
                gather(g4_s.ap().rearrange("(c p) d -> c (p d)", p=128),
                       gG4.ap().rearrange("(c p) d -> c (p d)", p=128), 11, [("g", 4)], [("tm_s", 3)], stf[:, :], "stf")
                for i in range(4):
                    DMA(P, "sp", mk_s.ap().rearrange("h f t -> (h f) t")[:, i * TPC:(i + 1) * TPC].rearrange("(k x) t -> k x t", x=rcMK),
                        rankview(gMK, rcMK, i), [("g", 5)], [("mk_s", i)])
                    DMA(P, "sp", mv_s.ap().rearrange("h p x -> (h p) x")[:, i * 2048:(i + 1) * 2048].rearrange("(k x) c -> k x c", x=rcMV),
                        rankview(gMV, rcMV, i), [("g", 6)], [("mv_s", i)])
                P.emit()
            if stop == "x1":
                return nc
            HT = L("HT", [128, S], BF16)
            build_mlstm(env=env, pre=dict(qT=qT_s, kt=kt_s, vt=vt_s, g4=g4_s, bif=W["bif"], og=og_s, gA=W["gA"],
                                          cmat=ext["cmat"], identb=ext["identb"], HT=HT))
            if stop == "m":
                return nc
            OT = L("OT", [512, TPC], BF16)
            build_attn(env=env, pre=dict(mq=A["MQ"], mk=mk_s, mv=mv_s, esel=ext["esel"], OT=OT))
            if stop == "t":
                return nc
            nc_, es_, C_, _ = _begin(env)
            with es_:
                idx = C_.sb([128, 16], I32, "idx")
                DMA(P, "sp", idx[:, :], ext["idx"][:, :], [], ["idx"])
                gHT, rcH = allgather("gHT_L%d" % l, HT.ap(), 128, S, BF16, [], "gHT")
                assert rcH == 8
                HT_own = L("HT_own", [512, TPC], BF16)
                src = gHT.ap().rearrange("r (i t) -> (r i) t", i=4)
                stb = Rot([(("stb", i), C_.sb([128, TPC], BF16, "stb")) for i in range(2)])
                for h in range(4):
                    tk_, tt_ = stb.next()
                    P.dma("pool", lambda e, h=h, tt_=tt_: e.indirect_dma_start(
                        out=tt_[:, :], out_offset=None, in_=src,
                        in_offset=bass.IndirectOffsetOnAxis(ap=idx[:, 5 + h:6 + h], axis=0)), ["idx", "gHT"], [tk_])
                    DMA(P, "sp", HT_own[h * 128:(h + 1) * 128, :], tt_[:, :], [tk_], [("HT_own", h)])
                P.emit()
            if stop == "x2":
                return nc
            x1 = L("x1", [TPC, D], F32)
            xn2T = L("xn2T", [D, TPC], BF16)
            aff = L("aff", [TPC, 16], F32)
            affT = L("affT", [16, TPC], F32)
            if l == 0:
                xin = L("xin", [TPC, D], F32)
                nc_, es_, C_, _ = _begin(env)
                with es_:
                    DMA(P, "sp", xin[:, :], ext["xe0"][HALO:HALO + TPC, :], [], ["xin"])
                    P.emit()
            else:
                xin = x_own
            build_merge(env=env, pre=dict(x=xin, HT=HT_own, UT=A["UT"], OT=OT, GTS=A["GTS"], w_a=W["w_a"], w_b=W["w_b"],
                                          w_c=W["w_c"], w_o=W["w_o"], gffn=W["gffn"], w_r=W["w_r"], identb=ext["identb"],
                                          ident32=ext["ident32"], x1=x1, xn2T=xn2T, aff=aff, affT=affT))
            if stop == "c1":
                return nc
            affT_s = L("affT_s", [16, S], F32)
            nc_, es_, C_, _ = _begin(env)
            with es_:
                gAf, rcA = allgather("gAf_L%d" % l, affT.ap(), 16, TPC, F32, [], "gAf")
                assert rcA == 16
                for i in range(4):
                    DMA(P, "sp", affT_s[:, i * TPC:(i + 1) * TPC], gAf[i * 16:(i + 1) * 16, :], ["gAf"], [("affT_s", i)])
                P.emit()
            thr = L("thr", [16, 2], F32)
            build_thr(env=env, pre=dict(affT=affT_s, thr=thr))
            if stop == "h":
                return nc
            x2 = out if l == 1 else L("x2", [TPC, D], F32)
            build_ffn(env=env, pre=dict(x1=x1, xn2T=xn2T, aff=aff, thr16=thr, ident32=ext["ident32"], wg=W["wg"], wu=W["wu"],
                                        wd=W["wd"], x2=x2))
            if l == 0:
                xe1 = L("xe1", [TPC + 2 * HALO, D], F32)
                nc_, es_, C_, _ = _begin(env)
                with es_:
                    idx = C_.sb([128, 16], I32, "idx")
                    zt = C_.sb([128, D], F32, "zt")
                    DMA(P, "sp", idx[:, :], ext["idx"][:, :], [], ["idx"])
                    MEMSET(P, "dve", zt[:, :], 0.0, ["zt"])
                    edges = L("edges", [384, D], F32)
                    DMA(P, "sp", edges[0:128, :], x2[0:128, :], [], ["edges"])
                    DMA(P, "sp", edges[128:256, :], x2[TPC - 128:TPC, :], [], ["edges"])
                    DMA(P, "sp", edges[256:384, :], zt[:, :], ["zt"], ["edges"])
                    DMA(P, "sp", xe1[HALO:HALO + TPC, :], x2[:, :], [], ["xe1m"])
                    gE, rcE = allgather("gE_L%d" % l, edges.ap(), 384, D, F32, ["edges"], "gE")
                    assert rcE == 64
                    hl = C_.sb([128, D], F32, "hl")
                    hr = C_.sb([128, D], F32, "hr")
                    P.dma("pool", lambda e: e.indirect_dma_start(
                        out=hl[:, :], out_offset=None, in_=gE[:, :],
                        in_offset=bass.IndirectOffsetOnAxis(ap=idx[:, 9:10], axis=0)), ["idx", "gE"], ["hl"])
                    P.dma("pool", lambda e: e.indirect_dma_start(
                        out=hr[:, :], out_offset=None, in_=gE[:, :],
                        in_offset=bass.IndirectOffsetOnAxis(ap=idx[:, 10:11], axis=0)), ["idx", "gE"], ["hr"])
                    DMA(P, "sp", xe1[0:HALO, :], hl[:, :], ["hl"], ["xe1l"])
                    DMA(P, "sp", xe1[HALO + TPC:, :], hr[:, :], ["hr"], ["xe1r"])
                    P.emit()
                xe = xe1
                x_own = x2
    return nc


def _grow(x, rc, r):
    return (x // rc) * (4 * rc) + r * rc + (x % rc)


def fused_idx(c):
    r = c % 4
    p = np.arange(128)
    idx = np.zeros((128, 16), np.int32)
    for i in range(4):
        idx[:, i] = _grow(r * 128 + p, 32, i)
    y = r * 32 + (p % 32)
    idx[:, 4] = _grow(y, 8, p // 32)
    idx[:, 11] = (p // 32) * 128 + y
    for h in range(4):
        idx[:, 5 + h] = _grow(p, 8, h) * 4 + r
    idx[:, 9] = _grow(128 + p, 64, r - 1) if r > 0 else _grow(256 + p, 64, r)
    idx[:, 10] = _grow(p, 64, r + 1) if r < 3 else _grow(256 + p, 64, r)
    return idx


def kernel(**inputs):
    prm = {k: np.asarray(v) for k, v in inputs.items()}
    x = np.ascontiguousarray(prm["x"], dtype=np.float32)
    nc = build_fused()
    CT, ST = rope_tables()
    cst = consts()
    mc = mlstm_consts()
    ac = attn_consts()
    i32 = np.eye(128, dtype=np.float32)
    lay = []
    for l in range(2):
        convp = np.zeros((128, 4, 34), np.float32)
        convp[:, :, 0:31] = prm["conv_w"][l].T.reshape(4, 128, 31).transpose(1, 0, 2)
        convp[:, :, 31] = prm["conv_b"][l].reshape(4, 128).T
        convp[:, :, 32] = prm["conv_ln_g"][l].reshape(4, 128).T
        convp[:, :, 33] = prm["conv_ln_b"][l].reshape(4, 128).T
        lay.append(dict(w_in=prm["w_in"][l], gmix=_gain_cols(prm["mix_norm_g"][l], 8), convp=convp,
                        gcq=_gain_cols(prm["cq_norm_g"][l], 3), gckv=_gain_cols(prm["ckv_norm_g"][l], 2),
                        w_uq=prm["w_uq"][l], w_ukv=prm["w_ukv"][l],
                        gqk=np.ascontiguousarray(np.stack([prm["q_norm_g"][l], prm["k_norm_g"][l]], axis=1)),
                        w_a=prm["w_a_out"][l], w_b=prm["w_b_out"][l], w_c=prm["w_c_out"][l], w_o=prm["w_out"][l],
                        gffn=_gain_cols(prm["ffn_norm_g"][l], 8), w_r=prm["w_router"][l],
                        wg=prm["w_e_gate"][l], wu=prm["w_e_up"][l], wd=prm["w_e_down"][l]))
    maps = []
    for c in range(NCORES):
        b, r = c // 4, c % 4
        s0 = r * TPC
        xe = np.zeros((TPC + 2 * HALO, D), np.float32)
        lo, hi = max(0, s0 - HALO), min(S, s0 + TPC + HALO)
        xe[lo - (s0 - HALO):hi - (s0 - HALO)] = x[b, lo:hi]
        m = dict(xe0=xe, identb=cst["identb"], ropeT=np.ascontiguousarray(np.stack([CT[:, s0:s0 + TPC], ST[:, s0:s0 + TPC]], axis=1)),
                 rmat=cst["rmat"], onesf=cst["onesf"], cmat=mc["cmat"], ident32=i32, esel=ac["esel"], idx=fused_idx(c))
        cols = [r, 4 + r, 8 + r, 12 + r]
        for l in range(2):
            for k_, v_ in lay[l].items():
                m["%s_%d" % (k_, l)] = v_
            m["bif_%d" % l] = np.ascontiguousarray(np.broadcast_to(prm["b_if"][l][cols], (128, 4)))
            m["gA_%d" % l] = np.ascontiguousarray(np.broadcast_to(prm["a_norm_g"][l][r], (128, 128)))
        maps.append(m)
    res = run_spmd(nc, maps)
    out = np.empty_like(x)
    for c in range(NCORES):
        b, r = c // 4, c % 4
        out[b, r * TPC:(r + 1) * TPC] = np.asarray(res[c]["out"])
    return out
```

```python
import math
from contextlib import ExitStack

import numpy as np
import ml_dtypes

import concourse.bass as bass
import concourse.mybir as mybir
from concourse.bass_utils import run_bass_kernel_spmd

F32 = mybir.dt.float32
BF16 = mybir.dt.bfloat16
I32 = mybir.dt.int32
AF = mybir.ActivationFunctionType
ALU = mybir.AluOpType
AX = mybir.AxisListType
NPBF = ml_dtypes.bfloat16

D = 1024
S = 16384
NB = 2
INW = 6832
EPS = 1e-6
NCORES = 8
TPC = S * NB // NCORES

O_AQ, O_AK, O_AV, O_AO, O_AG = 0, 512, 1024, 1536, 2048
O_GLU = 2064
O_CQ = 3088
O_CKV = 3472
O_CKR = 3728
O_GTS = 3760


class Prog:
    RING = 8

    def __init__(self, nc, es):
        self.nc = nc
        self.es = es
        self.ops = []
        self.engs = ["pe", "act", "dve", "pool", "sp"]
        self.csem = None
        self.rings = {}
        self.ccsem = None
        self.ccount = {e: 0 for e in self.engs}
        self.dcount = {e: 0 for e in self.engs}
        self.cccount = 0
        self.nstage = 0

    def cc(self, fn, r=(), w=()):
        self.ops.append(dict(eng="pool", fn=fn, r=tuple(r), w=tuple(w), dma=True, cc=True))

    def op(self, eng, fn, r=(), w=()):
        self.ops.append(dict(eng=eng, fn=fn, r=tuple(r), w=tuple(w), dma=False))

    def dma(self, eng, fn, r=(), w=()):
        self.ops.append(dict(eng=eng, fn=fn, r=tuple(r), w=tuple(w), dma=True))

    def emit(self):
        nc, es = self.nc, self.es
        ops = self.ops
        last_w = {}
        readers = {}
        deps = []
        for i, o in enumerate(ops):
            d = set()
            for k in o["r"]:
                if k in last_w:
                    d.add((last_w[k], "raw"))
            for k in o["w"]:
                if k in last_w:
                    d.add((last_w[k], "waw"))
                for j in readers.get(k, ()):
                    if j != i:
                        d.add((j, "war"))
            for k in o["r"]:
                lst = readers.setdefault(k, [])
                if not o["dma"]:
                    lst[:] = [j for j in lst if ops[j]["dma"] or ops[j]["eng"] != o["eng"]]
                lst.append(i)
            for k in o["w"]:
                last_w[k] = i
                readers[k] = []
            dd = set()
            for j, kind in d:
                p = ops[j]
                if (not p["dma"]) and (not o["dma"]) and p["eng"] == o["eng"]:
                    if o["eng"] == "pe":
                        continue
                    if kind == "war":
                        continue
                dd.add(j)
            deps.append(dd)
        needed = set()
        for dd in deps:
            needed |= dd
        engs = self.engs
        if self.csem is None:
            self.csem = {e: es.enter_context(nc.semaphore("c_" + e)) for e in engs}
            self.ccsem = es.enter_context(nc.semaphore("c_cc"))
        csem = self.csem
        rings = self.rings
        ccount = self.ccount
        dcount = self.dcount
        prev_end = dict(c={e: ccount[e] for e in engs}, d={e: dcount[e] for e in rings}, cc=self.cccount)
        lastc = {}
        for i, o in enumerate(ops):
            if not o["dma"]:
                lastc[o["eng"]] = i
        needed |= set(lastc.values())
        sig = {}
        prewait = {}
        for i, o in enumerate(ops):
            e = o["eng"]
            if o.get("cc"):
                self.cccount += 1
                sig[i] = (self.ccsem, self.cccount, 1)
                if self.cccount > 1:
                    prewait[i] = (self.ccsem, self.cccount - 1)
            elif o["dma"]:
                if e not in rings:
                    rings[e] = [es.enter_context(nc.semaphore("r_%s%d" % (e, k)))
                                for k in range(self.RING)]
                n = dcount[e]
                dcount[e] += 1
                sem = rings[e][n % self.RING]
                sig[i] = (sem, 16 * (n // self.RING + 1), 16)
                if n >= self.RING:
                    prewait[i] = (sem, 16 * (n // self.RING))
            elif i in needed:
                ccount[e] += 1
                sig[i] = (csem[e], ccount[e], 1)
        per = {e: [] for e in engs}
        for i, o in enumerate(ops):
            per[o["eng"]].append(i)
        self.stats = dict(n_ops=len(ops), ccount=ccount, dcount=dcount)

        nstage = self.nstage
        self.nstage += 1

        def run(e, engobj):
            waited = {}
            if nstage > 0:
                for e2 in engs:
                    if prev_end["c"][e2] > 0:
                        engobj.wait_ge(csem[e2], prev_end["c"][e2])
                for e2, n in prev_end["d"].items():
                    for k in range(self.RING):
                        cnt = (n - k + self.RING - 1) // self.RING if n > k else 0
                        if cnt > 0:
                            engobj.wait_ge(rings[e2][k], 16 * cnt)
                if prev_end["cc"] > 0:
                    engobj.wait_ge(self.ccsem, prev_end["cc"])
            for i in per[e]:
                o = ops[i]
                ws = [sig[j][:2] for j in deps[i]]
                if i in prewait:
                    ws.append(prewait[i])
                mx = {}
                for sem, val in ws:
                    key = id(sem)
                    if key not in mx or mx[key][1] < val:
                        mx[key] = (sem, val)
                for key, (sem, val) in mx.items():
                    if waited.get(key, 0) >= val:
                        continue
                    waited[key] = val
                    engobj.wait_ge(sem, val)
                ins = o["fn"](engobj)
                if i in sig:
                    sem, val, inc = sig[i]
                    ins.then_inc(sem, inc)
            if e in rings:
                n = dcount[e]
                for k in range(self.RING):
                    cnt = (n - k + self.RING - 1) // self.RING if n > k else 0
                    if cnt > 0:
                        engobj.wait_ge(rings[e][k], 16 * cnt)

        with nc.Block() as block:
            @block.tensor
            def _(t):
                run("pe", t)

            @block.scalar
            def _(t):
                run("act", t)

            @block.vector
            def _(t):
                run("dve", t)

            @block.gpsimd
            def _(t):
                run("pool", t)

            @block.sync
            def _(t):
                run("sp", t)
        self.ops = []


class Ctx:
    def __init__(self, nc, es, pre=None, tag=""):
        self.nc, self.es = nc, es
        self.n = 0
        self.pre = pre
        self.tag = tag

    def sb(self, shape, dt, name=None):
        self.n += 1
        t = self.es.enter_context(self.nc.sbuf_tensor("%s%s_%d" % (self.tag, name or "t", self.n), list(shape), dt))
        esz = 4 if dt in (F32, I32) else 2
        nbytes = int(np.prod(shape[1:])) * esz
        alloc = (nbytes + 31) // 32 * 32
        if alloc % 64 != 0:
            self.n += 1
            self.es.enter_context(self.nc.sbuf_tensor("%spad_%d" % (self.tag, self.n), [128, 8], F32))
        return t

    def ps(self, shape, dt, name=None):
        self.n += 1
        return self.es.enter_context(self.nc.psum_tensor("%s%s_%d" % (self.tag, name or "p", self.n), list(shape), dt))

    def dram(self, name, shape, dt, kind):
        if self.pre is not None:
            h = self.pre[name]
            assert list(h.shape) == list(shape), (name, h.shape, shape)
            return h
        return self.nc.dram_tensor(name, list(shape), dt, kind=kind)


class Rot:
    def __init__(self, items):
        self.items = items
        self.i = 0

    def next(self):
        it = self.items[self.i % len(self.items)]
        self.i += 1
        return it


class Env:
    def __init__(self):
        self.nc = bass.Bass("TRN2", target_bir_lowering=False)
        self.es = ExitStack()
        self.P = Prog(self.nc, self.es)
        self.nstage = 0


def _begin(env, pre=None):
    if env is None:
        nc = bass.Bass("TRN2", target_bir_lowering=False)
        es = ExitStack()
        return nc, es, Ctx(nc, es), Prog(nc, es)
    env.nstage += 1
    es = ExitStack()
    return env.nc, es, Ctx(env.nc, es, pre=pre, tag="s%d_" % env.nstage), env.P


def run_spmd(nc, in_maps):
    res = run_bass_kernel_spmd(nc, in_maps, core_ids=list(range(NCORES)))
    return res.results


def ACT(P, out, in_, func, r, w, **kw):
    P.op("act", lambda e: e.activation(out=out, in_=in_, func=func, **kw), r, w)


def TS(P, eng, out, in0, s1, s2, op0, op1, r, w):
    if op1 is None:
        P.op(eng, lambda e: e.tensor_scalar(out=out, in0=in0, scalar1=s1, scalar2=None, op0=op0), r, w)
    else:
        P.op(eng, lambda e: e.tensor_scalar(out=out, in0=in0, scalar1=s1, scalar2=s2, op0=op0, op1=op1), r, w)


def TT(P, eng, out, in0, in1, op, r, w):
    P.op(eng, lambda e: e.tensor_tensor(out=out, in0=in0, in1=in1, op=op), r, w)


def STT(P, eng, out, in0, scalar, in1, op0, op1, r, w):
    P.op(eng, lambda e: e.scalar_tensor_tensor(out=out, in0=in0, scalar=scalar, in1=in1, op0=op0, op1=op1), r, w)


def CP(P, eng, out, in_, r, w):
    if eng == "act":
        P.op(eng, lambda e: e.copy(out=out, in_=in_), r, w)
    else:
        P.op(eng, lambda e: e.tensor_copy(out=out, in_=in_), r, w)


def RSUM(P, eng, out, in_, r, w, axis=None):
    ax = axis if axis is not None else AX.X
    P.op(eng, lambda e: e.reduce_sum(out=out, in_=in_, axis=ax), r, w)


def RMAX(P, eng, out, in_, r, w, axis=None):
    ax = axis if axis is not None else AX.X
    P.op(eng, lambda e: e.reduce_max(out=out, in_=in_, axis=ax), r, w)


def MM(P, out, lhsT, rhs, start, stop, r, w):
    P.op("pe", lambda e: e.matmul(out, lhsT, rhs, start=start, stop=stop), r, w)


def TR(P, out, in_, ident, r, w):
    P.op("pe", lambda e: e.transpose(out, in_, ident), r, w)


def DMA(P, eng, out, in_, r, w):
    P.dma(eng, lambda e: e.dma_start(out=out, in_=in_), r, w)


def RECIP(P, eng, out, in_, r, w):
    P.op(eng, lambda e: e.reciprocal(out=out, in_=in_), r, w)


def MEMSET(P, eng, ap, val, w):
    P.op(eng, lambda e: e.memset(ap, val), (), w)


TP = 1024
HALO = 128
NTP = TP + 2 * HALO
NPASS = TPC // TP


def build_stageA(phases=("fm", "tm", "conv", "mla"), env=None, pre=None):
    nc, es, C, P = _begin(env, pre)
    with es:
        xe = C.dram("xe", [TPC + 2 * HALO, D], F32, "ExternalInput")
        w_in = C.dram("w_in", [D, INW], F32, "ExternalInput")
        gmix = C.dram("gmix", [128, 8], F32, "ExternalInput")
        identb = C.dram("identb", [128, 128], BF16, "ExternalInput")
        convp = C.dram("convp", [128, 4, 34], F32, "ExternalInput")
        gcq = C.dram("gcq", [128, 3], F32, "ExternalInput")
        gckv = C.dram("gckv", [128, 2], F32, "ExternalInput")
        w_uq = C.dram("w_uq", [384, 768], F32, "ExternalInput")
        w_ukv = C.dram("w_ukv", [256, 1024], F32, "ExternalInput")
        gqk = C.dram("gqk", [96, 2], F32, "ExternalInput")
        ropeT = C.dram("ropeT", [96, 2, TPC], F32, "ExternalInput")
        rmat = C.dram("rmat", [96, 96], BF16, "ExternalInput")
        onesf = C.dram("onesf", [128, 128], F32, "ExternalInput")

        QT = C.dram("QT", [512, TPC], BF16, "ExternalOutput")
        KT = C.dram("KT", [512, TPC], BF16, "ExternalOutput")
        Kt = C.dram("Kt", [4, TPC, 128], BF16, "ExternalOutput")
        Vt = C.dram("Vt", [4, TPC, 128], BF16, "ExternalOutput")
        OG = C.dram("OG", [4, TPC, 128], BF16, "ExternalOutput")
        G4 = C.dram("G4", [4, TPC, 4], F32, "ExternalOutput")
        GTS = C.dram("GTS", [TPC, 3072], BF16, "ExternalOutput")
        UT = C.dram("UT", [512, TPC], BF16, "ExternalOutput")
        MQ = C.dram("MQ", [8, 96, TPC], BF16, "ExternalOutput")
        MK = C.dram("MK", [8, 96, TPC], BF16, "ExternalOutput")
        MV = C.dram("MV", [8, 128, TPC // 128, 64], BF16, "ExternalOutput")

        w_v = w_in.ap().rearrange("(c p) n -> p c n", p=128)
        xnT = C.sb([128, 8, NTP], BF16, "xnT")
        f32t = Rot([(("f32t", i), C.sb([128, 512], F32, "f32t")) for i in range(4)])
        uT = C.sb([128, 4, NTP], BF16, "uT")
        cacc = [C.sb([128, 512], F32, "cacc") for g in range(4)]
        csq = [C.sb([128, 512], F32, "csq") for g in range(4)]
        cpar = C.sb([128, 4, 34], F32, "cpar")
        rope_sb = C.sb([96, 2, 512], F32, "rope")
        cql = C.sb([128, 3, 512], F32, "cql")
        ckl = C.sb([128, 2, 512], F32, "ckl")
        latsq = C.sb([128, 3, 512], BF16, "latsq")
        cqn = C.sb([128, 3, 512], BF16, "cqn")
        ckn = C.sb([128, 2, 512], BF16, "ckn")
        krt = C.sb([128, 512], F32, "krt")
        hx = Rot([(("hx", i), C.sb([128, 512], F32, "hx")) for i in range(2)])
        hsq = Rot([(("hsq", i), C.sb([128, 512], BF16, "hsq")) for i in range(2)])
        hxg = Rot([(("hxg", i), C.sb([128, 512], BF16, "hxg")) for i in range(2)])
        wuqb = C.sb([128, 3, 768], BF16, "wuqb")
        wukvb = C.sb([128, 2, 1024], BF16, "wukvb")
        wkrp = C.sb([128, 8, 128], BF16, "wkrp")
        gqk_sb = C.sb([96, 2], F32, "gqk")
        rmat_sb = C.sb([96, 96], BF16, "rmat")
        gcq_sb = C.sb([128, 3], F32, "gcqs")
        gckv_sb = C.sb([128, 2], F32, "gckvs")
        ident = C.sb([128, 128], BF16, "ident")
        gm = C.sb([128, 8], F32, "gm")
        onesb = C.sb([128, 128], BF16, "onesb")
        ones32 = C.sb([128, 128], F32, "ones32")
        xin = Rot([(("xin", i), C.sb([128, D], F32, "xin")) for i in range(2)])
        sqj = C.sb([128, D], F32, "sqj")
        xs = Rot([(("xs", i), C.sb([128, D], BF16, "xs")) for i in range(2)])
        stat = Rot([(("stat", i), C.sb([128, 2], F32, "stat")) for i in range(2)])
        wst = Rot([(("wst", i), C.sb([128, 8, 512], F32, "wst")) for i in range(3)])
        wb = Rot([(("wb", i), C.sb([128, 8, 512], BF16, "wb")) for i in range(3)])
        ob = Rot([(("ob", i), C.sb([128, 512], BF16, "ob")) for i in range(4)])
        g4sb = C.sb([128, NTP // 128, 16], F32, "g4sb")
        pacc = Rot([(("pacc", i), C.ps([128, 512], F32, "pacc")) for i in range(5)])
        ptr = Rot([(("ptr", i), C.ps([128, 1024], BF16, "ptr")) for i in range(2)])

        DMA(P, "sp", cpar[:], convp[:, :, :], [], ["cpar"])
        DMA(P, "sp", gqk_sb[:], gqk[:, :], [], ["gqk"])
        DMA(P, "sp", rmat_sb[:], rmat[:, :], [], ["rmat"])
        DMA(P, "sp", gcq_sb[:], gcq[:, :], [], ["gcqs"])
        DMA(P, "sp", gckv_sb[:], gckv[:, :], [], ["gckvs"])
        DMA(P, "sp", gm[:], gmix[:, :], [], ["gm"])
        if "mla" in phases:
            sk0, st0 = wst.items[0]
            st0f = st0[:].rearrange("p a b -> p (a b)")
            DMA(P, "sp", st0f[:, 0:2304].rearrange("p (a b) -> p a b", a=3),
                w_uq.ap().rearrange("(c p) n -> p c n", p=128), [], [sk0])
            for j in range(3):
                TS(P, "dve", wuqb[:, j, :], st0f[:, j * 768:(j + 1) * 768],
                   gcq_sb[:, j:j + 1], None, ALU.mult, None, [sk0, "gcqs"], ["wuqb"])
            sk1, st1 = wst.items[1]
            st1f = st1[:].rearrange("p a b -> p (a b)")
            DMA(P, "sp", st1f[:, 0:2048].rearrange("p (a b) -> p a b", a=2),
                w_ukv.ap().rearrange("(c p) n -> p c n", p=128), [], [sk1])
            for j in range(2):
                src = st1f[:, j * 1024:(j + 1) * 1024].rearrange("p (h x) -> p h x", h=8)
                TS(P, "dve", wukvb[:, j, 0:512].rearrange("p (h x) -> p h x", h=8), src[:, :, 0:64],
                   gckv_sb[:, j:j + 1], None, ALU.mult, None, [sk1, "gckvs"], ["wukvb"])
                TS(P, "dve", wukvb[:, j, 512:1024].rearrange("p (h x) -> p h x", h=8), src[:, :, 64:128],
                   gckv_sb[:, j:j + 1], None, ALU.mult, None, [sk1, "gckvs"], ["wukvb"])
            MEMSET(P, "pool", wkrp[:], 0.0, ["wkrp"])
            sk2_, st2_ = wst.items[0]
            DMA(P, "sp", st2_[:, :, 0:32], w_v[:, :, O_CKR:O_CKR + 32], [], [sk2_])
            for k in range(8):
                TS(P, "dve", wkrp[:, k, 64:96], st2_[:, k, 0:32], gm[:, k:k + 1], None, ALU.mult, None,
                   [sk2_, "gm"], ["wkrp"])
            MEMSET(P, "pool", krt[:], 0.0, ["krt"])
        DMA(P, "sp", ident[:], identb[:, :], [], ["ident"])
        DMA(P, "sp", gm[:], gmix[:, :], [], ["gm"])
        DMA(P, "sp", ones32[:], onesf[:, :], [], ["ones32"])
        CP(P, "dve", onesb[:], ones32[:], ["ones32"], ["onesb"])

        evac_i = [0]
        oq_i = [0]

        def oq():
            oq_i[0] += 1
            return ("sp", "pool")[oq_i[0] % 2]

        def load_w_impl(c0, ncols):
            sk, st = wst.next()
            bk, bt = wb.next()
            DMA(P, "sp", st[:, :, 0:ncols], w_v[:, :, c0:c0 + ncols], [], [sk])
            for k in range(8):
                ACT(P, bt[:, k, 0:ncols], st[:, k, 0:ncols], AF.Copy, [sk, "gm"], [(bk, k)], scale=gm[:, k:k + 1])
            return [(bk, k) for k in range(8)], bt

        wlist = []
        for _ps in range(NPASS):
            if "fm" in phases:
                wlist += [(O_AQ, 512), (O_AK, 512)]
            if "tm" in phases:
                wlist += [(O_AK, 512), (O_AV, 512), (O_AO, 512)] + [(O_GTS + j * 512, 512) for j in range(6)] + [(O_AG, 16)]
            if "conv" in phases:
                wlist += [(O_GLU, 512), (O_GLU + 512, 512)]
            if "mla" in phases:
                wlist += [(O_CQ, 384), (O_CKV, 288)]
        issued = []

        def nextw(c0, ncols):
            if not issued:
                issued.append((wlist[0], load_w_impl(*wlist.pop(0))))
            req, cur = issued.pop(0)
            assert req == (c0, ncols), (req, c0, ncols)
            if wlist:
                issued.append((wlist[0], load_w_impl(*wlist.pop(0))))
            return cur

        for ps_i in range(NPASS):
            t0 = ps_i * TP
            for t in range(NTP // 128):
                xk, xt = xin.next()
                sk2, stt = stat.next()
                xsk, xst = xs.next()
                pk, pt = ptr.next()
                r0 = t0 + t * 128
                DMA(P, "sp", xt[:], xe[r0:r0 + 128, :], [], [xk])
                ACT(P, sqj[:], xt[:], AF.Square, [xk], ["sqj"])
                RSUM(P, "dve", stt[:, 0:1], sqj[:], ["sqj"], [sk2])
                ACT(P, stt[:, 1:2], stt[:, 0:1], AF.Sqrt, [sk2], [sk2], scale=1.0 / D, bias=EPS)
                RECIP(P, "dve", stt[:, 1:2], stt[:, 1:2], [sk2], [sk2])
                ACT(P, xst[:], xt[:], AF.Copy, [xk, sk2], [xsk], scale=stt[:, 1:2])
                for k in range(8):
                    TR(P, pt[:, k * 128:(k + 1) * 128], xst[:, k * 128:(k + 1) * 128], ident[:],
                       [xsk, "ident"], [pk])
                CP(P, ("dve", "pool")[0], xnT[:, :, t * 128:(t + 1) * 128],
                   pt[:].rearrange("p (k t) -> p k t", k=8), [pk], [("xnT", t)])

            def xk_keys(tok0, ntok):
                return [("xnT", t) for t in range(tok0 // 128, (tok0 + ntok - 1) // 128 + 1)]

            if "fm" in phases:
                for (c0, dst) in ((O_AQ, QT), (O_AK, KT)):
                    wk, wt = nextw(c0, 512)
                    for cb in range(4):
                        for tb in range(TP // 512):
                            tk0 = HALO + tb * 512
                            ak, at = pacc.next()
                            for k in range(8):
                                MM(P, at[:, :], wt[:, k, cb * 128:(cb + 1) * 128], xnT[:, k, tk0:tk0 + 512],
                                   k == 0, k == 7, [wk[k]] + xk_keys(tk0, 512), [ak])
                            okk, ot = ob.next()
                            evac_i[0] += 1
                            CP(P, ("act", "dve")[evac_i[0] % 2], ot[:, :], at[:, :], [ak], [okk])
                            DMA(P, oq(), dst[cb * 128:(cb + 1) * 128, t0 + tb * 512:t0 + (tb + 1) * 512], ot[:, :],
                                [okk], [])

            if "tm" in phases:
                blocks = [(O_AK, Kt, 0, "copy"), (O_AV, Vt, 0, "copy"), (O_AO, OG, 0, "sig")]
                for j in range(6):
                    blocks.append((O_GTS + j * 512, GTS, j * 512, "sig"))
                for (c0, dst, dc0, mode) in blocks:
                    wk, wt = nextw(c0, 512)
                    for t in range(TP // 128):
                        tk0 = HALO + t * 128
                        ak, at = pacc.next()
                        for k in range(8):
                            MM(P, at[:, :], xnT[:, k, tk0:tk0 + 128], wt[:, k, :], k == 0, k == 7,
                               [wk[k]] + xk_keys(tk0, 128), [ak])
                        okk, ot = ob.next()
                        if mode == "sig":
                            ACT(P, ot[:, :], at[:, :], AF.Sigmoid, [ak], [okk])
                        else:
                            evac_i[0] += 1
                            CP(P, ("act", "dve")[evac_i[0] % 2], ot[:, :], at[:, :], [ak], [okk])
                        if dst is GTS:
                            DMA(P, oq(), dst[t0 + t * 128:t0 + (t + 1) * 128, dc0:dc0 + 512], ot[:, :], [okk], [])
                        else:
                            DMA(P, oq(), dst[:, t0 + t * 128:t0 + (t + 1) * 128, :].rearrange("h t d -> t h d"),
                                ot[:, :].rearrange("p (h d) -> p h d", h=4), [okk], [])
                wk, wt = nextw(O_AG, 16)
                for t in range(TP // 128):
                    tk0 = HALO + t * 128
                    ak, at = pacc.next()
                    for k in range(8):
                        MM(P, at[:, 0:16], xnT[:, k, tk0:tk0 + 128], wt[:, k, 0:16], k == 0, k == 7,
                           [wk[k]] + xk_keys(tk0, 128), [ak])
                    CP(P, "dve", g4sb[:, t, :].rearrange("p (h g) -> p h g", h=4), at[:, 0:16].rearrange("p (g h) -> p h g", h=4),
                       [ak], [("g4", t)])
                for h in range(4):
                    DMA(P, "sp", G4[h, t0:t0 + TP, :].rearrange("(t p) g -> p t g", p=128), g4sb[:, 0:TP // 128, h * 4:(h + 1) * 4],
                        [("g4", t) for t in range(TP // 128)], [])

            if "conv" in phases:
                wak, wat = nextw(O_GLU, 512)
                wgk, wgt = nextw(O_GLU + 512, 512)
                blks = [(b0, min(512, NTP - b0)) for b0 in range(0, NTP, 512)]
                for g in range(4):
                    for (b0, bn) in blks:
                        ak, at = pacc.next()
                        gk, gt = pacc.next()
                        for k in range(8):
                            MM(P, at[:, 0:bn], wat[:, k, g * 128:(g + 1) * 128], xnT[:, k, b0:b0 + bn],
                               k == 0, k == 7, [wak[k]] + xk_keys(b0, bn), [ak])
                        for k in range(8):
                            MM(P, gt[:, 0:bn], wgt[:, k, g * 128:(g + 1) * 128], xnT[:, k, b0:b0 + bn],
                               k == 0, k == 7, [wgk[k]] + xk_keys(b0, bn), [gk])
                        fk, ft = f32t.next()
                        ACT(P, ft[:, 0:bn], gt[:, 0:bn], AF.Sigmoid, [gk], [fk])
                        TT(P, "dve", uT[:, g, b0:b0 + bn], at[:, 0:bn], ft[:, 0:bn], ALU.mult, [ak, fk],
                           [("uT", g, b0 // 512)])
                for tb in range(TP // 512):
                    c0 = HALO + tb * 512 - 15
                    ukeys = lambda g: [("uT", g, j) for j in range(c0 // 512, (c0 + 542 - 1) // 512 + 1)]
                    for g in range(4):
                        eng = "dve"
                        ck = ("cacc", g)
                        ca = cacc[g]
                        TS(P, eng, ca[:, :], uT[:, g, c0:c0 + 512], cpar[:, g, 0:1], cpar[:, g, 31:32],
                           ALU.mult, ALU.add, ukeys(g) + ["cpar"], [ck])
                        for k in range(1, 31):
                            STT(P, eng, ca[:, :], uT[:, g, c0 + k:c0 + k + 512], cpar[:, g, k:k + 1], ca[:, :],
                                ALU.mult, ALU.add, ukeys(g) + ["cpar", ck], [ck])
                    mk, mt = pacc.next()
                    for g in range(4):
                        MM(P, mt[:, :], ones32[:, :], cacc[g][:, :], g == 0, g == 3, ["ones32", ("cacc", g)], [mk])
                    for g in range(4):
                        STT(P, "dve", cacc[g][:, :], mt[:, :], -1.0 / 512, cacc[g][:, :], ALU.mult, ALU.add,
                            [mk, ("cacc", g)], [("cacc", g)])
                        ACT(P, csq[g][:, :], cacc[g][:, :], AF.Square, [("cacc", g)], [("csq", g)])
                    vk, vt = pacc.next()
                    for g in range(4):
                        MM(P, vt[:, :], ones32[:, :], csq[g][:, :], g == 0, g == 3, ["ones32", ("csq", g)], [vk])
                    fk, ft = f32t.next()
                    ACT(P, ft[:, :], vt[:, :], AF.Sqrt, [vk], [fk], scale=1.0 / 512, bias=EPS)
                    RECIP(P, "dve", ft[:, :], ft[:, :], [fk], [fk])
                    for g in range(4):
                        TT(P, "dve", csq[g][:, :], cacc[g][:, :], ft[:, :], ALU.mult, [("cacc", g), fk], [("csq", g)])
                        TS(P, "pool", csq[g][:, :], csq[g][:, :], cpar[:, g, 32:33], cpar[:, g, 33:34],
                           ALU.mult, ALU.add, [("csq", g), "cpar"], [("csq", g)])
                        okk, ot = ob.next()
                        ACT(P, ot[:, :], csq[g][:, :], AF.Silu, [("csq", g)], [okk])
                        DMA(P, "sp", UT[g * 128:(g + 1) * 128, t0 + tb * 512:t0 + (tb + 1) * 512], ot[:, :], [okk], [])

            if "mla" in phases:
                wqk, wqt = nextw(O_CQ, 384)
                wkk, wkt = nextw(O_CKV, 288)
                for tb in range(TP // 512):
                    tk0 = HALO + tb * 512
                    g0 = t0 + tb * 512
                    DMA(P, "sp", rope_sb[:, :, :], ropeT[:, :, g0:g0 + 512], [], ["rope"])
                    for (wk_, wt_, nblk, lat, latn, lkey, dim) in ((wqk, wqt, 3, cql, cqn, "cq", 384.0),
                                                                 (wkk, wkt, 2, ckl, ckn, "ckv", 256.0)):
                        for j in range(nblk):
                            ak, at = pacc.next()
                            for k in range(8):
                                MM(P, at[:, :], wt_[:, k, j * 128:(j + 1) * 128], xnT[:, k, tk0:tk0 + 512],
                                   k == 0, k == 7, [wk_[k]] + xk_keys(tk0, 512), [ak])
                            CP(P, "dve", lat[:, j, :], at[:, :], [ak], [(lkey, j)])
                            ACT(P, latsq[:, j, :], lat[:, j, :], AF.Square, [(lkey, j)], [(lkey + "sq", j)])
                        sk_, st_ = pacc.next()
                        for j in range(nblk):
                            MM(P, st_[:, :], onesb[:, :], latsq[:, j, :], j == 0, j == nblk - 1,
                               ["onesb", (lkey + "sq", j)], [sk_])
                        fk, ft = f32t.next()
                        ACT(P, ft[:, :], st_[:, :], AF.Sqrt, [sk_], [fk], scale=1.0 / dim, bias=EPS)
                        RECIP(P, "dve", ft[:, :], ft[:, :], [fk], [fk])
                        for j in range(nblk):
                            if "dbg5" in phases:
                                TT(P, "dve", lat[:, j, :], lat[:, j, :], ft[:, :], ALU.mult,
                                   [(lkey, j), fk], [(lkey, j)])
                                CP(P, "act", latn[:, j, :], lat[:, j, :], [(lkey, j)], [(lkey + "n", j)])
                            else:
                                TT(P, "dve", latn[:, j, :], lat[:, j, :], ft[:, :], ALU.mult,
                                   [(lkey, j), fk], [(lkey + "n", j)])
                    if "nokr" not in phases:
                        ak, at = pacc.next()
                        for k in range(8):
                            MM(P, at[:, :], wkrp[:, k, :], xnT[:, k, tk0:tk0 + 512], k == 0, k == 7,
                               ["wkrp"] + xk_keys(tk0, 512), [ak])
                        CP(P, "act", krt[0:96, :], at[0:96, :], [ak], ["krt"])
                    cqn_keys = [("cqn", j) for j in range(3)]
                    ckn_keys = [("ckvn", j) for j in range(2)]
                    for h in (range(8) if "nomlah" not in phases else []):
                        for which in ("q", "k"):
                            ak, at = pacc.next()
                            xk_, xt_ = hx.next()
                            if which == "q":
                                for j in range(3):
                                    MM(P, at[0:96, :], wuqb[:, j, h * 96:(h + 1) * 96], cqn[:, j, :], j == 0, j == 2,
                                       ["wuqb"] + cqn_keys, [ak])
                                CP(P, "act", xt_[0:96, :], at[0:96, :], [ak], [xk_])
                            else:
                                for j in range(2):
                                    MM(P, at[0:64, :], wukvb[:, j, h * 64:(h + 1) * 64], ckn[:, j, :], j == 0, j == 1,
                                       ["wukvb"] + ckn_keys, [ak])
                                CP(P, "act", xt_[0:64, :], at[0:64, :], [ak], [xk_])
                                CP(P, "pool", xt_[64:96, :], krt[64:96, :], ["krt"], [xk_])
                            sqk, sqt = hsq.next()
                            ACT(P, sqt[0:96, :], xt_[0:96, :], AF.Square, [xk_], [sqk])
                            sk_, st_ = pacc.next()
                            MM(P, st_[0:96, :], onesb[0:96, 0:96], sqt[0:96, :], True, True, ["onesb", sqk], [sk_])
                            fk, ft = f32t.next()
                            ACT(P, ft[0:96, :], st_[0:96, :], AF.Sqrt, [sk_], [fk], scale=1.0 / 96, bias=EPS)
                            RECIP(P, "dve", ft[0:96, :], ft[0:96, :], [fk], [fk])
                            gcol = 0 if which == "q" else 1
                            xgk, xgt = hxg.next()
                            TS(P, "dve", xgt[0:96, :], xt_[0:96, :], gqk_sb[0:96, gcol:gcol + 1], None, ALU.mult, None,
                               [xk_, "gqk"], [xgk])
                            rk, rt = pacc.next()
                            MM(P, rt[0:96, :], rmat_sb[0:96, 0:96], xgt[0:96, :], True, True, ["rmat", xgk], [rk])
                            t1k, t1 = f32t.next()
                            TT(P, "pool", t1[0:96, :], xgt[0:96, :], rope_sb[:, 0, :], ALU.mult, [xgk, "rope"], [t1k])
                            t2k, t2 = f32t.next()
                            TT(P, "dve", t2[0:96, :], rt[0:96, :], rope_sb[:, 1, :], ALU.mult, [rk, "rope"], [t2k])
                            TT(P, "pool", t1[0:96, :], t1[0:96, :], t2[0:96, :], ALU.add, [t1k, t2k], [t1k])
                            okk, ot = ob.next()
                            TT(P, "dve", ot[0:96, :], t1[0:96, :], ft[0:96, :], ALU.mult, [t1k, fk], [okk])
                            dst = MQ if which == "q" else MK
                            DMA(P, "sp", dst[h, :, g0:g0 + 512], ot[0:96, :], [okk], [])
                    if "dupgrp" in phases:
                        for rep in range(2):
                            ak, at = pacc.next()
                            for k in range(8):
                                MM(P, at[:, :], wkt[:, k, 0:128], xnT[:, k, tk0:tk0 + 512],
                                   k == 0, k == 7, [wkk[k]] + xk_keys(tk0, 512), [ak])
                    for t in (range(4 if "v_one" not in phases else 1) if "nomlav" not in phases else []):
                        ak, at = pacc.next()
                        for j in range(2):
                            MM(P, at[:, :], (xnT[:, j, tk0 + t * 128:tk0 + (t + 1) * 128] if "dbg1" in phases else ckn[:, j, t * 128:(t + 1) * 128]),
                               (wkt[:, j, :] if "dbg2" in phases else (wukvb[:, j, 0:512] if "dbg4" in phases else wukvb[:, j, 512:1024])), j == 0, j == 1,
                               ["wukvb"] + ckn_keys, [ak])
                        okk, ot = ob.next()
                        if "v_noevac" in phases:
                            continue
                        CP(P, "dve", ot[:, :], at[:, :], [ak], [okk])
                        if "v_nodma" in phases:
                            continue
                        DMA(P, "sp", MV[:, :, (g0 + t * 128) // 128, :].rearrange("h p d -> p h d"),
                            ot[:, :].rearrange("p (h d) -> p h d", h=8), [okk], [])

        P.emit()
        stats = P.stats
    return nc, stats


def _gain_cols(g, nch):
    return np.ascontiguousarray(g.reshape(nch, 128).T)


def rope_tables():
    pos = np.arange(S, dtype=np.float32)
    inv = (10000.0 ** (-np.arange(0, 32, 2, dtype=np.float32) / np.float32(32))).astype(np.float32)
    ang = (pos[:, None] * inv[None, :]).astype(np.float32)
    c = np.cos(ang.astype(np.float64)).astype(np.float32)
    s = np.sin(ang.astype(np.float64)).astype(np.float32)
    CT = np.ones((96, S), np.float32)
    ST = np.zeros((96, S), np.float32)
    CT[64:80] = c.T
    CT[80:96] = c.T
    ST[64:80] = s.T
    ST[80:96] = s.T
    return CT, ST


def consts():
    R = np.zeros((96, 96), np.float32)
    for j in range(16):
        R[80 + j, 64 + j] = -1.0
        R[64 + j, 80 + j] = 1.0
    return dict(identb=np.eye(128, dtype=np.float32).astype(NPBF), rmat=R.astype(NPBF),
                onesf=np.ones((128, 128), np.float32))


def stageA_inmaps(x, prm, l):
    CT, ST = rope_tables()
    cst = consts()
    convp = np.zeros((128, 4, 34), np.float32)
    cw = prm["conv_w"][l]
    convp[:, :, 0:31] = cw.T.reshape(4, 128, 31).transpose(1, 0, 2)
    convp[:, :, 31] = prm["conv_b"][l].reshape(4, 128).T
    convp[:, :, 32] = prm["conv_ln_g"][l].reshape(4, 128).T
    convp[:, :, 33] = prm["conv_ln_b"][l].reshape(4, 128).T
    maps = []
    for c in range(NCORES):
        b, q = c // 4, c % 4
        s0 = q * TPC
        xe = np.zeros((TPC + 2 * HALO, D), np.float32)
        lo, hi = max(0, s0 - HALO), min(S, s0 + TPC + HALO)
        xe[lo - (s0 - HALO):hi - (s0 - HALO)] = x[b, lo:hi]
        rope = np.stack([CT[:, s0:s0 + TPC], ST[:, s0:s0 + TPC]], axis=1)
        maps.append(dict(
            xe=xe, w_in=prm["w_in"][l], gmix=_gain_cols(prm["mix_norm_g"][l], 8),
            identb=cst["identb"], convp=convp, gcq=_gain_cols(prm["cq_norm_g"][l], 3),
            gckv=_gain_cols(prm["ckv_norm_g"][l], 2), w_uq=prm["w_uq"][l], w_ukv=prm["w_ukv"][l],
            gqk=np.ascontiguousarray(np.stack([prm["q_norm_g"][l], prm["k_norm_g"][l]], axis=1)),
            ropeT=np.ascontiguousarray(rope), rmat=cst["rmat"], onesf=cst["onesf"]))
    return maps


def build_mlstm(nch=S // 128, env=None, pre=None):
    nc, es, C, P = _begin(env, pre)
    ns = nch * 128
    lnscale = math.log(128.0 ** -0.5)
    with es:
        qTd = C.dram("qT", [128, ns], BF16, "ExternalInput")
        ktd = C.dram("kt", [ns, 128], BF16, "ExternalInput")
        vtd = C.dram("vt", [ns, 128], BF16, "ExternalInput")
        g4d = C.dram("g4", [ns, 4], F32, "ExternalInput")
        bifd = C.dram("bif", [128, 4], F32, "ExternalInput")
        ogd = C.dram("og", [ns, 128], BF16, "ExternalInput")
        gAd = C.dram("gA", [128, 128], F32, "ExternalInput")
        cmat = C.dram("cmat", [128, 6, 128], F32, "ExternalInput")
        identbd = C.dram("identb", [128, 128], BF16, "ExternalInput")
        HT = C.dram("HT", [128, ns], BF16, "ExternalOutput")

        qT = C.sb([128, ns], BF16, "qT")
        kt = C.sb([128, nch, 128], BF16, "kt")
        vt = C.sb([128, nch, 129], BF16, "vt")
        hacc = C.sb([128, nch, 128], F32, "hacc")
        g4 = C.sb([128, nch, 4], F32, "g4")
        bif = C.sb([128, 4], F32, "bif")
        nbif = C.sb([128, 4], F32, "nbif")
        gA = C.sb([128, 128], F32, "gA")
        cm = C.sb([128, 6, 128], F32, "cm")
        identb = C.sb([128, 128], BF16, "identb")
        gt = {n: C.sb([128, nch], F32, n) for n in ("lf", "ib", "bcum", "gtot", "biasS", "wint", "wk", "dec", "tmp")}
        Cf = C.sb([128, 129], F32, "Cf")
        Cb = C.sb([128, 129], BF16, "Cb")
        LF = Rot([(("LF", i), C.sb([128, 128], F32, "LF")) for i in range(2)])
        Dm = Rot([(("Dm", i), C.sb([128, 128], F32, "Dm")) for i in range(2)])
        kTc = Rot([(("kTc", i), C.sb([128, 128], BF16, "kTc")) for i in range(2)])
        SD = Rot([(("SD", i), C.sb([128, 128], BF16, "SD")) for i in range(2)])
        isb = Rot([(("isb", i), C.sb([128, 129], F32, "isb")) for i in range(2)])
        num = Rot([(("num", i), C.sb([128, 129], F32, "num")) for i in range(2)])
        dn = Rot([(("dn", i), C.sb([128, 2], F32, "dn")) for i in range(2)])
        Vw = Rot([(("Vw", i), C.sb([128, 129], BF16, "Vw")) for i in range(2)])
        ogt = Rot([(("ogt", i), C.sb([128, 128], BF16, "ogt")) for i in range(2)])
        hn = Rot([(("hn", i), C.sb([128, 128], F32, "hn")) for i in range(2)])
        hb = Rot([(("hb", i), C.sb([128, 128], BF16, "hb")) for i in range(2)])
        hT = Rot([(("hT", i), C.sb([128, 128], BF16, "hT")) for i in range(2)])
        sq = C.sb([128, 128], F32, "sq")
        pA = Rot([(("pA", i), C.ps([128, 512], F32, "pA")) for i in range(6)])
        pB = Rot([(("pB", i), C.ps([128, 1024], BF16, "pB")) for i in range(2)])

        DMA(P, "sp", qT[:, :], qTd[:, :], [], ["qT"])
        DMA(P, "sp", kt[:, :, :], ktd.ap().rearrange("(c p) d -> p c d", p=128), [], ["kt"])
        DMA(P, "sp", vt[:, :, 0:128], vtd.ap().rearrange("(c p) d -> p c d", p=128), [], ["vt"])
        MEMSET(P, "pool", vt[:, :, 128:129], 1.0, ["vt1"])
        DMA(P, "sp", g4[:, :, :], g4d.ap().rearrange("(c p) g -> p c g", p=128), [], ["g4"])
        DMA(P, "sp", bif[:, :], bifd[:, :], [], ["bif"])
        DMA(P, "sp", gA[:, :], gAd[:, :], [], ["gA"])
        DMA(P, "sp", cm[:, :, :], cmat[:, :, :], [], ["cm"])
        DMA(P, "sp", identb[:, :], identbd[:, :], [], ["identb"])
        TS(P, "dve", nbif[:, :], bif[:, :], -1.0, None, ALU.mult, None, ["bif"], ["nbif"])
        ident32 = cm[:, 4, :]
        ones32 = cm[:, 5, :]
        for d in range(2):
            Ud = cm[:, d, :]
            NEGd = cm[:, 2 + d, :]
            ic, fc = 2 * d, 2 * d + 1
            ACT(P, gt["tmp"][:, :], g4[:, :, fc], AF.Exp, ["g4", "nbif"], ["tmp"], scale=-1.0, bias=nbif[:, fc:fc + 1])
            ACT(P, gt["tmp"][:, :], gt["tmp"][:, :], AF.Ln, ["tmp"], ["tmp"], bias=1.0)
            TS(P, "dve", gt["lf"][:, :], gt["tmp"][:, :], -1.0, None, ALU.mult, None, ["tmp"], ["lf"])
            TS(P, "dve", gt["ib"][:, :], g4[:, :, ic], bif[:, ic:ic + 1], lnscale, ALU.add, ALU.add, ["g4", "bif"], ["ib"])
            bk, bp = pA.next()
            MM(P, bp[:, 0:nch], Ud, gt["lf"][:, :], True, True, ["cm", "lf"], [bk])
            CP(P, "dve", gt["bcum"][:, :], bp[:, 0:nch], [bk], ["bcum"])
            gk, gp = pA.next()
            MM(P, gp[:, 0:nch], ones32, gt["lf"][:, :], True, True, ["cm", "lf"], [gk])
            CP(P, "dve", gt["gtot"][:, :], gp[:, 0:nch], [gk], ["gtot"])
            TT(P, "dve", gt["biasS"][:, :], gt["ib"][:, :], gt["bcum"][:, :], ALU.subtract, ["ib", "bcum"], ["biasS"])
            ACT(P, gt["wint"][:, :], gt["bcum"][:, :], AF.Exp, ["bcum"], ["wint"])
            TT(P, "dve", gt["tmp"][:, :], gt["biasS"][:, :], gt["gtot"][:, :], ALU.add, ["biasS", "gtot"], ["tmp"])
            ACT(P, gt["wk"][:, :], gt["tmp"][:, :], AF.Exp, ["tmp"], ["wk"])
            ACT(P, gt["dec"][:, :], gt["gtot"][:, :], AF.Exp, ["gtot"], ["dec"])
            MEMSET(P, "dve", Cf[:, :], 0.0, ["Cf"])
            MEMSET(P, "pool", Cb[:, :], 0.0, ["Cb"])
            order = range(nch) if d == 0 else range(nch - 1, -1, -1)
            for c in order:
                lk, lt = LF.next()
                ACT(P, lt[:, :], ones32, AF.Copy, ["cm", "lf"], [lk], scale=gt["lf"][:, c:c + 1])
                dk, dp = pA.next()
                MM(P, dp[:, 0:128], lt[:, :], Ud, True, False, [lk, "cm"], [dk])
                MM(P, dp[:, 0:128], ident32, NEGd, False, True, ["cm"], [dk])
                mk_, mt_ = Dm.next()
                ACT(P, mt_[:, :], dp[:, 0:128], AF.Exp, [dk, "biasS"], [mk_], bias=gt["biasS"][:, c:c + 1])
                tk, tp = pB.next()
                TR(P, tp[:, 0:128], kt[:, c, :], identb[:, :], ["kt", "identb"], [tk])
                kck, kct = kTc.next()
                CP(P, "dve", kct[:, :], tp[:, 0:128], [tk], [kck])
                sk, sp_ = pA.next()
                MM(P, sp_[:, 0:128], kct[:, :], qT[:, c * 128:(c + 1) * 128], True, True, [kck, "qT"], [sk])
                sdk, sdt = SD.next()
                TT(P, "dve", sdt[:, :], sp_[:, 0:128], mt_[:, :], ALU.mult, [sk, mk_], [sdk])
                nk_, np_ = pA.next()
                MM(P, np_[:, 0:129], sdt[:, :], vt[:, c, :], True, True, [sdk, "vt", "vt1"], [nk_])
                ik, ip = pA.next()
                MM(P, ip[:, 0:129], qT[:, c * 128:(c + 1) * 128], Cb[:, :], True, True, ["qT", "Cb"], [ik])
                isk, ist = isb.next()
                ACT(P, ist[:, :], ip[:, 0:129], AF.Copy, [ik, "wint"], [isk], scale=gt["wint"][:, c:c + 1])
                nmk, nmt = num.next()
                TT(P, "dve", nmt[:, :], np_[:, 0:129], ist[:, :], ALU.add, [nk_, isk], [nmk])
                dnk, dnt = dn.next()
                ACT(P, dnt[:, 0:1], nmt[:, 128:129], AF.Abs, [nmk], [dnk])
                TS(P, "dve", dnt[:, 0:1], dnt[:, 0:1], 1.0, None, ALU.max, None, [dnk], [dnk])
                RECIP(P, "dve", dnt[:, 1:2], dnt[:, 0:1], [dnk], [dnk])
                if d == 0:
                    TS(P, "dve", hacc[:, c, :], nmt[:, 0:128], dnt[:, 1:2], None, ALU.mult, None, [nmk, dnk], [("hacc", c)])
                else:
                    STT(P, "dve", hacc[:, c, :], nmt[:, 0:128], dnt[:, 1:2], hacc[:, c, :], ALU.mult, ALU.add,
                        [nmk, dnk, ("hacc", c)], [("hacc", c)])
                vwk, vwt = Vw.next()
                TS(P, "pool", vwt[:, :], vt[:, c, :], gt["wk"][:, c:c + 1], None, ALU.mult, None, ["vt", "vt1", "wk"], [vwk])
                ck, cp_ = pA.next()
                MM(P, cp_[:, 0:129], kt[:, c, :], vwt[:, :], True, True, ["kt", vwk], [ck])
                STT(P, "dve", Cf[:, :], Cf[:, :], gt["dec"][:, c:c + 1], cp_[:, 0:129], ALU.mult, ALU.add,
                    ["Cf", "dec", ck], ["Cf"])
                CP(P, "act", Cb[:, :], Cf[:, :], ["Cf"], ["Cb"])
        for c in range(nch):
            ogk, ogt_ = ogt.next()
            DMA(P, "sp", ogt_[:, :], ogd[c * 128:(c + 1) * 128, :], [], [ogk])
            dnk, dnt = dn.next()
            ACT(P, sq[:, :], hacc[:, c, :], AF.Square, [("hacc", c)], ["sq"])
            RSUM(P, "dve", dnt[:, 0:1], sq[:, :], ["sq"], [dnk])
            ACT(P, dnt[:, 1:2], dnt[:, 0:1], AF.Sqrt, [dnk], [dnk], scale=1.0 / 128, bias=EPS)
            RECIP(P, "dve", dnt[:, 1:2], dnt[:, 1:2], [dnk], [dnk])
            hk, ht = hn.next()
            STT(P, "dve", ht[:, :], hacc[:, c, :], dnt[:, 1:2], gA[:, :], ALU.mult, ALU.mult, [("hacc", c), dnk, "gA"], [hk])
            hbk, hbt = hb.next()
            TT(P, "pool", hbt[:, :], ht[:, :], ogt_[:, :], ALU.mult, [hk, ogk], [hbk])
            tk, tp = pB.next()
            TR(P, tp[:, 0:128], hbt[:, :], identb[:, :], [hbk, "identb"], [tk])
            htk, htt = hT.next()
            CP(P, "act", htt[:, :], tp[:, 0:128], [tk], [htk])
            DMA(P, "sp", HT[:, c * 128:(c + 1) * 128], htt[:, :], [htk], [])
        P.emit()
        stats = P.stats
    return nc, stats


def mlstm_consts():
    s_ = np.arange(128)[:, None]
    t_ = np.arange(128)[None, :]
    U = (s_ <= t_).astype(np.float32)
    cm = np.zeros((128, 6, 128), np.float32)
    cm[:, 0] = U
    cm[:, 1] = U.T
    cm[:, 2] = np.where(s_ <= t_, 0.0, -30000.0)
    cm[:, 3] = np.where(s_ >= t_, 0.0, -30000.0)
    cm[:, 4] = np.eye(128)
    cm[:, 5] = 1.0
    return dict(cmat=cm, identb=np.eye(128, dtype=np.float32).astype(NPBF))


def build_merge(ntile=TPC // 128, env=None, pre=None):
    nc, es, C, P = _begin(env, pre)
    nt = ntile * 128
    with es:
        xd = C.dram("x", [nt, D], F32, "ExternalInput")
        srcs = [C.dram(n, [512, nt], BF16, "ExternalInput") for n in ("HT", "UT", "OT")]
        gtsd = C.dram("GTS", [nt, 3072], BF16, "ExternalInput")
        wds = [C.dram(n, [512, D], F32, "ExternalInput") for n in ("w_a", "w_b", "w_c")]
        wod = C.dram("w_o", [D, D], F32, "ExternalInput")
        gfd = C.dram("gffn", [128, 8], F32, "ExternalInput")
        wrd = C.dram("w_r", [D, 16], F32, "ExternalInput")
        identbd = C.dram("identb", [128, 128], BF16, "ExternalInput")
        ident32d = C.dram("ident32", [128, 128], F32, "ExternalInput")
        x1d = C.dram("x1", [nt, D], F32, "ExternalOutput")
        xn2d = C.dram("xn2T", [D, nt], BF16, "ExternalOutput")
        affd = C.dram("aff", [nt, 16], F32, "ExternalOutput")
        affTd = C.dram("affT", [16, nt], F32, "ExternalOutput")
        affTs = C.sb([16, nt], F32, "affTs")

        wbr = [C.sb([128, 4, D], BF16, "wbr") for _ in range(3)]
        wo = C.sb([128, 8, D], BF16, "wo")
        wst = Rot([(("wst", i), C.sb([128, 4, D], F32, "wst")) for i in range(2)])
        wr = C.sb([128, 8, 16], F32, "wr")
        gf = C.sb([128, 8], F32, "gf")
        gfull = C.sb([128, 8, 128], F32, "gfull")
        identb = C.sb([128, 128], BF16, "identb")
        ident32 = C.sb([128, 128], F32, "ident32")
        srct = [Rot([((("src", b), i), C.sb([128, 4, 128], BF16, "src")) for i in range(2)]) for b in range(3)]
        gts = Rot([(("gts", i), C.sb([128, 3072], BF16, "gts")) for i in range(2)])
        xt = Rot([(("xt", i), C.sb([128, D], F32, "xt")) for i in range(2)])
        mg = C.sb([128, D], F32, "mg")
        tmpm = Rot([(("tmpm", i), C.sb([128, 512], F32, "tmpm")) for i in range(2)])
        mgb = C.sb([128, D], BF16, "mgb")
        mT = C.sb([128, 8, 128], BF16, "mT")
        x1 = Rot([(("x1", i), C.sb([128, D], F32, "x1")) for i in range(2)])
        sqj = C.sb([128, D], F32, "sqj")
        xs = C.sb([128, D], F32, "xs")
        st = Rot([(("st", i), C.sb([128, 16], F32, "st")) for i in range(2)])
        xT32 = C.sb([128, 8, 128], F32, "xT32")
        xTb = Rot([(("xTb", i), C.sb([128, 8, 128], BF16, "xTb")) for i in range(2)])
        lg = C.sb([128, 16], F32, "lg")
        ex = C.sb([128, 16], F32, "ex")
        affs = C.sb([128, ntile, 16], F32, "affs")
        pA = Rot([(("pA", i), C.ps([128, 512], F32, "pA")) for i in range(5)])
        pB = C.ps([128, 1024], BF16, "pB")

        DMA(P, "sp", identb[:, :], identbd[:, :], [], ["identb"])
        DMA(P, "sp", ident32[:, :], ident32d[:, :], [], ["ident32"])
        DMA(P, "sp", gf[:, :], gfd[:, :], [], ["gf"])
        DMA(P, "sp", wr[:, :, :], wrd.ap().rearrange("(c p) e -> p c e", p=128), [], ["wr"])
        for b in range(3):
            sk, stg = wst.next()
            DMA(P, "sp", stg[:, :, :], wds[b].ap().rearrange("(c p) n -> p c n", p=128), [], [sk])
            for c in range(4):
                CP(P, ("dve", "pool")[c % 2], wbr[b][:, c, :], stg[:, c, :], [sk], [("wbr", b)])
        for hh in range(2):
            sk, stg = wst.next()
            DMA(P, "sp", stg[:, :, :], wod.ap().rearrange("(c p) n -> p c n", p=128)[:, hh * 4:(hh + 1) * 4, :], [], [sk])
            for c in range(4):
                CP(P, ("dve", "pool")[c % 2], wo[:, hh * 4 + c, :], stg[:, c, :], [sk], ["wo"])
        for k in range(8):
            TS(P, "pool", gfull[:, k, :], ident32[:, :], 0.0, gf[:, k:k + 1], ALU.mult, ALU.add, ["ident32", "gf"], ["gfull"])
        for t in range(ntile):
            r0 = t * 128
            xk, xt_ = xt.next()
            DMA(P, "sp", xt_[:, :], xd[r0:r0 + 128, :], [], [xk])
            gk, gt_ = gts.next()
            DMA(P, "sp", gt_[:, :], gtsd[r0:r0 + 128, :], [], [gk])
            skeys = []
            stiles = []
            for b in range(3):
                k_, t_ = srct[b].next()
                DMA(P, "sp", t_[:, :, :], srcs[b].ap().rearrange("(c p) t -> p c t", p=128)[:, :, r0:r0 + 128], [], [k_])
                skeys.append(k_)
                stiles.append(t_)
            for b in range(3):
                for hf in range(2):
                    ak, at = pA.next()
                    for c in range(4):
                        MM(P, at[:, :], stiles[b][:, c, :], wbr[b][:, c, hf * 512:(hf + 1) * 512], c == 0, c == 3,
                           [skeys[b], ("wbr", b)], [ak])
                    gsl = gt_[:, b * 1024 + hf * 512:b * 1024 + (hf + 1) * 512]
                    if b == 0:
                        TT(P, "dve", mg[:, hf * 512:(hf + 1) * 512], at[:, :], gsl, ALU.mult, [ak, gk], [("mg", hf)])
                    else:
                        tk, tt_ = tmpm.next()
                        TT(P, "dve", tt_[:, :], at[:, :], gsl, ALU.mult, [ak, gk], [tk])
                        TT(P, "pool", mg[:, hf * 512:(hf + 1) * 512], mg[:, hf * 512:(hf + 1) * 512], tt_[:, :], ALU.add,
                           [("mg", hf), tk], [("mg", hf)])
            CP(P, "act", mgb[:, :], mg[:, :], [("mg", 0), ("mg", 1)], ["mgb"])
            for k in range(8):
                TR(P, pB[:, k * 128:(k + 1) * 128], mgb[:, k * 128:(k + 1) * 128], identb[:, :], ["mgb", "identb"], ["pB"])
            CP(P, "dve", mT[:, :, :], pB[:].rearrange("p (k t) -> p k t", k=8), ["pB"], ["mT"])
            x1k, x1t = x1.next()
            for hf in range(2):
                ak, at = pA.next()
                for k in range(8):
                    MM(P, at[:, :], mT[:, k, :], wo[:, k, hf * 512:(hf + 1) * 512], k == 0, k == 7, ["mT", "wo"], [ak])
                TT(P, "dve", x1t[:, hf * 512:(hf + 1) * 512], at[:, :], xt_[:, hf * 512:(hf + 1) * 512], ALU.add,
                   [ak, xk], [(x1k, hf)])
            DMA(P, "sp", x1d[r0:r0 + 128, :], x1t[:, :], [(x1k, 0), (x1k, 1)], [])
            sk_, st_ = st.next()
            ACT(P, sqj[:, :], x1t[:, :], AF.Square, [(x1k, 0), (x1k, 1)], ["sqj"])
            RSUM(P, "dve", st_[:, 0:1], sqj[:, :], ["sqj"], [sk_])
            ACT(P, st_[:, 1:2], st_[:, 0:1], AF.Sqrt, [sk_], [sk_], scale=1.0 / D, bias=EPS)
            RECIP(P, "dve", st_[:, 1:2], st_[:, 1:2], [sk_], [sk_])
            ACT(P, xs[:, :], x1t[:, :], AF.Copy, [(x1k, 0), (x1k, 1), sk_], ["xs"], scale=st_[:, 1:2])
            for hf in range(2):
                ak, at = pA.next()
                for k in range(4):
                    kk = hf * 4 + k
                    TR(P, at[:, k * 128:(k + 1) * 128], xs[:, kk * 128:(kk + 1) * 128], ident32[:, :], ["xs", "ident32"], [ak])
                TT(P, "dve", xT32[:, hf * 4:(hf + 1) * 4, :], at[:].rearrange("p (k t) -> p k t", k=4),
                   gfull[:, hf * 4:(hf + 1) * 4, :], ALU.mult, [ak, "gfull"], [("xT32", hf)])
            xbk, xbt = xTb.next()
            CP(P, "act", xbt[:, :, :], xT32[:, :, :], [("xT32", 0), ("xT32", 1)], [xbk])
            DMA(P, "sp", xn2d.ap().rearrange("(k p) t -> p k t", p=128)[:, :, r0:r0 + 128], xbt[:, :, :], [xbk], [])
            ak, at = pA.next()
            for k in range(8):
                MM(P, at[:, 0:16], xT32[:, k, :], wr[:, k, :], k == 0, k == 7, [("xT32", 0), ("xT32", 1), "wr"], [ak])
            CP(P, "dve", lg[:, :], at[:, 0:16], [ak], ["lg"])
            RMAX(P, "dve", st_[:, 2:3], lg[:, :], ["lg"], [sk_])
            TS(P, "dve", st_[:, 2:3], st_[:, 2:3], -1.0, None, ALU.mult, None, [sk_], [sk_])
            ACT(P, ex[:, :], lg[:, :], AF.Exp, ["lg", sk_], ["ex"], bias=st_[:, 2:3])
            RSUM(P, "dve", st_[:, 3:4], ex[:, :], ["ex"], [sk_])
            RECIP(P, "dve", st_[:, 3:4], st_[:, 3:4], [sk_], [sk_])
            TS(P, "dve", affs[:, t, :], ex[:, :], st_[:, 3:4], None, ALU.mult, None, ["ex", sk_], [("affs", t)])
            ak, at = pA.next()
            TR(P, at[0:16, 0:128], affs[:, t, :], ident32[:, :], [("affs", t), "ident32"], [ak])
            CP(P, "act", affTs[:, r0:r0 + 128], at[0:16, 0:128], [ak], [("affT", t)])
        DMA(P, "sp", affd.ap().rearrange("(t p) e -> p t e", p=128), affs[:, :, :], [("affs", t) for t in range(ntile)], [])
        DMA(P, "sp", affTd[:, :], affTs[:, :], [("affT", t) for t in range(ntile)], [])
        P.emit()
        stats = P.stats
    return nc, stats


def build_thr(ns=S, cap=2 * S // 16, iters=30, env=None, pre=None):
    nc, es, C, P = _begin(env, pre)
    with es:
        affT = C.dram("affT", [16, ns], F32, "ExternalInput")
        thr = C.dram("thr", [16, 2], F32, "ExternalOutput")
        a = C.sb([16, ns], F32, "a")
        junk = C.sb([16, ns], F32, "junk")
        lh = C.sb([16, 2], F32, "lh")
        w = C.sb([16, 8], F32, "w")
        DMA(P, "sp", a[:, :], affT[:, :], [], ["a"])
        MEMSET(P, "dve", lh[:, 0:1], 0.0, ["lh"])
        MEMSET(P, "dve", lh[:, 1:2], 1.0, ["lh"])
        for it in range(iters):
            TT(P, "dve", w[:, 0:1], lh[:, 0:1], lh[:, 1:2], ALU.add, ["lh"], ["w"])
            TS(P, "dve", w[:, 0:1], w[:, 0:1], 0.5, None, ALU.mult, None, ["w"], ["w"])
            P.op("dve", lambda e: e.tensor_scalar(out=junk[:, :], in0=a[:, :], scalar1=w[:, 0:1], scalar2=0.0,
                                                  op0=ALU.is_ge, op1=ALU.add, accum_out=w[:, 1:2]),
                 ["a", "w"], ["junk", "w"])
            TS(P, "dve", w[:, 2:3], w[:, 1:2], float(cap), None, ALU.is_ge, None, ["w"], ["w"])
            TT(P, "dve", w[:, 3:4], w[:, 0:1], lh[:, 0:1], ALU.subtract, ["w", "lh"], ["w"])
            TT(P, "dve", w[:, 4:5], lh[:, 1:2], w[:, 0:1], ALU.subtract, ["w", "lh"], ["w"])
            STT(P, "dve", lh[:, 0:1], w[:, 3:4], w[:, 2:3], lh[:, 0:1], ALU.mult, ALU.add, ["w", "lh"], ["lh"])
            STT(P, "dve", lh[:, 1:2], w[:, 4:5], w[:, 2:3], w[:, 0:1], ALU.mult, ALU.add, ["w", "lh"], ["lh"])
        DMA(P, "sp", thr[:, :], lh[:, :], ["lh"], [])
        P.emit()
        stats = P.stats
    return nc, stats


def build_ffn(nt=TPC, nexp=16, tb=1024, env=None, pre=None):
    nc, es, C, P = _begin(env, pre)
    FF = 1536
    ntile = nt // 128
    nblk = nt // tb
    with es:
        x1d = C.dram("x1", [nt, D], F32, "ExternalInput")
        xnd = C.dram("xn2T", [D, nt], BF16, "ExternalInput")
        affd = C.dram("aff", [nt, 16], F32, "ExternalInput")
        thrd = C.dram("thr_row", [128, 16], F32, "ExternalInput") if not (pre is not None and "thr16" in pre) else None
        wgd = C.dram("wg", [nexp, D, FF], F32, "ExternalInput")
        wud = C.dram("wu", [nexp, D, FF], F32, "ExternalInput")
        wdd = C.dram("wd", [nexp, FF, D], F32, "ExternalInput")
        x2d = C.dram("x2", [nt, D], F32, "ExternalOutput")

        xb = C.sb([128, 8, tb], BF16, "xb")
        acc = C.sb([128, tb // 128, D], F32, "acc")
        wgb = C.sb([128, 8, FF], BF16, "wgb")
        wub = C.sb([128, 8, FF], BF16, "wub")
        wdb = C.sb([128, 12, D], BF16, "wdb")
        stg = Rot([(("stg", i), C.sb([128, 4096], F32, "stg")) for i in range(2)])
        hT = C.sb([128, 12, tb], BF16, "hT")
        sg = Rot([(("sg", i), C.sb([128, 512], F32, "sg")) for i in range(2)])
        affs = C.sb([128, ntile, 16], F32, "affs")
        gw = C.sb([128, ntile, 16], F32, "gw")
        thr = C.sb([128, 16], F32, "thr")
        xo = Rot([(("xo", i), C.sb([128, D], F32, "xo")) for i in range(2)])
        pA = Rot([(("pA", i), C.ps([128, 512], F32, "pA")) for i in range(7)])

        if pre is not None and "thr16" in pre:
            t16d = pre["thr16"]
            i32d = pre["ident32"]
            t16 = C.sb([16, 2], F32, "t16")
            tbc = C.sb([16, 128], F32, "tbc")
            i16 = C.sb([16, 16], F32, "i16")
            DMA(P, "sp", t16[:, :], t16d[:, :], [], ["t16"])
            DMA(P, "sp", i16[:, :], i32d[0:16, 0:16], [], ["i16"])
            MEMSET(P, "dve", tbc[:, :], 1.0, ["tbc"])
            TS(P, "dve", tbc[:, :], tbc[:, :], t16[:, 0:1], None, ALU.mult, None, ["tbc", "t16"], ["tbc"])
            tk_, tp_ = pA.next()
            MM(P, tp_[:, 0:16], tbc[:, :], i16[:, :], True, True, ["tbc", "i16"], [tk_])
            CP(P, "dve", thr[:, :], tp_[:, 0:16], [tk_], ["thr"])
        else:
            DMA(P, "sp", thr[:, :], thrd[:, :], [], ["thr"])
        DMA(P, "sp", affs[:, :, :], affd.ap().rearrange("(t p) e -> p t e", p=128), [], ["affs"])
        for t in range(ntile):
            TT(P, "dve", gw[:, t, :], affs[:, t, :], thr[:, :], ALU.is_ge, ["affs", "thr"], ["gw"])
            TT(P, "dve", gw[:, t, :], gw[:, t, :], affs[:, t, :], ALU.mult, ["gw", "affs"], ["gw"])
        cv = [0]

        def conv(dst, src, r, w):
            eng = ("act", "dve", "act")[cv[0] % 3]
            cv[0] += 1
            CP(P, eng, dst, src, r, w)

        def load_gu(e, chs=(0, 1, 2)):
            for ch in chs:
                for (wd_, dstb, key) in ((wgd, wgb, "wgb"), (wud, wub, "wub")):
                    sk, st = stg.next()
                    sv = st[:, :].rearrange("p (c f) -> p c f", c=8)
                    DMA(P, "sp", sv, wd_[e].rearrange("(c p) f -> p c f", p=128)[:, :, ch * 512:(ch + 1) * 512], [], [sk])
                    for hh in range(2):
                        conv(dstb[:, hh * 4:(hh + 1) * 4, ch * 512:(ch + 1) * 512], sv[:, hh * 4:(hh + 1) * 4, :], [sk],
                             [(key, ch)])

        def load_d(e):
            for ch in range(3):
                sk, st = stg.next()
                sv = st[:, :].rearrange("p (c n) -> p c n", c=4)
                DMA(P, "sp", sv, wdd[e].rearrange("(c p) n -> p c n", p=128)[:, ch * 4:(ch + 1) * 4, :], [], [sk])
                for hh in range(2):
                    conv(wdb[:, ch * 4 + hh * 2:ch * 4 + hh * 2 + 2, :], sv[:, hh * 2:hh * 2 + 2, :], [sk], ["wdb"])

        first = True
        for blk in range(nblk):
            b0 = blk * tb
            DMA(P, "sp", xb[:, :, :], xnd.ap().rearrange("(k p) t -> p k t", p=128)[:, :, b0:b0 + tb], [], ["xb"])
            for e in range(nexp):
                if first:
                    load_gu(e)
                    load_d(e)
                    first = False
                nxt = (blk * nexp + e + 1)
                for f in range(12):
                    for tq in range(tb // 512):
                        gk, gp = pA.next()
                        uk, up = pA.next()
                        for k in range(8):
                            MM(P, gp[:, :], wgb[:, k, f * 128:(f + 1) * 128], xb[:, k, tq * 512:(tq + 1) * 512],
                               k == 0, k == 7, [("wgb", f // 4), "xb"], [gk])
                        for k in range(8):
                            MM(P, up[:, :], wub[:, k, f * 128:(f + 1) * 128], xb[:, k, tq * 512:(tq + 1) * 512],
                               k == 0, k == 7, [("wub", f // 4), "xb"], [uk])
                        sk, st = sg.next()
                        ACT(P, st[:, :], gp[:, :], AF.Silu, [gk], [sk])
                        TT(P, "dve", hT[:, f, tq * 512:(tq + 1) * 512], up[:, :], st[:, :], ALU.mult, [uk, sk], [("hT", f)])
                    if f % 4 == 3 and nxt < nblk * nexp:
                        load_gu(nxt % nexp, chs=(f // 4,))
                for tt in range(tb // 128):
                    gcol = gw[:, blk * (tb // 128) + tt, e:e + 1]
                    for hf in range(2):
                        yk, yp = pA.next()
                        for f in range(12):
                            MM(P, yp[:, :], hT[:, f, tt * 128:(tt + 1) * 128], wdb[:, f, hf * 512:(hf + 1) * 512],
                               f == 0, f == 11, [("hT", f), "wdb"], [yk])
                        asl = acc[:, tt, hf * 512:(hf + 1) * 512]
                        if e == 0:
                            TS(P, "dve", asl, yp[:, :], gcol, None, ALU.mult, None, [yk, "gw"], [("acc", tt, hf)])
                        else:
                            STT(P, "dve", asl, yp[:, :], gcol, asl, ALU.mult, ALU.add, [yk, "gw", ("acc", tt, hf)],
                                [("acc", tt, hf)])
                if nxt < nblk * nexp:
                    load_d(nxt % nexp)
            for tt in range(tb // 128):
                xk, xt_ = xo.next()
                r0 = b0 + tt * 128
                DMA(P, "sp", xt_[:, :], x1d[r0:r0 + 128, :], [], [xk])
                TT(P, "pool", xt_[:, :], xt_[:, :], acc[:, tt, :], ALU.add, [xk, ("acc", tt, 0), ("acc", tt, 1)], [xk])
                DMA(P, "sp", x2d[r0:r0 + 128, :], xt_[:, :], [xk], [])
        P.emit()
        stats = P.stats
    return nc, stats


def build_attn(nq=TPC, nk=S, nheads=8, env=None, pre=None):
    nc, es, C, P = _begin(env, pre)
    NKT = nk // 128
    NQB = nq // 512
    scale = 96.0 ** -0.5
    with es:
        mq = C.dram("mq", [nheads, 96, nq], BF16, "ExternalInput")
        mk = C.dram("mk", [nheads, 96, nk], BF16, "ExternalInput")
        mv = C.dram("mv", [nheads, 128, NKT * 64], BF16, "ExternalInput")
        esel = C.dram("esel", [65, 64], F32, "ExternalInput")
        OT = C.dram("OT", [nheads * 64, nq], BF16, "ExternalOutput")

        kT = Rot([(("kT", i), C.sb([96, nk], BF16, "kT")) for i in range(2)])
        vv = Rot([(("vv", i), C.sb([128, NKT, 65], BF16, "vv")) for i in range(2)])
        qT = Rot([(("qT", i), C.sb([96, nq], BF16, "qT")) for i in range(2)])
        pT = Rot([(("pT", i), C.sb([128, 512], BF16, "pT")) for i in range(4)])
        osb = C.sb([65, 512], F32, "osb")
        rbc = C.sb([64, 512], F32, "rbc")
        oo = Rot([(("oo", i), C.sb([64, 512], BF16, "oo")) for i in range(2)])
        es_sb = C.sb([65, 64], F32, "esel")
        sps = Rot([(("sps", i), C.ps([128, 512], F32, "sps")) for i in range(4)])
        ops_ = Rot([(("ops", i), C.ps([128, 512], F32, "ops")) for i in range(2)])
        bps = C.ps([128, 512], F32, "bps")
        DMA(P, "sp", es_sb[:], esel[:, :], [], ["esel"])
        for i in range(2):
            MEMSET(P, "pool", vv.items[i][1][:, :, 64:65], 1.0, [("vv1", i)])
        for h in range(nheads):
            kk, kt_ = kT.next()
            vk, vt_ = vv.next()
            qk, qt_ = qT.next()
            DMA(P, "sp", kt_[:, :], mk[h, :, :], [], [kk])
            DMA(P, "sp", vt_[:, :, 0:64], mv[h, :, :].rearrange("p (t d) -> p t d", d=64), [], [vk])
            DMA(P, "sp", qt_[:, :], mq[h, :, :], [], [qk])
            vkeys = [vk, ("vv1", (vv.i - 1) % 2)]
            for qb in range(NQB):
                ok_, ot_ = ops_.next()

                def s_mm(t, kt_=kt_, qt_=qt_, qb=qb, kk=kk, qk=qk):
                    sk, st = sps.next()
                    MM(P, st[:, :], kt_[:, t * 128:(t + 1) * 128], qt_[:, qb * 512:(qb + 1) * 512], True, True,
                       [kk, qk], [sk])
                    return sk, st
                pend = [s_mm(0)]
                if NKT > 1:
                    pend.append(s_mm(1))
                for t in range(NKT):
                    if t + 2 < NKT:
                        pend.append(s_mm(t + 2))
                    sk, st = pend.pop(0)
                    pk, pt = pT.next()
                    ACT(P, pt[:, :], st[:, :], AF.Exp, [sk], [pk], scale=scale)
                    MM(P, ot_[0:65, :], vt_[:, t, :], pt[:, :], t == 0, t == NKT - 1, vkeys + [pk], [ok_])
                CP(P, "dve", osb[:, :], ot_[0:65, :], [ok_], ["osb"])
                MM(P, bps[0:64, :], es_sb[:, :], osb[:, :], True, True, ["esel", "osb"], ["bps"])
                CP(P, "dve", rbc[:, :], bps[0:64, :], ["bps"], ["rbc"])
                RECIP(P, "dve", rbc[:, :], rbc[:, :], ["rbc"], ["rbc"])
                ook, oot = oo.next()
                TT(P, "dve", oot[:, :], osb[0:64, :], rbc[:, :], ALU.mult, ["osb", "rbc"], [ook])
                DMA(P, "sp", OT[h * 64:(h + 1) * 64, qb * 512:(qb + 1) * 512], oot[:, :], [ook], [])
        P.emit()
        stats = P.stats
    return nc, stats


def attn_consts():
    e = np.zeros((65, 64), np.float32)
    e[64, :] = 1.0
    return dict(esel=e)


RG = [[0, 1, 2, 3], [4, 5, 6, 7]]
LAYER_W = [("w_in", [D, INW], F32), ("gmix", [128, 8], F32), ("convp", [128, 4, 34], F32), ("gcq", [128, 3], F32),
           ("gckv", [128, 2], F32), ("w_uq", [384, 768], F32), ("w_ukv", [256, 1024], F32), ("gqk", [96, 2], F32),
           ("bif", [128, 4], F32), ("gA", [128, 128], F32), ("w_a", [512, D], F32), ("w_b", [512, D], F32),
           ("w_c", [512, D], F32), ("w_o", [D, D], F32), ("gffn", [128, 8], F32), ("w_r", [D, 16], F32),
           ("wg", [16, D, 1536], F32), ("wu", [16, D, 1536], F32), ("wd", [16, 1536, D], F32)]
CONSTS = [("identb", [128, 128], BF16), ("ropeT", [96, 2, TPC], F32), ("rmat", [96, 96], BF16), ("onesf", [128, 128], F32),
          ("cmat", [128, 6, 128], F32), ("ident32", [128, 128], F32), ("esel", [65, 64], F32), ("idx", [128, 16], I32)]


def build_fused(stop=None):
    env = Env()
    nc, P = env.nc, env.P
    BYP = ALU.bypass
    CCB = 256 * 1024
    with env.es:
        def DT(name, shape, dt, kind="Internal"):
            return nc.dram_tensor(name, list(shape), dt, kind=kind)

        def allgather(name, src2d, rows, cols, dt, rkeys, wkey):
            esz = 4 if dt in (F32, I32) else 2
            rc = max(1, min(rows, CCB // (cols * esz)))
            assert rows % rc == 0
            g = DT(name, [4 * rows, cols], dt)
            for k in range(rows // rc):
                P.cc(lambda e, k=k: e.collective_compute("AllGather", BYP, replica_groups=RG, ins=[src2d[k * rc:(k + 1) * rc, :]],
                                                         outs=[g[k * 4 * rc:(k + 1) * 4 * rc, :]]), rkeys, [wkey])
            return g, rc

        def rankview(g, rc, r):
            return g.ap().rearrange("(k r x) c -> r k x c", r=4, x=rc)[r]

        ext = {"xe0": DT("xe0", [TPC + 2 * HALO, D], F32, "ExternalInput")}
        for (n, sh, dt) in CONSTS:
            ext[n] = DT(n, sh, dt, "ExternalInput")
        for l in range(2):
            for (n, sh, dt) in LAYER_W:
                ext["%s_%d" % (n, l)] = DT("%s_%d" % (n, l), sh, dt, "ExternalInput")
        out = DT("out", [TPC, D], F32, "ExternalOutput")
        xe = ext["xe0"]
        x_own = None
        for l in range(2):
            W = {n: ext["%s_%d" % (n, l)] for (n, _, _) in LAYER_W}
            L = lambda n, sh, dt: DT("%s_L%d" % (n, l), sh, dt)
            A = dict(QT=L("QT", [512, TPC], BF16), KT=L("KT", [512, TPC], BF16), Kt=L("Kt", [4, TPC, 128], BF16),
                     Vt=L("Vt", [4, TPC, 128], BF16), OG=L("OG", [4, TPC, 128], BF16), G4=L("G4", [4, TPC, 4], F32),
                     GTS=L("GTS", [TPC, 3072], BF16), UT=L("UT", [512, TPC], BF16), MQ=L("MQ", [8, 96, TPC], BF16),
                     MK=L("MK", [8, 96, TPC], BF16), MV=L("MV", [8, 128, TPC // 128, 64], BF16))
            preA = dict(xe=xe, w_in=W["w_in"], gmix=W["gmix"], identb=ext["identb"], convp=W["convp"], gcq=W["gcq"],
                        gckv=W["gckv"], w_uq=W["w_uq"], w_ukv=W["w_ukv"], gqk=W["gqk"], ropeT=ext["ropeT"],
                        rmat=ext["rmat"], onesf=ext["onesf"], **A)
            build_stageA(env=env, pre=preA)
            nc_, es_, C_, _ = _begin(env)
            with es_:
                idx = C_.sb([128, 16], I32, "idx")
                DMA(P, "sp", idx[:, :], ext["idx"][:, :], [], ["idx"])
                gQT, rcQ = allgather("gQT_L%d" % l, A["QT"].ap(), 512, TPC, BF16, [], ("g", 0))
                gKt, rcK = allgather("gKt_L%d" % l, A["Kt"].ap().rearrange("h t d -> (h t) d"), 4 * TPC, 128, BF16, [], ("g", 1))
                gVt, _ = allgather("gVt_L%d" % l, A["Vt"].ap().rearrange("h t d -> (h t) d"), 4 * TPC, 128, BF16, [], ("g", 2))
                gOG, _ = allgather("gOG_L%d" % l, A["OG"].ap().rearrange("h t d -> (h t) d"), 4 * TPC, 128, BF16, [], ("g", 3))
                gG4, rcG = allgather("gG4_L%d" % l, A["G4"].ap().rearrange("h t g -> (h t) g"), 4 * TPC, 4, F32, [], ("g", 4))
                gMK, rcMK = allgather("gMK_L%d" % l, A["MK"].ap().rearrange("h f t -> (h f) t"), 768, TPC, BF16, [], ("g", 5))
                gMV, rcMV = allgather("gMV_L%d" % l, A["MV"].ap().rearrange("h p t d -> (h p) (t d)"), 1024, 2048, BF16, [], ("g", 6))
                assert (rcQ, rcK, rcG, rcMK, rcMV) == (32, 1024, 4 * TPC, 32, 64), (rcQ, rcK, rcG, rcMK, rcMV)
                qT_s = L("qT_s", [128, S], BF16)
                kt_s = L("kt_s", [S, 128], BF16)
                vt_s = L("vt_s", [S, 128], BF16)
                og_s = L("og_s", [S, 128], BF16)
                g4_s = L("g4_s", [S, 4], F32)
                mk_s = L("mk_s", [8, 96, S], BF16)
                mv_s = L("mv_s", [8, 128, (S // 128) * 64], BF16)
                stb = Rot([(("stb", i), C_.sb([128, 16384], BF16, "stb")) for i in range(2)])
                stf = C_.sb([128, 512], F32, "stf")

                def gather(dst_ap, src_ap, col, rkeys, wkeys, tile_ap, tkey):
                    P.dma("pool", lambda e: e.indirect_dma_start(
                        out=tile_ap, out_offset=None, in_=src_ap,
                        in_offset=bass.IndirectOffsetOnAxis(ap=idx[:, col:col + 1], axis=0)), ["idx"] + rkeys, [tkey])
                    DMA(P, "sp", dst_ap, tile_ap, [tkey], wkeys)
                for i in range(4):
                    tk_, tt_ = stb.next()
                    gather(qT_s[:, i * TPC:(i + 1) * TPC], gQT[:, :], i, [("g", 0)], [("qT_s", i)], tt_[:, 0:TPC], tk_)
                for j, (gsrc, dst) in enumerate(((gKt, kt_s), (gVt, vt_s), (gOG, og_s))):
                    tk_, tt_ = stb.next()
                    gather(dst.ap().rearrange("(c p) d -> c (p d)", p=128),
                           gsrc.ap().rearrange("(c p) d -> c (p d)", p=128), 4, [("g", 1 + j)], [("tm_s", j)], tt_[:, :], tk_)
                gather(g4_s.ap().rearrange("(c p) d -> c (p d)", p=128),
                       gG4.ap().rearrange("(c p) d -> c (p d)", p=128), 11, [("g", 4)], [("tm_s", 3)], stf[:, :], "stf")
                for i in range(4):
                    DMA(P, "sp", mk_s.ap().rearrange("h f t -> (h f) t")[:, i * TPC:(i + 1) * TPC].rearrange("(k x) t -> k x t", x=rcMK),
                        rankview(gMK, rcMK, i), [("g", 5)], [("mk_s", i)])
                    DMA(P, "sp", mv_s.ap().rearrange("h p x -> (h p) x")[:, i * 2048:(i + 1) * 2048].rearrange("(k x) c -> k x c", x=rcMV),
                        rankview(gMV, rcMV, i), [("g", 6)], [("mv_s", i)])
                P.emit()
            if stop == "x1":
                return nc
            HT = L("HT", [128, S], BF16)
            build_mlstm(env=env, pre=dict(qT=qT_s, kt=kt_s, vt=vt_s, g4=g4_s, bif=W["bif"], og=og_s, gA=W["gA"],
                                          cmat=ext["cmat"], identb=ext["identb"], HT=HT))
            if stop == "m":
                return nc
            OT = L("OT", [512, TPC], BF16)
            build_attn(env=env, pre=dict(mq=A["MQ"], mk=mk_s, mv=mv_s, esel=ext["esel"], OT=OT))
            if stop == "t":
                return nc
            nc_, es_, C_, _ = _begin(env)
            with es_:
                idx = C_.sb([128, 16], I32, "idx")
                DMA(P, "sp", idx[:, :], ext["idx"][:, :], [], ["idx"])
                gHT, rcH = allgather("gHT_L%d" % l, HT.ap(), 128, S, BF16, [], "gHT")
                assert rcH == 8
                HT_own = L("HT_own", [512, TPC], BF16)
                src = gHT.ap().rearrange("r (i t) -> (r i) t", i=4)
                stb = Rot([(("stb", i), C_.sb([128, TPC], BF16, "stb")) for i in range(2)])
                for h in range(4):
                    tk_, tt_ = stb.next()
                    P.dma("pool", lambda e, h=h, tt_=tt_: e.indirect_dma_start(
                        out=tt_[:, :], out_offset=None, in_=src,
                        in_offset=bass.IndirectOffsetOnAxis(ap=idx[:, 5 + h:6 + h], axis=0)), ["idx", "gHT"], [tk_])
                    DMA(P, "sp", HT_own[h * 128:(h + 1) * 128, :], tt_[:, :], [tk_], [("HT_own", h)])
                P.emit()
            if stop == "x2":
                return nc
            x1 = L("x1", [TPC, D], F32)
            xn2T = L("xn2T", [D, TPC], BF16)
            aff = L("aff", [TPC, 16], F32)
            affT = L("affT", [16, TPC], F32)
            if l == 0:
                xin = L("xin", [TPC, D], F32)
                nc_, es_, C_, _ = _begin(env)
                with es_:
                    DMA(P, "sp", xin[:, :], ext["xe0"][HALO:HALO + TPC, :], [], ["xin"])
                    P.emit()
            else:
                xin = x_own
            build_merge(env=env, pre=dict(x=xin, HT=HT_own, UT=A["UT"], OT=OT, GTS=A["GTS"], w_a=W["w_a"], w_b=W["w_b"],
                                          w_c=W["w_c"], w_o=W["w_o"], gffn=W["gffn"], w_r=W["w_r"], identb=ext["identb"],
                                          ident32=ext["ident32"], x1=x1, xn2T=xn2T, aff=aff, affT=affT))
            if stop == "c1":
                return nc
            affT_s = L("affT_s", [16, S], F32)
            nc_, es_, C_, _ = _begin(env)
            with es_:
                gAf, rcA = allgather("gAf_L%d" % l, affT.ap(), 16, TPC, F32, [], "gAf")
                assert rcA == 16
                for i in range(4):
                    DMA(P, "sp", affT_s[:, i * TPC:(i + 1) * TPC], gAf[i * 16:(i + 1) * 16, :], ["gAf"], [("affT_s", i)])
                P.emit()
            thr = L("thr", [16, 2], F32)
            build_thr(env=env, pre=dict(affT=affT_s, thr=thr))
            if stop == "h":
                return nc
            x2 = out if l == 1 else L("x2", [TPC, D], F32)
            build_ffn(env=env, pre=dict(x1=x1, xn2T=xn2T, aff=aff, thr16=thr, ident32=ext["ident32"], wg=W["wg"], wu=W["wu"],
                                        wd=W["wd"], x2=x2))
            if l == 0:
                xe1 = L("xe1", [TPC + 2 * HALO, D], F32)
                nc_, es_, C_, _ = _begin(env)
                with es_:
                    idx = C_.sb([128, 16], I32, "idx")
                    zt = C_.sb([128, D], F32, "zt")
                    DMA(P, "sp", idx[:, :], ext["idx"][:, :], [], ["idx"])
                    MEMSET(P, "dve", zt[:, :], 0.0, ["zt"])
                    edges = L("edges", [384, D], F32)
                    DMA(P, "sp", edges[0:128, :], x2[0:128, :], [], ["edges"])
                    DMA(P, "sp", edges[128:256, :], x2[TPC - 128:TPC, :], [], ["edges"])
                    DMA(P, "sp", edges[256:384, :], zt[:, :], ["zt"], ["edges"])
                    DMA(P, "sp", xe1[HALO:HALO + TPC, :], x2[:, :], [], ["xe1m"])
                    gE, rcE = allgather("gE_L%d" % l, edges.ap(), 384, D, F32, ["edges"], "gE")
                    assert rcE == 64
                    hl = C_.sb([128, D], F32, "hl")
                    hr = C_.sb([128, D], F32, "hr")
                    P.dma("pool", lambda e: e.indirect_dma_start(
                        out=hl[:, :], out_offset=None, in_=gE[:, :],
                        in_offset=bass.IndirectOffsetOnAxis(ap=idx[:, 9:10], axis=0)), ["idx", "gE"], ["hl"])
                    P.dma("pool", lambda e: e.indirect_dma_start(
                        out=hr[:, :], out_offset=None, in_=gE[:, :],
                        in_offset=bass.IndirectOffsetOnAxis(ap=idx[:, 10:11], axis=0)), ["idx", "gE"], ["hr"])
                    DMA(P, "sp", xe1[0:HALO, :], hl[:, :], ["hl"], ["xe1l"])
                    DMA(P, "sp", xe1[HALO + TPC:, :], hr[:, :], ["hr"], ["xe1r"])
                    P.emit()
                xe = xe1
                x_own = x2
    return nc


def _grow(x, rc, r):
    return (x // rc) * (4 * rc) + r * rc + (x % rc)


def fused_idx(c):
    r = c % 4
    p = np.arange(128)
    idx = np.zeros((128, 16), np.int32)
    for i in range(4):
        idx[:, i] = _grow(r * 128 + p, 32, i)
    y = r * 32 + (p % 32)
    idx[:, 4] = _grow(y, 8, p // 32)
    idx[:, 11] = (p // 32) * 128 + y
    for h in range(4):
        idx[:, 5 + h] = _grow(p, 8, h) * 4 + r
    idx[:, 9] = _grow(128 + p, 64, r - 1) if r > 0 else _grow(256 + p, 64, r)
    idx[:, 10] = _grow(p, 64, r + 1) if r < 3 else _grow(256 + p, 64, r)
    return idx


def kernel(**inputs):
    prm = {k: np.asarray(v) for k, v in inputs.items()}
    x = np.ascontiguousarray(prm["x"], dtype=np.float32)
    nc = build_fused()
    CT, ST = rope_tables()
    cst = consts()
    mc = mlstm_consts()
    ac = attn_consts()
    i32 = np.eye(128, dtype=np.float32)
    lay = []
    for l in range(2):
        convp = np.zeros((128, 4, 34), np.float32)
        convp[:, :, 0:31] = prm["conv_w"][l].T.reshape(4, 128, 31).transpose(1, 0, 2)
        convp[:, :, 31] = prm["conv_b"][l].reshape(4, 128).T
        convp[:, :, 32] = prm["conv_ln_g"][l].reshape(4, 128).T
        convp[:, :, 33] = prm["conv_ln_b"][l].reshape(4, 128).T
        lay.append(dict(w_in=prm["w_in"][l], gmix=_gain_cols(prm["mix_norm_g"][l], 8), convp=convp,
                        gcq=_gain_cols(prm["cq_norm_g"][l], 3), gckv=_gain_cols(prm["ckv_norm_g"][l], 2),
                        w_uq=prm["w_uq"][l], w_ukv=prm["w_ukv"][l],
                        gqk=np.ascontiguousarray(np.stack([prm["q_norm_g"][l], prm["k_norm_g"][l]], axis=1)),
                        w_a=prm["w_a_out"][l], w_b=prm["w_b_out"][l], w_c=prm["w_c_out"][l], w_o=prm["w_out"][l],
                        gffn=_gain_cols(prm["ffn_norm_g"][l], 8), w_r=prm["w_router"][l],
                        wg=prm["w_e_gate"][l], wu=prm["w_e_up"][l], wd=prm["w_e_down"][l]))
    maps = []
    for c in range(NCORES):
        b, r = c // 4, c % 4
        s0 = r * TPC
        xe = np.zeros((TPC + 2 * HALO, D), np.float32)
        lo, hi = max(0, s0 - HALO), min(S, s0 + TPC + HALO)
        xe[lo - (s0 - HALO):hi - (s0 - HALO)] = x[b, lo:hi]
        m = dict(xe0=xe, identb=cst["identb"], ropeT=np.ascontiguousarray(np.stack([CT[:, s0:s0 + TPC], ST[:, s0:s0 + TPC]], axis=1)),
                 rmat=cst["rmat"], onesf=cst["onesf"], cmat=mc["cmat"], ident32=i32, esel=ac["esel"], idx=fused_idx(c))
        cols = [r, 4 + r, 8 + r, 12 + r]
        for l in range(2):
            for k_, v_ in lay[l].items():
                m["%s_%d" % (k_, l)] = v_
            m["bif_%d" % l] = np.ascontiguousarray(np.broadcast_to(prm["b_if"][l][cols], (128, 4)))
            m["gA_%d" % l] = np.ascontiguousarray(np.broadcast_to(prm["a_norm_g"][l][r], (128, 128)))
        maps.append(m)
    res = run_spmd(nc, maps)
    out = np.empty_like(x)
    for c in range(NCORES):
        b, r = c // 4, c % 4
        out[b, r * TPC:(r + 1) * TPC] = np.asarray(res[c]["out"])
    return out
```

```python
import math
from contextlib import ExitStack

import numpy as np
import ml_dtypes

import concourse.bass as bass
import concourse.mybir as mybir
from concourse.bass_utils import run_bass_kernel_spmd

F32 = mybir.dt.float32
BF16 = mybir.dt.bfloat16
I32 = mybir.dt.int32
AF = mybir.ActivationFunctionType
ALU = mybir.AluOpType
AX = mybir.AxisListType
NPBF = ml_dtypes.bfloat16

D = 1024
S = 16384
NB = 2
INW = 6832
EPS = 1e-6
NCORES = 8
TPC = S * NB // NCORES

O_AQ, O_AK, O_AV, O_AO, O_AG = 0, 512, 1024, 1536, 2048
O_GLU = 2064
O_CQ = 3088
O_CKV = 3472
O_CKR = 3728
O_GTS = 3760


class Prog:
    RING = 8

    def __init__(self, nc, es):
        self.nc = nc
        self.es = es
        self.ops = []
        self.engs = ["pe", "act", "dve", "pool", "sp"]
        self.csem = None
        self.rings = {}
        self.ccsem = None
        self.ccount = {e: 0 for e in self.engs}
        self.dcount = {e: 0 for e in self.engs}
        self.cccount = 0
        self.nstage = 0

    def cc(self, fn, r=(), w=()):
        self.ops.append(dict(eng="pool", fn=fn, r=tuple(r), w=tuple(w), dma=True, cc=True))

    def op(self, eng, fn, r=(), w=()):
        self.ops.append(dict(eng=eng, fn=fn, r=tuple(r), w=tuple(w), dma=False))

    def dma(self, eng, fn, r=(), w=()):
        self.ops.append(dict(eng=eng, fn=fn, r=tuple(r), w=tuple(w), dma=True))

    def emit(self):
        nc, es = self.nc, self.es
        ops = self.ops
        last_w = {}
        readers = {}
        deps = []
        for i, o in enumerate(ops):
            d = set()
            for k in o["r"]:
                if k in last_w:
                    d.add((last_w[k], "raw"))
            for k in o["w"]:
                if k in last_w:
                    d.add((last_w[k], "waw"))
                for j in readers.get(k, ()):
                    if j != i:
                        d.add((j, "war"))
            for k in o["r"]:
                lst = readers.setdefault(k, [])
                if not o["dma"]:
                    lst[:] = [j for j in lst if ops[j]["dma"] or ops[j]["eng"] != o["eng"]]
                lst.append(i)
            for k in o["w"]:
                last_w[k] = i
                readers[k] = []
            dd = set()
            for j, kind in d:
                p = ops[j]
                if (not p["dma"]) and (not o["dma"]) and p["eng"] == o["eng"]:
                    if o["eng"] == "pe":
                        continue
                    if kind == "war":
                        continue
                dd.add(j)
            deps.append(dd)
        needed = set()
        for dd in deps:
            needed |= dd
        engs = self.engs
        if self.csem is None:
            self.csem = {e: es.enter_context(nc.semaphore("c_" + e)) for e in engs}
            self.ccsem = es.enter_context(nc.semaphore("c_cc"))
        csem = self.csem
        rings = self.rings
        ccount = self.ccount
        dcount = self.dcount
        prev_end = dict(c={e: ccount[e] for e in engs}, d={e: dcount[e] for e in rings}, cc=self.cccount)
        lastc = {}
        for i, o in enumerate(ops):
            if not o["dma"]:
                lastc[o["eng"]] = i
        needed |= set(lastc.values())
        sig = {}
        prewait = {}
        for i, o in enumerate(ops):
            e = o["eng"]
            if o.get("cc"):
                self.cccount += 1
                sig[i] = (self.ccsem, self.cccount, 1)
                if self.cccount > 1:
                    prewait[i] = (self.ccsem, self.cccount - 1)
            elif o["dma"]:
                if e not in rings:
                    rings[e] = [es.enter_context(nc.semaphore("r_%s%d" % (e, k)))
                                for k in range(self.RING)]
                n = dcount[e]
                dcount[e] += 1
                sem = rings[e][n % self.RING]
                sig[i] = (sem, 16 * (n // self.RING + 1), 16)
                if n >= self.RING:
                    prewait[i] = (sem, 16 * (n // self.RING))
            elif i in needed:
                ccount[e] += 1
                sig[i] = (csem[e], ccount[e], 1)
        per = {e: [] for e in engs}
        for i, o in enumerate(ops):
            per[o["eng"]].append(i)
        self.stats = dict(n_ops=len(ops), ccount=ccount, dcount=dcount)

        nstage = self.nstage
        self.nstage += 1

        def run(e, engobj):
            waited = {}
            if nstage > 0:
                for e2 in engs:
                    if prev_end["c"][e2] > 0:
                        engobj.wait_ge(csem[e2], prev_end["c"][e2])
                for e2, n in prev_end["d"].items():
                    for k in range(self.RING):
                        cnt = (n - k + self.RING - 1) // self.RING if n > k else 0
                        if cnt > 0:
                            engobj.wait_ge(rings[e2][k], 16 * cnt)
                if prev_end["cc"] > 0:
                    engobj.wait_ge(self.ccsem, prev_end["cc"])
            for i in per[e]:
                o = ops[i]
                ws = [sig[j][:2] for j in deps[i]]
                if i in prewait:
                    ws.append(prewait[i])
                mx = {}
                for sem, val in ws:
                    key = id(sem)
                    if key not in mx or mx[key][1] < val:
                        mx[key] = (sem, val)
                for key, (sem, val) in mx.items():
                    if waited.get(key, 0) >= val:
                        continue
                    waited[key] = val
                    engobj.wait_ge(sem, val)
                ins = o["fn"](engobj)
                if i in sig:
                    sem, val, inc = sig[i]
                    ins.then_inc(sem, inc)
            if e in rings:
                n = dcount[e]
                for k in range(self.RING):
                    cnt = (n - k + self.RING - 1) // self.RING if n > k else 0
                    if cnt > 0:
                        engobj.wait_ge(rings[e][k], 16 * cnt)

        with nc.Block() as block:
            @block.tensor
            def _(t):
                run("pe", t)

            @block.scalar
            def _(t):
                run("act", t)

            @block.vector
            def _(t):
                run("dve", t)

            @block.gpsimd
            def _(t):
                run("pool", t)

            @block.sync
            def _(t):
                run("sp", t)
        self.ops = []


class Ctx:
    def __init__(self, nc, es, pre=None, tag=""):
        self.nc, self.es = nc, es
        self.n = 0
        self.pre = pre
        self.tag = tag

    def sb(self, shape, dt, name=None):
        self.n += 1
        t = self.es.enter_context(self.nc.sbuf_tensor("%s%s_%d" % (self.tag, name or "t", self.n), list(shape), dt))
        esz = 4 if dt in (F32, I32) else 2
        nbytes = int(np.prod(shape[1:])) * esz
        alloc = (nbytes + 31) // 32 * 32
        if alloc % 64 != 0:
            self.n += 1
            self.es.enter_context(self.nc.sbuf_tensor("%spad_%d" % (self.tag, self.n), [128, 8], F32))
        return t

    def ps(self, shape, dt, name=None):
        self.n += 1
        return self.es.enter_context(self.nc.psum_tensor("%s%s_%d" % (self.tag, name or "p", self.n), list(shape), dt))

    def dram(self, name, shape, dt, kind):
        if self.pre is not None:
            h = self.pre[name]
            assert list(h.shape) == list(shape), (name, h.shape, shape)
            return h
        return self.nc.dram_tensor(name, list(shape), dt, kind=kind)


class Rot:
    def __init__(self, items):
        self.items = items
        self.i = 0

    def next(self):
        it = self.items[self.i % len(self.items)]
        self.i += 1
        return it


class Env:
    def __init__(self):
        self.nc = bass.Bass("TRN2", target_bir_lowering=False)
        self.es = ExitStack()
        self.P = Prog(self.nc, self.es)
        self.nstage = 0


def _begin(env, pre=None):
    if env is None:
        nc = bass.Bass("TRN2", target_bir_lowering=False)
        es = ExitStack()
        return nc, es, Ctx(nc, es), Prog(nc, es)
    env.nstage += 1
    es = ExitStack()
    return env.nc, es, Ctx(env.nc, es, pre=pre, tag="s%d_" % env.nstage), env.P


def run_spmd(nc, in_maps):
    res = run_bass_kernel_spmd(nc, in_maps, core_ids=list(range(NCORES)))
    return res.results


def ACT(P, out, in_, func, r, w, **kw):
    P.op("act", lambda e: e.activation(out=out, in_=in_, func=func, **kw), r, w)


def TS(P, eng, out, in0, s1, s2, op0, op1, r, w):
    if op1 is None:
        P.op(eng, lambda e: e.tensor_scalar(out=out, in0=in0, scalar1=s1, scalar2=None, op0=op0), r, w)
    else:
        P.op(eng, lambda e: e.tensor_scalar(out=out, in0=in0, scalar1=s1, scalar2=s2, op0=op0, op1=op1), r, w)


def TT(P, eng, out, in0, in1, op, r, w):
    P.op(eng, lambda e: e.tensor_tensor(out=out, in0=in0, in1=in1, op=op), r, w)


def STT(P, eng, out, in0, scalar, in1, op0, op1, r, w):
    P.op(eng, lambda e: e.scalar_tensor_tensor(out=out, in0=in0, scalar=scalar, in1=in1, op0=op0, op1=op1), r, w)


def CP(P, eng, out, in_, r, w):
    if eng == "act":
        P.op(eng, lambda e: e.copy(out=out, in_=in_), r, w)
    else:
        P.op(eng, lambda e: e.tensor_copy(out=out, in_=in_), r, w)


def RSUM(P, eng, out, in_, r, w, axis=None):
    ax = axis if axis is not None else AX.X
    P.op(eng, lambda e: e.reduce_sum(out=out, in_=in_, axis=ax), r, w)


def RMAX(P, eng, out, in_, r, w, axis=None):
    ax = axis if axis is not None else AX.X
    P.op(eng, lambda e: e.reduce_max(out=out, in_=in_, axis=ax), r, w)


def MM(P, out, lhsT, rhs, start, stop, r, w):
    P.op("pe", lambda e: e.matmul(out, lhsT, rhs, start=start, stop=stop), r, w)


def TR(P, out, in_, ident, r, w):
    P.op("pe", lambda e: e.transpose(out, in_, ident), r, w)


def DMA(P, eng, out, in_, r, w):
    P.dma(eng, lambda e: e.dma_start(out=out, in_=in_), r, w)


def RECIP(P, eng, out, in_, r, w):
    P.op(eng, lambda e: e.reciprocal(out=out, in_=in_), r, w)


def MEMSET(P, eng, ap, val, w):
    P.op(eng, lambda e: e.memset(ap, val), (), w)


TP = 1024
HALO = 128
NTP = TP + 2 * HALO
NPASS = TPC // TP


def build_stageA(phases=("fm", "tm", "conv", "mla"), env=None, pre=None):
    nc, es, C, P = _begin(env, pre)
    with es:
        xe = C.dram("xe", [TPC + 2 * HALO, D], F32, "ExternalInput")
        w_in = C.dram("w_in", [D, INW], F32, "ExternalInput")
        gmix = C.dram("gmix", [128, 8], F32, "ExternalInput")
        identb = C.dram("identb", [128, 128], BF16, "ExternalInput")
        convp = C.dram("convp", [128, 4, 34], F32, "ExternalInput")
        gcq = C.dram("gcq", [128, 3], F32, "ExternalInput")
        gckv = C.dram("gckv", [128, 2], F32, "ExternalInput")
        w_uq = C.dram("w_uq", [384, 768], F32, "ExternalInput")
        w_ukv = C.dram("w_ukv", [256, 1024], F32, "ExternalInput")
        gqk = C.dram("gqk", [96, 2], F32, "ExternalInput")
        ropeT = C.dram("ropeT", [96, 2, TPC], F32, "ExternalInput")
        rmat = C.dram("rmat", [96, 96], BF16, "ExternalInput")
        onesf = C.dram("onesf", [128, 128], F32, "ExternalInput")

        QT = C.dram("QT", [512, TPC], BF16, "ExternalOutput")
        KT = C.dram("KT", [512, TPC], BF16, "ExternalOutput")
        Kt = C.dram("Kt", [4, TPC, 128], BF16, "ExternalOutput")
        Vt = C.dram("Vt", [4, TPC, 128], BF16, "ExternalOutput")
        OG = C.dram("OG", [4, TPC, 128], BF16, "ExternalOutput")
        G4 = C.dram("G4", [4, TPC, 4], F32, "ExternalOutput")
        GTS = C.dram("GTS", [TPC, 3072], BF16, "ExternalOutput")
        UT = C.dram("UT", [512, TPC], BF16, "ExternalOutput")
        MQ = C.dram("MQ", [8, 96, TPC], BF16, "ExternalOutput")
        MK = C.dram("MK", [8, 96, TPC], BF16, "ExternalOutput")
        MV = C.dram("MV", [8, 128, TPC // 128, 64], BF16, "ExternalOutput")

        w_v = w_in.ap().rearrange("(c p) n -> p c n", p=128)
        xnT = C.sb([128, 8, NTP], BF16, "xnT")
        f32t = Rot([(("f32t", i), C.sb([128, 512], F32, "f32t")) for i in range(4)])
        uT = C.sb([128, 4, NTP], BF16, "uT")
        cacc = [C.sb([128, 512], F32, "cacc") for g in range(4)]
        csq = [C.sb([128, 512], F32, "csq") for g in range(4)]
        cpar = C.sb([128, 4, 34], F32, "cpar")
        rope_sb = C.sb([96, 2, 512], F32, "rope")
        cql = C.sb([128, 3, 512], F32, "cql")
        ckl = C.sb([128, 2, 512], F32, "ckl")
        latsq = C.sb([128, 3, 512], BF16, "latsq")
        cqn = C.sb([128, 3, 512], BF16, "cqn")
        ckn = C.sb([128, 2, 512], BF16, "ckn")
        krt = C.sb([128, 512], F32, "krt")
        hx = Rot([(("hx", i), C.sb([128, 512], F32, "hx")) for i in range(2)])
        hsq = Rot([(("hsq", i), C.sb([128, 512], BF16, "hsq")) for i in range(2)])
        hxg = Rot([(("hxg", i), C.sb([128, 512], BF16, "hxg")) for i in range(2)])
        wuqb = C.sb([128, 3, 768], BF16, "wuqb")
        wukvb = C.sb([128, 2, 1024], BF16, "wukvb")
        wkrp = C.sb([128, 8, 128], BF16, "wkrp")
        gqk_sb = C.sb([96, 2], F32, "gqk")
        rmat_sb = C.sb([96, 96], BF16, "rmat")
        gcq_sb = C.sb([128, 3], F32, "gcqs")
        gckv_sb = C.sb([128, 2], F32, "gckvs")
        ident = C.sb([128, 128], BF16, "ident")
        gm = C.sb([128, 8], F32, "gm")
        onesb = C.sb([128, 128], BF16, "onesb")
        ones32 = C.sb([128, 128], F32, "ones32")
        xin = Rot([(("xin", i), C.sb([128, D], F32, "xin")) for i in range(2)])
        sqj = C.sb([128, D], F32, "sqj")
        xs = Rot([(("xs", i), C.sb([128, D], BF16, "xs")) for i in range(2)])
        stat = Rot([(("stat", i), C.sb([128, 2], F32, "stat")) for i in range(2)])
        wst = Rot([(("wst", i), C.sb([128, 8, 512], F32, "wst")) for i in range(3)])
        wb = Rot([(("wb", i), C.sb([128, 8, 512], BF16, "wb")) for i in range(3)])
        ob = Rot([(("ob", i), C.sb([128, 512], BF16, "ob")) for i in range(4)])
        g4sb = C.sb([128, NTP // 128, 16], F32, "g4sb")
        pacc = Rot([(("pacc", i), C.ps([128, 512], F32, "pacc")) for i in range(5)])
        ptr = Rot([(("ptr", i), C.ps([128, 1024], BF16, "ptr")) for i in range(2)])

        DMA(P, "sp", cpar[:], convp[:, :, :], [], ["cpar"])
        DMA(P, "sp", gqk_sb[:], gqk[:, :], [], ["gqk"])
        DMA(P, "sp", rmat_sb[:], rmat[:, :], [], ["rmat"])
        DMA(P, "sp", gcq_sb[:], gcq[:, :], [], ["gcqs"])
        DMA(P, "sp", gckv_sb[:], gckv[:, :], [], ["gckvs"])
        DMA(P, "sp", gm[:], gmix[:, :], [], ["gm"])
        if "mla" in phases:
            sk0, st0 = wst.items[0]
            st0f = st0[:].rearrange("p a b -> p (a b)")
            DMA(P, "sp", st0f[:, 0:2304].rearrange("p (a b) -> p a b", a=3),
                w_uq.ap().rearrange("(c p) n -> p c n", p=128), [], [sk0])
            for j in range(3):
                TS(P, "dve", wuqb[:, j, :], st0f[:, j * 768:(j + 1) * 768],
                   gcq_sb[:, j:j + 1], None, ALU.mult, None, [sk0, "gcqs"], ["wuqb"])
            sk1, st1 = wst.items[1]
            st1f = st1[:].rearrange("p a b -> p (a b)")
            DMA(P, "sp", st1f[:, 0:2048].rearrange("p (a b) -> p a b", a=2),
                w_ukv.ap().rearrange("(c p) n -> p c n", p=128), [], [sk1])
            for j in range(2):
                src = st1f[:, j * 1024:(j + 1) * 1024].rearrange("p (h x) -> p h x", h=8)
                TS(P, "dve", wukvb[:, j, 0:512].rearrange("p (h x) -> p h x", h=8), src[:, :, 0:64],
                   gckv_sb[:, j:j + 1], None, ALU.mult, None, [sk1, "gckvs"], ["wukvb"])
                TS(P, "dve", wukvb[:, j, 512:1024].rearrange("p (h x) -> p h x", h=8), src[:, :, 64:128],
                   gckv_sb[:, j:j + 1], None, ALU.mult, None, [sk1, "gckvs"], ["wukvb"])
            MEMSET(P, "pool", wkrp[:], 0.0, ["wkrp"])
            sk2_, st2_ = wst.items[0]
            DMA(P, "sp", st2_[:, :, 0:32], w_v[:, :, O_CKR:O_CKR + 32], [], [sk2_])
            for k in range(8):
                TS(P, "dve", wkrp[:, k, 64:96], st2_[:, k, 0:32], gm[:, k:k + 1], None, ALU.mult, None,
                   [sk2_, "gm"], ["wkrp"])
            MEMSET(P, "pool", krt[:], 0.0, ["krt"])
        DMA(P, "sp", ident[:], identb[:, :], [], ["ident"])
        DMA(P, "sp", gm[:], gmix[:, :], [], ["gm"])
        DMA(P, "sp", ones32[:], onesf[:, :], [], ["ones32"])
        CP(P, "dve", onesb[:], ones32[:], ["ones32"], ["onesb"])

        evac_i = [0]

        def load_w_impl(c0, ncols):
            sk, st = wst.next()
            bk, bt = wb.next()
            DMA(P, "sp", st[:, :, 0:ncols], w_v[:, :, c0:c0 + ncols], [], [sk])
            for k in range(8):
                ACT(P, bt[:, k, 0:ncols], st[:, k, 0:ncols], AF.Copy, [sk, "gm"], [(bk, k)], scale=gm[:, k:k + 1])
            return [(bk, k) for k in range(8)], bt

        wlist = []
        for _ps in range(NPASS):
            if "fm" in phases:
                wlist += [(O_AQ, 512), (O_AK, 512)]
            if "tm" in phases:
                wlist += [(O_AK, 512), (O_AV, 512), (O_AO, 512)] + [(O_GTS + j * 512, 512) for j in range(6)] + [(O_AG, 16)]
            if "conv" in phases:
                wlist += [(O_GLU, 512), (O_GLU + 512, 512)]
            if "mla" in phases:
                wlist += [(O_CQ, 384), (O_CKV, 288)]
        issued = []

        def nextw(c0, ncols):
            if not issued:
                issued.append((wlist[0], load_w_impl(*wlist.pop(0))))
            req, cur = issued.pop(0)
            assert req == (c0, ncols), (req, c0, ncols)
            if wlist:
                issued.append((wlist[0], load_w_impl(*wlist.pop(0))))
            return cur

        for ps_i in range(NPASS):
            t0 = ps_i * TP
            for t in range(NTP // 128):
                xk, xt = xin.next()
                sk2, stt = stat.next()
                xsk, xst = xs.next()
                pk, pt = ptr.next()
                r0 = t0 + t * 128
                DMA(P, "sp", xt[:], xe[r0:r0 + 128, :], [], [xk])
                ACT(P, sqj[:], xt[:], AF.Square, [xk], ["sqj"])
                RSUM(P, "dve", stt[:, 0:1], sqj[:], ["sqj"], [sk2])
                ACT(P, stt[:, 1:2], stt[:, 0:1], AF.Sqrt, [sk2], [sk2], scale=1.0 / D, bias=EPS)
                RECIP(P, "dve", stt[:, 1:2], stt[:, 1:2], [sk2], [sk2])
                ACT(P, xst[:], xt[:], AF.Copy, [xk, sk2], [xsk], scale=stt[:, 1:2])
                for k in range(8):
                    TR(P, pt[:, k * 128:(k + 1) * 128], xst[:, k * 128:(k + 1) * 128], ident[:],
                       [xsk, "ident"], [pk])
                CP(P, ("dve", "pool")[0], xnT[:, :, t * 128:(t + 1) * 128],
                   pt[:].rearrange("p (k t) -> p k t", k=8), [pk], [("xnT", t)])

            def xk_keys(tok0, ntok):
                return [("xnT", t) for t in range(tok0 // 128, (tok0 + ntok - 1) // 128 + 1)]

            if "fm" in phases:
                for (c0, dst) in ((O_AQ, QT), (O_AK, KT)):
                    wk, wt = nextw(c0, 512)
                    for cb in range(4):
                        for tb in range(TP // 512):
                            tk0 = HALO + tb * 512
                            ak, at = pacc.next()
                            for k in range(8):
                                MM(P, at[:, :], wt[:, k, cb * 128:(cb + 1) * 128], xnT[:, k, tk0:tk0 + 512],
                                   k == 0, k == 7, [wk[k]] + xk_keys(tk0, 512), [ak])
                            okk, ot = ob.next()
                            evac_i[0] += 1
                            CP(P, ("act", "dve")[evac_i[0] % 2], ot[:, :], at[:, :], [ak], [okk])
                            DMA(P, "sp", dst[cb * 128:(cb + 1) * 128, t0 + tb * 512:t0 + (tb + 1) * 512], ot[:, :],
                                [okk], [])

            if "tm" in phases:
                blocks = [(O_AK, Kt, 0, "copy"), (O_AV, Vt, 0, "copy"), (O_AO, OG, 0, "sig")]
                for j in range(6):
                    blocks.append((O_GTS + j * 512, GTS, j * 512, "sig"))
                for (c0, dst, dc0, mode) in blocks:
                    wk, wt = nextw(c0, 512)
                    for t in range(TP // 128):
                        tk0 = HALO + t * 128
                        ak, at = pacc.next()
                        for k in range(8):
                            MM(P, at[:, :], xnT[:, k, tk0:tk0 + 128], wt[:, k, :], k == 0, k == 7,
                               [wk[k]] + xk_keys(tk0, 128), [ak])
                        okk, ot = ob.next()
                        if mode == "sig":
                            ACT(P, ot[:, :], at[:, :], AF.Sigmoid, [ak], [okk])
                        else:
                            evac_i[0] += 1
                            CP(P, ("act", "dve")[evac_i[0] % 2], ot[:, :], at[:, :], [ak], [okk])
                        if dst is GTS:
                            DMA(P, "sp", dst[t0 + t * 128:t0 + (t + 1) * 128, dc0:dc0 + 512], ot[:, :], [okk], [])
                        else:
                            DMA(P, "sp", dst[:, t0 + t * 128:t0 + (t + 1) * 128, :].rearrange("h t d -> t h d"),
                                ot[:, :].rearrange("p (h d) -> p h d", h=4), [okk], [])
                wk, wt = nextw(O_AG, 16)
                for t in range(TP // 128):
                    tk0 = HALO + t * 128
                    ak, at = pacc.next()
                    for k in range(8):
                        MM(P, at[:, 0:16], xnT[:, k, tk0:tk0 + 128], wt[:, k, 0:16], k == 0, k == 7,
                           [wk[k]] + xk_keys(tk0, 128), [ak])
                    CP(P, "dve", g4sb[:, t, :].rearrange("p (h g) -> p h g", h=4), at[:, 0:16].rearrange("p (g h) -> p h g", h=4),
                       [ak], [("g4", t)])
                for h in range(4):
                    DMA(P, "sp", G4[h, t0:t0 + TP, :].rearrange("(t p) g -> p t g", p=128), g4sb[:, 0:TP // 128, h * 4:(h + 1) * 4],
                        [("g4", t) for t in range(TP // 128)], [])

            if "conv" in phases:
                wak, wat = nextw(O_GLU, 512)
                wgk, wgt = nextw(O_GLU + 512, 512)
                blks = [(b0, min(512, NTP - b0)) for b0 in range(0, NTP, 512)]
                for g in range(4):
                    for (b0, bn) in blks:
                        ak, at = pacc.next()
                        gk, gt = pacc.next()
                        for k in range(8):
                            MM(P, at[:, 0:bn], wat[:, k, g * 128:(g + 1) * 128], xnT[:, k, b0:b0 + bn],
                               k == 0, k == 7, [wak[k]] + xk_keys(b0, bn), [ak])
                        for k in range(8):
                            MM(P, gt[:, 0:bn], wgt[:, k, g * 128:(g + 1) * 128], xnT[:, k, b0:b0 + bn],
                               k == 0, k == 7, [wgk[k]] + xk_keys(b0, bn), [gk])
                        fk, ft = f32t.next()
                        ACT(P, ft[:, 0:bn], gt[:, 0:bn], AF.Sigmoid, [gk], [fk])
                        TT(P, "dve", uT[:, g, b0:b0 + bn], at[:, 0:bn], ft[:, 0:bn], ALU.mult, [ak, fk],
                           [("uT", g, b0 // 512)])
                for tb in range(TP // 512):
                    c0 = HALO + tb * 512 - 15
                    ukeys = lambda g: [("uT", g, j) for j in range(c0 // 512, (c0 + 542 - 1) // 512 + 1)]
                    for g in range(4):
                        eng = "dve"
                        ck = ("cacc", g)
                        ca = cacc[g]
                        TS(P, eng, ca[:, :], uT[:, g, c0:c0 + 512], cpar[:, g, 0:1], cpar[:, g, 31:32],
                           ALU.mult, ALU.add, ukeys(g) + ["cpar"], [ck])
                        for k in range(1, 31):
                            STT(P, eng, ca[:, :], uT[:, g, c0 + k:c0 + k + 512], cpar[:, g, k:k + 1], ca[:, :],
                                ALU.mult, ALU.add, ukeys(g) + ["cpar", ck], [ck])
                    mk, mt = pacc.next()
                    for g in range(4):
                        MM(P, mt[:, :], ones32[:, :], cacc[g][:, :], g == 0, g == 3, ["ones32", ("cacc", g)], [mk])
                    for g in range(4):
                        STT(P, "dve", cacc[g][:, :], mt[:, :], -1.0 / 512, cacc[g][:, :], ALU.mult, ALU.add,
                            [mk, ("cacc", g)], [("cacc", g)])
                        ACT(P, csq[g][:, :], cacc[g][:, :], AF.Square, [("cacc", g)], [("csq", g)])
                    vk, vt = pacc.next()
                    for g in range(4):
                        MM(P, vt[:, :], ones32[:, :], csq[g][:, :], g == 0, g == 3, ["ones32", ("csq", g)], [vk])
                    fk, ft = f32t.next()
                    ACT(P, ft[:, :], vt[:, :], AF.Sqrt, [vk], [fk], scale=1.0 / 512, bias=EPS)
                    RECIP(P, "dve", ft[:, :], ft[:, :], [fk], [fk])
                    for g in range(4):
                        TT(P, "dve", csq[g][:, :], cacc[g][:, :], ft[:, :], ALU.mult, [("cacc", g), fk], [("csq", g)])
                        TS(P, "pool", csq[g][:, :], csq[g][:, :], cpar[:, g, 32:33], cpar[:, g, 33:34],
                           ALU.mult, ALU.add, [("csq", g), "cpar"], [("csq", g)])
                        okk, ot = ob.next()
                        ACT(P, ot[:, :], csq[g][:, :], AF.Silu, [("csq", g)], [okk])
                        DMA(P, "sp", UT[g * 128:(g + 1) * 128, t0 + tb * 512:t0 + (tb + 1) * 512], ot[:, :], [okk], [])

            if "mla" in phases:
                wqk, wqt = nextw(O_CQ, 384)
                wkk, wkt = nextw(O_CKV, 288)
                for tb in range(TP // 512):
                    tk0 = HALO + tb * 512
                    g0 = t0 + tb * 512
                    DMA(P, "sp", rope_sb[:, :, :], ropeT[:, :, g0:g0 + 512], [], ["rope"])
                    for (wk_, wt_, nblk, lat, latn, lkey, dim) in ((wqk, wqt, 3, cql, cqn, "cq", 384.0),
                                                                 (wkk, wkt, 2, ckl, ckn, "ckv", 256.0)):
                        for j in range(nblk):
                            ak, at = pacc.next()
                            for k in range(8):
                                MM(P, at[:, :], wt_[:, k, j * 128:(j + 1) * 128], xnT[:, k, tk0:tk0 + 512],
                                   k == 0, k == 7, [wk_[k]] + xk_keys(tk0, 512), [ak])
                            CP(P, "dve", lat[:, j, :], at[:, :], [ak], [(lkey, j)])
                            ACT(P, latsq[:, j, :], lat[:, j, :], AF.Square, [(lkey, j)], [(lkey + "sq", j)])
                        sk_, st_ = pacc.next()
                        for j in range(nblk):
                            MM(P, st_[:, :], onesb[:, :], latsq[:, j, :], j == 0, j == nblk - 1,
                               ["onesb", (lkey + "sq", j)], [sk_])
                        fk, ft = f32t.next()
                        ACT(P, ft[:, :], st_[:, :], AF.Sqrt, [sk_], [fk], scale=1.0 / dim, bias=EPS)
                        RECIP(P, "dve", ft[:, :], ft[:, :], [fk], [fk])
                        for j in range(nblk):
                            if "dbg5" in phases:
                                TT(P, "dve", lat[:, j, :], lat[:, j, :], ft[:, :], ALU.mult,
                                   [(lkey, j), fk], [(lkey, j)])
                                CP(P, "act", latn[:, j, :], lat[:, j, :], [(lkey, j)], [(lkey + "n", j)])
                            else:
                                TT(P, "dve", latn[:, j, :], lat[:, j, :], ft[:, :], ALU.mult,
                                   [(lkey, j), fk], [(lkey + "n", j)])
                    if "nokr" not in phases:
                        ak, at = pacc.next()
                        for k in range(8):
                            MM(P, at[:, :], wkrp[:, k, :], xnT[:, k, tk0:tk0 + 512], k == 0, k == 7,
                               ["wkrp"] + xk_keys(tk0, 512), [ak])
                        CP(P, "act", krt[0:96, :], at[0:96, :], [ak], ["krt"])
                    cqn_keys = [("cqn", j) for j in range(3)]
                    ckn_keys = [("ckvn", j) for j in range(2)]
                    for h in (range(8) if "nomlah" not in phases else []):
                        for which in ("q", "k"):
                            ak, at = pacc.next()
                            xk_, xt_ = hx.next()
                            if which == "q":
                                for j in range(3):
                                    MM(P, at[0:96, :], wuqb[:, j, h * 96:(h + 1) * 96], cqn[:, j, :], j == 0, j == 2,
                                       ["wuqb"] + cqn_keys, [ak])
                                CP(P, "act", xt_[0:96, :], at[0:96, :], [ak], [xk_])
                            else:
                                for j in range(2):
                                    MM(P, at[0:64, :], wukvb[:, j, h * 64:(h + 1) * 64], ckn[:, j, :], j == 0, j == 1,
                                       ["wukvb"] + ckn_keys, [ak])
                                CP(P, "act", xt_[0:64, :], at[0:64, :], [ak], [xk_])
                                CP(P, "pool", xt_[64:96, :], krt[64:96, :], ["krt"], [xk_])
                            sqk, sqt = hsq.next()
                            ACT(P, sqt[0:96, :], xt_[0:96, :], AF.Square, [xk_], [sqk])
                            sk_, st_ = pacc.next()
                            MM(P, st_[0:96, :], onesb[0:96, 0:96], sqt[0:96, :], True, True, ["onesb", sqk], [sk_])
                            fk, ft = f32t.next()
                            ACT(P, ft[0:96, :], st_[0:96, :], AF.Sqrt, [sk_], [fk], scale=1.0 / 96, bias=EPS)
                            RECIP(P, "dve", ft[0:96, :], ft[0:96, :], [fk], [fk])
                            gcol = 0 if which == "q" else 1
                            xgk, xgt = hxg.next()
                            TS(P, "dve", xgt[0:96, :], xt_[0:96, :], gqk_sb[0:96, gcol:gcol + 1], None, ALU.mult, None,
                               [xk_, "gqk"], [xgk])
                            rk, rt = pacc.next()
                            MM(P, rt[0:96, :], rmat_sb[0:96, 0:96], xgt[0:96, :], True, True, ["rmat", xgk], [rk])
                            t1k, t1 = f32t.next()
                            TT(P, "pool", t1[0:96, :], xgt[0:96, :], rope_sb[:, 0, :], ALU.mult, [xgk, "rope"], [t1k])
                            t2k, t2 = f32t.next()
                            TT(P, "dve", t2[0:96, :], rt[0:96, :], rope_sb[:, 1, :], ALU.mult, [rk, "rope"], [t2k])
                            TT(P, "pool", t1[0:96, :], t1[0:96, :], t2[0:96, :], ALU.add, [t1k, t2k], [t1k])
                            okk, ot = ob.next()
                            TT(P, "dve", ot[0:96, :], t1[0:96, :], ft[0:96, :], ALU.mult, [t1k, fk], [okk])
                            dst = MQ if which == "q" else MK
                            DMA(P, "sp", dst[h, :, g0:g0 + 512], ot[0:96, :], [okk], [])
                    if "dupgrp" in phases:
                        for rep in range(2):
                            ak, at = pacc.next()
                            for k in range(8):
                                MM(P, at[:, :], wkt[:, k, 0:128], xnT[:, k, tk0:tk0 + 512],
                                   k == 0, k == 7, [wkk[k]] + xk_keys(tk0, 512), [ak])
                    for t in (range(4 if "v_one" not in phases else 1) if "nomlav" not in phases else []):
                        ak, at = pacc.next()
                        for j in range(2):
                            MM(P, at[:, :], (xnT[:, j, tk0 + t * 128:tk0 + (t + 1) * 128] if "dbg1" in phases else ckn[:, j, t * 128:(t + 1) * 128]),
                               (wkt[:, j, :] if "dbg2" in phases else (wukvb[:, j, 0:512] if "dbg4" in phases else wukvb[:, j, 512:1024])), j == 0, j == 1,
                               ["wukvb"] + ckn_keys, [ak])
                        okk, ot = ob.next()
                        if "v_noevac" in phases:
                            continue
                        CP(P, "dve", ot[:, :], at[:, :], [ak], [okk])
                        if "v_nodma" in phases:
                            continue
                        DMA(P, "sp", MV[:, :, (g0 + t * 128) // 128, :].rearrange("h p d -> p h d"),
                            ot[:, :].rearrange("p (h d) -> p h d", h=8), [okk], [])

        P.emit()
        stats = P.stats
    return nc, stats


def _gain_cols(g, nch):
    return np.ascontiguousarray(g.reshape(nch, 128).T)


def rope_tables():
    pos = np.arange(S, dtype=np.float32)
    inv = (10000.0 ** (-np.arange(0, 32, 2, dtype=np.float32) / np.float32(32))).astype(np.float32)
    ang = (pos[:, None] * inv[None, :]).astype(np.float32)
    c = np.cos(ang.astype(np.float64)).astype(np.float32)
    s = np.sin(ang.astype(np.float64)).astype(np.float32)
    CT = np.ones((96, S), np.float32)
    ST = np.zeros((96, S), np.float32)
    CT[64:80] = c.T
    CT[80:96] = c.T
    ST[64:80] = s.T
    ST[80:96] = s.T
    return CT, ST


def consts():
    R = np.zeros((96, 96), np.float32)
    for j in range(16):
        R[80 + j, 64 + j] = -1.0
        R[64 + j, 80 + j] = 1.0
    return dict(identb=np.eye(128, dtype=np.float32).astype(NPBF), rmat=R.astype(NPBF),
                onesf=np.ones((128, 128), np.float32))


def stageA_inmaps(x, prm, l):
    CT, ST = rope_tables()
    cst = consts()
    convp = np.zeros((128, 4, 34), np.float32)
    cw = prm["conv_w"][l]
    convp[:, :, 0:31] = cw.T.reshape(4, 128, 31).transpose(1, 0, 2)
    convp[:, :, 31] = prm["conv_b"][l].reshape(4, 128).T
    convp[:, :, 32] = prm["conv_ln_g"][l].reshape(4, 128).T
    convp[:, :, 33] = prm["conv_ln_b"][l].reshape(4, 128).T
    maps = []
    for c in range(NCORES):
        b, q = c // 4, c % 4
        s0 = q * TPC
        xe = np.zeros((TPC + 2 * HALO, D), np.float32)
        lo, hi = max(0, s0 - HALO), min(S, s0 + TPC + HALO)
        xe[lo - (s0 - HALO):hi - (s0 - HALO)] = x[b, lo:hi]
        rope = np.stack([CT[:, s0:s0 + TPC], ST[:, s0:s0 + TPC]], axis=1)
        maps.append(dict(
            xe=xe, w_in=prm["w_in"][l], gmix=_gain_cols(prm["mix_norm_g"][l], 8),
            identb=cst["identb"], convp=convp, gcq=_gain_cols(prm["cq_norm_g"][l], 3),
            gckv=_gain_cols(prm["ckv_norm_g"][l], 2), w_uq=prm["w_uq"][l], w_ukv=prm["w_ukv"][l],
            gqk=np.ascontiguousarray(np.stack([prm["q_norm_g"][l], prm["k_norm_g"][l]], axis=1)),
            ropeT=np.ascontiguousarray(rope), rmat=cst["rmat"], onesf=cst["onesf"]))
    return maps


def build_mlstm(nch=S // 128, env=None, pre=None):
    nc, es, C, P = _begin(env, pre)
    ns = nch * 128
    lnscale = math.log(128.0 ** -0.5)
    with es:
        qTd = C.dram("qT", [128, ns], BF16, "ExternalInput")
        ktd = C.dram("kt", [ns, 128], BF16, "ExternalInput")
        vtd = C.dram("vt", [ns, 128], BF16, "ExternalInput")
        g4d = C.dram("g4", [ns, 4], F32, "ExternalInput")
        bifd = C.dram("bif", [128, 4], F32, "ExternalInput")
        ogd = C.dram("og", [ns, 128], BF16, "ExternalInput")
        gAd = C.dram("gA", [128, 128], F32, "ExternalInput")
        cmat = C.dram("cmat", [128, 6, 128], F32, "ExternalInput")
        identbd = C.dram("identb", [128, 128], BF16, "ExternalInput")
        HT = C.dram("HT", [128, ns], BF16, "ExternalOutput")

        qT = C.sb([128, ns], BF16, "qT")
        kt = C.sb([128, nch, 128], BF16, "kt")
        vt = C.sb([128, nch, 129], BF16, "vt")
        hacc = C.sb([128, nch, 128], F32, "hacc")
        g4 = C.sb([128, nch, 4], F32, "g4")
        bif = C.sb([128, 4], F32, "bif")
        nbif = C.sb([128, 4], F32, "nbif")
        gA = C.sb([128, 128], F32, "gA")
        cm = C.sb([128, 6, 128], F32, "cm")
        identb = C.sb([128, 128], BF16, "identb")
        gt = {n: C.sb([128, nch], F32, n) for n in ("lf", "ib", "bcum", "gtot", "biasS", "wint", "wk", "dec", "tmp")}
        Cf = C.sb([128, 129], F32, "Cf")
        Cb = C.sb([128, 129], BF16, "Cb")
        LF = Rot([(("LF", i), C.sb([128, 128], F32, "LF")) for i in range(2)])
        Dm = Rot([(("Dm", i), C.sb([128, 128], F32, "Dm")) for i in range(2)])
        kTc = Rot([(("kTc", i), C.sb([128, 128], BF16, "kTc")) for i in range(2)])
        SD = Rot([(("SD", i), C.sb([128, 128], BF16, "SD")) for i in range(2)])
        isb = Rot([(("isb", i), C.sb([128, 129], F32, "isb")) for i in range(2)])
        num = Rot([(("num", i), C.sb([128, 129], F32, "num")) for i in range(2)])
        dn = Rot([(("dn", i), C.sb([128, 2], F32, "dn")) for i in range(2)])
        Vw = Rot([(("Vw", i), C.sb([128, 129], BF16, "Vw")) for i in range(2)])
        ogt = Rot([(("ogt", i), C.sb([128, 128], BF16, "ogt")) for i in range(2)])
        hn = Rot([(("hn", i), C.sb([128, 128], F32, "hn")) for i in range(2)])
        hb = Rot([(("hb", i), C.sb([128, 128], BF16, "hb")) for i in range(2)])
        hT = Rot([(("hT", i), C.sb([128, 128], BF16, "hT")) for i in range(2)])
        sq = C.sb([128, 128], F32, "sq")
        pA = Rot([(("pA", i), C.ps([128, 512], F32, "pA")) for i in range(6)])
        pB = Rot([(("pB", i), C.ps([128, 1024], BF16, "pB")) for i in range(2)])

        DMA(P, "sp", qT[:, :], qTd[:, :], [], ["qT"])
        DMA(P, "sp", kt[:, :, :], ktd.ap().rearrange("(c p) d -> p c d", p=128), [], ["kt"])
        DMA(P, "sp", vt[:, :, 0:128], vtd.ap().rearrange("(c p) d -> p c d", p=128), [], ["vt"])
        MEMSET(P, "pool", vt[:, :, 128:129], 1.0, ["vt1"])
        DMA(P, "sp", g4[:, :, :], g4d.ap().rearrange("(c p) g -> p c g", p=128), [], ["g4"])
        DMA(P, "sp", bif[:, :], bifd[:, :], [], ["bif"])
        DMA(P, "sp", gA[:, :], gAd[:, :], [], ["gA"])
        DMA(P, "sp", cm[:, :, :], cmat[:, :, :], [], ["cm"])
        DMA(P, "sp", identb[:, :], identbd[:, :], [], ["identb"])
        TS(P, "dve", nbif[:, :], bif[:, :], -1.0, None, ALU.mult, None, ["bif"], ["nbif"])
        ident32 = cm[:, 4, :]
        ones32 = cm[:, 5, :]
        for d in range(2):
            Ud = cm[:, d, :]
            NEGd = cm[:, 2 + d, :]
            ic, fc = 2 * d, 2 * d + 1
            ACT(P, gt["tmp"][:, :], g4[:, :, fc], AF.Exp, ["g4", "nbif"], ["tmp"], scale=-1.0, bias=nbif[:, fc:fc + 1])
            ACT(P, gt["tmp"][:, :], gt["tmp"][:, :], AF.Ln, ["tmp"], ["tmp"], bias=1.0)
            TS(P, "dve", gt["lf"][:, :], gt["tmp"][:, :], -1.0, None, ALU.mult, None, ["tmp"], ["lf"])
            TS(P, "dve", gt["ib"][:, :], g4[:, :, ic], bif[:, ic:ic + 1], lnscale, ALU.add, ALU.add, ["g4", "bif"], ["ib"])
            bk, bp = pA.next()
            MM(P, bp[:, 0:nch], Ud, gt["lf"][:, :], True, True, ["cm", "lf"], [bk])
            CP(P, "dve", gt["bcum"][:, :], bp[:, 0:nch], [bk], ["bcum"])
            gk, gp = pA.next()
            MM(P, gp[:, 0:nch], ones32, gt["lf"][:, :], True, True, ["cm", "lf"], [gk])
            CP(P, "dve", gt["gtot"][:, :], gp[:, 0:nch], [gk], ["gtot"])
            TT(P, "dve", gt["biasS"][:, :], gt["ib"][:, :], gt["bcum"][:, :], ALU.subtract, ["ib", "bcum"], ["biasS"])
            ACT(P, gt["wint"][:, :], gt["bcum"][:, :], AF.Exp, ["bcum"], ["wint"])
            TT(P, "dve", gt["tmp"][:, :], gt["biasS"][:, :], gt["gtot"][:, :], ALU.add, ["biasS", "gtot"], ["tmp"])
            ACT(P, gt["wk"][:, :], gt["tmp"][:, :], AF.Exp, ["tmp"], ["wk"])
            ACT(P, gt["dec"][:, :], gt["gtot"][:, :], AF.Exp, ["gtot"], ["dec"])
            MEMSET(P, "dve", Cf[:, :], 0.0, ["Cf"])
            MEMSET(P, "pool", Cb[:, :], 0.0, ["Cb"])
            order = range(nch) if d == 0 else range(nch - 1, -1, -1)
            for c in order:
                lk, lt = LF.next()
                ACT(P, lt[:, :], ones32, AF.Copy, ["cm", "lf"], [lk], scale=gt["lf"][:, c:c + 1])
                dk, dp = pA.next()
                MM(P, dp[:, 0:128], lt[:, :], Ud, True, False, [lk, "cm"], [dk])
                MM(P, dp[:, 0:128], ident32, NEGd, False, True, ["cm"], [dk])
                mk_, mt_ = Dm.next()
                ACT(P, mt_[:, :], dp[:, 0:128], AF.Exp, [dk, "biasS"], [mk_], bias=gt["biasS"][:, c:c + 1])
                tk, tp = pB.next()
                TR(P, tp[:, 0:128], kt[:, c, :], identb[:, :], ["kt", "identb"], [tk])
                kck, kct = kTc.next()
                CP(P, "dve", kct[:, :], tp[:, 0:128], [tk], [kck])
                sk, sp_ = pA.next()
                MM(P, sp_[:, 0:128], kct[:, :], qT[:, c * 128:(c + 1) * 128], True, True, [kck, "qT"], [sk])
                sdk, sdt = SD.next()
                TT(P, "dve", sdt[:, :], sp_[:, 0:128], mt_[:, :], ALU.mult, [sk, mk_], [sdk])
                nk_, np_ = pA.next()
                MM(P, np_[:, 0:129], sdt[:, :], vt[:, c, :], True, True, [sdk, "vt", "vt1"], [nk_])
                ik, ip = pA.next()
                MM(P, ip[:, 0:129], qT[:, c * 128:(c + 1) * 128], Cb[:, :], True, True, ["qT", "Cb"], [ik])
                isk, ist = isb.next()
                ACT(P, ist[:, :], ip[:, 0:129], AF.Copy, [ik, "wint"], [isk], scale=gt["wint"][:, c:c + 1])
                nmk, nmt = num.next()
                TT(P, "dve", nmt[:, :], np_[:, 0:129], ist[:, :], ALU.add, [nk_, isk], [nmk])
                dnk, dnt = dn.next()
                ACT(P, dnt[:, 0:1], nmt[:, 128:129], AF.Abs, [nmk], [dnk])
                TS(P, "dve", dnt[:, 0:1], dnt[:, 0:1], 1.0, None, ALU.max, None, [dnk], [dnk])
                RECIP(P, "dve", dnt[:, 1:2], dnt[:, 0:1], [dnk], [dnk])
                if d == 0:
                    TS(P, "dve", hacc[:, c, :], nmt[:, 0:128], dnt[:, 1:2], None, ALU.mult, None, [nmk, dnk], [("hacc", c)])
                else:
                    STT(P, "dve", hacc[:, c, :], nmt[:, 0:128], dnt[:, 1:2], hacc[:, c, :], ALU.mult, ALU.add,
                        [nmk, dnk, ("hacc", c)], [("hacc", c)])
                vwk, vwt = Vw.next()
                TS(P, "pool", vwt[:, :], vt[:, c, :], gt["wk"][:, c:c + 1], None, ALU.mult, None, ["vt", "vt1", "wk"], [vwk])
                ck, cp_ = pA.next()
                MM(P, cp_[:, 0:129], kt[:, c, :], vwt[:, :], True, True, ["kt", vwk], [ck])
                STT(P, "dve", Cf[:, :], Cf[:, :], gt["dec"][:, c:c + 1], cp_[:, 0:129], ALU.mult, ALU.add,
                    ["Cf", "dec", ck], ["Cf"])
                CP(P, "act", Cb[:, :], Cf[:, :], ["Cf"], ["Cb"])
        for c in range(nch):
            ogk, ogt_ = ogt.next()
            DMA(P, "sp", ogt_[:, :], ogd[c * 128:(c + 1) * 128, :], [], [ogk])
            dnk, dnt = dn.next()
            ACT(P, sq[:, :], hacc[:, c, :], AF.Square, [("hacc", c)], ["sq"])
            RSUM(P, "dve", dnt[:, 0:1], sq[:, :], ["sq"], [dnk])
            ACT(P, dnt[:, 1:2], dnt[:, 0:1], AF.Sqrt, [dnk], [dnk], scale=1.0 / 128, bias=EPS)
            RECIP(P, "dve", dnt[:, 1:2], dnt[:, 1:2], [dnk], [dnk])
            hk, ht = hn.next()
            STT(P, "dve", ht[:, :], hacc[:, c, :], dnt[:, 1:2], gA[:, :], ALU.mult, ALU.mult, [("hacc", c), dnk, "gA"], [hk])
            hbk, hbt = hb.next()
            TT(P, "pool", hbt[:, :], ht[:, :], ogt_[:, :], ALU.mult, [hk, ogk], [hbk])
            tk, tp = pB.next()
            TR(P, tp[:, 0:128], hbt[:, :], identb[:, :], [hbk, "identb"], [tk])
            htk, htt = hT.next()
            CP(P, "act", htt[:, :], tp[:, 0:128], [tk], [htk])
            DMA(P, "sp", HT[:, c * 128:(c + 1) * 128], htt[:, :], [htk], [])
        P.emit()
        stats = P.stats
    return nc, stats


def mlstm_consts():
    s_ = np.arange(128)[:, None]
    t_ = np.arange(128)[None, :]
    U = (s_ <= t_).astype(np.float32)
    cm = np.zeros((128, 6, 128), np.float32)
    cm[:, 0] = U
    cm[:, 1] = U.T
    cm[:, 2] = np.where(s_ <= t_, 0.0, -30000.0)
    cm[:, 3] = np.where(s_ >= t_, 0.0, -30000.0)
    cm[:, 4] = np.eye(128)
    cm[:, 5] = 1.0
    return dict(cmat=cm, identb=np.eye(128, dtype=np.float32).astype(NPBF))


def build_merge(ntile=TPC // 128, env=None, pre=None):
    nc, es, C, P = _begin(env, pre)
    nt = ntile * 128
    with es:
        xd = C.dram("x", [nt, D], F32, "ExternalInput")
        srcs = [C.dram(n, [512, nt], BF16, "ExternalInput") for n in ("HT", "UT", "OT")]
        gtsd = C.dram("GTS", [nt, 3072], BF16, "ExternalInput")
        wds = [C.dram(n, [512, D], F32, "ExternalInput") for n in ("w_a", "w_b", "w_c")]
        wod = C.dram("w_o", [D, D], F32, "ExternalInput")
        gfd = C.dram("gffn", [128, 8], F32, "ExternalInput")
        wrd = C.dram("w_r", [D, 16], F32, "ExternalInput")
        identbd = C.dram("identb", [128, 128], BF16, "ExternalInput")
        ident32d = C.dram("ident32", [128, 128], F32, "ExternalInput")
        x1d = C.dram("x1", [nt, D], F32, "ExternalOutput")
        xn2d = C.dram("xn2T", [D, nt], BF16, "ExternalOutput")
        affd = C.dram("aff", [nt, 16], F32, "ExternalOutput")
        affTd = C.dram("affT", [16, nt], F32, "ExternalOutput")
        affTs = C.sb([16, nt], F32, "affTs")

        wbr = [C.sb([128, 4, D], BF16, "wbr") for _ in range(3)]
        wo = C.sb([128, 8, D], BF16, "wo")
        wst = Rot([(("wst", i), C.sb([128, 4, D], F32, "wst")) for i in range(2)])
        wr = C.sb([128, 8, 16], F32, "wr")
        gf = C.sb([128, 8], F32, "gf")
        gfull = C.sb([128, 8, 128], F32, "gfull")
        identb = C.sb([128, 128], BF16, "identb")
        ident32 = C.sb([128, 128], F32, "ident32")
        srct = [Rot([((("src", b), i), C.sb([128, 4, 128], BF16, "src")) for i in range(2)]) for b in range(3)]
        gts = Rot([(("gts", i), C.sb([128, 3072], BF16, "gts")) for i in range(2)])
        xt = Rot([(("xt", i), C.sb([128, D], F32, "xt")) for i in range(2)])
        mg = C.sb([128, D], F32, "mg")
        tmpm = Rot([(("tmpm", i), C.sb([128, 512], F32, "tmpm")) for i in range(2)])
        mgb = C.sb([128, D], BF16, "mgb")
        mT = C.sb([128, 8, 128], BF16, "mT")
        x1 = Rot([(("x1", i), C.sb([128, D], F32, "x1")) for i in range(2)])
        sqj = C.sb([128, D], F32, "sqj")
        xs = C.sb([128, D], F32, "xs")
        st = Rot([(("st", i), C.sb([128, 16], F32, "st")) for i in range(2)])
        xT32 = C.sb([128, 8, 128], F32, "xT32")
        xTb = Rot([(("xTb", i), C.sb([128, 8, 128], BF16, "xTb")) for i in range(2)])
        lg = C.sb([128, 16], F32, "lg")
        ex = C.sb([128, 16], F32, "ex")
        affs = C.sb([128, ntile, 16], F32, "affs")
        pA = Rot([(("pA", i), C.ps([128, 512], F32, "pA")) for i in range(5)])
        pB = C.ps([128, 1024], BF16, "pB")

        DMA(P, "sp", identb[:, :], identbd[:, :], [], ["identb"])
        DMA(P, "sp", ident32[:, :], ident32d[:, :], [], ["ident32"])
        DMA(P, "sp", gf[:, :], gfd[:, :], [], ["gf"])
        DMA(P, "sp", wr[:, :, :], wrd.ap().rearrange("(c p) e -> p c e", p=128), [], ["wr"])
        for b in range(3):
            sk, stg = wst.next()
            DMA(P, "sp", stg[:, :, :], wds[b].ap().rearrange("(c p) n -> p c n", p=128), [], [sk])
            for c in range(4):
                CP(P, ("dve", "pool")[c % 2], wbr[b][:, c, :], stg[:, c, :], [sk], [("wbr", b)])
        for hh in range(2):
            sk, stg = wst.next()
            DMA(P, "sp", stg[:, :, :], wod.ap().rearrange("(c p) n -> p c n", p=128)[:, hh * 4:(hh + 1) * 4, :], [], [sk])
            for c in range(4):
                CP(P, ("dve", "pool")[c % 2], wo[:, hh * 4 + c, :], stg[:, c, :], [sk], ["wo"])
        for k in range(8):
            TS(P, "pool", gfull[:, k, :], ident32[:, :], 0.0, gf[:, k:k + 1], ALU.mult, ALU.add, ["ident32", "gf"], ["gfull"])
        for t in range(ntile):
            r0 = t * 128
            xk, xt_ = xt.next()
            DMA(P, "sp", xt_[:, :], xd[r0:r0 + 128, :], [], [xk])
            gk, gt_ = gts.next()
            DMA(P, "sp", gt_[:, :], gtsd[r0:r0 + 128, :], [], [gk])
            skeys = []
            stiles = []
            for b in range(3):
                k_, t_ = srct[b].next()
                DMA(P, "sp", t_[:, :, :], srcs[b].ap().rearrange("(c p) t -> p c t", p=128)[:, :, r0:r0 + 128], [], [k_])
                skeys.append(k_)
                stiles.append(t_)
            for b in range(3):
                for hf in range(2):
                    ak, at = pA.next()
                    for c in range(4):
                        MM(P, at[:, :], stiles[b][:, c, :], wbr[b][:, c, hf * 512:(hf + 1) * 512], c == 0, c == 3,
                           [skeys[b], ("wbr", b)], [ak])
                    gsl = gt_[:, b * 1024 + hf * 512:b * 1024 + (hf + 1) * 512]
                    if b == 0:
                        TT(P, "dve", mg[:, hf * 512:(hf + 1) * 512], at[:, :], gsl, ALU.mult, [ak, gk], [("mg", hf)])
                    else:
                        tk, tt_ = tmpm.next()
                        TT(P, "dve", tt_[:, :], at[:, :], gsl, ALU.mult, [ak, gk], [tk])
                        TT(P, "pool", mg[:, hf * 512:(hf + 1) * 512], mg[:, hf * 512:(hf + 1) * 512], tt_[:, :], ALU.add,
                           [("mg", hf), tk], [("mg", hf)])
            CP(P, "act", mgb[:, :], mg[:, :], [("mg", 0), ("mg", 1)], ["mgb"])
            for k in range(8):
                TR(P, pB[:, k * 128:(k + 1) * 128], mgb[:, k * 128:(k + 1) * 128], identb[:, :], ["mgb", "identb"], ["pB"])
            CP(P, "dve", mT[:, :, :], pB[:].rearrange("p (k t) -> p k t", k=8), ["pB"], ["mT"])
            x1k, x1t = x1.next()
            for hf in range(2):
                ak, at = pA.next()
                for k in range(8):
                    MM(P, at[:, :], mT[:, k, :], wo[:, k, hf * 512:(hf + 1) * 512], k == 0, k == 7, ["mT", "wo"], [ak])
                TT(P, "dve", x1t[:, hf * 512:(hf + 1) * 512], at[:, :], xt_[:, hf * 512:(hf + 1) * 512], ALU.add,
                   [ak, xk], [(x1k, hf)])
            DMA(P, "sp", x1d[r0:r0 + 128, :], x1t[:, :], [(x1k, 0), (x1k, 1)], [])
            sk_, st_ = st.next()
            ACT(P, sqj[:, :], x1t[:, :], AF.Square, [(x1k, 0), (x1k, 1)], ["sqj"])
            RSUM(P, "dve", st_[:, 0:1], sqj[:, :], ["sqj"], [sk_])
            ACT(P, st_[:, 1:2], st_[:, 0:1], AF.Sqrt, [sk_], [sk_], scale=1.0 / D, bias=EPS)
            RECIP(P, "dve", st_[:, 1:2], st_[:, 1:2], [sk_], [sk_])
            ACT(P, xs[:, :], x1t[:, :], AF.Copy, [(x1k, 0), (x1k, 1), sk_], ["xs"], scale=st_[:, 1:2])
            for hf in range(2):
                ak, at = pA.next()
                for k in range(4):
                    kk = hf * 4 + k
                    TR(P, at[:, k * 128:(k + 1) * 128], xs[:, kk * 128:(kk + 1) * 128], ident32[:, :], ["xs", "ident32"], [ak])
                TT(P, "dve", xT32[:, hf * 4:(hf + 1) * 4, :], at[:].rearrange("p (k t) -> p k t", k=4),
                   gfull[:, hf * 4:(hf + 1) * 4, :], ALU.mult, [ak, "gfull"], [("xT32", hf)])
            xbk, xbt = xTb.next()
            CP(P, "act", xbt[:, :, :], xT32[:, :, :], [("xT32", 0), ("xT32", 1)], [xbk])
            DMA(P, "sp", xn2d.ap().rearrange("(k p) t -> p k t", p=128)[:, :, r0:r0 + 128], xbt[:, :, :], [xbk], [])
            ak, at = pA.next()
            for k in range(8):
                MM(P, at[:, 0:16], xT32[:, k, :], wr[:, k, :], k == 0, k == 7, [("xT32", 0), ("xT32", 1), "wr"], [ak])
            CP(P, "dve", lg[:, :], at[:, 0:16], [ak], ["lg"])
            RMAX(P, "dve", st_[:, 2:3], lg[:, :], ["lg"], [sk_])
            TS(P, "dve", st_[:, 2:3], st_[:, 2:3], -1.0, None, ALU.mult, None, [sk_], [sk_])
            ACT(P, ex[:, :], lg[:, :], AF.Exp, ["lg", sk_], ["ex"], bias=st_[:, 2:3])
            RSUM(P, "dve", st_[:, 3:4], ex[:, :], ["ex"], [sk_])
            RECIP(P, "dve", st_[:, 3:4], st_[:, 3:4], [sk_], [sk_])
            TS(P, "dve", affs[:, t, :], ex[:, :], st_[:, 3:4], None, ALU.mult, None, ["ex", sk_], [("affs", t)])
            ak, at = pA.next()
            TR(P, at[0:16, 0:128], affs[:, t, :], ident32[:, :], [("affs", t), "ident32"], [ak])
            CP(P, "act", affTs[:, r0:r0 + 128], at[0:16, 0:128], [ak], [("affT", t)])
        DMA(P, "sp", affd.ap().rearrange("(t p) e -> p t e", p=128), affs[:, :, :], [("affs", t) for t in range(ntile)], [])
        DMA(P, "sp", affTd[:, :], affTs[:, :], [("affT", t) for t in range(ntile)], [])
        P.emit()
        stats = P.stats
    return nc, stats


def build_thr(ns=S, cap=2 * S // 16, iters=30, env=None, pre=None):
    nc, es, C, P = _begin(env, pre)
    with es:
        affT = C.dram("affT", [16, ns], F32, "ExternalInput")
        thr = C.dram("thr", [16, 2], F32, "ExternalOutput")
        a = C.sb([16, ns], F32, "a")
        junk = C.sb([16, ns], F32, "junk")
        lh = C.sb([16, 2], F32, "lh")
        w = C.sb([16, 8], F32, "w")
        DMA(P, "sp", a[:, :], affT[:, :], [], ["a"])
        MEMSET(P, "dve", lh[:, 0:1], 0.0, ["lh"])
        MEMSET(P, "dve", lh[:, 1:2], 1.0, ["lh"])
        for it in range(iters):
            TT(P, "dve", w[:, 0:1], lh[:, 0:1], lh[:, 1:2], ALU.add, ["lh"], ["w"])
            TS(P, "dve", w[:, 0:1], w[:, 0:1], 0.5, None, ALU.mult, None, ["w"], ["w"])
            P.op("dve", lambda e: e.tensor_scalar(out=junk[:, :], in0=a[:, :], scalar1=w[:, 0:1], scalar2=0.0,
                                                  op0=ALU.is_ge, op1=ALU.add, accum_out=w[:, 1:2]),
                 ["a", "w"], ["junk", "w"])
            TS(P, "dve", w[:, 2:3], w[:, 1:2], float(cap), None, ALU.is_ge, None, ["w"], ["w"])
            TT(P, "dve", w[:, 3:4], w[:, 0:1], lh[:, 0:1], ALU.subtract, ["w", "lh"], ["w"])
            TT(P, "dve", w[:, 4:5], lh[:, 1:2], w[:, 0:1], ALU.subtract, ["w", "lh"], ["w"])
            STT(P, "dve", lh[:, 0:1], w[:, 3:4], w[:, 2:3], lh[:, 0:1], ALU.mult, ALU.add, ["w", "lh"], ["lh"])
            STT(P, "dve", lh[:, 1:2], w[:, 4:5], w[:, 2:3], w[:, 0:1], ALU.mult, ALU.add, ["w", "lh"], ["lh"])
        DMA(P, "sp", thr[:, :], lh[:, :], ["lh"], [])
        P.emit()
        stats = P.stats
    return nc, stats


def build_ffn(nt=TPC, nexp=16, tb=1024, env=None, pre=None):
    nc, es, C, P = _begin(env, pre)
    FF = 1536
    ntile = nt // 128
    nblk = nt // tb
    with es:
        x1d = C.dram("x1", [nt, D], F32, "ExternalInput")
        xnd = C.dram("xn2T", [D, nt], BF16, "ExternalInput")
        affd = C.dram("aff", [nt, 16], F32, "ExternalInput")
        thrd = C.dram("thr_row", [128, 16], F32, "ExternalInput") if not (pre is not None and "thr16" in pre) else None
        wgd = C.dram("wg", [nexp, D, FF], F32, "ExternalInput")
        wud = C.dram("wu", [nexp, D, FF], F32, "ExternalInput")
        wdd = C.dram("wd", [nexp, FF, D], F32, "ExternalInput")
        x2d = C.dram("x2", [nt, D], F32, "ExternalOutput")

        xb = C.sb([128, 8, tb], BF16, "xb")
        acc = C.sb([128, tb // 128, D], F32, "acc")
        wgb = C.sb([128, 8, FF], BF16, "wgb")
        wub = C.sb([128, 8, FF], BF16, "wub")
        wdb = C.sb([128, 12, D], BF16, "wdb")
        stg = Rot([(("stg", i), C.sb([128, 4096], F32, "stg")) for i in range(2)])
        hT = C.sb([128, 12, tb], BF16, "hT")
        sg = Rot([(("sg", i), C.sb([128, 512], F32, "sg")) for i in range(2)])
        affs = C.sb([128, ntile, 16], F32, "affs")
        gw = C.sb([128, ntile, 16], F32, "gw")
        thr = C.sb([128, 16], F32, "thr")
        xo = Rot([(("xo", i), C.sb([128, D], F32, "xo")) for i in range(2)])
        pA = Rot([(("pA", i), C.ps([128, 512], F32, "pA")) for i in range(7)])

        if pre is not None and "thr16" in pre:
            t16d = pre["thr16"]
            i32d = pre["ident32"]
            t16 = C.sb([16, 2], F32, "t16")
            tbc = C.sb([16, 128], F32, "tbc")
            i16 = C.sb([16, 16], F32, "i16")
            DMA(P, "sp", t16[:, :], t16d[:, :], [], ["t16"])
            DMA(P, "sp", i16[:, :], i32d[0:16, 0:16], [], ["i16"])
            MEMSET(P, "dve", tbc[:, :], 1.0, ["tbc"])
            TS(P, "dve", tbc[:, :], tbc[:, :], t16[:, 0:1], None, ALU.mult, None, ["tbc", "t16"], ["tbc"])
            tk_, tp_ = pA.next()
            MM(P, tp_[:, 0:16], tbc[:, :], i16[:, :], True, True, ["tbc", "i16"], [tk_])
            CP(P, "dve", thr[:, :], tp_[:, 0:16], [tk_], ["thr"])
        else:
            DMA(P, "sp", thr[:, :], thrd[:, :], [], ["thr"])
        DMA(P, "sp", affs[:, :, :], affd.ap().rearrange("(t p) e -> p t e", p=128), [], ["affs"])
        for t in range(ntile):
            TT(P, "dve", gw[:, t, :], affs[:, t, :], thr[:, :], ALU.is_ge, ["affs", "thr"], ["gw"])
            TT(P, "dve", gw[:, t, :], gw[:, t, :], affs[:, t, :], ALU.mult, ["gw", "affs"], ["gw"])
        cv = [0]

        def conv(dst, src, r, w):
            eng = ("act", "dve", "act")[cv[0] % 3]
            cv[0] += 1
            CP(P, eng, dst, src, r, w)

        def load_gu(e, chs=(0, 1, 2)):
            for ch in chs:
                for (wd_, dstb, key) in ((wgd, wgb, "wgb"), (wud, wub, "wub")):
                    sk, st = stg.next()
                    sv = st[:, :].rearrange("p (c f) -> p c f", c=8)
                    DMA(P, "sp", sv, wd_[e].rearrange("(c p) f -> p c f", p=128)[:, :, ch * 512:(ch + 1) * 512], [], [sk])
                    for hh in range(2):
                        conv(dstb[:, hh * 4:(hh + 1) * 4, ch * 512:(ch + 1) * 512], sv[:, hh * 4:(hh + 1) * 4, :], [sk],
                             [(key, ch)])

        def load_d(e):
            for ch in range(3):
                sk, st = stg.next()
                sv = st[:, :].rearrange("p (c n) -> p c n", c=4)
                DMA(P, "sp", sv, wdd[e].rearrange("(c p) n -> p c n", p=128)[:, ch * 4:(ch + 1) * 4, :], [], [sk])
                for hh in range(2):
                    conv(wdb[:, ch * 4 + hh * 2:ch * 4 + hh * 2 + 2, :], sv[:, hh * 2:hh * 2 + 2, :], [sk], ["wdb"])

        first = True
        for blk in range(nblk):
            b0 = blk * tb
            DMA(P, "sp", xb[:, :, :], xnd.ap().rearrange("(k p) t -> p k t", p=128)[:, :, b0:b0 + tb], [], ["xb"])
            for e in range(nexp):
                if first:
                    load_gu(e)
                    load_d(e)
                    first = False
                nxt = (blk * nexp + e + 1)
                for f in range(12):
                    for tq in range(tb // 512):
                        gk, gp = pA.next()
                        uk, up = pA.next()
                        for k in range(8):
                            MM(P, gp[:, :], wgb[:, k, f * 128:(f + 1) * 128], xb[:, k, tq * 512:(tq + 1) * 512],
                               k == 0, k == 7, [("wgb", f // 4), "xb"], [gk])
                        for k in range(8):
                            MM(P, up[:, :], wub[:, k, f * 128:(f + 1) * 128], xb[:, k, tq * 512:(tq + 1) * 512],
                               k == 0, k == 7, [("wub", f // 4), "xb"], [uk])
                        sk, st = sg.next()
                        ACT(P, st[:, :], gp[:, :], AF.Silu, [gk], [sk])
                        TT(P, "dve", hT[:, f, tq * 512:(tq + 1) * 512], up[:, :], st[:, :], ALU.mult, [uk, sk], [("hT", f)])
                    if f % 4 == 3 and nxt < nblk * nexp:
                        load_gu(nxt % nexp, chs=(f // 4,))
                for tt in range(tb // 128):
                    gcol = gw[:, blk * (tb // 128) + tt, e:e + 1]
                    for hf in range(2):
                        yk, yp = pA.next()
                        for f in range(12):
                            MM(P, yp[:, :], hT[:, f, tt * 128:(tt + 1) * 128], wdb[:, f, hf * 512:(hf + 1) * 512],
                               f == 0, f == 11, [("hT", f), "wdb"], [yk])
                        asl = acc[:, tt, hf * 512:(hf + 1) * 512]
                        if e == 0:
                            TS(P, "dve", asl, yp[:, :], gcol, None, ALU.mult, None, [yk, "gw"], [("acc", tt, hf)])
                        else:
                            STT(P, "dve", asl, yp[:, :], gcol, asl, ALU.mult, ALU.add, [yk, "gw", ("acc", tt, hf)],
                                [("acc", tt, hf)])
                if nxt < nblk * nexp:
                    load_d(nxt % nexp)
            for tt in range(tb // 128):
                xk, xt_ = xo.next()
                r0 = b0 + tt * 128
                DMA(P, "sp", xt_[:, :], x1d[r0:r0 + 128, :], [], [xk])
                TT(P, "pool", xt_[:, :], xt_[:, :], acc[:, tt, :], ALU.add, [xk, ("acc", tt, 0), ("acc", tt, 1)], [xk])
                DMA(P, "sp", x2d[r0:r0 + 128, :], xt_[:, :], [xk], [])
        P.emit()
        stats = P.stats
    return nc, stats


def build_attn(nq=TPC, nk=S, nheads=8, env=None, pre=None, hook=None):
    nc, es, C, P = _begin(env, pre)
    NKT = nk // 128
    NQB = nq // 512
    scale = 96.0 ** -0.5
    with es:
        mq = C.dram("mq", [nheads, 96, nq], BF16, "ExternalInput")
        mk = C.dram("mk", [nheads, 96, nk], BF16, "ExternalInput")
        mv = C.dram("mv", [nheads, 128, NKT * 64], BF16, "ExternalInput")
        esel = C.dram("esel", [65, 64], F32, "ExternalInput")
        OT = C.dram("OT", [nheads * 64, nq], BF16, "ExternalOutput")

        kT = Rot([(("kT", i), C.sb([96, nk], BF16, "kT")) for i in range(2)])
        vv = Rot([(("vv", i), C.sb([128, NKT, 65], BF16, "vv")) for i in range(2)])
        qT = Rot([(("qT", i), C.sb([96, nq], BF16, "qT")) for i in range(2)])
        pT = Rot([(("pT", i), C.sb([128, 512], BF16, "pT")) for i in range(4)])
        osb = C.sb([65, 512], F32, "osb")
        rbc = C.sb([64, 512], F32, "rbc")
        oo = Rot([(("oo", i), C.sb([64, 512], BF16, "oo")) for i in range(2)])
        es_sb = C.sb([65, 64], F32, "esel")
        sps = Rot([(("sps", i), C.ps([128, 512], F32, "sps")) for i in range(4)])
        ops_ = Rot([(("ops", i), C.ps([128, 512], F32, "ops")) for i in range(2)])
        bps = C.ps([128, 512], F32, "bps")
        DMA(P, "sp", es_sb[:], esel[:, :], [], ["esel"])
        for i in range(2):
            MEMSET(P, "pool", vv.items[i][1][:, :, 64:65], 1.0, [("vv1", i)])
        if hook is not None:
            hook(C, P)
        for h in range(nheads):
            kk, kt_ = kT.next()
            vk, vt_ = vv.next()
            qk, qt_ = qT.next()
            DMA(P, "sp", kt_[:, :], mk[h, :, :], [("mk_s", h)], [kk])
            DMA(P, "sp", vt_[:, :, 0:64], mv[h, :, :].rearrange("p (t d) -> p t d", d=64), [("mv_s", h)], [vk])
            DMA(P, "sp", qt_[:, :], mq[h, :, :], [], [qk])
            vkeys = [vk, ("vv1", (vv.i - 1) % 2)]
            for qb in range(NQB):
                ok_, ot_ = ops_.next()

                def s_mm(t, kt_=kt_, qt_=qt_, qb=qb, kk=kk, qk=qk):
                    sk, st = sps.next()
                    MM(P, st[:, :], kt_[:, t * 128:(t + 1) * 128], qt_[:, qb * 512:(qb + 1) * 512], True, True,
                       [kk, qk], [sk])
                    return sk, st
                pend = [s_mm(0)]
                if NKT > 1:
                    pend.append(s_mm(1))
                for t in range(NKT):
                    if t + 2 < NKT:
                        pend.append(s_mm(t + 2))
                    sk, st = pend.pop(0)
                    pk, pt = pT.next()
                    ACT(P, pt[:, :], st[:, :], AF.Exp, [sk], [pk], scale=scale)
                    MM(P, ot_[0:65, :], vt_[:, t, :], pt[:, :], t == 0, t == NKT - 1, vkeys + [pk], [ok_])
                CP(P, "dve", osb[:, :], ot_[0:65, :], [ok_], ["osb"])
                MM(P, bps[0:64, :], es_sb[:, :], osb[:, :], True, True, ["esel", "osb"], ["bps"])
                CP(P, "dve", rbc[:, :], bps[0:64, :], ["bps"], ["rbc"])
                RECIP(P, "dve", rbc[:, :], rbc[:, :], ["rbc"], ["rbc"])
                ook, oot = oo.next()
                TT(P, "dve", oot[:, :], osb[0:64, :], rbc[:, :], ALU.mult, ["osb", "rbc"], [ook])
                DMA(P, "sp", OT[h * 64:(h + 1) * 64, qb * 512:(qb + 1) * 512], oot[:, :], [ook], [])
        P.emit()
        stats = P.stats
    return nc, stats


def attn_consts():
    e = np.zeros((65, 64), np.float32)
    e[64, :] = 1.0
    return dict(esel=e)


RG = [[0, 1, 2, 3], [4, 5, 6, 7]]
LAYER_W = [("w_in", [D, INW], F32), ("gmix", [128, 8], F32), ("convp", [128, 4, 34], F32), ("gcq", [128, 3], F32),
           ("gckv", [128, 2], F32), ("w_uq", [384, 768], F32), ("w_ukv", [256, 1024], F32), ("gqk", [96, 2], F32),
           ("bif", [128, 4], F32), ("gA", [128, 128], F32), ("w_a", [512, D], F32), ("w_b", [512, D], F32),
           ("w_c", [512, D], F32), ("w_o", [D, D], F32), ("gffn", [128, 8], F32), ("w_r", [D, 16], F32),
           ("wg", [16, D, 1536], F32), ("wu", [16, D, 1536], F32), ("wd", [16, 1536, D], F32)]
CONSTS = [("identb", [128, 128], BF16), ("ropeT", [96, 2, TPC], F32), ("rmat", [96, 96], BF16), ("onesf", [128, 128], F32),
          ("cmat", [128, 6, 128], F32), ("ident32", [128, 128], F32), ("esel", [65, 64], F32), ("idx", [128, 16], I32)]


def build_fused(stop=None):
    env = Env()
    nc, P = env.nc, env.P
    BYP = ALU.bypass
    CCB = 256 * 1024
    with env.es:
        def DT(name, shape, dt, kind="Internal"):
            return nc.dram_tensor(name, list(shape), dt, kind=kind)

        def allgather(name, src2d, rows, cols, dt, rkeys, wkey):
            esz = 4 if dt in (F32, I32) else 2
            rc = max(1, min(rows, CCB // (cols * esz)))
            assert rows % rc == 0
            g = DT(name, [4 * rows, cols], dt)
            for k in range(rows // rc):
                P.cc(lambda e, k=k: e.collective_compute("AllGather", BYP, replica_groups=RG, ins=[src2d[k * rc:(k + 1) * rc, :]],
                                                         outs=[g[k * 4 * rc:(k + 1) * 4 * rc, :]]), rkeys, [wkey])
            return g, rc

        def rankview(g, rc, r):
            return g.ap().rearrange("(k r x) c -> r k x c", r=4, x=rc)[r]

        ext = {"xe0": DT("xe0", [TPC + 2 * HALO, D], F32, "ExternalInput")}
        for (n, sh, dt) in CONSTS:
            ext[n] = DT(n, sh, dt, "ExternalInput")
        for l in range(2):
            for (n, sh, dt) in LAYER_W:
                ext["%s_%d" % (n, l)] = DT("%s_%d" % (n, l), sh, dt, "ExternalInput")
        out = DT("out", [TPC, D], F32, "ExternalOutput")
        xe = ext["xe0"]
        x_own = None
        for l in range(2):
            W = {n: ext["%s_%d" % (n, l)] for (n, _, _) in LAYER_W}
            L = lambda n, sh, dt: DT("%s_L%d" % (n, l), sh, dt)
            A = dict(QT=L("QT", [512, TPC], BF16), KT=L("KT", [512, TPC], BF16), Kt=L("Kt", [4, TPC, 128], BF16),
                     Vt=L("Vt", [4, TPC, 128], BF16), OG=L("OG", [4, TPC, 128], BF16), G4=L("G4", [4, TPC, 4], F32),
                     GTS=L("GTS", [TPC, 3072], BF16), UT=L("UT", [512, TPC], BF16), MQ=L("MQ", [8, 96, TPC], BF16),
                     MK=L("MK", [8, 96, TPC], BF16), MV=L("MV", [8, 128, TPC // 128, 64], BF16))
            preA = dict(xe=xe, w_in=W["w_in"], gmix=W["gmix"], identb=ext["identb"], convp=W["convp"], gcq=W["gcq"],
                        gckv=W["gckv"], w_uq=W["w_uq"], w_ukv=W["w_ukv"], gqk=W["gqk"], ropeT=ext["ropeT"],
                        rmat=ext["rmat"], onesf=ext["onesf"], **A)
            build_stageA(env=env, pre=preA)
            nc_, es_, C_, _ = _begin(env)
            with es_:
                idx = C_.sb([128, 16], I32, "idx")
                DMA(P, "sp", idx[:, :], ext["idx"][:, :], [], ["idx"])
                P.emit()
            mk_s = L("mk_s", [8, 96, S], BF16)
            mv_s = L("mv_s", [8, 128, (S // 128) * 64], BF16)

            def exchange_kv(C_, P_, A=A, l=l, mk_s=mk_s, mv_s=mv_s):
                srcK = A["MK"].ap().rearrange("h f t -> (h f) t")
                srcV = A["MV"].ap().rearrange("h p t d -> (h p) (t d)")
                gK = DT("gMK_L%d" % l, [4 * 768, TPC], BF16)
                gV = DT("gMV_L%d" % l, [4 * 1024, 2048], BF16)
                for h in range(8):
                    for k in range(3 * h, 3 * h + 3):
                        P_.cc(lambda e, k=k: e.collective_compute("AllGather", BYP, replica_groups=RG, ins=[srcK[k * 32:(k + 1) * 32, :]],
                                                                  outs=[gK[k * 128:(k + 1) * 128, :]]), [], [("gK", h)])
                    for k in range(2 * h, 2 * h + 2):
                        P_.cc(lambda e, k=k: e.collective_compute("AllGather", BYP, replica_groups=RG, ins=[srcV[k * 64:(k + 1) * 64, :]],
                                                                  outs=[gV[k * 256:(k + 1) * 256, :]]), [], [("gV", h)])
                    for i in range(4):
                        DMA(P_, "pool", mk_s[h, :, i * TPC:(i + 1) * TPC].rearrange("(k x) t -> k x t", x=32),
                            gK.ap().rearrange("(k r x) c -> r k x c", r=4, x=32)[i][3 * h:3 * h + 3], [("gK", h)], [("mk_s", h)])
                        DMA(P_, "pool", mv_s[h, :, i * 2048:(i + 1) * 2048].rearrange("(k x) c -> k x c", x=64),
                            gV.ap().rearrange("(k r x) c -> r k x c", r=4, x=64)[i][2 * h:2 * h + 2], [("gV", h)], [("mv_s", h)])
            if stop == "x1":
                return nc
            qT_s = L("qT_s", [128, S], BF16)
            kt_s = L("kt_s", [S, 128], BF16)
            vt_s = L("vt_s", [S, 128], BF16)
            og_s = L("og_s", [S, 128], BF16)
            g4_s = L("g4_s", [S, 4], F32)

            def exchange_heads(C_, P_, A=A, l=l, qT_s=qT_s, kt_s=kt_s, vt_s=vt_s, og_s=og_s, g4_s=g4_s):
                idx = C_.sb([128, 16], I32, "idx")
                DMA(P_, "pool", idx[:, :], ext["idx"][:, :], [], ["idx"])
                gQT, rcQ = allgather("gQT_L%d" % l, A["QT"].ap(), 512, TPC, BF16, [], ("g", 0))
                gKt, rcK = allgather("gKt_L%d" % l, A["Kt"].ap().rearrange("h t d -> (h t) d"), 4 * TPC, 128, BF16, [], ("g", 1))
                gVt, _ = allgather("gVt_L%d" % l, A["Vt"].ap().rearrange("h t d -> (h t) d"), 4 * TPC, 128, BF16, [], ("g", 2))
                gOG, _ = allgather("gOG_L%d" % l, A["OG"].ap().rearrange("h t d -> (h t) d"), 4 * TPC, 128, BF16, [], ("g", 3))
                gG4, rcG = allgather("gG4_L%d" % l, A["G4"].ap().rearrange("h t g -> (h t) g"), 4 * TPC, 4, F32, [], ("g", 4))
                assert (rcQ, rcK, rcG) == (32, 1024, 4 * TPC), (rcQ, rcK, rcG)
                stb = Rot([(("stb", i), C_.sb([128, 16384], BF16, "stb")) for i in range(2)])
                stf = C_.sb([128, 512], F32, "stf")

                def gather(dst_ap, src_ap, col, rkeys, wkeys, tile_ap, tkey):
                    P_.dma("pool", lambda e: e.indirect_dma_start(
                        out=tile_ap, out_offset=None, in_=src_ap,
                        in_offset=bass.IndirectOffsetOnAxis(ap=idx[:, col:col + 1], axis=0)), ["idx"] + rkeys, [tkey])
                    DMA(P_, "pool", dst_ap, tile_ap, [tkey], wkeys)
                for i in range(4):
                    tk_, tt_ = stb.next()
                    gather(qT_s[:, i * TPC:(i + 1) * TPC], gQT[:, :], i, [("g", 0)], [("qT_s", i)], tt_[:, 0:TPC], tk_)
                for j, (gsrc, dst) in enumerate(((gKt, kt_s), (gVt, vt_s), (gOG, og_s))):
                    tk_, tt_ = stb.next()
                    gather(dst.ap().rearrange("(c p) d -> c (p d)", p=128),
                           gsrc.ap().rearrange("(c p) d -> c (p d)", p=128), 4, [("g", 1 + j)], [("tm_s", j)], tt_[:, :], tk_)
                gather(g4_s.ap().rearrange("(c p) d -> c (p d)", p=128),
                       gG4.ap().rearrange("(c p) d -> c (p d)", p=128), 11, [("g", 4)], [("tm_s", 3)], stf[:, :], "stf")

            OT = L("OT", [512, TPC], BF16)
            def both(C_, P_, f1=exchange_kv, f2=exchange_heads):
                f1(C_, P_)
                f2(C_, P_)
            build_attn(env=env, pre=dict(mq=A["MQ"], mk=mk_s, mv=mv_s, esel=ext["esel"], OT=OT), hook=both)
            if stop == "t":
                return nc
            HT = L("HT", [128, S], BF16)
            build_mlstm(env=env, pre=dict(qT=qT_s, kt=kt_s, vt=vt_s, g4=g4_s, bif=W["bif"], og=og_s, gA=W["gA"],
                                          cmat=ext["cmat"], identb=ext["identb"], HT=HT))
            if stop == "m":
                return nc
            nc_, es_, C_, _ = _begin(env)
            with es_:
                idx = C_.sb([128, 16], I32, "idx")
                DMA(P, "sp", idx[:, :], ext["idx"][:, :], [], ["idx"])
                gHT, rcH = allgather("gHT_L%d" % l, HT.ap(), 128, S, BF16, [], "gHT")
                assert rcH == 8
                HT_own = L("HT_own", [512, TPC], BF16)
                src = gHT.ap().rearrange("r (i t) -> (r i) t", i=4)
                stb = Rot([(("stb", i), C_.sb([128, TPC], BF16, "stb")) for i in range(2)])
                for h in range(4):
                    tk_, tt_ = stb.next()
                    P.dma("pool", lambda e, h=h, tt_=tt_: e.indirect_dma_start(
                        out=tt_[:, :], out_offset=None, in_=src,
                        in_offset=bass.IndirectOffsetOnAxis(ap=idx[:, 5 + h:6 + h], axis=0)), ["idx", "gHT"], [tk_])
                    DMA(P, "sp", HT_own[h * 128:(h + 1) * 128, :], tt_[:, :], [tk_], [("HT_own", h)])
                P.emit()
            if stop == "x2":
                return nc
            x1 = L("x1", [TPC, D], F32)
            xn2T = L("xn2T", [D, TPC], BF16)
            aff = L("aff", [TPC, 16], F32)
            affT = L("affT", [16, TPC], F32)
            if l == 0:
                xin = L("xin", [TPC, D], F32)
                nc_, es_, C_, _ = _begin(env)
                with es_:
                    DMA(P, "sp", xin[:, :], ext["xe0"][HALO:HALO + TPC, :], [], ["xin"])
                    P.emit()
            else:
                xin = x_own
            build_merge(env=env, pre=dict(x=xin, HT=HT_own, UT=A["UT"], OT=OT, GTS=A["GTS"], w_a=W["w_a"], w_b=W["w_b"],
                                          w_c=W["w_c"], w_o=W["w_o"], gffn=W["gffn"], w_r=W["w_r"], identb=ext["identb"],
                                          ident32=ext["ident32"], x1=x1, xn2T=xn2T, aff=aff, affT=affT))
            if stop == "c1":
                return nc
            affT_s = L("affT_s", [16, S], F32)
            nc_, es_, C_, _ = _begin(env)
            with es_:
                gAf, rcA = allgather("gAf_L%d" % l, affT.ap(), 16, TPC, F32, [], "gAf")
                assert rcA == 16
                for i in range(4):
                    DMA(P, "sp", affT_s[:, i * TPC:(i + 1) * TPC], gAf[i * 16:(i + 1) * 16, :], ["gAf"], [("affT_s", i)])
                P.emit()
            thr = L("thr", [16, 2], F32)
            build_thr(env=env, pre=dict(affT=affT_s, thr=thr))
            if stop == "h":
                return nc
            x2 = out if l == 1 else L("x2", [TPC, D], F32)
            build_ffn(env=env, pre=dict(x1=x1, xn2T=xn2T, aff=aff, thr16=thr, ident32=ext["ident32"], wg=W["wg"], wu=W["wu"],
                                        wd=W["wd"], x2=x2))
            if l == 0:
                xe1 = L("xe1", [TPC + 2 * HALO, D], F32)
                nc_, es_, C_, _ = _begin(env)
                with es_:
                    idx = C_.sb([128, 16], I32, "idx")
                    zt = C_.sb([128, D], F32, "zt")
                    DMA(P, "sp", idx[:, :], ext["idx"][:, :], [], ["idx"])
                    MEMSET(P, "dve", zt[:, :], 0.0, ["zt"])
                    edges = L("edges", [384, D], F32)
                    DMA(P, "sp", edges[0:128, :], x2[0:128, :], [], ["edges"])
                    DMA(P, "sp", edges[128:256, :], x2[TPC - 128:TPC, :], [], ["edges"])
                    DMA(P, "sp", edges[256:384, :], zt[:, :], ["zt"], ["edges"])
                    DMA(P, "sp", xe1[HALO:HALO + TPC, :], x2[:, :], [], ["xe1m"])
                    gE, rcE = allgather("gE_L%d" % l, edges.ap(), 384, D, F32, ["edges"], "gE")
                    assert rcE == 64
                    hl = C_.sb([128, D], F32, "hl")
                    hr = C_.sb([128, D], F32, "hr")
                    P.dma("pool", lambda e: e.indirect_dma_start(
                        out=hl[:, :], out_offset=None, in_=gE[:, :],
                        in_offset=bass.IndirectOffsetOnAxis(ap=idx[:, 9:10], axis=0)), ["idx", "gE"], ["hl"])
                    P.dma("pool", lambda e: e.indirect_dma_start(
                        out=hr[:, :], out_offset=None, in_=gE[:, :],
                        in_offset=bass.IndirectOffsetOnAxis(ap=idx[:, 10:11], axis=0)), ["idx", "gE"], ["hr"])
                    DMA(P, "sp", xe1[0:HALO, :], hl[:, :], ["hl"], ["xe1l"])
                    DMA(P, "sp", xe1[HALO + TPC:, :], hr[:, :], ["hr"], ["xe1r"])
                    P.emit()
                xe = xe1
                x_own = x2
    return nc


def _grow(x, rc, r):
    return (x // rc) * (4 * rc) + r * rc + (x % rc)


def fused_idx(c):
    r = c % 4
    p = np.arange(128)
    idx = np.zeros((128, 16), np.int32)
    for i in range(4):
        idx[:, i] = _grow(r * 128 + p, 32, i)
    y = r * 32 + (p % 32)
    idx[:, 4] = _grow(y, 8, p // 32)
    idx[:, 11] = (p // 32) * 128 + y
    for h in range(4):
        idx[:, 5 + h] = _grow(p, 8, h) * 4 + r
    idx[:, 9] = _grow(128 + p, 64, r - 1) if r > 0 else _grow(256 + p, 64, r)
    idx[:, 10] = _grow(p, 64, r + 1) if r < 3 else _grow(256 + p, 64, r)
    return idx


def kernel(**inputs):
    prm = {k: np.asarray(v) for k, v in inputs.items()}
    x = np.ascontiguousarray(prm["x"], dtype=np.float32)
    nc = build_fused()
    CT, ST = rope_tables()
    cst = consts()
    mc = mlstm_consts()
    ac = attn_consts()
    i32 = np.eye(128, dtype=np.float32)
    lay = []
    for l in range(2):
        convp = np.zeros((128, 4, 34), np.float32)
        convp[:, :, 0:31] = prm["conv_w"][l].T.reshape(4, 128, 31).transpose(1, 0, 2)
        convp[:, :, 31] = prm["conv_b"][l].reshape(4, 128).T
        convp[:, :, 32] = prm["conv_ln_g"][l].reshape(4, 128).T
        convp[:, :, 33] = prm["conv_ln_b"][l].reshape(4, 128).T
        lay.append(dict(w_in=prm["w_in"][l], gmix=_gain_cols(prm["mix_norm_g"][l], 8), convp=convp,
                        gcq=_gain_cols(prm["cq_norm_g"][l], 3), gckv=_gain_cols(prm["ckv_norm_g"][l], 2),
                        w_uq=prm["w_uq"][l], w_ukv=prm["w_ukv"][l],
                        gqk=np.ascontiguousarray(np.stack([prm["q_norm_g"][l], prm["k_norm_g"][l]], axis=1)),
                        w_a=prm["w_a_out"][l], w_b=prm["w_b_out"][l], w_c=prm["w_c_out"][l], w_o=prm["w_out"][l],
                        gffn=_gain_cols(prm["ffn_norm_g"][l], 8), w_r=prm["w_router"][l],
                        wg=prm["w_e_gate"][l], wu=prm["w_e_up"][l], wd=prm["w_e_down"][l]))
    maps = []
    for c in range(NCORES):
        b, r = c // 4, c % 4
        s0 = r * TPC
        xe = np.zeros((TPC + 2 * HALO, D), np.float32)
        lo, hi = max(0, s0 - HALO), min(S, s0 + TPC + HALO)
        xe[lo - (s0 - HALO):hi - (s0 - HALO)] = x[b, lo:hi]
        m = dict(xe0=xe, identb=cst["identb"], ropeT=np.ascontiguousarray(np.stack([CT[:, s0:s0 + TPC], ST[:, s0:s0 + TPC]], axis=1)),
                 rmat=cst["rmat"], onesf=cst["onesf"], cmat=mc["cmat"], ident32=i32, esel=ac["esel"], idx=fused_idx(c))
        cols = [r, 4 + r, 8 + r, 12 + r]
        for l in range(2):
            for k_, v_ in lay[l].items():
                m["%s_%d" % (k_, l)] = v_
            m["bif_%d" % l] = np.ascontiguousarray(np.broadcast_to(prm["b_if"][l][cols], (128, 4)))
            m["gA_%d" % l] = np.ascontiguousarray(np.broadcast_to(prm["a_norm_g"][l][r], (128, 128)))
        maps.append(m)
    res = run_spmd(nc, maps)
    out = np.empty_like(x)
    for c in range(NCORES):
        b, r = c // 4, c % 4
        out[b, r * TPC:(r + 1) * TPC] = np.asarray(res[c]["out"])
    return out
```

```python
import math
from contextlib import ExitStack

import numpy as np
import ml_dtypes

import concourse.bass as bass
import concourse.mybir as mybir
from concourse.bass_utils import run_bass_kernel_spmd

F32 = mybir.dt.float32
BF16 = mybir.dt.bfloat16
I32 = mybir.dt.int32
AF = mybir.ActivationFunctionType
ALU = mybir.AluOpType
AX = mybir.AxisListType
NPBF = ml_dtypes.bfloat16

D = 1024
S = 16384
NB = 2
INW = 6832
EPS = 1e-6
NCORES = 8
TPC = S * NB // NCORES

O_AQ, O_AK, O_AV, O_AO, O_AG = 0, 512, 1024, 1536, 2048
O_GLU = 2064
O_CQ = 3088
O_CKV = 3472
O_CKR = 3728
O_GTS = 3760


class Prog:
    RING = 8

    def __init__(self, nc, es):
        self.nc = nc
        self.es = es
        self.ops = []
        self.engs = ["pe", "act", "dve", "pool", "sp"]
        self.csem = None
        self.rings = {}
        self.ccsem = None
        self.ccount = {e: 0 for e in self.engs}
        self.dcount = {e: 0 for e in self.engs}
        self.cccount = 0
        self.nstage = 0

    def cc(self, fn, r=(), w=()):
        self.ops.append(dict(eng="pool", fn=fn, r=tuple(r), w=tuple(w), dma=True, cc=True))

    def op(self, eng, fn, r=(), w=()):
        self.ops.append(dict(eng=eng, fn=fn, r=tuple(r), w=tuple(w), dma=False))

    def dma(self, eng, fn, r=(), w=()):
        self.ops.append(dict(eng=eng, fn=fn, r=tuple(r), w=tuple(w), dma=True))

    def emit(self):
        nc, es = self.nc, self.es
        ops = self.ops
        last_w = {}
        readers = {}
        deps = []
        for i, o in enumerate(ops):
            d = set()
            for k in o["r"]:
                if k in last_w:
                    d.add((last_w[k], "raw"))
            for k in o["w"]:
                if k in last_w:
                    d.add((last_w[k], "waw"))
                for j in readers.get(k, ()):
                    if j != i:
                        d.add((j, "war"))
            for k in o["r"]:
                lst = readers.setdefault(k, [])
                if not o["dma"]:
                    lst[:] = [j for j in lst if ops[j]["dma"] or ops[j]["eng"] != o["eng"]]
                lst.append(i)
            for k in o["w"]:
                last_w[k] = i
                readers[k] = []
            dd = set()
            for j, kind in d:
                p = ops[j]
                if (not p["dma"]) and (not o["dma"]) and p["eng"] == o["eng"]:
                    if o["eng"] == "pe":
                        continue
                    if kind == "war":
                        continue
                dd.add(j)
            deps.append(dd)
        needed = set()
        for dd in deps:
            needed |= dd
        engs = self.engs
        if self.csem is None:
            self.csem = {e: es.enter_context(nc.semaphore("c_" + e)) for e in engs}
            self.ccsem = es.enter_context(nc.semaphore("c_cc"))
        csem = self.csem
        rings = self.rings
        ccount = self.ccount
        dcount = self.dcount
        prev_end = dict(c={e: ccount[e] for e in engs}, d={e: dcount[e] for e in rings}, cc=self.cccount)
        lastc = {}
        for i, o in enumerate(ops):
            if not o["dma"]:
                lastc[o["eng"]] = i
        needed |= set(lastc.values())
        sig = {}
        prewait = {}
        for i, o in enumerate(ops):
            e = o["eng"]
            if o.get("cc"):
                self.cccount += 1
                sig[i] = (self.ccsem, self.cccount, 1)
                if self.cccount > 1:
                    prewait[i] = (self.ccsem, self.cccount - 1)
            elif o["dma"]:
                if e not in rings:
                    rings[e] = [es.enter_context(nc.semaphore("r_%s%d" % (e, k)))
                                for k in range(self.RING)]
                n = dcount[e]
                dcount[e] += 1
                sem = rings[e][n % self.RING]
                sig[i] = (sem, 16 * (n // self.RING + 1), 16)
                if n >= self.RING:
                    prewait[i] = (sem, 16 * (n // self.RING))
            elif i in needed:
                ccount[e] += 1
                sig[i] = (csem[e], ccount[e], 1)
        per = {e: [] for e in engs}
        for i, o in enumerate(ops):
            per[o["eng"]].append(i)
        self.stats = dict(n_ops=len(ops), ccount=ccount, dcount=dcount)

        nstage = self.nstage
        self.nstage += 1

        def run(e, engobj):
            waited = {}
            if nstage > 0:
                for e2 in engs:
                    if prev_end["c"][e2] > 0:
                        engobj.wait_ge(csem[e2], prev_end["c"][e2])
                for e2, n in prev_end["d"].items():
                    for k in range(self.RING):
                        cnt = (n - k + self.RING - 1) // self.RING if n > k else 0
                        if cnt > 0:
                            engobj.wait_ge(rings[e2][k], 16 * cnt)
                if prev_end["cc"] > 0:
                    engobj.wait_ge(self.ccsem, prev_end["cc"])
            for i in per[e]:
                o = ops[i]
                ws = [sig[j][:2] for j in deps[i]]
                if i in prewait:
                    ws.append(prewait[i])
                mx = {}
                for sem, val in ws:
                    key = id(sem)
                    if key not in mx or mx[key][1] < val:
                        mx[key] = (sem, val)
                for key, (sem, val) in mx.items():
                    if waited.get(key, 0) >= val:
                        continue
                    waited[key] = val
                    engobj.wait_ge(sem, val)
                ins = o["fn"](engobj)
                if i in sig:
                    sem, val, inc = sig[i]
                    ins.then_inc(sem, inc)
            if e in rings:
                n = dcount[e]
                for k in range(self.RING):
                    cnt = (n - k + self.RING - 1) // self.RING if n > k else 0
                    if cnt > 0:
                        engobj.wait_ge(rings[e][k], 16 * cnt)

        with nc.Block() as block:
            @block.tensor
            def _(t):
                run("pe", t)

            @block.scalar
            def _(t):
                run("act", t)

            @block.vector
            def _(t):
                run("dve", t)

            @block.gpsimd
            def _(t):
                run("pool", t)

            @block.sync
            def _(t):
                run("sp", t)
        self.ops = []


class Ctx:
    def __init__(self, nc, es, pre=None, tag=""):
        self.nc, self.es = nc, es
        self.n = 0
        self.pre = pre
        self.tag = tag

    def sb(self, shape, dt, name=None):
        self.n += 1
        t = self.es.enter_context(self.nc.sbuf_tensor("%s%s_%d" % (self.tag, name or "t", self.n), list(shape), dt))
        esz = 4 if dt in (F32, I32) else 2
        nbytes = int(np.prod(shape[1:])) * esz
        alloc = (nbytes + 31) // 32 * 32
        if alloc % 64 != 0:
            self.n += 1
            self.es.enter_context(self.nc.sbuf_tensor("%spad_%d" % (self.tag, self.n), [128, 8], F32))
        return t

    def ps(self, shape, dt, name=None):
        self.n += 1
        return self.es.enter_context(self.nc.psum_tensor("%s%s_%d" % (self.tag, name or "p", self.n), list(shape), dt))

    def dram(self, name, shape, dt, kind):
        if self.pre is not None:
            h = self.pre[name]
            assert list(h.shape) == list(shape), (name, h.shape, shape)
            return h
        return self.nc.dram_tensor(name, list(shape), dt, kind=kind)


class Rot:
    def __init__(self, items):
        self.items = items
        self.i = 0

    def next(self):
        it = self.items[self.i % len(self.items)]
        self.i += 1
        return it


class Env:
    def __init__(self):
        self.nc = bass.Bass("TRN2", target_bir_lowering=False)
        self.es = ExitStack()
        self.P = Prog(self.nc, self.es)
        self.nstage = 0


def _begin(env, pre=None):
    if env is None:
        nc = bass.Bass("TRN2", target_bir_lowering=False)
        es = ExitStack()
        return nc, es, Ctx(nc, es), Prog(nc, es)
    env.nstage += 1
    es = ExitStack()
    return env.nc, es, Ctx(env.nc, es, pre=pre, tag="s%d_" % env.nstage), env.P


def run_spmd(nc, in_maps):
    res = run_bass_kernel_spmd(nc, in_maps, core_ids=list(range(NCORES)))
    return res.results


def ACT(P, out, in_, func, r, w, **kw):
    P.op("act", lambda e: e.activation(out=out, in_=in_, func=func, **kw), r, w)


def TS(P, eng, out, in0, s1, s2, op0, op1, r, w):
    if op1 is None:
        P.op(eng, lambda e: e.tensor_scalar(out=out, in0=in0, scalar1=s1, scalar2=None, op0=op0), r, w)
    else:
        P.op(eng, lambda e: e.tensor_scalar(out=out, in0=in0, scalar1=s1, scalar2=s2, op0=op0, op1=op1), r, w)


def TT(P, eng, out, in0, in1, op, r, w):
    P.op(eng, lambda e: e.tensor_tensor(out=out, in0=in0, in1=in1, op=op), r, w)


def STT(P, eng, out, in0, scalar, in1, op0, op1, r, w):
    P.op(eng, lambda e: e.scalar_tensor_tensor(out=out, in0=in0, scalar=scalar, in1=in1, op0=op0, op1=op1), r, w)


def CP(P, eng, out, in_, r, w):
    if eng == "act":
        P.op(eng, lambda e: e.copy(out=out, in_=in_), r, w)
    else:
        P.op(eng, lambda e: e.tensor_copy(out=out, in_=in_), r, w)


def RSUM(P, eng, out, in_, r, w, axis=None):
    ax = axis if axis is not None else AX.X
    P.op(eng, lambda e: e.reduce_sum(out=out, in_=in_, axis=ax), r, w)


def RMAX(P, eng, out, in_, r, w, axis=None):
    ax = axis if axis is not None else AX.X
    P.op(eng, lambda e: e.reduce_max(out=out, in_=in_, axis=ax), r, w)


def MM(P, out, lhsT, rhs, start, stop, r, w):
    P.op("pe", lambda e: e.matmul(out, lhsT, rhs, start=start, stop=stop), r, w)


def TR(P, out, in_, ident, r, w):
    P.op("pe", lambda e: e.transpose(out, in_, ident), r, w)


def DMA(P, eng, out, in_, r, w):
    P.dma(eng, lambda e: e.dma_start(out=out, in_=in_), r, w)


def RECIP(P, eng, out, in_, r, w):
    P.op(eng, lambda e: e.reciprocal(out=out, in_=in_), r, w)


def MEMSET(P, eng, ap, val, w):
    P.op(eng, lambda e: e.memset(ap, val), (), w)


TP = 1024
HALO = 128
NTP = TP + 2 * HALO
NPASS = TPC // TP


def build_stageA(phases=("fm", "tm", "conv", "mla"), env=None, pre=None):
    nc, es, C, P = _begin(env, pre)
    with es:
        xe = C.dram("xe", [TPC + 2 * HALO, D], F32, "ExternalInput")
        w_in = C.dram("w_in", [D, INW], F32, "ExternalInput")
        gmix = C.dram("gmix", [128, 8], F32, "ExternalInput")
        identb = C.dram("identb", [128, 128], BF16, "ExternalInput")
        convp = C.dram("convp", [128, 4, 34], F32, "ExternalInput")
        gcq = C.dram("gcq", [128, 3], F32, "ExternalInput")
        gckv = C.dram("gckv", [128, 2], F32, "ExternalInput")
        w_uq = C.dram("w_uq", [384, 768], F32, "ExternalInput")
        w_ukv = C.dram("w_ukv", [256, 1024], F32, "ExternalInput")
        gqk = C.dram("gqk", [96, 2], F32, "ExternalInput")
        ropeT = C.dram("ropeT", [96, 2, TPC], F32, "ExternalInput")
        rmat = C.dram("rmat", [96, 96], BF16, "ExternalInput")
        onesf = C.dram("onesf", [128, 128], F32, "ExternalInput")

        QT = C.dram("QT", [512, TPC], BF16, "ExternalOutput")
        KT = C.dram("KT", [512, TPC], BF16, "ExternalOutput")
        Kt = C.dram("Kt", [4, TPC, 128], BF16, "ExternalOutput")
        Vt = C.dram("Vt", [4, TPC, 128], BF16, "ExternalOutput")
        OG = C.dram("OG", [4, TPC, 128], BF16, "ExternalOutput")
        G4 = C.dram("G4", [4, TPC, 4], F32, "ExternalOutput")
        GTS = C.dram("GTS", [TPC, 3072], BF16, "ExternalOutput")
        UT = C.dram("UT", [512, TPC], BF16, "ExternalOutput")
        MQ = C.dram("MQ", [8, 96, TPC], BF16, "ExternalOutput")
        MK = C.dram("MK", [8, 96, TPC], BF16, "ExternalOutput")
        MV = C.dram("MV", [8, 128, TPC // 128, 64], BF16, "ExternalOutput")

        w_v = w_in.ap().rearrange("(c p) n -> p c n", p=128)
        xnT = C.sb([128, 8, NTP], BF16, "xnT")
        f32t = Rot([(("f32t", i), C.sb([128, 512], F32, "f32t")) for i in range(4)])
        uT = C.sb([128, 4, NTP], BF16, "uT")
        cacc = [C.sb([128, 512], F32, "cacc") for g in range(4)]
        csq = [C.sb([128, 512], F32, "csq") for g in range(4)]
        cpar = C.sb([128, 4, 34], F32, "cpar")
        rope_sb = C.sb([96, 2, 512], F32, "rope")
        cql = C.sb([128, 3, 512], F32, "cql")
        ckl = C.sb([128, 2, 512], F32, "ckl")
        latsq = C.sb([128, 3, 512], BF16, "latsq")
        cqn = C.sb([128, 3, 512], BF16, "cqn")
        ckn = C.sb([128, 2, 512], BF16, "ckn")
        krt = C.sb([128, 512], F32, "krt")
        hx = Rot([(("hx", i), C.sb([128, 512], F32, "hx")) for i in range(2)])
        hsq = Rot([(("hsq", i), C.sb([128, 512], BF16, "hsq")) for i in range(2)])
        hxg = Rot([(("hxg", i), C.sb([128, 512], BF16, "hxg")) for i in range(2)])
        wuqb = C.sb([128, 3, 768], BF16, "wuqb")
        wukvb = C.sb([128, 2, 1024], BF16, "wukvb")
        wkrp = C.sb([128, 8, 128], BF16, "wkrp")
        gqk_sb = C.sb([96, 2], F32, "gqk")
        rmat_sb = C.sb([96, 96], BF16, "rmat")
        gcq_sb = C.sb([128, 3], F32, "gcqs")
        gckv_sb = C.sb([128, 2], F32, "gckvs")
        ident = C.sb([128, 128], BF16, "ident")
        gm = C.sb([128, 8], F32, "gm")
        onesb = C.sb([128, 128], BF16, "onesb")
        ones32 = C.sb([128, 128], F32, "ones32")
        xin = Rot([(("xin", i), C.sb([128, D], F32, "xin")) for i in range(2)])
        sqj = C.sb([128, D], F32, "sqj")
        xs = Rot([(("xs", i), C.sb([128, D], BF16, "xs")) for i in range(2)])
        stat = Rot([(("stat", i), C.sb([128, 2], F32, "stat")) for i in range(2)])
        wst = Rot([(("wst", i), C.sb([128, 8, 512], F32, "wst")) for i in range(3)])
        wb = Rot([(("wb", i), C.sb([128, 8, 512], BF16, "wb")) for i in range(3)])
        ob = Rot([(("ob", i), C.sb([128, 512], BF16, "ob")) for i in range(4)])
        g4sb = C.sb([128, NTP // 128, 16], F32, "g4sb")
        pacc = Rot([(("pacc", i), C.ps([128, 512], F32, "pacc")) for i in range(5)])
        ptr = Rot([(("ptr", i), C.ps([128, 1024], BF16, "ptr")) for i in range(2)])

        DMA(P, "sp", cpar[:], convp[:, :, :], [], ["cpar"])
        DMA(P, "sp", gqk_sb[:], gqk[:, :], [], ["gqk"])
        DMA(P, "sp", rmat_sb[:], rmat[:, :], [], ["rmat"])
        DMA(P, "sp", gcq_sb[:], gcq[:, :], [], ["gcqs"])
        DMA(P, "sp", gckv_sb[:], gckv[:, :], [], ["gckvs"])
        DMA(P, "sp", gm[:], gmix[:, :], [], ["gm"])
        if "mla" in phases:
            sk0, st0 = wst.items[0]
            st0f = st0[:].rearrange("p a b -> p (a b)")
            DMA(P, "sp", st0f[:, 0:2304].rearrange("p (a b) -> p a b", a=3),
                w_uq.ap().rearrange("(c p) n -> p c n", p=128), [], [sk0])
            for j in range(3):
                TS(P, "dve", wuqb[:, j, :], st0f[:, j * 768:(j + 1) * 768],
                   gcq_sb[:, j:j + 1], None, ALU.mult, None, [sk0, "gcqs"], ["wuqb"])
            sk1, st1 = wst.items[1]
            st1f = st1[:].rearrange("p a b -> p (a b)")
            DMA(P, "sp", st1f[:, 0:2048].rearrange("p (a b) -> p a b", a=2),
                w_ukv.ap().rearrange("(c p) n -> p c n", p=128), [], [sk1])
            for j in range(2):
                src = st1f[:, j * 1024:(j + 1) * 1024].rearrange("p (h x) -> p h x", h=8)
                TS(P, "dve", wukvb[:, j, 0:512].rearrange("p (h x) -> p h x", h=8), src[:, :, 0:64],
                   gckv_sb[:, j:j + 1], None, ALU.mult, None, [sk1, "gckvs"], ["wukvb"])
                TS(P, "dve", wukvb[:, j, 512:1024].rearrange("p (h x) -> p h x", h=8), src[:, :, 64:128],
                   gckv_sb[:, j:j + 1], None, ALU.mult, None, [sk1, "gckvs"], ["wukvb"])
            MEMSET(P, "pool", wkrp[:], 0.0, ["wkrp"])
            sk2_, st2_ = wst.items[0]
            DMA(P, "sp", st2_[:, :, 0:32], w_v[:, :, O_CKR:O_CKR + 32], [], [sk2_])
            for k in range(8):
                TS(P, "dve", wkrp[:, k, 64:96], st2_[:, k, 0:32], gm[:, k:k + 1], None, ALU.mult, None,
                   [sk2_, "gm"], ["wkrp"])
            MEMSET(P, "pool", krt[:], 0.0, ["krt"])
        DMA(P, "sp", ident[:], identb[:, :], [], ["ident"])
        DMA(P, "sp", gm[:], gmix[:, :], [], ["gm"])
        DMA(P, "sp", ones32[:], onesf[:, :], [], ["ones32"])
        CP(P, "dve", onesb[:], ones32[:], ["ones32"], ["onesb"])

        evac_i = [0]

        def load_w_impl(c0, ncols):
            sk, st = wst.next()
            bk, bt = wb.next()
            DMA(P, "sp", st[:, :, 0:ncols], w_v[:, :, c0:c0 + ncols], [], [sk])
            for k in range(8):
                ACT(P, bt[:, k, 0:ncols], st[:, k, 0:ncols], AF.Copy, [sk, "gm"], [(bk, k)], scale=gm[:, k:k + 1])
            return [(bk, k) for k in range(8)], bt

        wlist = []
        for _ps in range(NPASS):
            if "fm" in phases:
                wlist += [(O_AQ, 512), (O_AK, 512)]
            if "tm" in phases:
                wlist += [(O_AK, 512), (O_AV, 512), (O_AO, 512)] + [(O_GTS + j * 512, 512) for j in range(6)] + [(O_AG, 16)]
            if "conv" in phases:
                wlist += [(O_GLU, 512), (O_GLU + 512, 512)]
            if "mla" in phases:
                wlist += [(O_CQ, 384), (O_CKV, 288)]
        issued = []

        def nextw(c0, ncols):
            if not issued:
                issued.append((wlist[0], load_w_impl(*wlist.pop(0))))
            req, cur = issued.pop(0)
            assert req == (c0, ncols), (req, c0, ncols)
            if wlist:
                issued.append((wlist[0], load_w_impl(*wlist.pop(0))))
            return cur

        for ps_i in range(NPASS):
            t0 = ps_i * TP
            for t in range(NTP // 128):
                xk, xt = xin.next()
                sk2, stt = stat.next()
                xsk, xst = xs.next()
                pk, pt = ptr.next()
                r0 = t0 + t * 128
                DMA(P, "sp", xt[:], xe[r0:r0 + 128, :], [], [xk])
                ACT(P, sqj[:], xt[:], AF.Square, [xk], ["sqj"])
                RSUM(P, "dve", stt[:, 0:1], sqj[:], ["sqj"], [sk2])
                ACT(P, stt[:, 1:2], stt[:, 0:1], AF.Sqrt, [sk2], [sk2], scale=1.0 / D, bias=EPS)
                RECIP(P, "dve", stt[:, 1:2], stt[:, 1:2], [sk2], [sk2])
                ACT(P, xst[:], xt[:], AF.Copy, [xk, sk2], [xsk], scale=stt[:, 1:2])
                for k in range(8):
                    TR(P, pt[:, k * 128:(k + 1) * 128], xst[:, k * 128:(k + 1) * 128], ident[:],
                       [xsk, "ident"], [pk])
                CP(P, ("dve", "pool")[0], xnT[:, :, t * 128:(t + 1) * 128],
                   pt[:].rearrange("p (k t) -> p k t", k=8), [pk], [("xnT", t)])

            def xk_keys(tok0, ntok):
                return [("xnT", t) for t in range(tok0 // 128, (tok0 + ntok - 1) // 128 + 1)]

            if "fm" in phases:
                for (c0, dst) in ((O_AQ, QT), (O_AK, KT)):
                    wk, wt = nextw(c0, 512)
                    for cb in range(4):
                        for tb in range(TP // 512):
                            tk0 = HALO + tb * 512
                            ak, at = pacc.next()
                            for k in range(8):
                                MM(P, at[:, :], wt[:, k, cb * 128:(cb + 1) * 128], xnT[:, k, tk0:tk0 + 512],
                                   k == 0, k == 7, [wk[k]] + xk_keys(tk0, 512), [ak])
                            okk, ot = ob.next()
                            evac_i[0] += 1
                            CP(P, ("act", "dve")[evac_i[0] % 2], ot[:, :], at[:, :], [ak], [okk])
                            DMA(P, "sp", dst[cb * 128:(cb + 1) * 128, t0 + tb * 512:t0 + (tb + 1) * 512], ot[:, :],
                                [okk], [])

            if "tm" in phases:
                blocks = [(O_AK, Kt, 0, "copy"), (O_AV, Vt, 0, "copy"), (O_AO, OG, 0, "sig")]
                for j in range(6):
                    blocks.append((O_GTS + j * 512, GTS, j * 512, "sig"))
                for (c0, dst, dc0, mode) in blocks:
                    wk, wt = nextw(c0, 512)
                    for t in range(TP // 128):
                        tk0 = HALO + t * 128
                        ak, at = pacc.next()
                        for k in range(8):
                            MM(P, at[:, :], xnT[:, k, tk0:tk0 + 128], wt[:, k, :], k == 0, k == 7,
                               [wk[k]] + xk_keys(tk0, 128), [ak])
                        okk, ot = ob.next()
                        if mode == "sig":
                            ACT(P, ot[:, :], at[:, :], AF.Sigmoid, [ak], [okk])
                        else:
                            evac_i[0] += 1
                            CP(P, ("act", "dve")[evac_i[0] % 2], ot[:, :], at[:, :], [ak], [okk])
                        if dst is GTS:
                            DMA(P, "sp", dst[t0 + t * 128:t0 + (t + 1) * 128, dc0:dc0 + 512], ot[:, :], [okk], [])
                        else:
                            DMA(P, "sp", dst[:, t0 + t * 128:t0 + (t + 1) * 128, :].rearrange("h t d -> t h d"),
                                ot[:, :].rearrange("p (h d) -> p h d", h=4), [okk], [])
                wk, wt = nextw(O_AG, 16)
                for t in range(TP // 128):
                    tk0 = HALO + t * 128
                    ak, at = pacc.next()
                    for k in range(8):
                        MM(P, at[:, 0:16], xnT[:, k, tk0:tk0 + 128], wt[:, k, 0:16], k == 0, k == 7,
                           [wk[k]] + xk_keys(tk0, 128), [ak])
                    CP(P, "dve", g4sb[:, t, :].rearrange("p (h g) -> p h g", h=4), at[:, 0:16].rearrange("p (g h) -> p h g", h=4),
                       [ak], [("g4", t)])
                for h in range(4):
                    DMA(P, "sp", G4[h, t0:t0 + TP, :].rearrange("(t p) g -> p t g", p=128), g4sb[:, 0:TP // 128, h * 4:(h + 1) * 4],
                        [("g4", t) for t in range(TP // 128)], [])

            if "conv" in phases:
                wak, wat = nextw(O_GLU, 512)
                wgk, wgt = nextw(O_GLU + 512, 512)
                blks = [(b0, min(512, NTP - b0)) for b0 in range(0, NTP, 512)]
                for g in range(4):
                    for (b0, bn) in blks:
                        ak, at = pacc.next()
                        gk, gt = pacc.next()
                        for k in range(8):
                            MM(P, at[:, 0:bn], wat[:, k, g * 128:(g + 1) * 128], xnT[:, k, b0:b0 + bn],
                               k == 0, k == 7, [wak[k]] + xk_keys(b0, bn), [ak])
                        for k in range(8):
                            MM(P, gt[:, 0:bn], wgt[:, k, g * 128:(g + 1) * 128], xnT[:, k, b0:b0 + bn],
                               k == 0, k == 7, [wgk[k]] + xk_keys(b0, bn), [gk])
                        fk, ft = f32t.next()
                        ACT(P, ft[:, 0:bn], gt[:, 0:bn], AF.Sigmoid, [gk], [fk])
                        TT(P, "dve", uT[:, g, b0:b0 + bn], at[:, 0:bn], ft[:, 0:bn], ALU.mult, [ak, fk],
                           [("uT", g, b0 // 512)])
                for tb in range(TP // 512):
                    c0 = HALO + tb * 512 - 15
                    ukeys = lambda g: [("uT", g, j) for j in range(c0 // 512, (c0 + 542 - 1) // 512 + 1)]
                    for g in range(4):
                        eng = "dve"
                        ck = ("cacc", g)
                        ca = cacc[g]
                        TS(P, eng, ca[:, :], uT[:, g, c0:c0 + 512], cpar[:, g, 0:1], cpar[:, g, 31:32],
                           ALU.mult, ALU.add, ukeys(g) + ["cpar"], [ck])
                        for k in range(1, 31):
                            STT(P, eng, ca[:, :], uT[:, g, c0 + k:c0 + k + 512], cpar[:, g, k:k + 1], ca[:, :],
                                ALU.mult, ALU.add, ukeys(g) + ["cpar", ck], [ck])
                    mk, mt = pacc.next()
                    for g in range(4):
                        MM(P, mt[:, :], ones32[:, :], cacc[g][:, :], g == 0, g == 3, ["ones32", ("cacc", g)], [mk])
                    for g in range(4):
                        STT(P, "dve", cacc[g][:, :], mt[:, :], -1.0 / 512, cacc[g][:, :], ALU.mult, ALU.add,
                            [mk, ("cacc", g)], [("cacc", g)])
                        ACT(P, csq[g][:, :], cacc[g][:, :], AF.Square, [("cacc", g)], [("csq", g)])
                    vk, vt = pacc.next()
                    for g in range(4):
                        MM(P, vt[:, :], ones32[:, :], csq[g][:, :], g == 0, g == 3, ["ones32", ("csq", g)], [vk])
                    fk, ft = f32t.next()
                    ACT(P, ft[:, :], vt[:, :], AF.Sqrt, [vk], [fk], scale=1.0 / 512, bias=EPS)
                    RECIP(P, "dve", ft[:, :], ft[:, :], [fk], [fk])
                    for g in range(4):
                        TT(P, "dve", csq[g][:, :], cacc[g][:, :], ft[:, :], ALU.mult, [("cacc", g), fk], [("csq", g)])
                        TS(P, "pool", csq[g][:, :], csq[g][:, :], cpar[:, g, 32:33], cpar[:, g, 33:34],
                           ALU.mult, ALU.add, [("csq", g), "cpar"], [("csq", g)])
                        okk, ot = ob.next()
                        ACT(P, ot[:, :], csq[g][:, :], AF.Silu, [("csq", g)], [okk])
                        DMA(P, "sp", UT[g * 128:(g + 1) * 128, t0 + tb * 512:t0 + (tb + 1) * 512], ot[:, :], [okk], [])

            if "mla" in phases:
                wqk, wqt = nextw(O_CQ, 384)
                wkk, wkt = nextw(O_CKV, 288)
                for tb in range(TP // 512):
                    tk0 = HALO + tb * 512
                    g0 = t0 + tb * 512
                    DMA(P, "sp", rope_sb[:, :, :], ropeT[:, :, g0:g0 + 512], [], ["rope"])
                    for (wk_, wt_, nblk, lat, latn, lkey, dim) in ((wqk, wqt, 3, cql, cqn, "cq", 384.0),
                                                                 (wkk, wkt, 2, ckl, ckn, "ckv", 256.0)):
                        for j in range(nblk):
                            ak, at = pacc.next()
                            for k in range(8):
                                MM(P, at[:, :], wt_[:, k, j * 128:(j + 1) * 128], xnT[:, k, tk0:tk0 + 512],
                                   k == 0, k == 7, [wk_[k]] + xk_keys(tk0, 512), [ak])
                            CP(P, "dve", lat[:, j, :], at[:, :], [ak], [(lkey, j)])
                            ACT(P, latsq[:, j, :], lat[:, j, :], AF.Square, [(lkey, j)], [(lkey + "sq", j)])
                        sk_, st_ = pacc.next()
                        for j in range(nblk):
                            MM(P, st_[:, :], onesb[:, :], latsq[:, j, :], j == 0, j == nblk - 1,
                               ["onesb", (lkey + "sq", j)], [sk_])
                        fk, ft = f32t.next()
                        ACT(P, ft[:, :], st_[:, :], AF.Sqrt, [sk_], [fk], scale=1.0 / dim, bias=EPS)
                        RECIP(P, "dve", ft[:, :], ft[:, :], [fk], [fk])
                        for j in range(nblk):
                            if "dbg5" in phases:
                                TT(P, "dve", lat[:, j, :], lat[:, j, :], ft[:, :], ALU.mult,
                                   [(lkey, j), fk], [(lkey, j)])
                                CP(P, "act", latn[:, j, :], lat[:, j, :], [(lkey, j)], [(lkey + "n", j)])
                            else:
                                TT(P, "dve", latn[:, j, :], lat[:, j, :], ft[:, :], ALU.mult,
                                   [(lkey, j), fk], [(lkey + "n", j)])
                    if "nokr" not in phases:
                        ak, at = pacc.next()
                        for k in range(8):
                            MM(P, at[:, :], wkrp[:, k, :], xnT[:, k, tk0:tk0 + 512], k == 0, k == 7,
                               ["wkrp"] + xk_keys(tk0, 512), [ak])
                        CP(P, "act", krt[0:96, :], at[0:96, :], [ak], ["krt"])
                    cqn_keys = [("cqn", j) for j in range(3)]
                    ckn_keys = [("ckvn", j) for j in range(2)]
                    for h in (range(8) if "nomlah" not in phases else []):
                        for which in ("q", "k"):
                            ak, at = pacc.next()
                            xk_, xt_ = hx.next()
                            if which == "q":
                                for j in range(3):
                                    MM(P, at[0:96, :], wuqb[:, j, h * 96:(h + 1) * 96], cqn[:, j, :], j == 0, j == 2,
                                       ["wuqb"] + cqn_keys, [ak])
                                CP(P, "act", xt_[0:96, :], at[0:96, :], [ak], [xk_])
                            else:
                                for j in range(2):
                                    MM(P, at[0:64, :], wukvb[:, j, h * 64:(h + 1) * 64], ckn[:, j, :], j == 0, j == 1,
                                       ["wukvb"] + ckn_keys, [ak])
                                CP(P, "act", xt_[0:64, :], at[0:64, :], [ak], [xk_])
                                CP(P, "pool", xt_[64:96, :], krt[64:96, :], ["krt"], [xk_])
                            sqk, sqt = hsq.next()
                            ACT(P, sqt[0:96, :], xt_[0:96, :], AF.Square, [xk_], [sqk])
                            sk_, st_ = pacc.next()
                            MM(P, st_[0:96, :], onesb[0:96, 0:96], sqt[0:96, :], True, True, ["onesb", sqk], [sk_])
                            fk, ft = f32t.next()
                            ACT(P, ft[0:96, :], st_[0:96, :], AF.Sqrt, [sk_], [fk], scale=1.0 / 96, bias=EPS)
                            RECIP(P, "dve", ft[0:96, :], ft[0:96, :], [fk], [fk])
                            gcol = 0 if which == "q" else 1
                            xgk, xgt = hxg.next()
                            TS(P, "dve", xgt[0:96, :], xt_[0:96, :], gqk_sb[0:96, gcol:gcol + 1], None, ALU.mult, None,
                               [xk_, "gqk"], [xgk])
                            rk, rt = pacc.next()
                            MM(P, rt[0:96, :], rmat_sb[0:96, 0:96], xgt[0:96, :], True, True, ["rmat", xgk], [rk])
                            t1k, t1 = f32t.next()
                            TT(P, "pool", t1[0:96, :], xgt[0:96, :], rope_sb[:, 0, :], ALU.mult, [xgk, "rope"], [t1k])
                            t2k, t2 = f32t.next()
                            TT(P, "dve", t2[0:96, :], rt[0:96, :], rope_sb[:, 1, :], ALU.mult, [rk, "rope"], [t2k])
                            TT(P, "pool", t1[0:96, :], t1[0:96, :], t2[0:96, :], ALU.add, [t1k, t2k], [t1k])
                            okk, ot = ob.next()
                            TT(P, "dve", ot[0:96, :], t1[0:96, :], ft[0:96, :], ALU.mult, [t1k, fk], [okk])
                            dst = MQ if which == "q" else MK
                            DMA(P, "sp", dst[h, :, g0:g0 + 512], ot[0:96, :], [okk], [])
                    if "dupgrp" in phases:
                        for rep in range(2):
                            ak, at = pacc.next()
                            for k in range(8):
                                MM(P, at[:, :], wkt[:, k, 0:128], xnT[:, k, tk0:tk0 + 512],
                                   k == 0, k == 7, [wkk[k]] + xk_keys(tk0, 512), [ak])
                    for t in (range(4 if "v_one" not in phases else 1) if "nomlav" not in phases else []):
                        ak, at = pacc.next()
                        for j in range(2):
                            MM(P, at[:, :], (xnT[:, j, tk0 + t * 128:tk0 + (t + 1) * 128] if "dbg1" in phases else ckn[:, j, t * 128:(t + 1) * 128]),
                               (wkt[:, j, :] if "dbg2" in phases else (wukvb[:, j, 0:512] if "dbg4" in phases else wukvb[:, j, 512:1024])), j == 0, j == 1,
                               ["wukvb"] + ckn_keys, [ak])
                        okk, ot = ob.next()
                        if "v_noevac" in phases:
                            continue
                        CP(P, "dve", ot[:, :], at[:, :], [ak], [okk])
                        if "v_nodma" in phases:
                            continue
                        DMA(P, "sp", MV[:, :, (g0 + t * 128) // 128, :].rearrange("h p d -> p h d"),
                            ot[:, :].rearrange("p (h d) -> p h d", h=8), [okk], [])

        P.emit()
        stats = P.stats
    return nc, stats


def _gain_cols(g, nch):
    return np.ascontiguousarray(g.reshape(nch, 128).T)


def rope_tables():
    pos = np.arange(S, dtype=np.float32)
    inv = (10000.0 ** (-np.arange(0, 32, 2, dtype=np.float32) / np.float32(32))).astype(np.float32)
    ang = (pos[:, None] * inv[None, :]).astype(np.float32)
    c = np.cos(ang.astype(np.float64)).astype(np.float32)
    s = np.sin(ang.astype(np.float64)).astype(np.float32)
    CT = np.ones((96, S), np.float32)
    ST = np.zeros((96, S), np.float32)
    CT[64:80] = c.T
    CT[80:96] = c.T
    ST[64:80] = s.T
    ST[80:96] = s.T
    return CT, ST


def consts():
    R = np.zeros((96, 96), np.float32)
    for j in range(16):
        R[80 + j, 64 + j] = -1.0
        R[64 + j, 80 + j] = 1.0
    return dict(identb=np.eye(128, dtype=np.float32).astype(NPBF), rmat=R.astype(NPBF),
                onesf=np.ones((128, 128), np.float32))


def stageA_inmaps(x, prm, l):
    CT, ST = rope_tables()
    cst = consts()
    convp = np.zeros((128, 4, 34), np.float32)
    cw = prm["conv_w"][l]
    convp[:, :, 0:31] = cw.T.reshape(4, 128, 31).transpose(1, 0, 2)
    convp[:, :, 31] = prm["conv_b"][l].reshape(4, 128).T
    convp[:, :, 32] = prm["conv_ln_g"][l].reshape(4, 128).T
    convp[:, :, 33] = prm["conv_ln_b"][l].reshape(4, 128).T
    maps = []
    for c in range(NCORES):
        b, q = c // 4, c % 4
        s0 = q * TPC
        xe = np.zeros((TPC + 2 * HALO, D), np.float32)
        lo, hi = max(0, s0 - HALO), min(S, s0 + TPC + HALO)
        xe[lo - (s0 - HALO):hi - (s0 - HALO)] = x[b, lo:hi]
        rope = np.stack([CT[:, s0:s0 + TPC], ST[:, s0:s0 + TPC]], axis=1)
        maps.append(dict(
            xe=xe, w_in=prm["w_in"][l], gmix=_gain_cols(prm["mix_norm_g"][l], 8),
            identb=cst["identb"], convp=convp, gcq=_gain_cols(prm["cq_norm_g"][l], 3),
            gckv=_gain_cols(prm["ckv_norm_g"][l], 2), w_uq=prm["w_uq"][l], w_ukv=prm["w_ukv"][l],
            gqk=np.ascontiguousarray(np.stack([prm["q_norm_g"][l], prm["k_norm_g"][l]], axis=1)),
            ropeT=np.ascontiguousarray(rope), rmat=cst["rmat"], onesf=cst["onesf"]))
    return maps


def build_mlstm(nch=S // 128, env=None, pre=None):
    nc, es, C, P = _begin(env, pre)
    ns = nch * 128
    lnscale = math.log(128.0 ** -0.5)
    with es:
        qTd = C.dram("qT", [128, ns], BF16, "ExternalInput")
        ktd = C.dram("kt", [ns, 128], BF16, "ExternalInput")
        vtd = C.dram("vt", [ns, 128], BF16, "ExternalInput")
        g4d = C.dram("g4", [ns, 4], F32, "ExternalInput")
        bifd = C.dram("bif", [128, 4], F32, "ExternalInput")
        ogd = C.dram("og", [ns, 128], BF16, "ExternalInput")
        gAd = C.dram("gA", [128, 128], F32, "ExternalInput")
        cmat = C.dram("cmat", [128, 6, 128], F32, "ExternalInput")
        identbd = C.dram("identb", [128, 128], BF16, "ExternalInput")
        HT = C.dram("HT", [128, ns], BF16, "ExternalOutput")

        qT = C.sb([128, ns], BF16, "qT")
        kt = C.sb([128, nch, 128], BF16, "kt")
        vt = C.sb([128, nch, 129], BF16, "vt")
        hacc = C.sb([128, nch, 128], F32, "hacc")
        g4 = C.sb([128, nch, 4], F32, "g4")
        bif = C.sb([128, 4], F32, "bif")
        nbif = C.sb([128, 4], F32, "nbif")
        gA = C.sb([128, 128], F32, "gA")
        cm = C.sb([128, 6, 128], F32, "cm")
        identb = C.sb([128, 128], BF16, "identb")
        gt = {n: C.sb([128, nch], F32, n) for n in ("lf", "ib", "bcum", "gtot", "biasS", "wint", "wk", "dec", "tmp")}
        Cf = C.sb([128, 129], F32, "Cf")
        Cb = C.sb([128, 129], BF16, "Cb")
        LF = Rot([(("LF", i), C.sb([128, 128], F32, "LF")) for i in range(2)])
        Dm = Rot([(("Dm", i), C.sb([128, 128], F32, "Dm")) for i in range(2)])
        kTc = Rot([(("kTc", i), C.sb([128, 128], BF16, "kTc")) for i in range(2)])
        SD = Rot([(("SD", i), C.sb([128, 128], BF16, "SD")) for i in range(2)])
        isb = Rot([(("isb", i), C.sb([128, 129], F32, "isb")) for i in range(2)])
        num = Rot([(("num", i), C.sb([128, 129], F32, "num")) for i in range(2)])
        dn = Rot([(("dn", i), C.sb([128, 2], F32, "dn")) for i in range(2)])
        Vw = Rot([(("Vw", i), C.sb([128, 129], BF16, "Vw")) for i in range(2)])
        ogt = Rot([(("ogt", i), C.sb([128, 128], BF16, "ogt")) for i in range(2)])
        hn = Rot([(("hn", i), C.sb([128, 128], F32, "hn")) for i in range(2)])
        hb = Rot([(("hb", i), C.sb([128, 128], BF16, "hb")) for i in range(2)])
        hT = Rot([(("hT", i), C.sb([128, 128], BF16, "hT")) for i in range(2)])
        sq = C.sb([128, 128], F32, "sq")
        pA = Rot([(("pA", i), C.ps([128, 512], F32, "pA")) for i in range(6)])
        pB = Rot([(("pB", i), C.ps([128, 1024], BF16, "pB")) for i in range(2)])

        DMA(P, "sp", qT[:, :], qTd[:, :], [], ["qT"])
        DMA(P, "sp", kt[:, :, :], ktd.ap().rearrange("(c p) d -> p c d", p=128), [], ["kt"])
        DMA(P, "sp", vt[:, :, 0:128], vtd.ap().rearrange("(c p) d -> p c d", p=128), [], ["vt"])
        MEMSET(P, "pool", vt[:, :, 128:129], 1.0, ["vt1"])
        DMA(P, "sp", g4[:, :, :], g4d.ap().rearrange("(c p) g -> p c g", p=128), [], ["g4"])
        DMA(P, "sp", bif[:, :], bifd[:, :], [], ["bif"])
        DMA(P, "sp", gA[:, :], gAd[:, :], [], ["gA"])
        DMA(P, "sp", cm[:, :, :], cmat[:, :, :], [], ["cm"])
        DMA(P, "sp", identb[:, :], identbd[:, :], [], ["identb"])
        TS(P, "dve", nbif[:, :], bif[:, :], -1.0, None, ALU.mult, None, ["bif"], ["nbif"])
        ident32 = cm[:, 4, :]
        ones32 = cm[:, 5, :]
        for d in range(2):
            Ud = cm[:, d, :]
            NEGd = cm[:, 2 + d, :]
            ic, fc = 2 * d, 2 * d + 1
            ACT(P, gt["tmp"][:, :], g4[:, :, fc], AF.Exp, ["g4", "nbif"], ["tmp"], scale=-1.0, bias=nbif[:, fc:fc + 1])
            ACT(P, gt["tmp"][:, :], gt["tmp"][:, :], AF.Ln, ["tmp"], ["tmp"], bias=1.0)
            TS(P, "dve", gt["lf"][:, :], gt["tmp"][:, :], -1.0, None, ALU.mult, None, ["tmp"], ["lf"])
            TS(P, "dve", gt["ib"][:, :], g4[:, :, ic], bif[:, ic:ic + 1], lnscale, ALU.add, ALU.add, ["g4", "bif"], ["ib"])
            bk, bp = pA.next()
            MM(P, bp[:, 0:nch], Ud, gt["lf"][:, :], True, True, ["cm", "lf"], [bk])
            CP(P, "dve", gt["bcum"][:, :], bp[:, 0:nch], [bk], ["bcum"])
            gk, gp = pA.next()
            MM(P, gp[:, 0:nch], ones32, gt["lf"][:, :], True, True, ["cm", "lf"], [gk])
            CP(P, "dve", gt["gtot"][:, :], gp[:, 0:nch], [gk], ["gtot"])
            TT(P, "dve", gt["biasS"][:, :], gt["ib"][:, :], gt["bcum"][:, :], ALU.subtract, ["ib", "bcum"], ["biasS"])
            ACT(P, gt["wint"][:, :], gt["bcum"][:, :], AF.Exp, ["bcum"], ["wint"])
            TT(P, "dve", gt["tmp"][:, :], gt["biasS"][:, :], gt["gtot"][:, :], ALU.add, ["biasS", "gtot"], ["tmp"])
            ACT(P, gt["wk"][:, :], gt["tmp"][:, :], AF.Exp, ["tmp"], ["wk"])
            ACT(P, gt["dec"][:, :], gt["gtot"][:, :], AF.Exp, ["gtot"], ["dec"])
            MEMSET(P, "dve", Cf[:, :], 0.0, ["Cf"])
            MEMSET(P, "pool", Cb[:, :], 0.0, ["Cb"])
            order = range(nch) if d == 0 else range(nch - 1, -1, -1)
            for c in order:
                lk, lt = LF.next()
                ACT(P, lt[:, :], ones32, AF.Copy, ["cm", "lf"], [lk], scale=gt["lf"][:, c:c + 1])
                dk, dp = pA.next()
                MM(P, dp[:, 0:128], lt[:, :], Ud, True, False, [lk, "cm"], [dk])
                MM(P, dp[:, 0:128], ident32, NEGd, False, True, ["cm"], [dk])
                mk_, mt_ = Dm.next()
                ACT(P, mt_[:, :], dp[:, 0:128], AF.Exp, [dk, "biasS"], [mk_], bias=gt["biasS"][:, c:c + 1])
                tk, tp = pB.next()
                TR(P, tp[:, 0:128], kt[:, c, :], identb[:, :], ["kt", "identb"], [tk])
                kck, kct = kTc.next()
                CP(P, "dve", kct[:, :], tp[:, 0:128], [tk], [kck])
                sk, sp_ = pA.next()
                MM(P, sp_[:, 0:128], kct[:, :], qT[:, c * 128:(c + 1) * 128], True, True, [kck, "qT"], [sk])
                sdk, sdt = SD.next()
                TT(P, "dve", sdt[:, :], sp_[:, 0:128], mt_[:, :], ALU.mult, [sk, mk_], [sdk])
                nk_, np_ = pA.next()
                MM(P, np_[:, 0:129], sdt[:, :], vt[:, c, :], True, True, [sdk, "vt", "vt1"], [nk_])
                ik, ip = pA.next()
                MM(P, ip[:, 0:129], qT[:, c * 128:(c + 1) * 128], Cb[:, :], True, True, ["qT", "Cb"], [ik])
                isk, ist = isb.next()
                ACT(P, ist[:, :], ip[:, 0:129], AF.Copy, [ik, "wint"], [isk], scale=gt["wint"][:, c:c + 1])
                nmk, nmt = num.next()
                TT(P, "dve", nmt[:, :], np_[:, 0:129], ist[:, :], ALU.add, [nk_, isk], [nmk])
                dnk, dnt = dn.next()
                ACT(P, dnt[:, 0:1], nmt[:, 128:129], AF.Abs, [nmk], [dnk])
                TS(P, "dve", dnt[:, 0:1], dnt[:, 0:1], 1.0, None, ALU.max, None, [dnk], [dnk])
                RECIP(P, "dve", dnt[:, 1:2], dnt[:, 0:1], [dnk], [dnk])
                if d == 0:
                    TS(P, "dve", hacc[:, c, :], nmt[:, 0:128], dnt[:, 1:2], None, ALU.mult, None, [nmk, dnk], [("hacc", c)])
                else:
                    STT(P, "dve", hacc[:, c, :], nmt[:, 0:128], dnt[:, 1:2], hacc[:, c, :], ALU.mult, ALU.add,
                        [nmk, dnk, ("hacc", c)], [("hacc", c)])
                vwk, vwt = Vw.next()
                TS(P, "pool", vwt[:, :], vt[:, c, :], gt["wk"][:, c:c + 1], None, ALU.mult, None, ["vt", "vt1", "wk"], [vwk])
                ck, cp_ = pA.next()
                MM(P, cp_[:, 0:129], kt[:, c, :], vwt[:, :], True, True, ["kt", vwk], [ck])
                STT(P, "dve", Cf[:, :], Cf[:, :], gt["dec"][:, c:c + 1], cp_[:, 0:129], ALU.mult, ALU.add,
                    ["Cf", "dec", ck], ["Cf"])
                CP(P, "act", Cb[:, :], Cf[:, :], ["Cf"], ["Cb"])
        for c in range(nch):
            ogk, ogt_ = ogt.next()
            DMA(P, "sp", ogt_[:, :], ogd[c * 128:(c + 1) * 128, :], [], [ogk])
            dnk, dnt = dn.next()
            ACT(P, sq[:, :], hacc[:, c, :], AF.Square, [("hacc", c)], ["sq"])
            RSUM(P, "dve", dnt[:, 0:1], sq[:, :], ["sq"], [dnk])
            ACT(P, dnt[:, 1:2], dnt[:, 0:1], AF.Sqrt, [dnk], [dnk], scale=1.0 / 128, bias=EPS)
            RECIP(P, "dve", dnt[:, 1:2], dnt[:, 1:2], [dnk], [dnk])
            hk, ht = hn.next()
            STT(P, "dve", ht[:, :], hacc[:, c, :], dnt[:, 1:2], gA[:, :], ALU.mult, ALU.mult, [("hacc", c), dnk, "gA"], [hk])
            hbk, hbt = hb.next()
            TT(P, "pool", hbt[:, :], ht[:, :], ogt_[:, :], ALU.mult, [hk, ogk], [hbk])
            tk, tp = pB.next()
            TR(P, tp[:, 0:128], hbt[:, :], identb[:, :], [hbk, "identb"], [tk])
            htk, htt = hT.next()
            CP(P, "act", htt[:, :], tp[:, 0:128], [tk], [htk])
            DMA(P, "sp", HT[:, c * 128:(c + 1) * 128], htt[:, :], [htk], [])
        P.emit()
        stats = P.stats
    return nc, stats


def mlstm_consts():
    s_ = np.arange(128)[:, None]
    t_ = np.arange(128)[None, :]
    U = (s_ <= t_).astype(np.float32)
    cm = np.zeros((128, 6, 128), np.float32)
    cm[:, 0] = U
    cm[:, 1] = U.T
    cm[:, 2] = np.where(s_ <= t_, 0.0, -30000.0)
    cm[:, 3] = np.where(s_ >= t_, 0.0, -30000.0)
    cm[:, 4] = np.eye(128)
    cm[:, 5] = 1.0
    return dict(cmat=cm, identb=np.eye(128, dtype=np.float32).astype(NPBF))


def build_merge(ntile=TPC // 128, env=None, pre=None):
    nc, es, C, P = _begin(env, pre)
    nt = ntile * 128
    with es:
        xd = C.dram("x", [nt, D], F32, "ExternalInput")
        srcs = [C.dram(n, [512, nt], BF16, "ExternalInput") for n in ("HT", "UT", "OT")]
        gtsd = C.dram("GTS", [nt, 3072], BF16, "ExternalInput")
        wds = [C.dram(n, [512, D], F32, "ExternalInput") for n in ("w_a", "w_b", "w_c")]
        wod = C.dram("w_o", [D, D], F32, "ExternalInput")
        gfd = C.dram("gffn", [128, 8], F32, "ExternalInput")
        wrd = C.dram("w_r", [D, 16], F32, "ExternalInput")
        identbd = C.dram("identb", [128, 128], BF16, "ExternalInput")
        ident32d = C.dram("ident32", [128, 128], F32, "ExternalInput")
        x1d = C.dram("x1", [nt, D], F32, "ExternalOutput")
        xn2d = C.dram("xn2T", [D, nt], BF16, "ExternalOutput")
        affd = C.dram("aff", [nt, 16], F32, "ExternalOutput")
        affTd = C.dram("affT", [16, nt], F32, "ExternalOutput")
        affTs = C.sb([16, nt], F32, "affTs")

        wbr = [C.sb([128, 4, D], BF16, "wbr") for _ in range(3)]
        wo = C.sb([128, 8, D], BF16, "wo")
        wst = Rot([(("wst", i), C.sb([128, 4, D], F32, "wst")) for i in range(2)])
        wr = C.sb([128, 8, 16], F32, "wr")
        gf = C.sb([128, 8], F32, "gf")
        gfull = C.sb([128, 8, 128], F32, "gfull")
        identb = C.sb([128, 128], BF16, "identb")
        ident32 = C.sb([128, 128], F32, "ident32")
        srct = [Rot([((("src", b), i), C.sb([128, 4, 128], BF16, "src")) for i in range(2)]) for b in range(3)]
        gts = Rot([(("gts", i), C.sb([128, 3072], BF16, "gts")) for i in range(2)])
        xt = Rot([(("xt", i), C.sb([128, D], F32, "xt")) for i in range(2)])
        mgR = Rot([(("mg", i), C.sb([128, D], F32, "mg")) for i in range(2)])
        tmpm = Rot([(("tmpm", i), C.sb([128, 512], F32, "tmpm")) for i in range(2)])
        mgbR = Rot([(("mgb", i), C.sb([128, D], BF16, "mgb")) for i in range(2)])
        mTR = Rot([(("mT", i), C.sb([128, 8, 128], BF16, "mT")) for i in range(2)])
        x1 = Rot([(("x1", i), C.sb([128, D], F32, "x1")) for i in range(2)])
        sqR = Rot([(("sqj", i), C.sb([128, D], F32, "sqj")) for i in range(2)])
        xsR = Rot([(("xs", i), C.sb([128, D], F32, "xs")) for i in range(2)])
        st = Rot([(("st", i), C.sb([128, 16], F32, "st")) for i in range(2)])
        xTR = Rot([(("xT32", i), C.sb([128, 8, 128], F32, "xT32")) for i in range(2)])
        xTb = Rot([(("xTb", i), C.sb([128, 8, 128], BF16, "xTb")) for i in range(2)])
        lgR = Rot([(("lg", i), C.sb([128, 16], F32, "lg")) for i in range(2)])
        exR = Rot([(("ex", i), C.sb([128, 16], F32, "ex")) for i in range(2)])
        affs = C.sb([128, ntile, 16], F32, "affs")
        pA = Rot([(("pA", i), C.ps([128, 512], F32, "pA")) for i in range(5)])
        pBR = Rot([(("pB", i), C.ps([128, 1024], BF16, "pB")) for i in range(2)])

        DMA(P, "sp", identb[:, :], identbd[:, :], [], ["identb"])
        DMA(P, "sp", ident32[:, :], ident32d[:, :], [], ["ident32"])
        DMA(P, "sp", gf[:, :], gfd[:, :], [], ["gf"])
        DMA(P, "sp", wr[:, :, :], wrd.ap().rearrange("(c p) e -> p c e", p=128), [], ["wr"])
        for b in range(3):
            sk, stg = wst.next()
            DMA(P, "sp", stg[:, :, :], wds[b].ap().rearrange("(c p) n -> p c n", p=128), [], [sk])
            for c in range(4):
                CP(P, ("dve", "pool")[c % 2], wbr[b][:, c, :], stg[:, c, :], [sk], [("wbr", b)])
        for hh in range(2):
            sk, stg = wst.next()
            DMA(P, "sp", stg[:, :, :], wod.ap().rearrange("(c p) n -> p c n", p=128)[:, hh * 4:(hh + 1) * 4, :], [], [sk])
            for c in range(4):
                CP(P, ("dve", "pool")[c % 2], wo[:, hh * 4 + c, :], stg[:, c, :], [sk], ["wo"])
        for k in range(8):
            TS(P, "pool", gfull[:, k, :], ident32[:, :], 0.0, gf[:, k:k + 1], ALU.mult, ALU.add, ["ident32", "gf"], ["gfull"])
        for t in range(ntile):
            r0 = t * 128
            mgk, mg = mgR.next()
            mgbk, mgb = mgbR.next()
            mTk, mT = mTR.next()
            sqk, sqj = sqR.next()
            xsk, xs = xsR.next()
            xTk, xT32 = xTR.next()
            lgk, lg = lgR.next()
            exk, ex = exR.next()
            pBk, pB = pBR.next()
            xk, xt_ = xt.next()
            DMA(P, "sp", xt_[:, :], xd[r0:r0 + 128, :], [], [xk])
            gk, gt_ = gts.next()
            DMA(P, "sp", gt_[:, :], gtsd[r0:r0 + 128, :], [], [gk])
            skeys = []
            stiles = []
            for b in range(3):
                k_, t_ = srct[b].next()
                DMA(P, "sp", t_[:, :, :], srcs[b].ap().rearrange("(c p) t -> p c t", p=128)[:, :, r0:r0 + 128], [], [k_])
                skeys.append(k_)
                stiles.append(t_)
            for b in range(3):
                for hf in range(2):
                    ak, at = pA.next()
                    for c in range(4):
                        MM(P, at[:, :], stiles[b][:, c, :], wbr[b][:, c, hf * 512:(hf + 1) * 512], c == 0, c == 3,
                           [skeys[b], ("wbr", b)], [ak])
                    gsl = gt_[:, b * 1024 + hf * 512:b * 1024 + (hf + 1) * 512]
                    if b == 0:
                        TT(P, "dve", mg[:, hf * 512:(hf + 1) * 512], at[:, :], gsl, ALU.mult, [ak, gk], [(mgk, hf)])
                    else:
                        tk, tt_ = tmpm.next()
                        TT(P, "dve", tt_[:, :], at[:, :], gsl, ALU.mult, [ak, gk], [tk])
                        TT(P, "pool", mg[:, hf * 512:(hf + 1) * 512], mg[:, hf * 512:(hf + 1) * 512], tt_[:, :], ALU.add,
                           [(mgk, hf), tk], [(mgk, hf)])
            CP(P, "act", mgb[:, :], mg[:, :], [(mgk, 0), (mgk, 1)], [mgbk])
            for k in range(8):
                TR(P, pB[:, k * 128:(k + 1) * 128], mgb[:, k * 128:(k + 1) * 128], identb[:, :], [mgbk, "identb"], [pBk])
            CP(P, "dve", mT[:, :, :], pB[:].rearrange("p (k t) -> p k t", k=8), [pBk], [mTk])
            x1k, x1t = x1.next()
            for hf in range(2):
                ak, at = pA.next()
                for k in range(8):
                    MM(P, at[:, :], mT[:, k, :], wo[:, k, hf * 512:(hf + 1) * 512], k == 0, k == 7, [mTk, "wo"], [ak])
                TT(P, "dve", x1t[:, hf * 512:(hf + 1) * 512], at[:, :], xt_[:, hf * 512:(hf + 1) * 512], ALU.add,
                   [ak, xk], [(x1k, hf)])
            DMA(P, "sp", x1d[r0:r0 + 128, :], x1t[:, :], [(x1k, 0), (x1k, 1)], [])
            sk_, st_ = st.next()
            ACT(P, sqj[:, :], x1t[:, :], AF.Square, [(x1k, 0), (x1k, 1)], [sqk])
            RSUM(P, "dve", st_[:, 0:1], sqj[:, :], [sqk], [sk_])
            ACT(P, st_[:, 1:2], st_[:, 0:1], AF.Sqrt, [sk_], [sk_], scale=1.0 / D, bias=EPS)
            RECIP(P, "dve", st_[:, 1:2], st_[:, 1:2], [sk_], [sk_])
            ACT(P, xs[:, :], x1t[:, :], AF.Copy, [(x1k, 0), (x1k, 1), sk_], [xsk], scale=st_[:, 1:2])
            for hf in range(2):
                ak, at = pA.next()
                for k in range(4):
                    kk = hf * 4 + k
                    TR(P, at[:, k * 128:(k + 1) * 128], xs[:, kk * 128:(kk + 1) * 128], ident32[:, :], [xsk, "ident32"], [ak])
                TT(P, "dve", xT32[:, hf * 4:(hf + 1) * 4, :], at[:].rearrange("p (k t) -> p k t", k=4),
                   gfull[:, hf * 4:(hf + 1) * 4, :], ALU.mult, [ak, "gfull"], [(xTk, hf)])
            xbk, xbt = xTb.next()
            CP(P, "act", xbt[:, :, :], xT32[:, :, :], [(xTk, 0), (xTk, 1)], [xbk])
            DMA(P, "sp", xn2d.ap().rearrange("(k p) t -> p k t", p=128)[:, :, r0:r0 + 128], xbt[:, :, :], [xbk], [])
            ak, at = pA.next()
            for k in range(8):
                MM(P, at[:, 0:16], xT32[:, k, :], wr[:, k, :], k == 0, k == 7, [(xTk, 0), (xTk, 1), "wr"], [ak])
            CP(P, "dve", lg[:, :], at[:, 0:16], [ak], [lgk])
            RMAX(P, "dve", st_[:, 2:3], lg[:, :], [lgk], [sk_])
            TS(P, "dve", st_[:, 2:3], st_[:, 2:3], -1.0, None, ALU.mult, None, [sk_], [sk_])
            ACT(P, ex[:, :], lg[:, :], AF.Exp, [lgk, sk_], [exk], bias=st_[:, 2:3])
            RSUM(P, "dve", st_[:, 3:4], ex[:, :], [exk], [sk_])
            RECIP(P, "dve", st_[:, 3:4], st_[:, 3:4], [sk_], [sk_])
            TS(P, "dve", affs[:, t, :], ex[:, :], st_[:, 3:4], None, ALU.mult, None, [exk, sk_], [("affs", t)])
            ak, at = pA.next()
            TR(P, at[0:16, 0:128], affs[:, t, :], ident32[:, :], [("affs", t), "ident32"], [ak])
            CP(P, "act", affTs[:, r0:r0 + 128], at[0:16, 0:128], [ak], [("affT", t)])
        DMA(P, "sp", affd.ap().rearrange("(t p) e -> p t e", p=128), affs[:, :, :], [("affs", t) for t in range(ntile)], [])
        DMA(P, "sp", affTd[:, :], affTs[:, :], [("affT", t) for t in range(ntile)], [])
        P.emit()
        stats = P.stats
    return nc, stats


def build_thr(ns=S, cap=2 * S // 16, iters=30, env=None, pre=None):
    nc, es, C, P = _begin(env, pre)
    with es:
        affT = C.dram("affT", [16, ns], F32, "ExternalInput")
        thr = C.dram("thr", [16, 2], F32, "ExternalOutput")
        a = C.sb([16, ns], F32, "a")
        junk = C.sb([16, ns], F32, "junk")
        lh = C.sb([16, 2], F32, "lh")
        w = C.sb([16, 8], F32, "w")
        DMA(P, "sp", a[:, :], affT[:, :], [], ["a"])
        MEMSET(P, "dve", lh[:, 0:1], 0.0, ["lh"])
        MEMSET(P, "dve", lh[:, 1:2], 1.0, ["lh"])
        for it in range(iters):
            TT(P, "dve", w[:, 0:1], lh[:, 0:1], lh[:, 1:2], ALU.add, ["lh"], ["w"])
            TS(P, "dve", w[:, 0:1], w[:, 0:1], 0.5, None, ALU.mult, None, ["w"], ["w"])
            P.op("dve", lambda e: e.tensor_scalar(out=junk[:, :], in0=a[:, :], scalar1=w[:, 0:1], scalar2=0.0,
                                                  op0=ALU.is_ge, op1=ALU.add, accum_out=w[:, 1:2]),
                 ["a", "w"], ["junk", "w"])
            TS(P, "dve", w[:, 2:3], w[:, 1:2], float(cap), None, ALU.is_ge, None, ["w"], ["w"])
            TT(P, "dve", w[:, 3:4], w[:, 0:1], lh[:, 0:1], ALU.subtract, ["w", "lh"], ["w"])
            TT(P, "dve", w[:, 4:5], lh[:, 1:2], w[:, 0:1], ALU.subtract, ["w", "lh"], ["w"])
            STT(P, "dve", lh[:, 0:1], w[:, 3:4], w[:, 2:3], lh[:, 0:1], ALU.mult, ALU.add, ["w", "lh"], ["lh"])
            STT(P, "dve", lh[:, 1:2], w[:, 4:5], w[:, 2:3], w[:, 0:1], ALU.mult, ALU.add, ["w", "lh"], ["lh"])
        DMA(P, "sp", thr[:, :], lh[:, :], ["lh"], [])
        P.emit()
        stats = P.stats
    return nc, stats


def build_ffn(nt=TPC, nexp=16, tb=1024, env=None, pre=None):
    nc, es, C, P = _begin(env, pre)
    FF = 1536
    ntile = nt // 128
    nblk = nt // tb
    with es:
        x1d = C.dram("x1", [nt, D], F32, "ExternalInput")
        xnd = C.dram("xn2T", [D, nt], BF16, "ExternalInput")
        affd = C.dram("aff", [nt, 16], F32, "ExternalInput")
        thrd = C.dram("thr_row", [128, 16], F32, "ExternalInput") if not (pre is not None and "thr16" in pre) else None
        wgd = C.dram("wg", [nexp, D, FF], F32, "ExternalInput")
        wud = C.dram("wu", [nexp, D, FF], F32, "ExternalInput")
        wdd = C.dram("wd", [nexp, FF, D], F32, "ExternalInput")
        x2d = C.dram("x2", [nt, D], F32, "ExternalOutput")

        xb = C.sb([128, 8, tb], BF16, "xb")
        acc = C.sb([128, tb // 128, D], F32, "acc")
        wgb = C.sb([128, 8, FF], BF16, "wgb")
        wub = C.sb([128, 8, FF], BF16, "wub")
        wdb = C.sb([128, 12, D], BF16, "wdb")
        stg = Rot([(("stg", i), C.sb([128, 4096], F32, "stg")) for i in range(2)])
        hT = C.sb([128, 12, tb], BF16, "hT")
        sg = Rot([(("sg", i), C.sb([128, 512], F32, "sg")) for i in range(2)])
        affs = C.sb([128, ntile, 16], F32, "affs")
        gw = C.sb([128, ntile, 16], F32, "gw")
        thr = C.sb([128, 16], F32, "thr")
        xo = Rot([(("xo", i), C.sb([128, D], F32, "xo")) for i in range(2)])
        pA = Rot([(("pA", i), C.ps([128, 512], F32, "pA")) for i in range(7)])

        if pre is not None and "thr16" in pre:
            t16d = pre["thr16"]
            i32d = pre["ident32"]
            t16 = C.sb([16, 2], F32, "t16")
            tbc = C.sb([16, 128], F32, "tbc")
            i16 = C.sb([16, 16], F32, "i16")
            DMA(P, "sp", t16[:, :], t16d[:, :], [], ["t16"])
            DMA(P, "sp", i16[:, :], i32d[0:16, 0:16], [], ["i16"])
            MEMSET(P, "dve", tbc[:, :], 1.0, ["tbc"])
            TS(P, "dve", tbc[:, :], tbc[:, :], t16[:, 0:1], None, ALU.mult, None, ["tbc", "t16"], ["tbc"])
            tk_, tp_ = pA.next()
            MM(P, tp_[:, 0:16], tbc[:, :], i16[:, :], True, True, ["tbc", "i16"], [tk_])
            CP(P, "dve", thr[:, :], tp_[:, 0:16], [tk_], ["thr"])
        else:
            DMA(P, "sp", thr[:, :], thrd[:, :], [], ["thr"])
        DMA(P, "sp", affs[:, :, :], affd.ap().rearrange("(t p) e -> p t e", p=128), [], ["affs"])
        for t in range(ntile):
            TT(P, "dve", gw[:, t, :], affs[:, t, :], thr[:, :], ALU.is_ge, ["affs", "thr"], ["gw"])
            TT(P, "dve", gw[:, t, :], gw[:, t, :], affs[:, t, :], ALU.mult, ["gw", "affs"], ["gw"])
        cv = [0]

        def conv(dst, src, r, w):
            eng = ("act", "dve", "act")[cv[0] % 3]
            cv[0] += 1
            CP(P, eng, dst, src, r, w)

        def load_gu(e, chs=(0, 1, 2)):
            for ch in chs:
                for (wd_, dstb, key) in ((wgd, wgb, "wgb"), (wud, wub, "wub")):
                    sk, st = stg.next()
                    sv = st[:, :].rearrange("p (c f) -> p c f", c=8)
                    DMA(P, "sp", sv, wd_[e].rearrange("(c p) f -> p c f", p=128)[:, :, ch * 512:(ch + 1) * 512], [], [sk])
                    for hh in range(2):
                        conv(dstb[:, hh * 4:(hh + 1) * 4, ch * 512:(ch + 1) * 512], sv[:, hh * 4:(hh + 1) * 4, :], [sk],
                             [(key, ch)])

        def load_d(e):
            for ch in range(3):
                sk, st = stg.next()
                sv = st[:, :].rearrange("p (c n) -> p c n", c=4)
                DMA(P, "sp", sv, wdd[e].rearrange("(c p) n -> p c n", p=128)[:, ch * 4:(ch + 1) * 4, :], [], [sk])
                for hh in range(2):
                    conv(wdb[:, ch * 4 + hh * 2:ch * 4 + hh * 2 + 2, :], sv[:, hh * 2:hh * 2 + 2, :], [sk], ["wdb"])

        first = True
        for blk in range(nblk):
            b0 = blk * tb
            DMA(P, "sp", xb[:, :, :], xnd.ap().rearrange("(k p) t -> p k t", p=128)[:, :, b0:b0 + tb], [], ["xb"])
            for e in range(nexp):
                if first:
                    load_gu(e)
                    load_d(e)
                    first = False
                nxt = (blk * nexp + e + 1)
                for f in range(12):
                    for tq in range(tb // 512):
                        gk, gp = pA.next()
                        uk, up = pA.next()
                        for k in range(8):
                            MM(P, gp[:, :], wgb[:, k, f * 128:(f + 1) * 128], xb[:, k, tq * 512:(tq + 1) * 512],
                               k == 0, k == 7, [("wgb", f // 4), "xb"], [gk])
                        for k in range(8):
                            MM(P, up[:, :], wub[:, k, f * 128:(f + 1) * 128], xb[:, k, tq * 512:(tq + 1) * 512],
                               k == 0, k == 7, [("wub", f // 4), "xb"], [uk])
                        sk, st = sg.next()
                        ACT(P, st[:, :], gp[:, :], AF.Silu, [gk], [sk])
                        TT(P, "dve", hT[:, f, tq * 512:(tq + 1) * 512], up[:, :], st[:, :], ALU.mult, [uk, sk], [("hT", f)])
                    if f % 4 == 3 and nxt < nblk * nexp:
                        load_gu(nxt % nexp, chs=(f // 4,))
                for tt in range(tb // 128):
                    gcol = gw[:, blk * (tb // 128) + tt, e:e + 1]
                    for hf in range(2):
                        yk, yp = pA.next()
                        for f in range(12):
                            MM(P, yp[:, :], hT[:, f, tt * 128:(tt + 1) * 128], wdb[:, f, hf * 512:(hf + 1) * 512],
                               f == 0, f == 11, [("hT", f), "wdb"], [yk])
                        asl = acc[:, tt, hf * 512:(hf + 1) * 512]
                        if e == 0:
                            TS(P, "dve", asl, yp[:, :], gcol, None, ALU.mult, None, [yk, "gw"], [("acc", tt, hf)])
                        else:
                            STT(P, "dve", asl, yp[:, :], gcol, asl, ALU.mult, ALU.add, [yk, "gw", ("acc", tt, hf)],
                                [("acc", tt, hf)])
                if nxt < nblk * nexp:
                    load_d(nxt % nexp)
            for tt in range(tb // 128):
                xk, xt_ = xo.next()
                r0 = b0 + tt * 128
                DMA(P, "sp", xt_[:, :], x1d[r0:r0 + 128, :], [], [xk])
                TT(P, "pool", xt_[:, :], xt_[:, :], acc[:, tt, :], ALU.add, [xk, ("acc", tt, 0), ("acc", tt, 1)], [xk])
                DMA(P, "sp", x2d[r0:r0 + 128, :], xt_[:, :], [xk], [])
        P.emit()
        stats = P.stats
    return nc, stats


def build_attn(nq=TPC, nk=S, nheads=8, env=None, pre=None, hook=None):
    nc, es, C, P = _begin(env, pre)
    NKT = nk // 128
    NQB = nq // 512
    scale = 96.0 ** -0.5
    with es:
        mq = C.dram("mq", [nheads, 96, nq], BF16, "ExternalInput")
        mk = C.dram("mk", [nheads, 96, nk], BF16, "ExternalInput")
        mv = C.dram("mv", [nheads, 128, NKT * 64], BF16, "ExternalInput")
        esel = C.dram("esel", [65, 64], F32, "ExternalInput")
        OT = C.dram("OT", [nheads * 64, nq], BF16, "ExternalOutput")

        kT = Rot([(("kT", i), C.sb([96, nk], BF16, "kT")) for i in range(2)])
        vv = Rot([(("vv", i), C.sb([128, NKT, 65], BF16, "vv")) for i in range(2)])
        qT = Rot([(("qT", i), C.sb([96, nq], BF16, "qT")) for i in range(2)])
        pT = Rot([(("pT", i), C.sb([128, 512], BF16, "pT")) for i in range(4)])
        osb = C.sb([65, 512], F32, "osb")
        rbc = C.sb([64, 512], F32, "rbc")
        oo = Rot([(("oo", i), C.sb([64, 512], BF16, "oo")) for i in range(2)])
        es_sb = C.sb([65, 64], F32, "esel")
        sps = Rot([(("sps", i), C.ps([128, 512], F32, "sps")) for i in range(4)])
        ops_ = Rot([(("ops", i), C.ps([128, 512], F32, "ops")) for i in range(2)])
        bps = C.ps([128, 512], F32, "bps")
        DMA(P, "sp", es_sb[:], esel[:, :], [], ["esel"])
        for i in range(2):
            MEMSET(P, "pool", vv.items[i][1][:, :, 64:65], 1.0, [("vv1", i)])
        if hook is not None:
            hook(C, P)
        for h in range(nheads):
            kk, kt_ = kT.next()
            vk, vt_ = vv.next()
            qk, qt_ = qT.next()
            DMA(P, "sp", kt_[:, :], mk[h, :, :], [("mk_s", h)], [kk])
            DMA(P, "sp", vt_[:, :, 0:64], mv[h, :, :].rearrange("p (t d) -> p t d", d=64), [("mv_s", h)], [vk])
            DMA(P, "sp", qt_[:, :], mq[h, :, :], [], [qk])
            vkeys = [vk, ("vv1", (vv.i - 1) % 2)]
            for qb in range(NQB):
                ok_, ot_ = ops_.next()

                def s_mm(t, kt_=kt_, qt_=qt_, qb=qb, kk=kk, qk=qk):
                    sk, st = sps.next()
                    MM(P, st[:, :], kt_[:, t * 128:(t + 1) * 128], qt_[:, qb * 512:(qb + 1) * 512], True, True,
                       [kk, qk], [sk])
                    return sk, st
                pend = [s_mm(0)]
                if NKT > 1:
                    pend.append(s_mm(1))
                for t in range(NKT):
                    if t + 2 < NKT:
                        pend.append(s_mm(t + 2))
                    sk, st = pend.pop(0)
                    pk, pt = pT.next()
                    ACT(P, pt[:, :], st[:, :], AF.Exp, [sk], [pk], scale=scale)
                    MM(P, ot_[0:65, :], vt_[:, t, :], pt[:, :], t == 0, t == NKT - 1, vkeys + [pk], [ok_])
                CP(P, "dve", osb[:, :], ot_[0:65, :], [ok_], ["osb"])
                MM(P, bps[0:64, :], es_sb[:, :], osb[:, :], True, True, ["esel", "osb"], ["bps"])
                CP(P, "dve", rbc[:, :], bps[0:64, :], ["bps"], ["rbc"])
                RECIP(P, "dve", rbc[:, :], rbc[:, :], ["rbc"], ["rbc"])
                ook, oot = oo.next()
                TT(P, "dve", oot[:, :], osb[0:64, :], rbc[:, :], ALU.mult, ["osb", "rbc"], [ook])
                DMA(P, "sp", OT[h * 64:(h + 1) * 64, qb * 512:(qb + 1) * 512], oot[:, :], [ook], [])
        P.emit()
        stats = P.stats
    return nc, stats


def attn_consts():
    e = np.zeros((65, 64), np.float32)
    e[64, :] = 1.0
    return dict(esel=e)


RG = [[0, 1, 2, 3], [4, 5, 6, 7]]
LAYER_W = [("w_in", [D, INW], F32), ("gmix", [128, 8], F32), ("convp", [128, 4, 34], F32), ("gcq", [128, 3], F32),
           ("gckv", [128, 2], F32), ("w_uq", [384, 768], F32), ("w_ukv", [256, 1024], F32), ("gqk", [96, 2], F32),
           ("bif", [128, 4], F32), ("gA", [128, 128], F32), ("w_a", [512, D], F32), ("w_b", [512, D], F32),
           ("w_c", [512, D], F32), ("w_o", [D, D], F32), ("gffn", [128, 8], F32), ("w_r", [D, 16], F32),
           ("wg", [16, D, 1536], F32), ("wu", [16, D, 1536], F32), ("wd", [16, 1536, D], F32)]
CONSTS = [("identb", [128, 128], BF16), ("ropeT", [96, 2, TPC], F32), ("rmat", [96, 96], BF16), ("onesf", [128, 128], F32),
          ("cmat", [128, 6, 128], F32), ("ident32", [128, 128], F32), ("esel", [65, 64], F32), ("idx", [128, 16], I32)]


def build_fused(stop=None):
    env = Env()
    nc, P = env.nc, env.P
    BYP = ALU.bypass
    CCB = 256 * 1024
    with env.es:
        def DT(name, shape, dt, kind="Internal"):
            return nc.dram_tensor(name, list(shape), dt, kind=kind)

        def allgather(name, src2d, rows, cols, dt, rkeys, wkey):
            esz = 4 if dt in (F32, I32) else 2
            rc = max(1, min(rows, CCB // (cols * esz)))
            assert rows % rc == 0
            g = DT(name, [4 * rows, cols], dt)
            for k in range(rows // rc):
                P.cc(lambda e, k=k: e.collective_compute("AllGather", BYP, replica_groups=RG, ins=[src2d[k * rc:(k + 1) * rc, :]],
                                                         outs=[g[k * 4 * rc:(k + 1) * 4 * rc, :]]), rkeys, [wkey])
            return g, rc

        def rankview(g, rc, r):
            return g.ap().rearrange("(k r x) c -> r k x c", r=4, x=rc)[r]

        ext = {"xe0": DT("xe0", [TPC + 2 * HALO, D], F32, "ExternalInput")}
        for (n, sh, dt) in CONSTS:
            ext[n] = DT(n, sh, dt, "ExternalInput")
        for l in range(2):
            for (n, sh, dt) in LAYER_W:
                ext["%s_%d" % (n, l)] = DT("%s_%d" % (n, l), sh, dt, "ExternalInput")
        out = DT("out", [TPC, D], F32, "ExternalOutput")
        xe = ext["xe0"]
        x_own = None
        for l in range(2):
            W = {n: ext["%s_%d" % (n, l)] for (n, _, _) in LAYER_W}
            L = lambda n, sh, dt: DT("%s_L%d" % (n, l), sh, dt)
            A = dict(QT=L("QT", [512, TPC], BF16), KT=L("KT", [512, TPC], BF16), Kt=L("Kt", [4, TPC, 128], BF16),
                     Vt=L("Vt", [4, TPC, 128], BF16), OG=L("OG", [4, TPC, 128], BF16), G4=L("G4", [4, TPC, 4], F32),
                     GTS=L("GTS", [TPC, 3072], BF16), UT=L("UT", [512, TPC], BF16), MQ=L("MQ", [8, 96, TPC], BF16),
                     MK=L("MK", [8, 96, TPC], BF16), MV=L("MV", [8, 128, TPC // 128, 64], BF16))
            preA = dict(xe=xe, w_in=W["w_in"], gmix=W["gmix"], identb=ext["identb"], convp=W["convp"], gcq=W["gcq"],
                        gckv=W["gckv"], w_uq=W["w_uq"], w_ukv=W["w_ukv"], gqk=W["gqk"], ropeT=ext["ropeT"],
                        rmat=ext["rmat"], onesf=ext["onesf"], **A)
            build_stageA(env=env, pre=preA)
            nc_, es_, C_, _ = _begin(env)
            with es_:
                idx = C_.sb([128, 16], I32, "idx")
                DMA(P, "sp", idx[:, :], ext["idx"][:, :], [], ["idx"])
                P.emit()
            mk_s = L("mk_s", [8, 96, S], BF16)
            mv_s = L("mv_s", [8, 128, (S // 128) * 64], BF16)

            def exchange_kv(C_, P_, A=A, l=l, mk_s=mk_s, mv_s=mv_s):
                srcK = A["MK"].ap().rearrange("h f t -> (h f) t")
                srcV = A["MV"].ap().rearrange("h p t d -> (h p) (t d)")
                gK = DT("gMK_L%d" % l, [4 * 768, TPC], BF16)
                gV = DT("gMV_L%d" % l, [4 * 1024, 2048], BF16)
                for h in range(8):
                    for k in range(3 * h, 3 * h + 3):
                        P_.cc(lambda e, k=k: e.collective_compute("AllGather", BYP, replica_groups=RG, ins=[srcK[k * 32:(k + 1) * 32, :]],
                                                                  outs=[gK[k * 128:(k + 1) * 128, :]]), [], [("gK", h)])
                    for k in range(2 * h, 2 * h + 2):
                        P_.cc(lambda e, k=k: e.collective_compute("AllGather", BYP, replica_groups=RG, ins=[srcV[k * 64:(k + 1) * 64, :]],
                                                                  outs=[gV[k * 256:(k + 1) * 256, :]]), [], [("gV", h)])
                    for i in range(4):
                        DMA(P_, "pool", mk_s[h, :, i * TPC:(i + 1) * TPC].rearrange("(k x) t -> k x t", x=32),
                            gK.ap().rearrange("(k r x) c -> r k x c", r=4, x=32)[i][3 * h:3 * h + 3], [("gK", h)], [("mk_s", h)])
                        DMA(P_, "pool", mv_s[h, :, i * 2048:(i + 1) * 2048].rearrange("(k x) c -> k x c", x=64),
                            gV.ap().rearrange("(k r x) c -> r k x c", r=4, x=64)[i][2 * h:2 * h + 2], [("gV", h)], [("mv_s", h)])
            if stop == "x1":
                return nc
            qT_s = L("qT_s", [128, S], BF16)
            kt_s = L("kt_s", [S, 128], BF16)
            vt_s = L("vt_s", [S, 128], BF16)
            og_s = L("og_s", [S, 128], BF16)
            g4_s = L("g4_s", [S, 4], F32)

            def exchange_heads(C_, P_, A=A, l=l, qT_s=qT_s, kt_s=kt_s, vt_s=vt_s, og_s=og_s, g4_s=g4_s):
                idx = C_.sb([128, 16], I32, "idx")
                DMA(P_, "pool", idx[:, :], ext["idx"][:, :], [], ["idx"])
                gQT, rcQ = allgather("gQT_L%d" % l, A["QT"].ap(), 512, TPC, BF16, [], ("g", 0))
                gKt, rcK = allgather("gKt_L%d" % l, A["Kt"].ap().rearrange("h t d -> (h t) d"), 4 * TPC, 128, BF16, [], ("g", 1))
                gVt, _ = allgather("gVt_L%d" % l, A["Vt"].ap().rearrange("h t d -> (h t) d"), 4 * TPC, 128, BF16, [], ("g", 2))
                gOG, _ = allgather("gOG_L%d" % l, A["OG"].ap().rearrange("h t d -> (h t) d"), 4 * TPC, 128, BF16, [], ("g", 3))
                gG4, rcG = allgather("gG4_L%d" % l, A["G4"].ap().rearrange("h t g -> (h t) g"), 4 * TPC, 4, F32, [], ("g", 4))
                assert (rcQ, rcK, rcG) == (32, 1024, 4 * TPC), (rcQ, rcK, rcG)
                stb = Rot([(("stb", i), C_.sb([128, 16384], BF16, "stb")) for i in range(2)])
                stf = C_.sb([128, 512], F32, "stf")

                def gather(dst_ap, src_ap, col, rkeys, wkeys, tile_ap, tkey):
                    P_.dma("pool", lambda e: e.indirect_dma_start(
                        out=tile_ap, out_offset=None, in_=src_ap,
                        in_offset=bass.IndirectOffsetOnAxis(ap=idx[:, col:col + 1], axis=0)), ["idx"] + rkeys, [tkey])
                    DMA(P_, "pool", dst_ap, tile_ap, [tkey], wkeys)
                for i in range(4):
                    tk_, tt_ = stb.next()
                    gather(qT_s[:, i * TPC:(i + 1) * TPC], gQT[:, :], i, [("g", 0)], [("qT_s", i)], tt_[:, 0:TPC], tk_)
                for j, (gsrc, dst) in enumerate(((gKt, kt_s), (gVt, vt_s), (gOG, og_s))):
                    tk_, tt_ = stb.next()
                    gather(dst.ap().rearrange("(c p) d -> c (p d)", p=128),
                           gsrc.ap().rearrange("(c p) d -> c (p d)", p=128), 4, [("g", 1 + j)], [("tm_s", j)], tt_[:, :], tk_)
                gather(g4_s.ap().rearrange("(c p) d -> c (p d)", p=128),
                       gG4.ap().rearrange("(c p) d -> c (p d)", p=128), 11, [("g", 4)], [("tm_s", 3)], stf[:, :], "stf")

            OT = L("OT", [512, TPC], BF16)
            def both(C_, P_, f1=exchange_kv, f2=exchange_heads):
                f1(C_, P_)
                f2(C_, P_)
            build_attn(env=env, pre=dict(mq=A["MQ"], mk=mk_s, mv=mv_s, esel=ext["esel"], OT=OT), hook=both)
            if stop == "t":
                return nc
            HT = L("HT", [128, S], BF16)
            build_mlstm(env=env, pre=dict(qT=qT_s, kt=kt_s, vt=vt_s, g4=g4_s, bif=W["bif"], og=og_s, gA=W["gA"],
                                          cmat=ext["cmat"], identb=ext["identb"], HT=HT))
            if stop == "m":
                return nc
            nc_, es_, C_, _ = _begin(env)
            with es_:
                idx = C_.sb([128, 16], I32, "idx")
                DMA(P, "sp", idx[:, :], ext["idx"][:, :], [], ["idx"])
                gHT, rcH = allgather("gHT_L%d" % l, HT.ap(), 128, S, BF16, [], "gHT")
                assert rcH == 8
                HT_own = L("HT_own", [512, TPC], BF16)
                src = gHT.ap().rearrange("r (i t) -> (r i) t", i=4)
                stb = Rot([(("stb", i), C_.sb([128, TPC], BF16, "stb")) for i in range(2)])
                for h in range(4):
                    tk_, tt_ = stb.next()
                    P.dma("pool", lambda e, h=h, tt_=tt_: e.indirect_dma_start(
                        out=tt_[:, :], out_offset=None, in_=src,
                        in_offset=bass.IndirectOffsetOnAxis(ap=idx[:, 5 + h:6 + h], axis=0)), ["idx", "gHT"], [tk_])
                    DMA(P, "sp", HT_own[h * 128:(h + 1) * 128, :], tt_[:, :], [tk_], [("HT_own", h)])
                P.emit()
            if stop == "x2":
                return nc
            x1 = L("x1", [TPC, D], F32)
            xn2T = L("xn2T", [D, TPC], BF16)
            aff = L("aff", [TPC, 16], F32)
            affT = L("affT", [16, TPC], F32)
            if l == 0:
                xin = L("xin", [TPC, D], F32)
                nc_, es_, C_, _ = _begin(env)
                with es_:
                    DMA(P, "sp", xin[:, :], ext["xe0"][HALO:HALO + TPC, :], [], ["xin"])
                    P.emit()
            else:
                xin = x_own
            build_merge(env=env, pre=dict(x=xin, HT=HT_own, UT=A["UT"], OT=OT, GTS=A["GTS"], w_a=W["w_a"], w_b=W["w_b"],
                                          w_c=W["w_c"], w_o=W["w_o"], gffn=W["gffn"], w_r=W["w_r"], identb=ext["identb"],
                                          ident32=ext["ident32"], x1=x1, xn2T=xn2T, aff=aff, affT=affT))
            if stop == "c1":
                return nc
            affT_s = L("affT_s", [16, S], F32)
            nc_, es_, C_, _ = _begin(env)
            with es_:
                gAf, rcA = allgather("gAf_L%d" % l, affT.ap(), 16, TPC, F32, [], "gAf")
                assert rcA == 16
                for i in range(4):
                    DMA(P, "sp", affT_s[:, i * TPC:(i + 1) * TPC], gAf[i * 16:(i + 1) * 16, :], ["gAf"], [("affT_s", i)])
                P.emit()
            thr = L("thr", [16, 2], F32)
            build_thr(env=env, pre=dict(affT=affT_s, thr=thr))
            if stop == "h":
                return nc
            x2 = out if l == 1 else L("x2", [TPC, D], F32)
            build_ffn(env=env, pre=dict(x1=x1, xn2T=xn2T, aff=aff, thr16=thr, ident32=ext["ident32"], wg=W["wg"], wu=W["wu"],
                                        wd=W["wd"], x2=x2))
            if l == 0:
                xe1 = L("xe1", [TPC + 2 * HALO, D], F32)
                nc_, es_, C_, _ = _begin(env)
                with es_:
                    idx = C_.sb([128, 16], I32, "idx")
                    zt = C_.sb([128, D], F32, "zt")
                    DMA(P, "sp", idx[:, :], ext["idx"][:, :], [], ["idx"])
                    MEMSET(P, "dve", zt[:, :], 0.0, ["zt"])
                    edges = L("edges", [384, D], F32)
                    DMA(P, "sp", edges[0:128, :], x2[0:128, :], [], ["edges"])
                    DMA(P, "sp", edges[128:256, :], x2[TPC - 128:TPC, :], [], ["edges"])
                    DMA(P, "sp", edges[256:384, :], zt[:, :], ["zt"], ["edges"])
                    DMA(P, "sp", xe1[HALO:HALO + TPC, :], x2[:, :], [], ["xe1m"])
                    gE, rcE = allgather("gE_L%d" % l, edges.ap(), 384, D, F32, ["edges"], "gE")
                    assert rcE == 64
                    hl = C_.sb([128, D], F32, "hl")
                    hr = C_.sb([128, D], F32, "hr")
                    P.dma("pool", lambda e: e.indirect_dma_start(
                        out=hl[:, :], out_offset=None, in_=gE[:, :],
                        in_offset=bass.IndirectOffsetOnAxis(ap=idx[:, 9:10], axis=0)), ["idx", "gE"], ["hl"])
                    P.dma("pool", lambda e: e.indirect_dma_start(
                        out=hr[:, :], out_offset=None, in_=gE[:, :],
                        in_offset=bass.IndirectOffsetOnAxis(ap=idx[:, 10:11], axis=0)), ["idx", "gE"], ["hr"])
                    DMA(P, "sp", xe1[0:HALO, :], hl[:, :], ["hl"], ["xe1l"])
                    DMA(P, "sp", xe1[HALO + TPC:, :], hr[:, :], ["hr"], ["xe1r"])
                    P.emit()
                xe = xe1
                x_own = x2
    return nc


def _grow(x, rc, r):
    return (x // rc) * (4 * rc) + r * rc + (x % rc)


def fused_idx(c):
    r = c % 4
    p = np.arange(128)
    idx = np.zeros((128, 16), np.int32)
    for i in range(4):
        idx[:, i] = _grow(r * 128 + p, 32, i)
    y = r * 32 + (p % 32)
    idx[:, 4] = _grow(y, 8, p // 32)
    idx[:, 11] = (p // 32) * 128 + y
    for h in range(4):
        idx[:, 5 + h] = _grow(p, 8, h) * 4 + r
    idx[:, 9] = _grow(128 + p, 64, r - 1) if r > 0 else _grow(256 + p, 64, r)
    idx[:, 10] = _grow(p, 64, r + 1) if r < 3 else _grow(256 + p, 64, r)
    return idx


def kernel(**inputs):
    prm = {k: np.asarray(v) for k, v in inputs.items()}
    x = np.ascontiguousarray(prm["x"], dtype=np.float32)
    nc = build_fused()
    CT, ST = rope_tables()
    cst = consts()
    mc = mlstm_consts()
    ac = attn_consts()
    i32 = np.eye(128, dtype=np.float32)
    lay = []
    for l in range(2):
        convp = np.zeros((128, 4, 34), np.float32)
        convp[:, :, 0:31] = prm["conv_w"][l].T.reshape(4, 128, 31).transpose(1, 0, 2)
        convp[:, :, 31] = prm["conv_b"][l].reshape(4, 128).T
        convp[:, :, 32] = prm["conv_ln_g"][l].reshape(4, 128).T
        convp[:, :, 33] = prm["conv_ln_b"][l].reshape(4, 128).T
        lay.append(dict(w_in=prm["w_in"][l], gmix=_gain_cols(prm["mix_norm_g"][l], 8), convp=convp,
                        gcq=_gain_cols(prm["cq_norm_g"][l], 3), gckv=_gain_cols(prm["ckv_norm_g"][l], 2),
                        w_uq=prm["w_uq"][l], w_ukv=prm["w_ukv"][l],
                        gqk=np.ascontiguousarray(np.stack([prm["q_norm_g"][l], prm["k_norm_g"][l]], axis=1)),
                        w_a=prm["w_a_out"][l], w_b=prm["w_b_out"][l], w_c=prm["w_c_out"][l], w_o=prm["w_out"][l],
                        gffn=_gain_cols(prm["ffn_norm_g"][l], 8), w_r=prm["w_router"][l],
                        wg=prm["w_e_gate"][l], wu=prm["w_e_up"][l], wd=prm["w_e_down"][l]))
    maps = []
    for c in range(NCORES):
        b, r = c // 4, c % 4
        s0 = r * TPC
        xe = np.zeros((TPC + 2 * HALO, D), np.float32)
        lo, hi = max(0, s0 - HALO), min(S, s0 + TPC + HALO)
        xe[lo - (s0 - HALO):hi - (s0 - HALO)] = x[b, lo:hi]
        m = dict(xe0=xe, identb=cst["identb"], ropeT=np.ascontiguousarray(np.stack([CT[:, s0:s0 + TPC], ST[:, s0:s0 + TPC]], axis=1)),
                 rmat=cst["rmat"], onesf=cst["onesf"], cmat=mc["cmat"], ident32=i32, esel=ac["esel"], idx=fused_idx(c))
        cols = [r, 4 + r, 8 + r, 12 + r]
        for l in range(2):
            for k_, v_ in lay[l].items():
                m["%s_%d" % (k_, l)] = v_
            m["bif_%d" % l] = np.ascontiguousarray(np.broadcast_to(prm["b_if"][l][cols], (128, 4)))
            m["gA_%d" % l] = np.ascontiguousarray(np.broadcast_to(prm["a_norm_g"][l][r], (128, 128)))
        maps.append(m)
    res = run_spmd(nc, maps)
    out = np.empty_like(x)
    for c in range(NCORES):
        b, r = c // 4, c % 4
        out[b, r * TPC:(r + 1) * TPC] = np.asarray(res[c]["out"])
    return out
```
